# Optimizing a Trainium2 kernel written in Bass

```python
import jax
import jax.numpy as jnp
from jax import lax
import numpy as np

D_MODEL = 1024
BATCH = 1
SEQ = 16384
DEPTH = 1

GRID_W = 64
CTX_LEN = 256
D_MIX = D_MODEL
D_MLSTM = D_MIX // 2
MLSTM_HEADS = 4
MLSTM_DH = D_MLSTM // MLSTM_HEADS
QKV_BLOCK = 4
N_QKV_BLOCKS = D_MLSTM // QKV_BLOCK
CONV_K = 3
CHUNK = 128
D_FOURIER = D_MIX - D_MLSTM
FOURIER_GROUPS = 4
FOURIER_CG = D_FOURIER // FOURIER_GROUPS
D_IN_PROJ = 2 * D_MLSTM + D_FOURIER
N_EXPERT_GROUPS = 4
EXPERTS_PER_GROUP = 4
N_EXPERTS = N_EXPERT_GROUPS * EXPERTS_PER_GROUP
TOP_K = 2
D_EXPERT = D_MODEL // 2
N_MOD = 6
EPS = 1e-6
POS_BASE = 10000.0
F32 = jnp.float32

kernel_name = 'hybrid_mlstm_fourier_hmoe_prefix_block'


def rmsnorm(x, g):
    x32 = x.astype(F32)
    y = x32 * lax.rsqrt(jnp.mean(x32 * x32, axis=-1, keepdims=True) + EPS)
    return (y * g.astype(F32)).astype(x.dtype)


def modulate(h, shift, scale):
    return h * (1 + scale) + shift


def sincos_2d(rows, cols):
    quarter = D_MODEL // 4
    freq = 1.0 / (POS_BASE ** (jnp.arange(quarter, dtype=F32) / quarter))
    r = jnp.arange(rows, dtype=F32)[:, None] * freq
    cl = jnp.arange(cols, dtype=F32)[:, None] * freq
    er = jnp.concatenate([jnp.sin(r), jnp.cos(r)], axis=-1)
    ec = jnp.concatenate([jnp.sin(cl), jnp.cos(cl)], axis=-1)
    half = D_MODEL // 2
    emb = jnp.concatenate([jnp.broadcast_to(er[:, None, :], (rows, cols, half)),
                           jnp.broadcast_to(ec[None, :, :], (rows, cols, half))], axis=-1)
    return emb.reshape(rows * cols, D_MODEL)


def short_conv(x, w, b):
    y = lax.conv_general_dilated(x, w[:, None, :], window_strides=(1,), padding='SAME',
                                 dimension_numbers=('NWC', 'WIO', 'NWC'),
                                 feature_group_count=x.shape[-1])
    return y + b


def headwise(x, w):
    y = jnp.einsum('btni,nio->btno', x.reshape(x.shape[:-1] + (N_QKV_BLOCKS, QKV_BLOCK)), w)
    return y.reshape(x.shape)


def to_heads(t):
    B, T, _ = t.shape
    return t.astype(F32).reshape(B, T, MLSTM_HEADS, MLSTM_DH).transpose(0, 2, 1, 3)


def flip_seq(t):
    return jnp.flip(t, axis=2)


def mixer_features(h, lp):
    proj = h @ lp['w_in']
    x_m = proj[..., :D_MLSTM]
    z = proj[..., D_MLSTM:2 * D_MLSTM]
    u = proj[..., 2 * D_MLSTM:]
    act = jax.nn.silu(short_conv(x_m, lp['conv_w'], lp['conv_b']))
    q = headwise(act, lp['w_q'])
    k = headwise(act, lp['w_k'])
    v = headwise(x_m, lp['w_v'])
    qkv = jnp.concatenate([q, k, v], axis=-1)

    def gates(w, b):
        g = (qkv @ w + b).astype(F32).transpose(0, 2, 1)
        return g[:, :MLSTM_HEADS], jax.nn.log_sigmoid(g[:, MLSTM_HEADS:])

    heads = (to_heads(q) * (MLSTM_DH ** -0.5), to_heads(k), to_heads(v))
    return heads, gates(lp['w_if_fwd'], lp['b_if_fwd']), gates(lp['w_if_bwd'], lp['b_if_bwd']), act, z, u


def mlstm_scan(k, v, ig, lf, state):
    B, H, T, DH = k.shape
    nc = T // CHUNK
    kc = k.reshape(B, H, nc, CHUNK, DH)
    vc = v.reshape(B, H, nc, CHUNK, DH)
    b = jnp.cumsum(lf.reshape(B, H, nc, CHUNK), axis=-1)
    btot = b[..., -1]
    a = btot[..., None] - b + ig.reshape(B, H, nc, CHUNK)
    amax = jnp.max(a, axis=-1)

    def step(carry, xs):
        C, n, m = carry
        k_, v_, a_, amax_, bt_ = xs
        m_new = jnp.maximum(bt_ + m, amax_)
        decay = jnp.exp(bt_ + m - m_new)
        kw = k_ * jnp.exp(a_ - m_new[..., None])[..., None]
        C_new = decay[..., None, None] * C + jnp.einsum('bhlk,bhlv->bhkv', kw, v_)
        n_new = decay[..., None] * n + jnp.sum(kw, axis=2)
        return (C_new, n_new, m_new), (C, n, m)

    xs = tuple(jnp.moveaxis(t, 2, 0) for t in (kc, vc, a, amax, btot))
    final, starts = lax.scan(step, state, xs)
    starts = tuple(jnp.moveaxis(s, 0, 2) for s in starts)
    return starts, final


def mlstm_out(q, k, v, ig, lf, starts):
    C0, n0, m0 = starts
    B, H, T, DH = q.shape
    nc = T // CHUNK
    qc = q.reshape(B, H, nc, CHUNK, DH)
    kc = k.reshape(B, H, nc, CHUNK, DH)
    vc = v.reshape(B, H, nc, CHUNK, DH)
    igc = ig.reshape(B, H, nc, CHUNK)
    b = jnp.cumsum(lf.reshape(B, H, nc, CHUNK), axis=-1)
    tri = jnp.tril(jnp.ones((CHUNK, CHUNK), dtype=bool))
    logd = jnp.where(tri, b[..., :, None] - b[..., None, :] + igc[..., None, :], -jnp.inf)
    m_inter = b + m0[..., None]
    m_t = jnp.maximum(m_inter, jnp.max(logd, axis=-1))
    w_inter = jnp.exp(m_inter - m_t)
    s = jnp.einsum('bhctd,bhcsd->bhcts', qc, kc) * jnp.exp(logd - m_t[..., None])
    num = jnp.einsum('bhcts,bhcsd->bhctd', s, vc) + w_inter[..., None] * jnp.einsum('bhctk,bhckv->bhctv', qc, C0)
    nq = jnp.sum(s, axis=-1) + w_inter * jnp.einsum('bhctk,bhck->bhct', qc, n0)
    h = num / jnp.maximum(jnp.abs(nq), jnp.exp(-m_t))[..., None]
    return h.reshape(B, H, T, DH)


def mlstm_direction(ctx_in, lat_in, with_ctx):
    qc, kc, vc, igc, lfc = ctx_in
    ql, kl, vl, igl, lfl = lat_in
    B = kl.shape[0]
    zero = (jnp.zeros((B, MLSTM_HEADS, MLSTM_DH, MLSTM_DH), F32),
            jnp.zeros((B, MLSTM_HEADS, MLSTM_DH), F32),
            jnp.zeros((B, MLSTM_HEADS), F32))
    ctx_starts, ctx_final = mlstm_scan(kc, vc, igc, lfc, zero)
    lat_starts, _ = mlstm_scan(kl, vl, igl, lfl, ctx_final)
    h_lat = mlstm_out(ql, kl, vl, igl, lfl, lat_starts)
    h_ctx = mlstm_out(qc, kc, vc, igc, lfc, ctx_starts) if with_ctx else None
    return h_lat, h_ctx


def mlstm_readout(hf, hb, act, z, norm_w, skip):
    h = hf + hb
    mu = jnp.mean(h, axis=-1, keepdims=True)
    var = jnp.mean(jnp.square(h - mu), axis=-1, keepdims=True)
    hn = (h - mu) * lax.rsqrt(var + EPS)
    B, H, T, DH = hn.shape
    hn = hn.transpose(0, 2, 1, 3).reshape(B, T, D_MLSTM) * norm_w.astype(F32)
    out = (hn + skip.astype(F32) * act.astype(F32)) * jax.nn.silu(z.astype(F32))
    return out.astype(z.dtype)


def fourier_mix(u, w_fourier):
    B, T, _ = u.shape
    ug = u.astype(F32).reshape(B, T, FOURIER_GROUPS, FOURIER_CG)
    f = jnp.fft.fft2(ug, axes=(1, 3), norm='ortho').real
    y = jnp.einsum('btgc,gcd->btgd', f, w_fourier.astype(F32))
    return y.reshape(B, T, D_FOURIER).astype(u.dtype)


def token_mix(h, hc, lp, with_ctx):
    qkv_l, gf_l, gb_l, act_l, z_l, u_l = mixer_features(h, lp)
    qkv_c, gf_c, gb_c, act_c, z_c, u_c = mixer_features(hc, lp)
    hf_l, hf_c = mlstm_direction(qkv_c + gf_c, qkv_l + gf_l, with_ctx)
    hb_l, hb_c = mlstm_direction(tuple(flip_seq(t) for t in qkv_c + gb_c),
                                 tuple(flip_seq(t) for t in qkv_l + gb_l), with_ctx)

    def merge(hf, hb, act, z, u):
        m = mlstm_readout(hf, flip_seq(hb), act, z, lp['mlstm_norm_w'], lp['mlstm_skip'])
        f = fourier_mix(u, lp['w_fourier'])
        return jnp.concatenate([m, f], axis=-1) @ lp['w_out']

    y_l = merge(hf_l, hb_l, act_l, z_l, u_l)
    y_c = merge(hf_c, hb_c, act_c, z_c, u_c) if with_ctx else None
    return y_l, y_c


def hier_moe(h, lp):
    B, T, _ = h.shape
    g_logits = (h @ lp['w_router_group'] + lp['b_router_group']).astype(F32)
    p_group = jax.nn.softmax(g_logits, axis=-1)
    g_sel = jnp.argmax(g_logits, axis=-1)
    p_sel = jnp.take_along_axis(p_group, g_sel[..., None], axis=-1)
    e_logits = (h @ lp['w_router_expert'] + lp['b_router_expert']).astype(F32)
    e_logits = e_logits.reshape(B, T, N_EXPERT_GROUPS, EXPERTS_PER_GROUP)
    e_sel = jnp.take_along_axis(e_logits, g_sel[..., None, None], axis=2)[..., 0, :]
    top_v, top_i = lax.top_k(e_sel, TOP_K)
    w_top = jax.nn.softmax(top_v, axis=-1) * p_sel
    expert_id = g_sel[..., None] * EXPERTS_PER_GROUP + top_i
    combine = jnp.sum(jax.nn.one_hot(expert_id, N_EXPERTS, dtype=F32) * w_top[..., None], axis=-2)
    combine = combine.astype(h.dtype)
    y = jnp.zeros_like(h)
    for e in range(N_EXPERTS):
        a = jax.nn.silu(h @ lp['w_gate'][e]) * (h @ lp['w_up'][e])
        y = y + combine[..., e, None] * (a @ lp['w_down'][e])
    return y


def setup_inputs(seed: int = 0) -> dict:
    key = jax.random.key(seed)
    ks = jax.random.split(key, 40)
    L = DEPTH
    H = MLSTM_HEADS

    def nrm(k, shape, s):
        return jax.random.normal(k, shape, F32) * s

    def b_if(k1, k2):
        return jnp.concatenate([nrm(k1, (L, H), 0.1),
                                jnp.linspace(3.0, 6.0, H, dtype=F32)[None, :] + nrm(k2, (L, H), 0.1)], axis=-1)

    return {
        'x': nrm(ks[0], (BATCH, SEQ, D_MODEL), 1.0),
        'c': nrm(ks[1], (BATCH, D_MODEL), 1.0),
        'ctx': nrm(ks[2], (BATCH, CTX_LEN, D_MODEL), 1.0),
        'c_ctx': nrm(ks[3], (D_MODEL,), 1.0),
        'w_ada': nrm(ks[4], (L, D_MODEL, N_MOD * D_MODEL), 0.5 * D_MODEL ** -0.5),
        'b_ada': nrm(ks[5], (L, N_MOD * D_MODEL), 0.01),
        'g_pre_mix': 1.0 + nrm(ks[6], (L, D_MODEL), 0.05),
        'g_post_mix': 1.0 + nrm(ks[7], (L, D_MODEL), 0.05),
        'g_pre_ffn': 1.0 + nrm(ks[8], (L, D_MODEL), 0.05),
        'g_post_ffn': 1.0 + nrm(ks[9], (L, D_MODEL), 0.05),
        'w_in': nrm(ks[10], (L, D_MODEL, D_IN_PROJ), D_MODEL ** -0.5),
        'conv_w': nrm(ks[11], (L, CONV_K, D_MLSTM), CONV_K ** -0.5),
        'conv_b': nrm(ks[12], (L, D_MLSTM), 0.01),
        'w_q': nrm(ks[13], (L, N_QKV_BLOCKS, QKV_BLOCK, QKV_BLOCK), QKV_BLOCK ** -0.5),
        'w_k': nrm(ks[14], (L, N_QKV_BLOCKS, QKV_BLOCK, QKV_BLOCK), QKV_BLOCK ** -0.5),
        'w_v': nrm(ks[15], (L, N_QKV_BLOCKS, QKV_BLOCK, QKV_BLOCK), QKV_BLOCK ** -0.5),
        'w_if_fwd': nrm(ks[16], (L, 3 * D_MLSTM, 2 * H), 0.02),
        'b_if_fwd': b_if(ks[17], ks[18]),
        'w_if_bwd': nrm(ks[19], (L, 3 * D_MLSTM, 2 * H), 0.02),
        'b_if_bwd': b_if(ks[20], ks[21]),
        'mlstm_norm_w': 1.0 + nrm(ks[22], (L, D_MLSTM), 0.05),
        'mlstm_skip': 1.0 + nrm(ks[23], (L, D_MLSTM), 0.05),
        'w_fourier': nrm(ks[24], (L, FOURIER_GROUPS, FOURIER_CG, FOURIER_CG), FOURIER_CG ** -0.5),
        'w_out': nrm(ks[25], (L, D_MIX, D_MODEL), D_MIX ** -0.5),
        'w_router_group': nrm(ks[26], (L, D_MODEL, N_EXPERT_GROUPS), D_MODEL ** -0.5),
        'b_router_group': nrm(ks[27], (L, N_EXPERT_GROUPS), 0.01),
        'w_router_expert': nrm(ks[28], (L, D_MODEL, N_EXPERTS), D_MODEL ** -0.5),
        'b_router_expert': nrm(ks[29], (L, N_EXPERTS), 0.01),
        'w_gate': nrm(ks[30], (L, N_EXPERTS, D_MODEL, D_EXPERT), D_MODEL ** -0.5),
        'w_up': nrm(ks[31], (L, N_EXPERTS, D_MODEL, D_EXPERT), D_MODEL ** -0.5),
        'w_down': nrm(ks[32], (L, N_EXPERTS, D_EXPERT, D_MODEL), D_EXPERT ** -0.5),
    }


def reference(x, c, ctx, c_ctx, w_ada, b_ada, g_pre_mix, g_post_mix, g_pre_ffn, g_post_ffn,
              w_in, conv_w, conv_b, w_q, w_k, w_v, w_if_fwd, b_if_fwd, w_if_bwd, b_if_bwd,
              mlstm_norm_w, mlstm_skip, w_fourier, w_out, w_router_group, b_router_group,
              w_router_expert, b_router_expert, w_gate, w_up, w_down):
    T = x.shape[1]
    rows = T // GRID_W
    x = x + sincos_2d(rows, GRID_W).astype(x.dtype)[None]
    xc = ctx
    for l in range(DEPTH):
        with_ctx = l + 1 < DEPTH
        lp = {
            'w_in': w_in[l], 'conv_w': conv_w[l], 'conv_b': conv_b[l],
            'w_q': w_q[l], 'w_k': w_k[l], 'w_v': w_v[l],
            'w_if_fwd': w_if_fwd[l], 'b_if_fwd': b_if_fwd[l],
            'w_if_bwd': w_if_bwd[l], 'b_if_bwd': b_if_bwd[l],
            'mlstm_norm_w': mlstm_norm_w[l], 'mlstm_skip': mlstm_skip[l],
            'w_fourier': w_fourier[l], 'w_out': w_out[l],
            'w_router_group': w_router_group[l], 'b_router_group': b_router_group[l],
            'w_router_expert': w_router_expert[l], 'b_router_expert': b_router_expert[l],
            'w_gate': w_gate[l], 'w_up': w_up[l], 'w_down': w_down[l],
        }
        mod = [m[:, None, :] for m in jnp.split(jax.nn.silu(c) @ w_ada[l] + b_ada[l], N_MOD, axis=-1)]
        mod_c = [m[None, :] for m in jnp.split(jax.nn.silu(c_ctx) @ w_ada[l] + b_ada[l], N_MOD, axis=-1)]
        shift1, scale1, gate1, shift2, scale2, gate2 = mod
        h = modulate(rmsnorm(x, g_pre_mix[l]), shift1, scale1)
        hc = modulate(rmsnorm(xc, g_pre_mix[l]), mod_c[0], mod_c[1])
        y, yc = token_mix(h, hc, lp, with_ctx)
        x = x + gate1 * rmsnorm(y, g_post_mix[l])
        h2 = modulate(rmsnorm(x, g_pre_ffn[l]), shift2, scale2)
        x = x + gate2 * rmsnorm(hier_moe(h2, lp), g_post_ffn[l])
        if with_ctx:
            xc = xc + mod_c[2] * rmsnorm(yc, g_post_mix[l])
            hc2 = modulate(rmsnorm(xc, g_pre_ffn[l]), mod_c[3], mod_c[4])
            xc = xc + mod_c[5] * rmsnorm(hier_moe(hc2, lp), g_post_ffn[l])
    return x
```

```python
import math
from contextlib import ExitStack
import numpy as np
import concourse.bass as bass
import concourse.mybir as mybir
from concourse.bass_utils import run_bass_kernel_spmd

F32 = mybir.dt.float32
BF16 = mybir.dt.bfloat16
I32 = mybir.dt.int32
AF = mybir.ActivationFunctionType
ALU = mybir.AluOpType

NCORES = 8
T = 16384
D = 1024
NT = T // 128
OWN = NT // NCORES
NG = NT // 4
EPS = 1e-6
TWO_PI = 2.0 * math.pi
NHALO = 2 * NG
DBG = {}


class Buf:
    __slots__ = ("name", "w", "r")

    def __init__(self, name):
        self.name = name
        self.w = None
        self.r = {}


class Prog:
    def __init__(self, nc, stack, ndma=28):
        self.nc = nc
        self.engs = {"pe": nc.tensor, "act": nc.scalar, "dve": nc.vector, "pool": nc.gpsimd, "sp": nc.sync}
        self.ops = {k: [] for k in self.engs}
        self.sem = {k: stack.enter_context(nc.semaphore("s_" + k)) for k in self.engs}
        self.cnt = {k: 0 for k in self.engs}
        self.waited = {k: {} for k in self.engs}
        self.dsem = [stack.enter_context(nc.semaphore("d%d" % i)) for i in range(ndma)]
        self.dcnt = [0] * ndma
        self.ring = {"sp": list(range(0, ndma - 8)), "act": list(range(0, ndma - 8)), "pool": list(range(ndma - 8, ndma))}
        self.rpos = {"sp": 0, "pool": 0}

    def _collect(self, e, reads, writes, sync_same=True):
        waits = {}

        def need(tok):
            if tok is None:
                return
            sem, val, eng = tok
            if eng == e and not sync_same:
                return
            key = id(sem)
            if self.waited[e].get(key, 0) >= val:
                return
            if key not in waits or waits[key][1] < val:
                waits[key] = (sem, val)

        for b in reads:
            need(b.w)
        for b in writes:
            need(b.w)
            for t in b.r.values():
                need(t)
        for key, (sem, val) in waits.items():
            self.waited[e][key] = val
        return list(waits.values())

    def _commit(self, tok, reads, writes):
        key = id(tok[0])
        for b in reads:
            old = b.r.get(key)
            if old is None or old[1] < tok[1]:
                b.r[key] = tok
        for b in writes:
            b.w = tok
            b.r = {}

    def op(self, e, fn, reads=(), writes=(), sync_same=True):
        waits = self._collect(e, reads, writes, sync_same)
        self.cnt[e] += 1
        tok = (self.sem[e], self.cnt[e], e)
        self.ops[e].append((waits, fn, (self.sem[e], 1)))
        self._commit(tok, reads, writes)
        return tok

    def dma(self, e, out, in_, reads=(), writes=()):
        rk = "pool" if e == "pool" else "sp"
        ring = self.ring[rk]
        i = ring[self.rpos[rk] % len(ring)]
        self.rpos[rk] += 1
        sem = self.dsem[i]
        waits = self._collect(e, reads, writes)
        prev = self.dcnt[i] * 16
        if prev > 0 and self.waited[e].get(id(sem), 0) < prev:
            waits.append((sem, prev))
            self.waited[e][id(sem)] = prev
        self.dcnt[i] += 1
        tok = (sem, self.dcnt[i] * 16, "dma")
        self.ops[e].append((waits, (lambda eng, o=out, i_=in_: eng.dma_start(out=o, in_=i_)), (sem, 16)))
        self._commit(tok, reads, writes)
        return tok

    def barrier(self):
        toks = [(self.sem[k], self.cnt[k]) for k in self.engs if self.cnt[k] > 0]
        toks += [(self.dsem[i], self.dcnt[i] * 16) for i in range(len(self.dsem)) if self.dcnt[i] > 0]
        for e in self.engs:
            waits = []
            for sem, val in toks:
                if self.waited[e].get(id(sem), 0) < val:
                    waits.append((sem, val))
                    self.waited[e][id(sem)] = val
            if waits:
                self.ops[e].append((waits, None, None))

    def emit(self, block, final_waits):
        def replay(e):
            def body(eng):
                for waits, fn, inc in self.ops[e]:
                    for sem, val in waits:
                        eng.wait_ge(sem, val)
                    if fn is not None:
                        ins = fn(eng)
                        ins.then_inc(inc[0], inc[1])
                if e == "sp":
                    for sem, val in final_waits:
                        eng.wait_ge(sem, val)
            return body

        block.tensor(replay("pe"))
        block.scalar(replay("act"))
        block.vector(replay("dve"))
        block.gpsimd(replay("pool"))
        block.sync(replay("sp"))


def build(stage=99, dbg=False, ngroups=NG, cut=99):
    nc = bass.Bass("TRN2", target_bir_lowering=False)
    DBG.clear()

    def din(name, shape, dt=F32):
        return nc.dram_tensor(name, list(shape), dt, kind="ExternalInput")

    x_rot = din("x_rot", [ngroups * 512, D])
    x_halo = din("x_halo", [128, D])
    ctx_in = din("ctx", [256, D])
    cT = din("cT", [128, 16])
    w_ada = din("w_ada", [D, 6 * D])
    b_ada = din("b_ada", [1, 6 * D])
    gains = din("gains", [4, D])
    w_in = din("w_in", [D, 1536])
    convw = din("convw", [128, 16])
    w_qkv = din("w_qkv", [3, 512, 4])
    w_qkvT = din("w_qkvT", [3, 512, 4])
    w_if = din("w_if", [1536, 16])
    b_if = din("b_if", [1, 16])
    nrm_skip = din("nrm_skip", [2, 512])
    w_fourier = din("w_fourier", [4, 128, 128])
    w_out = din("w_out", [D, D])
    w_router = din("w_router", [D, 20])
    b_router = din("b_router", [1, 20])
    moe_small = stage < 8
    w_gate = din("w_gate", [16, D, 512] if not moe_small else [1, 8, 8])
    w_up = din("w_up", [16, D, 512] if not moe_small else [1, 8, 8])
    w_down = din("w_down", [16, 512, D] if not moe_small else [1, 8, 8])
    consts = din("consts", [128, 5 * 128])
    tabs = din("tabs", [128, 1024])
    dftidx = din("dftidx", [128, 5 * 128])
    out_d = nc.dram_tensor("out", [OWN * 128, D], F32, kind="ExternalOutput")

    posr_d = nc.dram_tensor("posr_d", [256, 512], F32)
    u_d = nc.dram_tensor("u_d", [512, T], BF16)
    az_d = nc.dram_tensor("az_d", [2, OWN * 128, 512], BF16)
    hs_d = nc.dram_tensor("hs_d", [OWN * 128, 512], F32)
    mod_d = nc.dram_tensor("mod_d", [1, 6 * D], F32)

    dbg_outs = {}

    def dbg_out(name, shape):
        DBG[name] = tuple(shape)
        dbg_outs[name] = nc.dram_tensor("dbg_" + name, list(shape), F32, kind="ExternalOutput")
        return dbg_outs[name]

    stack = ExitStack()
    with stack:
        P = Prog(nc, stack)
        used = [0]

        def sb(name, shape, dt=F32):
            t = stack.enter_context(nc.sbuf_tensor(name, list(shape), dt))
            return t

        def psum(name, shape, dt=F32):
            return stack.enter_context(nc.psum_tensor(name, list(shape), dt))

        def act(out, in_, func, reads, writes, eng="act", **kw):
            return P.op("act", lambda e: e.activation(out=out, in_=in_, func=func, **kw), reads, writes)

        def tt(eng, out, in0, in1, op, reads, writes):
            return P.op(eng, lambda e: e.tensor_tensor(out=out, in0=in0, in1=in1, op=op), reads, writes)

        def ts(eng, out, in0, s1, s2, op0, op1, reads, writes):
            if op1 is None:
                return P.op(eng, lambda e: e.tensor_scalar(out=out, in0=in0, scalar1=s1, scalar2=None, op0=op0), reads, writes)
            return P.op(eng, lambda e: e.tensor_scalar(out=out, in0=in0, scalar1=s1, scalar2=s2, op0=op0, op1=op1), reads, writes)

        def stt(eng, out, in0, scalar, in1, op0, op1, reads, writes):
            return P.op(eng, lambda e: e.scalar_tensor_tensor(out=out, in0=in0, scalar=scalar, in1=in1, op0=op0, op1=op1), reads, writes)

        def cp(eng, out, in_, reads, writes):
            if eng == "act":
                return P.op("act", lambda e: e.activation(out=out, in_=in_, func=AF.Copy), reads, writes)
            return P.op(eng, lambda e: e.tensor_copy(out=out, in_=in_), reads, writes)

        def mm(out, lhsT, rhs, start, stop, reads, writes):
            return P.op("pe", lambda e: e.matmul(out, lhsT=lhsT, rhs=rhs, start=start, stop=stop), reads, writes, sync_same=False)

        def tr(out, in_, ident, reads, writes):
            return P.op("pe", lambda e: e.transpose(out=out, in_=in_, identity=ident), reads, writes, sync_same=False)

        def memset(eng, ap, val, writes):
            return P.op(eng, lambda e: e.memset(ap, val), (), writes)

        def dump(name, ap, shape, reads):
            if not dbg:
                return
            dd = dbg_out(name, shape)
            P.dma("pool", dd.ap(), ap, reads=reads, writes=[Buf("dbg")])

        def bc_rows(dram_t, row, n, parts=128, off=0):
            width = dram_t.shape[-1]
            return bass.AP(dram_t, row * width + off, [[0, parts], [1, n]])

        PS = [psum("ps%d" % i, [128, 512]) for i in range(8)]
        PB = [Buf("ps%d" % i) for i in range(8)]

        CONST = sb("CONST", [128, 640])
        bCONST = Buf("CONST")
        P.dma("sp", CONST[:, :], consts.ap(), writes=[bCONST])
        IDF = CONST[:, 0:128]
        TRIU = CONST[:, 128:256]
        TRIL = CONST[:, 256:384]
        ONES = CONST[:, 384:512]
        BDM = CONST[:, 512:640]
        IDB = sb("IDB", [128, 128], BF16)
        bIDB = Buf("IDB")
        cp("dve", IDB[:, :], IDF, [bCONST], [bIDB])
        TABS = sb("TABS", [128, 1024])
        bTABS = Buf("TABS")
        P.dma("sp", TABS[:, :], tabs.ap(), writes=[bTABS])
        NEGH = TABS[:, 5:6]
        POSC = sb("POSC", [128, 512])
        bPOSC = Buf("POSC")

        final = []
        stm = ExitStack()

        def sbm(name, shape, dt=F32):
            return stm.enter_context(nc.sbuf_tensor(name, list(shape), dt))

        QTF = sbm("QTF", [128, 4 * OWN * 128], BF16)
        QT = QTF[:, :].rearrange("p (c t) -> p c t", c=4)
        bQT = Buf("QT")
        KTF = sbm("KTF", [128, 4 * OWN * 128], BF16)
        KT = KTF[:, :].rearrange("p (c t) -> p c t", c=4)
        bKT = Buf("KT")
        WINC = KTF[:, 0:4096].rearrange("p (k n) -> p k n", n=512)
        bWINC = bKT
        KTMO = sbm("KTMO", [128, OWN, 512], BF16)
        bKTMO = [Buf("KTMO%d" % i) for i in range(OWN)]
        VAO = sbm("VAO", [128, OWN, 4, 129], BF16)
        bVAO = [Buf("VAO%d" % i) for i in range(OWN)]
        OWNG = sbm("OWNG", [128, OWN, 24])
        bOWNG = Buf("OWNG")
        CF = sbm("CF", [128, 4, 129])
        bCF = Buf("CF")
        CB = sbm("CB", [128, 4, 129])
        bCB = Buf("CB")
        sts = ExitStack()

        def sbs(name, shape, dt=F32):
            return sts.enter_context(nc.sbuf_tensor(name, list(shape), dt))

        WIN = sbs("WIN", [128, 8, 1536], BF16)
        bWIN = Buf("WIN")
        BCOL = sbs("BCOL", [128, 16])
        bBCOL = Buf("BCOL")
        BZ = sbs("BZ", [128, 512])
        bBZ = Buf("BZ")
        bMODD = Buf("mod_d")

        def interleave(gens):
            gens = list(gens)
            while gens:
                for g_ in list(gens):
                    try:
                        next(g_)
                    except StopIteration:
                        gens.remove(g_)

        with ExitStack() as st0:
            def sb0(name, shape, dt=F32):
                return st0.enter_context(nc.sbuf_tensor(name, list(shape), dt))
            MOD = sb0("MOD", [128, 6 * D])
            bMOD = Buf("MOD")
            MODC = sb0("MODC", [128, 2 * D])
            bMODC = Buf("MODC")
            st0a = ExitStack()

            def sb0a(name, shape, dt=F32):
                return st0a.enter_context(nc.sbuf_tensor(name, list(shape), dt))
            CT = sb0a("CT", [128, 16])
            bCT = Buf("CT")
            P.dma("sp", CT[:, :], cT.ap(), writes=[bCT])
            SC = sb0a("SC", [128, 16])
            bSC = Buf("SC")
            act(SC[:, :], CT[:, :], AF.Silu, [bCT], [bSC])
            REP = sb0a("REP", [128, 16, 128])
            bREP = Buf("REP")
            for j in range(16):
                cp("dve", REP[:, j, :], SC[:, j:j + 1].broadcast_to([128, 128]), [bSC], [bREP])
            WA = [sb0a("WA%d" % i, [128, 8, 512]) for i in range(2)]
            bWA = [Buf("WA%d" % i) for i in range(2)]
            BA = [sb0a("BA%d" % i, [128, 512]) for i in range(2)]
            bBA = [Buf("BA%d" % i) for i in range(2)]
            w_ada_v = w_ada.ap().rearrange("(k p) n -> p k n", p=128)
            for blk in range(12):
                s = blk % 2
                P.dma("sp", WA[s][:, :, :], w_ada_v[:, :, blk * 512:(blk + 1) * 512], writes=[bWA[s]])
                P.dma("sp", BA[s][:, :], bc_rows(b_ada, 0, 512, off=blk * 512), writes=[bBA[s]])
                pb = blk % 2
                for kc in range(8):
                    mm(PS[pb][:, :], REP[:, kc, :], WA[s][:, kc, :], kc == 0, kc == 7, [bREP, bWA[s]], [PB[pb]])
                tt("dve", MOD[:, blk * 512:(blk + 1) * 512], PS[pb][:, :], BA[s][:, :], ALU.add, [PB[pb], bBA[s]], [bMOD])
                if blk < 4:
                    pc = 2 + blk % 2
                    for kc in range(8):
                        mm(PS[pc][:, :], REP[:, 8 + kc, :], WA[s][:, kc, :], kc == 0, kc == 7, [bREP, bWA[s]], [PB[pc]])
                    tt("dve", MODC[:, blk * 512:(blk + 1) * 512], PS[pc][:, :], BA[s][:, :], ALU.add, [PB[pc], bBA[s]], [bMODC])
            GB = sb0a("GB", [128, 4, D])
            bGB = Buf("GB")
            for i in range(4):
                P.dma("sp", GB[:, i, :], bc_rows(gains, i, D), writes=[bGB])
            stt("dve", MOD[:, D:2 * D], MOD[:, D:2 * D], 1.0, GB[:, 0, :], ALU.add, ALU.mult, [bMOD, bGB], [bMOD])
            tt("dve", MOD[:, 2 * D:3 * D], MOD[:, 2 * D:3 * D], GB[:, 1, :], ALU.mult, [bMOD, bGB], [bMOD])
            stt("dve", MOD[:, 4 * D:5 * D], MOD[:, 4 * D:5 * D], 1.0, GB[:, 2, :], ALU.add, ALU.mult, [bMOD, bGB], [bMOD])
            tt("dve", MOD[:, 5 * D:6 * D], MOD[:, 5 * D:6 * D], GB[:, 3, :], ALU.mult, [bMOD, bGB], [bMOD])
            stt("dve", MODC[:, D:2 * D], MODC[:, D:2 * D], 1.0, GB[:, 0, :], ALU.add, ALU.mult, [bMODC, bGB], [bMODC])
            P.dma("sp", mod_d.ap(), MOD[0:1, :], reads=[bMOD], writes=[bMODD])
            dump("mod", MOD[0:1, :], [1, 6 * D], [bMOD])
            dump("modc", MODC[0:1, :], [1, 2 * D], [bMODC])
            P.barrier()
            st0a.close()
            REPS = sb0("REPS", [128, 2, 8, 128])
            bREPS = Buf("REPS")
            GC = sb0("GC", [128, 16])
            bGC = Buf("GC")
            for kc in range(8):
                blk = slice(kc * 128, (kc + 1) * 128)
                tr(PS[0][:, 0:128], MOD[:, blk], IDF, [bMOD, bCONST], [PB[0]])
                tr(PS[0][:, 128:256], MOD[:, D + kc * 128:D + (kc + 1) * 128], IDF, [bMOD, bCONST], [PB[0]])
                tr(PS[0][:, 256:384], MODC[:, blk], IDF, [bMODC, bCONST], [PB[0]])
                tr(PS[0][:, 384:512], MODC[:, D + kc * 128:D + (kc + 1) * 128], IDF, [bMODC, bCONST], [PB[0]])
                cp("dve", REPS[:, 0, kc, :], PS[0][:, 0:128], [PB[0]], [bREPS])
                cp("dve", REPS[:, 1, kc, :], PS[0][:, 256:384], [PB[0]], [bREPS])
                cp("dve", GC[:, kc:kc + 1], PS[0][:, 128:129], [PB[0]], [bGC])
                cp("dve", GC[:, 8 + kc:9 + kc], PS[0][:, 384:385], [PB[0]], [bGC])
            WST = [sb0("WST%d" % i, [128, 1536]) for i in range(2)]
            bWST = [Buf("WST%d" % i) for i in range(2)]
            for kc in range(8):
                w_, bw_ = WST[kc % 2], bWST[kc % 2]
                P.dma("sp", w_[:, :], w_in.ap()[kc * 128:(kc + 1) * 128, :], writes=[bw_])
                ts("dve", WIN[:, kc, :], w_[:, :], GC[:, kc:kc + 1], None, ALU.mult, None, [bw_, bGC], [bWIN])
                ts("pool", WINC[:, kc, :], w_[:, 0:512], GC[:, 8 + kc:9 + kc], None, ALU.mult, None, [bw_, bGC], [bWINC])
                for blk in range(3):
                    mm(PS[2 + blk][:, :], REPS[:, 0, kc, :], w_[:, blk * 512:(blk + 1) * 512], kc == 0, kc == 7, [bREPS, bw_], [PB[2 + blk]])
                mm(PS[5][:, :], REPS[:, 1, kc, :], w_[:, 0:512], kc == 0, kc == 7, [bREPS, bw_], [PB[5]])
            BROW = sb0("BROW", [128, 4, 512])
            bBROW = Buf("BROW")
            for i in range(4):
                cp("dve", BROW[:, i, :], PS[2 + i][:, :], [PB[2 + i]], [bBROW])
            cp("dve", BZ[:, :], BROW[:, 1, :], [bBROW], [bBZ])
            for i, src in ((0, 0), (1, 2), (2, 3)):
                for j in range(4):
                    tr(PS[0][:, j * 128:(j + 1) * 128], BROW[:, src, j * 128:(j + 1) * 128], IDF, [bBROW, bCONST], [PB[0]])
                for j in range(4):
                    cp("dve", BCOL[:, i * 4 + j:i * 4 + j + 1], PS[0][:, j * 128:j * 128 + 1], [PB[0]], [bBCOL])
            P.barrier()

        BDB = sbs("BDB", [128, 3, 4, 128], BF16)
        bBDB = Buf("BDB")
        AW = sbs("AW", [128, 4, 32], BF16)
        bAW = Buf("AW")
        BIF = sbs("BIF", [128, 16])
        bBIF = Buf("BIF")
        P.dma("sp", BIF[:, :], bc_rows(b_if, 0, 16), writes=[bBIF])
        CW = sbs("CW", [128, 16])
        bCW = Buf("CW")
        P.dma("sp", CW[:, :], convw.ap(), writes=[bCW])
        XMH = sbs("XMH", [128, 4, 64])
        bXMH = Buf("XMH")
        bPOSRD = Buf("posr_d")
        NTB = 4
        XB = [sbs("XB%d" % i, [128, D]) for i in range(NTB)]
        bXB = [Buf("XB%d" % i) for i in range(NTB)]
        PRB = [sbs("PRB%d" % i, [128, 512]) for i in range(NTB)]
        bPRB = [Buf("PRB%d" % i) for i in range(NTB)]
        X0B = [sbs("X0B%d" % i, [128, D], BF16) for i in range(NTB)]
        bX0B = [Buf("X0B%d" % i) for i in range(NTB)]
        JUNK = [sbs("JUNK%d" % i, [128, D], BF16) for i in range(2)]
        bJUNK = [Buf("JUNK%d" % i) for i in range(2)]
        DG = [sbs("DG%d" % i, [128, 128], BF16) for i in range(NTB)]
        bDG = [Buf("DG%d" % i) for i in range(NTB)]
        SS = sbs("SS", [128, 4 * NTB])
        bSS = [Buf("SS%d" % i) for i in range(NTB)]
        HT = sbs("HT", [128, 8, 512], BF16)
        bHTt = [Buf("HT%d" % i) for i in range(4)]
        PT = PS[0][:, :].bitcast(BF16).rearrange("p (k t) -> p k t", t=128)

        with ExitStack() as st1:
            def sb1(name, shape, dt=F32):
                return st1.enter_context(nc.sbuf_tensor(name, list(shape), dt))
            FREQ = sb1("FREQ", [128, 256])
            bFREQ = Buf("FREQ")
            act(FREQ[:, :], TABS[:, 512:768], AF.Exp, [bTABS], [bFREQ], scale=-math.log(10000.0) / 256.0)
            WS = sb1("WS", [128, 6, 4, 4])
            bWS = Buf("WS")
            for w in range(3):
                P.dma("sp", WS[:, w, :, :], bass.AP(w_qkv, w * 2048, [[4, 128], [512, 4], [1, 4]]), writes=[bWS])
                P.dma("sp", WS[:, 3 + w, :, :], bass.AP(w_qkvT, w * 2048, [[4, 128], [512, 4], [1, 4]]), writes=[bWS])
            BDF = sb1("BDF", [128, 6, 4, 128])
            bBDF = Buf("BDF")
            BDM3 = BDM.rearrange("p (r o) -> p r o", o=4)
            for w in range(6):
                for cc in range(4):
                    tt("dve", BDF[:, w, cc, :].rearrange("p (r o) -> p r o", o=4),
                       WS[:, w, cc, None, :].broadcast_to([128, 32, 4]), BDM3, ALU.mult, [bWS, bCONST], [bBDF])
            cp("dve", BDB[:, :, :, :], BDF[:, 0:3, :, :], [bBDF], [bBDB])
            WIF = sb1("WIF", [128, 12, 16])
            bWIF = Buf("WIF")
            P.dma("sp", WIF[:, :, :], w_if.ap().rearrange("(k p) n -> p k n", p=128), writes=[bWIF])
            for cc in range(4):
                mm(PS[1][:, cc * 32:cc * 32 + 16], BDF[:, 3, cc, :], WIF[:, cc, :], True, False, [bBDF, bWIF], [PB[1]])
                mm(PS[1][:, cc * 32:cc * 32 + 16], BDF[:, 4, cc, :], WIF[:, 4 + cc, :], False, True, [bBDF, bWIF], [PB[1]])
                mm(PS[1][:, cc * 32 + 16:cc * 32 + 32], BDF[:, 5, cc, :], WIF[:, 8 + cc, :], True, True, [bBDF, bWIF], [PB[1]])
            cp("dve", AW[:, :, :], PS[1][:, 0:128].rearrange("p (c n) -> p c n", n=32), [PB[1]], [bAW])

            ANG = sb1("ANG", [128, 512])
            bANG = Buf("ANG")
            KI = sb1("KI", [128, 512], I32)
            bKI = Buf("KI")
            MSK = sb1("MSK", [128, 512])
            bMSK = Buf("MSK")

            def sincos(out, bout, idx):
                ts("dve", ANG[:, 0:256], FREQ[:, :], idx, None, ALU.mult, None, [bFREQ, bTABS], [bANG])
                ts("dve", ANG[:, 256:512], ANG[:, 0:256], math.pi / 2, None, ALU.add, None, [bANG], [bANG])
                ts("dve", KI[:, :], ANG[:, :], 1.0 / TWO_PI, None, ALU.mult, None, [bANG], [bKI])
                stt("dve", ANG[:, :], KI[:, :], -TWO_PI, ANG[:, :], ALU.mult, ALU.add, [bKI, bANG], [bANG])
                ts("dve", MSK[:, :], ANG[:, :], math.pi, TWO_PI, ALU.is_gt, ALU.mult, [bANG], [bMSK])
                tt("dve", ANG[:, :], ANG[:, :], MSK[:, :], ALU.subtract, [bANG, bMSK], [bANG])
                ts("dve", MSK[:, :], ANG[:, :], -math.pi, TWO_PI, ALU.is_lt, ALU.mult, [bANG], [bMSK])
                tt("dve", ANG[:, :], ANG[:, :], MSK[:, :], ALU.add, [bANG, bMSK], [bANG])
                ts("dve", ANG[:, :], ANG[:, :], math.pi, -math.pi, ALU.min, ALU.max, [bANG], [bANG])
                act(out, ANG[:, :], AF.Sin, [bANG], [bout])

            PR = sb1("PR", [128, 2, 512])
            bPR = Buf("PR")
            for a in range(2):
                sincos(PR[:, a, :], bPR, TABS[:, a:a + 1])
            P.dma("sp", posr_d.ap().rearrange("(p a) n -> p a n", a=2), PR[:, :, :], reads=[bPR], writes=[bPOSRD])
            sincos(POSC[:, :], bPOSC, TABS[:, 2:3])
            POSH = sb1("POSH", [128, D])
            bPOSH = Buf("POSH")
            sincos(POSH[:, 0:512], bPOSH, TABS[:, 3:4])
            sincos(POSH[:, 512:1024], bPOSH, TABS[:, 4:5])

            tile_ctr = [0]

            def tile_front(xsrc, pos_mode, ht_dst, bht):
                k = tile_ctr[0]
                tile_ctr[0] += 1
                q = k % NTB
                X, bX = XB[q], bXB[q]
                xb_, bxb_ = X0B[q], bX0B[q]
                dg, bdg = DG[q], bDG[q]
                bss = bSS[q]
                P.dma("sp", X[:, :], xsrc, writes=[bX])
                if pos_mode is not None and pos_mode[0] == "rolled":
                    i = pos_mode[1]
                    PRt, bPRt = PRB[q], bPRB[q]
                    P.dma("sp", PRt[0:64, :], bc_rows(posr_d, 2 * i, 512, parts=64), reads=[bPOSRD], writes=[bPRt])
                    P.dma("sp", PRt[64:128, :], bc_rows(posr_d, 2 * i + 1, 512, parts=64), reads=[bPOSRD], writes=[bPRt])
                    yield
                    tt("pool", xb_[:, 0:512], X[:, 0:512], PRt[:, :], ALU.add, [bX, bPRt], [bxb_])
                    tt("pool", xb_[:, 512:1024], X[:, 512:1024], POSC[:, :], ALU.add, [bX, bPOSC], [bxb_])
                elif pos_mode is not None:
                    tt("pool", xb_[:, :], X[:, :], POSH[:, :], ALU.add, [bX, bPOSH], [bxb_])
                else:
                    yield
                    cp("pool", xb_[:, :], X[:, :], [bX], [bxb_])
                yield
                sc = SS[:, q * 4:q * 4 + 4]
                act(JUNK[k % 2][:, :], xb_[:, :], AF.Square, [bxb_], [bJUNK[k % 2], bss], accum_out=sc[:, 0:1])
                yield
                ts("pool", sc[:, 1:2], sc[:, 0:1], 1.0 / D, EPS, ALU.mult, ALU.add, [bss], [bss])
                tt("pool", sc[:, 2:3], sc[:, 1:2], NEGH, ALU.pow, [bss, bTABS], [bss])
                yield
                ts("dve", dg[:, :], IDF, sc[:, 2:3], None, ALU.mult, None, [bCONST, bss], [bdg])
                yield
                for kc in range(8):
                    pb = kc // 4
                    mm(PS[pb][:, (kc % 4) * 128:(kc % 4 + 1) * 128], xb_[:, kc * 128:(kc + 1) * 128], dg[:, :], True, True, [bxb_, bdg], [PB[pb]])
                cp("act", ht_dst[:, 0:4, :], PS[0][:, :].rearrange("p (k t) -> p k t", t=128), [PB[0]], [bht])
                cp("dve", ht_dst[:, 4:8, :], PS[1][:, :].rearrange("p (k t) -> p k t", t=128), [PB[1]], [bht])
                yield

            HTH = sb1("HTH", [128, 8, 128], BF16)
            bHTH = Buf("HTH")
            interleave([tile_front(x_halo.ap(), ("halo",), HTH[:, :, :], bHTH)])
            for cc in range(4):
                for kc in range(8):
                    mm(PS[2][:, cc * 64:cc * 64 + 64], WIN[:, kc, cc * 128:(cc + 1) * 128], HTH[:, kc, 0:64], kc == 0, kc == 7, [bWIN, bHTH], [PB[2]])
            XMHF = sb1("XMHF", [128, 4, 64])
            bXMHF = Buf("XMHF")
            tt("dve", XMHF[:, :, :], PS[2][:, 0:256].rearrange("p (c n) -> p c n", n=64),
               BCOL[:, 0:4, None].broadcast_to([128, 4, 64]), ALU.add, [PB[2], bBCOL], [bXMHF])
            tt("dve", XMH[:, :, :], XMHF[:, :, :], TABS[:, None, 384:448].broadcast_to([128, 4, 64]), ALU.mult, [bXMHF, bTABS], [bXMH])
            dump("xmh", XMH[:, :, :], [128, 4, 64], [bXMH])
            P.barrier()

        XM = sbs("XM", [128, 4, 514])
        bXM = [Buf("XM%d" % i) for i in range(4)]
        ACC = [sbs("ACC%d" % i, [128, 512]) for i in range(2)]
        bACC = [Buf("ACC%d" % i) for i in range(2)]
        ACTT = [sbs("ACTT%d" % i, [128, 4, 512], BF16) for i in range(2)]
        bACTT = [[Buf("ACTT%d_%d" % (j, i)) for i in range(4)] for j in range(2)]
        XMB = [sbs("XMB%d" % i, [128, 4, 512], BF16) for i in range(2)]
        bXMB = [[Buf("XMB%d_%d" % (j, i)) for i in range(4)] for j in range(2)]
        UT = sbs("UT", [128, 4, 512], BF16)
        bUT = Buf("UT")
        bUD = Buf("u_d")
        KTMG = sbs("KTMG", [128, 4, 512], BF16)
        bKTMG = [Buf("KTMG%d" % i) for i in range(4)]
        VAG = sbs("VAG", [128, 4, 4, 129], BF16)
        bVAG = [Buf("VAG%d" % i) for i in range(4)]
        VS = [sbs("VS%d" % i, [128, 8, 129], BF16) for i in range(2)]
        bVS = [Buf("VS%d" % i) for i in range(2)]
        CBS = sbs("CBS", [128, 4, 129])
        bCBS = Buf("CBS")
        memset("pool", CBS[:, :, :], 0.0, [bCBS])
        CCB = sbs("CCB", [128, 4, 129])
        bCCB = Buf("CCB")
        GT = sbs("GT", [128, 4, 16])
        bGT = Buf("GT")
        GW = sbs("GW", [128, 4, 40])
        bGW = Buf("GW")
        EXI = sbs("EXI", [128, 4, 16])
        bEXI = Buf("EXI")
        EXO = sbs("EXO", [128, 4, 16])
        bEXO = Buf("EXO")
        PALL = sbs("PALL", [128, 5, 4])
        bPALL = Buf("PALL")
        MBT = sbs("MBT", [128, 4, 4])
        bMBT = Buf("MBT")
        WV = sbs("WV", [128, 4, 8])
        bWV = Buf("WV")
        STG = [sbs("STG%d" % i, [128, 512], BF16) for i in range(2)]
        bSTG = [Buf("STG%d" % i) for i in range(2)]
        ZT = sbs("ZT", [128, 512])
        bZT = Buf("ZT")
        bAZ = Buf("az_d")
        stg_ctr = [0]

        memset("pool", VAG[:, :, :, :], 1.0, bVAG)
        memset("pool", VAO[:, :, :, :], 1.0, bVAO)
        memset("pool", PALL[:, :, :], 0.0, [bPALL])

        PSG = PS[6][:, 384:448].rearrange("p (t g) -> p t g", g=16)
        bPSG = PB[6]
        PSB = PS[6][:, 448:512].rearrange("p (t g) -> p t g", g=16)
        bPSB = PB[6]
        DCF = [PS[5][:, 0:129], PS[5][:, 129:258], PS[5][:, 258:387], PS[6][:, 0:129]]
        bDCF = [PB[5], PB[5], PB[5], PB[6]]
        ACCB = [PS[7][:, 0:129], PS[7][:, 129:258], PS[7][:, 258:387], PS[6][:, 129:258]]
        bACCB = [PB[7], PB[7], PB[7], PB[6]]
        u_v = u_d.ap().rearrange("(c p) t -> p c t", p=128)
        LN_QS = math.log(128.0 ** -0.5)

        def front(gi, kind, par):
            n = 2 if kind == "ctx" else 4
            ntok = n * 128
            tgens = []
            for t_ in range(n):
                dst = HT[:, :, t_ * 128:(t_ + 1) * 128]
                if kind == "ctx":
                    tgens.append(tile_front(ctx_in.ap()[t_ * 128:(t_ + 1) * 128, :], None, dst, bHTt[t_]))
                else:
                    i = gi * 4 + t_
                    tgens.append(tile_front(x_rot.ap()[i * 128:(i + 1) * 128, :], ("rolled", i), dst, bHTt[t_]))
            while tgens:
                for g_ in list(tgens):
                    try:
                        next(g_)
                    except StopIteration:
                        tgens.remove(g_)
                yield
            W_, bW_, boff = (WINC, bWINC, 8) if kind == "ctx" else (WIN, bWIN, 0)
            att, batt, xmb, bxmb = ACTT[par], bACTT[par], XMB[par], bXMB[par]
            for cc in range(4):
                pb = 2 + cc % 2
                for kc in range(8):
                    mm(PS[pb][:, 0:ntok], W_[:, kc, cc * 128:(cc + 1) * 128], HT[:, kc, 0:ntok], kc == 0, kc == 7, [bW_] + bHTt, [PB[pb]])
                bias = BCOL[:, boff + cc:boff + cc + 1]
                act(XM[:, cc, 1:1 + ntok], PS[pb][:, 0:ntok], AF.Identity, [PB[pb], bBCOL], [bXM[cc]], bias=bias)
                act(xmb[:, cc, 0:ntok], PS[pb][:, 0:ntok], AF.Identity, [PB[pb], bBCOL], [bxmb[cc]], bias=bias)
                if kind == "ctx":
                    memset("pool", XM[:, cc, 0:1], 0.0, [bXM[cc]])
                    memset("pool", XM[:, cc, 1 + ntok:2 + ntok], 0.0, [bXM[cc]])
                else:
                    cp("pool", XM[:, cc, 0:1], XMH[:, cc, 2 * gi:2 * gi + 1], [bXMH], [bXM[cc]])
                    cp("pool", XM[:, cc, 513:514], XMH[:, cc, 2 * gi + 1:2 * gi + 2], [bXMH], [bXM[cc]])
                yield
                A_, bA_ = ACC[cc % 2], bACC[cc % 2]
                ts("dve", A_[:, 0:ntok], XM[:, cc, 1:1 + ntok], CW[:, cc * 3 + 1:cc * 3 + 2], CW[:, 12 + cc:13 + cc], ALU.mult, ALU.add, [bXM[cc], bCW], [bA_])
                stt("dve", A_[:, 0:ntok], XM[:, cc, 0:ntok], CW[:, cc * 3:cc * 3 + 1], A_[:, 0:ntok], ALU.mult, ALU.add, [bXM[cc], bCW, bA_], [bA_])
                stt("dve", A_[:, 0:ntok], XM[:, cc, 2:2 + ntok], CW[:, cc * 3 + 2:cc * 3 + 3], A_[:, 0:ntok], ALU.mult, ALU.add, [bXM[cc], bCW, bA_], [bA_])
                act(att[:, cc, 0:ntok], A_[:, 0:ntok], AF.Silu, [bA_], [batt[cc]])
                yield
            if kind == "own" and gi == 0:
                dump("actT", att[:, :, 0:128], [128, 4, 128], batt)
            if kind != "ctx":
                for cc in range(4):
                    pb = 2 + cc % 2
                    for kc in range(8):
                        mm(PS[pb][:, :], WIN[:, kc, 1024 + cc * 128:1024 + (cc + 1) * 128], HT[:, kc, :], kc == 0, kc == 7, [bWIN] + bHTt, [PB[pb]])
                    act(UT[:, cc, :], PS[pb][:, :], AF.Identity, [PB[pb], bBCOL], [bUT], bias=BCOL[:, 4 + cc:5 + cc])
                    yield
                P.dma("sp", u_v[:, :, gi * 512:(gi + 1) * 512], UT[:, :, :], reads=[bUT], writes=[bUD])
            if kind == "own":
                for w, dstT, bdst in ((0, QT, bQT), (1, KT, bKT)):
                    for cc in range(4):
                        pb = 2 + cc % 2
                        mm(PS[pb][:, :], BDB[:, w, cc, :], att[:, cc, :], True, True, [bBDB, batt[cc]], [PB[pb]])
                        cp("act" if cc % 2 else "dve", dstT[:, cc, gi * 512:(gi + 1) * 512], PS[pb][:, :], [PB[pb]], [bdst])
                    yield
                for t_ in range(4):
                    i = gi * 4 + t_
                    sl = slice(t_ * 128, (t_ + 1) * 128)
                    pb = 2 + t_ % 2
                    for kc in range(8):
                        mm(PS[pb][:, :], HT[:, kc, sl], WIN[:, kc, 512:1024], kc == 0, kc == 7, bHTt + [bWIN], [PB[pb]])
                    tt("dve", ZT[:, :], PS[pb][:, :], BZ[:, :], ALU.add, [PB[pb], bBZ], [bZT])
                    s_ = stg_ctr[0] % 2
                    stg_ctr[0] += 1
                    act(STG[s_][:, :], ZT[:, :], AF.Silu, [bZT], [bSTG[s_]])
                    P.dma("sp", az_d.ap()[1, i * 128:(i + 1) * 128, :], STG[s_][:, :], reads=[bSTG[s_]], writes=[bAZ])
                    for cc in range(4):
                        tr(PT[:, cc, :], att[:, cc, sl], IDB[:, :], [batt[cc], bIDB], [PB[0]])
                    s_ = stg_ctr[0] % 2
                    stg_ctr[0] += 1
                    cp("dve", STG[s_][:, :].rearrange("p (c t) -> p c t", t=128), PT[:, 0:4, :], [PB[0]], [bSTG[s_]])
                    P.dma("sp", az_d.ap()[0, i * 128:(i + 1) * 128, :], STG[s_][:, :], reads=[bSTG[s_]], writes=[bAZ])
                    yield

        def back(gi, kind, par):
            n = 2 if kind == "ctx" else 4
            att, batt, xmb, bxmb = ACTT[par], bACTT[par], XMB[par], bXMB[par]
            for t_ in range(n):
                sl = slice(t_ * 128, (t_ + 1) * 128)
                if kind == "own":
                    i = gi * 4 + t_
                    ktm, bktm, va, bva = KTMO[:, i, :], bKTMO[i], VAO[:, i, :, :], bVAO[i]
                else:
                    ktm, bktm, va, bva = KTMG[:, t_, :], bKTMG[t_], VAG[:, t_, :, :], bVAG[t_]
                for cc in range(4):
                    mm(PS[4][:, cc * 128:(cc + 1) * 128], att[:, cc, sl], BDB[:, 1, cc, :], True, True, [batt[cc], bBDB], [PB[4]])
                cp("act", ktm, PS[4][:, :], [PB[4]], [bktm])
                yield
                for cc in range(4):
                    mm(PS[4][:, cc * 128:(cc + 1) * 128], xmb[:, cc, sl], BDB[:, 2, cc, :], True, True, [bxmb[cc], bBDB], [PB[4]])
                cp("act", va[:, :, 0:128], PS[4][:, :].rearrange("p (h d) -> p h d", d=128), [PB[4]], [bva])
                for cc in range(4):
                    mm(PSG[:, t_, :], att[:, cc, sl], AW[:, cc, 0:16], cc == 0, False, [batt[cc], bAW], [bPSG])
                for cc in range(4):
                    mm(PSG[:, t_, :], xmb[:, cc, sl], AW[:, cc, 16:32], False, cc == 3, [bxmb[cc], bAW], [bPSG])
                yield
            tt("dve", GT[:, 0:n, :], PSG[:, 0:n, :], BIF[:, None, :].broadcast_to([128, n, 16]), ALU.add, [bPSG, bBIF], [bGT])
            stt("dve", GW[:, 0:n, 0:8], GT[:, 0:n, 8:16], -1.0, GT[:, 0:n, 8:16], ALU.mult, ALU.max, [bGT], [bGW])
            yield
            act(GW[:, 0:n, 8:16], GW[:, 0:n, 0:8], AF.Exp, [bGW], [bGW], scale=-1.0)
            yield
            act(GW[:, 0:n, 16:24], GW[:, 0:n, 8:16], AF.Ln, [bGW], [bGW], bias=1.0)
            ts("dve", GW[:, 0:n, 24:32], GT[:, 0:n, 8:16], 0.0, None, ALU.min, None, [bGT], [bGW])
            yield
            tt("dve", GW[:, 0:n, 32:40], GW[:, 0:n, 24:32], GW[:, 0:n, 16:24], ALU.subtract, [bGW], [bGW])
            yield
            for t_ in range(n):
                mm(PSB[:, t_, 0:4], TRIU, GW[:, t_, 32:36], True, True, [bCONST, bGW], [bPSB])
                mm(PSB[:, t_, 4:8], TRIL, GW[:, t_, 36:40], True, True, [bCONST, bGW], [bPSB])
                mm(PSB[:, t_, 8:16], ONES, GW[:, t_, 32:40], True, True, [bCONST, bGW], [bPSB])
            yield
            if kind == "own":
                i0 = gi * 4
                if gi == 0:
                    dump("gt", GT[:, :, :], [128, 4, 16], [bGT])
                    dump("lf", GW[:, :, 32:40], [128, 4, 8], [bGW])
                tt("dve", EXI[:, :, 0:8], GT[:, :, 0:8], PSB[:, :, 0:8], ALU.subtract, [bGT, bPSB], [bEXI])
                yield
                act(OWNG[:, i0:i0 + 4, 0:8], EXI[:, :, 0:8], AF.Exp, [bEXI], [bOWNG])
                act(OWNG[:, i0:i0 + 4, 8:16], PSB[:, :, 0:8], AF.Exp, [bPSB], [bOWNG], bias=LN_QS)
                act(OWNG[:, i0:i0 + 4, 16:24], PSB[:, :, 8:16], AF.Exp, [bPSB], [bOWNG])
                yield
                return
            tt("dve", EXI[:, 0:n, 0:8], GT[:, 0:n, 0:8], PSB[:, 0:n, 8:16], ALU.add, [bGT, bPSB], [bEXI])
            tt("dve", EXI[:, 0:n, 0:8], EXI[:, 0:n, 0:8], PSB[:, 0:n, 0:8], ALU.subtract, [bEXI, bPSB], [bEXI])
            yield
            if kind == "ctx":
                cp("dve", EXI[:, 0:2, 8:16], PSB[:, 0:2, 8:16], [bPSB], [bEXI])
                yield
                act(EXO[:, 0:2, :], EXI[:, 0:2, :], AF.Exp, [bEXI], [bEXO])
                yield
                tt("dve", WV[:, 0, 0:4], EXO[:, 0, 0:4], EXO[:, 1, 8:12], ALU.mult, [bEXO], [bWV])
                cp("dve", WV[:, 1, 0:4], EXO[:, 1, 0:4], [bEXO], [bWV])
                cp("dve", WV[:, 0, 4:8], EXO[:, 0, 4:8], [bEXO], [bWV])
                tt("dve", WV[:, 1, 4:8], EXO[:, 1, 4:8], EXO[:, 0, 12:16], ALU.mult, [bEXO], [bWV])
                yield
            else:
                MF = TABS[:, 128 + 4 * gi:132 + 4 * gi]
                MB = TABS[:, 256 + 4 * gi:260 + 4 * gi]
                MF3 = MF[:, :, None].broadcast_to([128, 4, 4])
                MB3 = MB[:, :, None].broadcast_to([128, 4, 4])
                tt("dve", EXI[:, :, 8:12], PSB[:, :, 8:12], MF3, ALU.mult, [bPSB, bTABS], [bEXI])
                tt("dve", MBT[:, :, :], PSB[:, :, 12:16], MB3, ALU.mult, [bPSB, bTABS], [bMBT])
                yield
                for t_ in range(4):
                    tt("dve", PALL[:, t_ + 1, :], PALL[:, t_, :], MBT[:, t_, :], ALU.add, [bPALL, bMBT], [bPALL])
                    yield
                cp("dve", EXI[:, :, 12:16], PALL[:, 0:4, :], [bPALL], [bEXI])
                yield
                act(EXO[:, :, :], EXI[:, :, :], AF.Exp, [bEXI], [bEXO])
                yield
                tt("dve", WV[:, :, 0:4], EXO[:, :, 0:4], MF3, ALU.mult, [bEXO, bTABS], [bWV])
                tt("dve", WV[:, :, 4:8], EXO[:, :, 4:8], EXO[:, :, 12:16], ALU.mult, [bEXO], [bWV])
                yield
                tt("dve", WV[:, :, 4:8], WV[:, :, 4:8], MB3, ALU.mult, [bWV, bTABS], [bWV])
                cp("dve", PALL[:, 0, :], PALL[:, 4, :], [bPALL], [bPALL])
                yield
            for t_ in range(n):
                s_ = t_ % 2
                tt("pool", VS[s_][:, 0:4, :], VAG[:, t_, :, :], WV[:, t_, 0:4, None].broadcast_to([128, 4, 129]), ALU.mult, [bVAG[t_], bWV], [bVS[s_]])
                tt("pool", VS[s_][:, 4:8, :], VAG[:, t_, :, :], WV[:, t_, 4:8, None].broadcast_to([128, 4, 129]), ALU.mult, [bVAG[t_], bWV], [bVS[s_]])
                yield
                for h in range(4):
                    klhs = KTMG[:, t_, h * 128:(h + 1) * 128]
                    mm(DCF[h], klhs, VS[s_][:, h, :], True, True, [bKTMG[t_], bVS[s_]], [bDCF[h]])
                    mm(ACCB[h], klhs, VS[s_][:, 4 + h, :], True, True, [bKTMG[t_], bVS[s_]], [bACCB[h]])
                yield
                dcf3 = PS[5][:, 0:387].rearrange("p (h d) -> p h d", d=129)
                acb3 = PS[7][:, 0:387].rearrange("p (h d) -> p h d", d=129)
                if kind == "ctx":
                    if t_ == 0:
                        cp("dve", CF[:, 0:3, :], dcf3, [PB[5]], [bCF])
                        cp("dve", CF[:, 3, :], DCF[3], [PB[6]], [bCF])
                        cp("dve", CCB[:, 0:3, :], acb3, [PB[7]], [bCCB])
                        cp("dve", CCB[:, 3, :], ACCB[3], [PB[6]], [bCCB])
                    else:
                        tt("dve", CF[:, 0:3, :], CF[:, 0:3, :], dcf3, ALU.add, [bCF, PB[5]], [bCF])
                        tt("dve", CF[:, 3, :], CF[:, 3, :], DCF[3], ALU.add, [bCF, PB[6]], [bCF])
                        tt("dve", CCB[:, 0:3, :], CCB[:, 0:3, :], acb3, ALU.add, [bCCB, PB[7]], [bCCB])
                        tt("dve", CCB[:, 3, :], CCB[:, 3, :], ACCB[3], ALU.add, [bCCB, PB[6]], [bCCB])
                else:
                    tt("dve", CF[:, :, :], CF[:, :, :], EXO[:, t_, 8:12, None].broadcast_to([128, 4, 129]), ALU.mult, [bCF, bEXO], [bCF])
                    tt("dve", CF[:, 0:3, :], CF[:, 0:3, :], dcf3, ALU.add, [bCF, PB[5]], [bCF])
                    tt("dve", CF[:, 3, :], CF[:, 3, :], DCF[3], ALU.add, [bCF, PB[6]], [bCF])
                    tt("dve", CBS[:, 0:3, :], CBS[:, 0:3, :], acb3, ALU.add, [bCBS, PB[7]], [bCBS])
                    tt("dve", CBS[:, 3, :], CBS[:, 3, :], ACCB[3], ALU.add, [bCBS, PB[6]], [bCBS])
                yield

        seq = [("ctx", 0)] + [("own", g) for g in range(min(OWN // 4, cut))]
        if cut > 4:
            seq += [("oth", g) for g in range(OWN // 4, ngroups)]
        prev = None
        def interleave_w(gw):
            gw = list(gw)
            while gw:
                for item in list(gw):
                    g_, w_ = item
                    for _ in range(w_):
                        try:
                            next(g_)
                        except StopIteration:
                            gw.remove(item)
                            break

        for idx, (kind, gi) in enumerate(seq):
            gens = [(front(gi, kind, idx % 2), 1)]
            if prev is not None:
                gens.append((back(*prev), 2))
            interleave_w(gens)
            prev = (gi, kind, idx % 2)
        interleave([back(*prev)])
        dump("cf_ctx", CF[:, :, :], [128, 4, 129], [bCF])
        act(EXO[:, 0, 0:4], PALL[:, 0, :], AF.Exp, [bPALL], [bEXO])
        for h in range(4):
            if ngroups > OWN // 4 and cut > 4:
                stt("dve", CB[:, h, :], CCB[:, h, :], EXO[:, 0, h:h + 1], CBS[:, h, :], ALU.mult, ALU.add, [bCCB, bEXO, bCBS], [bCB])
            else:
                cp("dve", CB[:, h, :], CCB[:, h, :], [bCCB], [bCB])
        dump("cf_in", CF[:, :, :], [128, 4, 129], [bCF])
        dump("cb_in", CB[:, :, :], [128, 4, 129], [bCB])
        dump("ktm0", KTMO[:, 0, :], [128, 512], [bKTMO[0]])
        dump("va0", VAO[:, 0, :, :], [128, 4, 129], [bVAO[0]])
        dump("owng", OWNG[:, :, :], [128, OWN, 24], [bOWNG])
        dump("qT", QT[:, :, 0:128], [128, 4, 128], [bQT])

        if stage <= 2:
            o_b = Buf("out")
            P.barrier()
            for i in range(OWN):
                t = P.dma("sp", out_d.ap()[i * 128:(i + 1) * 128, 0:512], POSC[:, 0:512], reads=[bPOSC], writes=[o_b])
                final.append((t[0], t[1]))
            P.barrier()
            with nc.Block() as block:
                P.emit(block, final)
            sts.close()
            stm.close()
            return nc

        P.barrier()
        sts.close()
        st6 = ExitStack()

        def sb6(name, shape, dt=F32):
            return st6.enter_context(nc.sbuf_tensor(name, list(shape), dt))

        HD = [sb6("HD%d" % i, [128, OWN, 512], BF16) for i in range(2)]
        bHD = [[Buf("HD%d_%d" % (i, c)) for c in range(OWN)] for i in range(2)]
        HS = [sb6("HS%d" % i, [128, 512]) for i in range(2)]
        bHS = [Buf("HS%d" % i) for i in range(2)]
        SM = [sb6("SM%d" % i, [128, 128], BF16) for i in range(8)]
        bSM = [Buf("SM%d" % i) for i in range(8)]
        VP = [sb6("VP%d" % i, [128, 129], BF16) for i in range(8)]
        bVP = [Buf("VP%d" % i) for i in range(8)]
        CSB = sb6("CSB", [128, 8, 129], BF16)
        bCSB = [Buf("CSB%d" % i) for i in range(8)]
        RD = [sb6("RD%d" % i, [128, 8]) for i in range(8)]
        bRD = [Buf("RD%d" % i) for i in range(8)]
        bST = [Buf("ST%d" % i) for i in range(8)]
        bHSD = Buf("hs_d")

        def chain(d, h):
            q = d * 4 + h
            ST = CF if d == 0 else CB
            bsrc = bCF if d == 0 else bCB
            hsl = slice(h * 128, (h + 1) * 128)
            cp("act", CSB[:, q, :], ST[:, h, :], [bsrc, bST[q]], [bCSB[q], bST[q]])
            yield
            order = range(OWN) if d == 0 else range(OWN - 1, -1, -1)
            for c in order:
                tsl = slice(c * 128, (c + 1) * 128)
                pS, pN, pC = PS[q][:, 0:128], PS[q][:, 128:257], PS[q][:, 257:386]
                mm(pS, KT[:, h, tsl], QT[:, h, tsl], True, True, [bKT, bQT], [PB[q]])
                ts("pool", VP[q][:, :], VAO[:, c, h, :], OWNG[:, c, q:q + 1], None, ALU.mult, None, [bVAO[c], bOWNG], [bVP[q]])
                yield
                tt("dve", SM[q][:, :], pS, TRIU if d == 0 else TRIL, ALU.mult, [PB[q], bCONST], [bSM[q]])
                yield
                mm(pN, SM[q][:, :], VP[q][:, :], True, False, [bSM[q], bVP[q]], [PB[q]])
                mm(pN, QT[:, h, tsl], CSB[:, q, :], False, True, [bQT, bCSB[q]], [PB[q]])
                mm(pC, KTMO[:, c, hsl], VP[q][:, :], True, True, [bKTMO[c], bVP[q]], [PB[q]])
                yield
                eq = OWNG[:, c, 8 + q:9 + q]
                r = RD[q]
                ts("dve", r[:, 0:1], PS[q][:, 256:257], eq, None, ALU.mult, None, [PB[q], bOWNG], [bRD[q]])
                stt("dve", r[:, 1:2], r[:, 0:1], -1.0, r[:, 0:1], ALU.mult, ALU.max, [bRD[q]], [bRD[q]])
                yield
                ts("dve", r[:, 2:3], r[:, 1:2], 1.0, None, ALU.max, None, [bRD[q]], [bRD[q]])
                P.op("dve", lambda e, o=r[:, 3:4], i_=r[:, 2:3]: e.reciprocal(out=o, in_=i_), [bRD[q]], [bRD[q]])
                yield
                tt("dve", r[:, 4:5], r[:, 3:4], eq, ALU.mult, [bRD[q], bOWNG], [bRD[q]])
                tt("dve", ST[:, h, :], ST[:, h, :], pC, ALU.add, [bST[q], PB[q]], [bST[q]])
                yield
                act(HD[d][:, c, hsl], PS[q][:, 128:256], AF.Copy, [PB[q], bRD[q]], [bHD[d][c]], scale=r[:, 4:5])
                ts("dve", ST[:, h, :], ST[:, h, :], OWNG[:, c, 16 + q:17 + q], None, ALU.mult, None, [bST[q], bOWNG], [bST[q]])
                yield
                cp("act", CSB[:, q, :], ST[:, h, :], [bST[q]], [bCSB[q]])
                yield

        interleave([chain(d, h) for d in range(2) for h in range(4)])
        for c in range(OWN):
            tt("pool", HS[c % 2][:, :], HD[0][:, c, :], HD[1][:, c, :], ALU.add, [bHD[0][c], bHD[1][c]], [bHS[c % 2]])
            P.dma("sp", hs_d.ap()[c * 128:(c + 1) * 128, :], HS[c % 2][:, :], reads=[bHS[c % 2]], writes=[bHSD])
            if c in (0, 7, 15):
                dump("hs%d" % c, HS[c % 2][:, :], [128, 512], [bHS[c % 2]])

        if stage <= 3:
            o_b = Buf("out")
            P.barrier()
            for i in range(OWN):
                t = P.dma("sp", out_d.ap()[i * 128:(i + 1) * 128, 0:512], POSC[:, 0:512], reads=[bPOSC], writes=[o_b])
                final.append((t[0], t[1]))
            P.barrier()
            with nc.Block() as block:
                P.emit(block, final)
            st6.close()
            stm.close()
            return nc

        P.barrier()
        st6.close()
        stm.close()

        H2T = sb("H2T", [128, 8, OWN * 128], BF16)
        bH2T = Buf("H2T")
        COMB = sb("COMB", [128, OWN, 16])
        bCOMB = Buf("COMB")
        st7 = ExitStack()

        def sb7(name, shape, dt=F32):
            return st7.enter_context(nc.sbuf_tensor(name, list(shape), dt))

        XCS = sb7("XCS", [128, 512, 2, 16], BF16)
        bXCS = Buf("XCS")
        CS128 = sb7("CS128", [128, 384], BF16)
        bCS = Buf("CS128")
        DI = sb7("DI", [128, 640])
        bDI = Buf("DI")
        P.dma("sp", DI[:, :], dftidx.ap(), writes=[bDI])
        CSF = sb7("CSF", [128, 384])
        bCSF = Buf("CSF")
        act(CSF[:, 0:256], DI[:, 0:256], AF.Sin, [bDI], [bCSF], scale=TWO_PI / 128.0)
        ts("dve", CSF[:, 256:384], CSF[:, 128:256], -1.0, None, ALU.mult, None, [bCSF], [bCSF])
        cp("dve", CS128[:, :], CSF[:, :], [bCSF], [bCS])
        CMY = sb7("CMY", [128, 48], BF16)
        bCMY = Buf("CMY")
        act(CSF[:, 0:32], DI[:, 512:544], AF.Sin, [bDI, bCS], [bCSF], scale=TWO_PI / 128.0)
        ts("dve", CSF[:, 32:48], CSF[:, 16:32], -1.0, None, ALU.mult, None, [bCSF], [bCSF])
        cp("dve", CMY[:, :], CSF[:, 0:48], [bCSF], [bCMY])
        with ExitStack() as st8:
            def sb8(name, shape, dt=F32):
                return st8.enter_context(nc.sbuf_tensor(name, list(shape), dt))
            TW = sb8("TW", [128, 256], BF16)
            bTW = Buf("TW")
            TWF = sb8("TWF", [128, 256])
            bTWF = Buf("TWF")
            act(TWF[:, :], DI[:, 256:512], AF.Sin, [bDI], [bTWF], scale=TWO_PI / 16384.0)
            ts("dve", TW[:, :], TWF[:, :], 1.0 / math.sqrt(16384.0 * 128.0), None, ALU.mult, None, [bTWF], [bTW])
            TC3 = TW[:, None, 0:128].broadcast_to([128, 8, 128])
            TS3 = TW[:, None, 128:256].broadcast_to([128, 8, 128])
            NW_ = 3
            UL = [sb8("UL%d" % i, [128, 16, 128], BF16) for i in range(3)]
            bUL = [Buf("UL%d" % i) for i in range(3)]
            YS = [sb8("YS%d" % i, [128, 8, 256], BF16) for i in range(NW_)]
            bYS = [Buf("YS%d" % i) for i in range(NW_)]
            MT_ = [[sb8("MTW%d_%d" % (j, i), [128, 8, 128], BF16) for i in range(4)] for j in range(NW_)]
            bMT_ = [[Buf("MTW%d_%d" % (j, i)) for i in range(4)] for j in range(NW_)]
            PQ = [sb8("PQ%d" % i, [128, 2, 8, 128], BF16) for i in range(NW_)]
            bPQ = [Buf("PQ%d" % i) for i in range(NW_)]

            def fft_half(hb):
                ub, half = hb // 2, hb % 2
                u_, bu_ = UL[ub % 3], bUL[ub % 3]
                w_ = hb % NW_
                if half == 0:
                    P.dma("sp", u_[:, :, :], bass.AP(u_d, ub * 16 * T, [[128, 128], [T, 16], [1, 128]]), reads=[bUD], writes=[bu_])
                    yield
                y_, by_ = YS[w_], bYS[w_]
                for pr in range(4):
                    pb = 1 + (hb * 4 + pr) % 4
                    for cc in range(2):
                        chl = half * 8 + pr * 2 + cc
                        mm(PS[pb][:, cc * 256:(cc + 1) * 256], u_[:, chl, :], CS128[:, 0:256], True, True, [bu_, bCS], [PB[pb]])
                    cp("act", y_[:, pr * 2:pr * 2 + 2, :], PS[pb][:, :].rearrange("p (c n) -> p c n", n=256), [PB[pb]], [by_])
                    yield
                yr = y_[:, :, 0:128]
                ys_ = y_[:, :, 128:256]
                pq, bpq = PQ[w_], bPQ[w_]
                m_, bm_ = MT_[w_], bMT_[w_]
                tt("dve", m_[0][:, :, :], yr, TC3, ALU.mult, [by_, bTW], [bm_[0]])
                tt("pool", m_[2][:, :, :], yr, TS3, ALU.mult, [by_, bTW], [bm_[2]])
                yield
                tt("dve", m_[1][:, :, :], ys_, TS3, ALU.mult, [by_, bTW], [bm_[1]])
                tt("pool", m_[3][:, :, :], ys_, TC3, ALU.mult, [by_, bTW], [bm_[3]])
                yield
                tt("dve", pq[:, 0, :, :], m_[0][:, :, :], m_[1][:, :, :], ALU.subtract, [bm_[0], bm_[1]], [bpq])
                yield
                tt("dve", pq[:, 1, :, :], m_[2][:, :, :], m_[3][:, :, :], ALU.add, [bm_[2], bm_[3]], [bpq])
                yield
                pb = 5 + hb % 3
                for cc in range(8):
                    o_c = PS[pb][:, cc * 32:cc * 32 + 16]
                    o_s = PS[pb][:, cc * 32 + 16:cc * 32 + 32]
                    mm(o_c, pq[:, 0, cc, :], CMY[:, 0:16], True, False, [bpq, bCMY], [PB[pb]])
                    mm(o_c, pq[:, 1, cc, :], CMY[:, 32:48], False, True, [bpq, bCMY], [PB[pb]])
                    mm(o_s, pq[:, 0, cc, :], CMY[:, 16:32], True, False, [bpq, bCMY], [PB[pb]])
                    mm(o_s, pq[:, 1, cc, :], CMY[:, 0:16], False, True, [bpq, bCMY], [PB[pb]])
                    if cc % 4 == 3:
                        yield
                cp("act", XCS[:, hb * 8:hb * 8 + 8, :, :], PS[pb][:, 0:256].rearrange("p (c s k) -> p c s k", s=2, k=16), [PB[pb]], [bXCS])
                yield

            def window(genfs, w):
                pend = list(genfs)
                act_ = []
                while pend or act_:
                    while pend and len(act_) < w:
                        act_.append(pend.pop(0)())
                    for g_ in list(act_):
                        try:
                            next(g_)
                        except StopIteration:
                            act_.remove(g_)

            window([(lambda hb=hb: fft_half(hb)) for hb in range(64)], NW_)
            P.barrier()
        dump("xcs", XCS[:, :, :, :], [128, 512, 2, 16], [bXCS])

        if stage <= 4:
            o_b = Buf("out")
            P.barrier()
            for i in range(OWN):
                t = P.dma("sp", out_d.ap()[i * 128:(i + 1) * 128, 0:512], POSC[:, 0:512], reads=[bPOSC], writes=[o_b])
                final.append((t[0], t[1]))
            P.barrier()
            with nc.Block() as block:
                P.emit(block, final)
            st7.close()
            return nc

        st9 = ExitStack()

        def sb9(name, shape, dt=F32):
            return st9.enter_context(nc.sbuf_tensor(name, list(shape), dt))

        WOB = sb9("WOB", [128, 8, D], BF16)
        bWOB = Buf("WOB")
        P.dma("pool", WOB[:, :, :], w_out.ap().rearrange("(k p) n -> p k n", p=128), writes=[bWOB])
        WFB = sb9("WFB", [128, 4, 128], BF16)
        bWFB = Buf("WFB")
        P.dma("pool", WFB[:, :, :], w_fourier.ap().rearrange("g c d -> c g d"), writes=[bWFB])
        NWSK = sb9("NWSK", [128, 2, 512])
        bNWSK = Buf("NWSK")
        for i in range(2):
            P.dma("sp", NWSK[:, i, :], bc_rows(nrm_skip, i, 512), writes=[bNWSK])
        WRF = sb9("WRF", [128, 8, 20])
        bWRF = Buf("WRF")
        P.dma("sp", WRF[:, :, :], w_router.ap().rearrange("(k p) n -> p k n", p=128), writes=[bWRF])
        WRH = sb9("WRH", [128, 8, 20], BF16)
        WRL = sb9("WRL", [128, 8, 20], BF16)
        bWR = Buf("WR")
        cp("dve", WRH[:, :, :], WRF[:, :, :], [bWRF], [bWR])
        tt("dve", WRL[:, :, :], WRF[:, :, :], WRH[:, :, :], ALU.subtract, [bWRF, bWR], [bWR])
        MODL = sb9("MODL", [128, 3, D])
        bMODL = Buf("MODL")
        for i_, off_ in enumerate((2 * D, 3 * D, 4 * D)):
            P.dma("sp", MODL[:, i_, :], bc_rows(mod_d, 0, D, off=off_), reads=[bMODD], writes=[bMODL])
        GP1, S2, G2 = MODL[:, 0, :], MODL[:, 1, :], MODL[:, 2, :]
        bMOD = bMODL
        BR = sb9("BR", [128, 20])
        bBR = Buf("BR")
        P.dma("sp", BR[:, :], bc_rows(b_router, 0, 20), writes=[bBR])
        HSt = [sb9("HSt%d" % i, [128, 512]) for i in range(2)]
        bHSt = [Buf("HSt%d" % i) for i in range(2)]
        AZ = [sb9("AZ%d" % i, [128, 2, 512], BF16) for i in range(2)]
        bAZ_ = [Buf("AZ%d" % i) for i in range(2)]
        SM__2 = [sb9("SM__%d" % i, [128, 32]) for i in range(2)]
        bSM__2 = [Buf("SM__%d" % i) for i in range(2)]
        CEN_2 = [sb9("CEN_%d" % i, [128, 512]) for i in range(2)]
        bCEN_2 = [Buf("CEN_%d" % i) for i in range(2)]
        SQ_2 = [sb9("SQ_%d" % i, [128, 512]) for i in range(2)]
        bSQ_2 = [Buf("SQ_%d" % i) for i in range(2)]
        T1_2 = [sb9("T1_%d" % i, [128, 512]) for i in range(2)]
        bT1_2 = [Buf("T1_%d" % i) for i in range(2)]
        T2_2 = [sb9("T2_%d" % i, [128, 512]) for i in range(2)]
        bT2_2 = [Buf("T2_%d" % i) for i in range(2)]
        MBF_2 = [sb9("MBF_%d" % i, [128, 512], BF16) for i in range(2)]
        bMBF_2 = [Buf("MBF_%d" % i) for i in range(2)]
        MTt_2 = [sb9("MTt_%d" % i, [128, 4, 128], BF16) for i in range(2)]
        bMTt_2 = [Buf("MTt_%d" % i) for i in range(2)]
        XT_2 = [sb9("XT_%d" % i, [128, 8, 128], BF16) for i in range(2)]
        bXT_2 = [Buf("XT_%d" % i) for i in range(2)]
        FTB_2 = [sb9("FTB_%d" % i, [128, 4, 128], BF16) for i in range(2)]
        bFTB_2 = [Buf("FTB_%d" % i) for i in range(2)]
        YFT_2 = [sb9("YFT_%d" % i, [128, 4, 128], BF16) for i in range(2)]
        bYFT_2 = [Buf("YFT_%d" % i) for i in range(2)]
        JK_2 = [sb9("JK_%d" % i, [128, D], BF16) for i in range(2)]
        bJK_2 = [Buf("JK_%d" % i) for i in range(2)]
        SY_2 = [sb9("SY_%d" % i, [128, 8]) for i in range(2)]
        bSY_2 = [Buf("SY_%d" % i) for i in range(2)]
        TT__2 = [sb9("TT__%d" % i, [128, D]) for i in range(2)]
        bTT_2 = [Buf("TT__%d" % i) for i in range(2)]
        H2_2 = [sb9("H2_%d" % i, [128, D]) for i in range(2)]
        bH2_2 = [Buf("H2_%d" % i) for i in range(2)]
        H2H_2 = [sb9("H2H_%d" % i, [128, D], BF16) for i in range(2)]
        bH2H_2 = [Buf("H2H_%d" % i) for i in range(2)]
        H2Lw_2 = [sb9("H2Lw_%d" % i, [128, D], BF16) for i in range(2)]
        bH2Lw_2 = [Buf("H2Lw_%d" % i) for i in range(2)]
        H2LT_2 = [sb9("H2LT_%d" % i, [128, 8, 128], BF16) for i in range(2)]
        bH2LT_2 = [Buf("H2LT_%d" % i) for i in range(2)]
        LG_2 = [sb9("LG_%d" % i, [128, 20]) for i in range(2)]
        bLG_2 = [Buf("LG_%d" % i) for i in range(2)]
        RT_2 = [sb9("RT_%d" % i, [128, 96]) for i in range(2)]
        bRT_2 = [Buf("RT_%d" % i) for i in range(2)]
        XR = [sb9("XR%d" % i, [128, D]) for i in range(2)]
        bXR = [Buf("XR%d" % i) for i in range(2)]
        PRr = [sb9("PRr%d" % i, [128, 512]) for i in range(2)]
        bPRr = [Buf("PRr%d" % i) for i in range(2)]
        X1 = [sb9("X1%d" % i, [128, D]) for i in range(2)]
        bX1 = [Buf("X1%d" % i) for i in range(2)]
        bOUT = Buf("out_d")
        BIG = 30000.0
        AX = mybir.AxisListType.X

        def red(eng, out, in_, op, reads, writes):
            return P.op(eng, lambda e: e.tensor_reduce(out=out, in_=in_, axis=AX, op=op), reads, writes)

        def s6_tile(c):
            s_ = c % 2
            rows = slice(c * 128, (c + 1) * 128)
            bk = (0, 1, 2, 3) if s_ == 0 else (4, 5, 6, 7)
            PTl = PS[bk[0]][:, :].bitcast(BF16).rearrange("p (k t) -> p k t", t=128)
            SM_, bSM_ = SM__2[s_], bSM__2[s_]
            CEN, bCEN = CEN_2[s_], bCEN_2[s_]
            SQ, bSQ = SQ_2[s_], bSQ_2[s_]
            T1, bT1 = T1_2[s_], bT1_2[s_]
            T2, bT2 = T2_2[s_], bT2_2[s_]
            MBF, bMBF = MBF_2[s_], bMBF_2[s_]
            MTt, bMTt = MTt_2[s_], bMTt_2[s_]
            XT, bXT = XT_2[s_], bXT_2[s_]
            FTB, bFTB = FTB_2[s_], bFTB_2[s_]
            YFT, bYFT = YFT_2[s_], bYFT_2[s_]
            JK, bJK = JK_2[s_], bJK_2[s_]
            SY, bSY = SY_2[s_], bSY_2[s_]
            TT_, bTT = TT__2[s_], bTT_2[s_]
            H2, bH2 = H2_2[s_], bH2_2[s_]
            H2H, bH2H = H2H_2[s_], bH2H_2[s_]
            H2Lw, bH2Lw = H2Lw_2[s_], bH2Lw_2[s_]
            H2LT, bH2LT = H2LT_2[s_], bH2LT_2[s_]
            LG, bLG = LG_2[s_], bLG_2[s_]
            RT, bRT = RT_2[s_], bRT_2[s_]
            hs, bhs, az, baz = HSt[s_], bHSt[s_], AZ[s_], bAZ_[s_]
            P.dma("sp", hs[:, :], hs_d.ap()[rows, :], reads=[bHSD], writes=[bhs])
            P.dma("sp", az[:, 0, :], az_d.ap()[0, rows, :], reads=[bAZ], writes=[baz])
            P.dma("sp", az[:, 1, :], az_d.ap()[1, rows, :], reads=[bAZ], writes=[baz])
            hs3 = hs[:, :].rearrange("p (h d) -> p h d", d=128)
            cen3 = CEN[:, :].rearrange("p (h d) -> p h d", d=128)
            sq3 = SQ[:, :].rearrange("p (h d) -> p h d", d=128)
            yield
            red("dve", SM_[:, 0:4], hs3, ALU.add, [bhs], [bSM_])
            ts("dve", SM_[:, 4:8], SM_[:, 0:4], 1.0 / 128.0, None, ALU.mult, None, [bSM_], [bSM_])
            tt("dve", cen3, hs3, SM_[:, 4:8, None].broadcast_to([128, 4, 128]), ALU.subtract, [bhs, bSM_], [bCEN])
            yield
            tt("pool", SQ[:, :], CEN[:, :], CEN[:, :], ALU.mult, [bCEN], [bSQ])
            red("dve", SM_[:, 8:12], sq3, ALU.add, [bSQ], [bSM_])
            yield
            ts("pool", SM_[:, 12:16], SM_[:, 8:12], 1.0 / 128.0, EPS, ALU.mult, ALU.add, [bSM_], [bSM_])
            tt("pool", SM_[:, 16:20], SM_[:, 12:16], NEGH.broadcast_to([128, 4]), ALU.pow, [bSM_, bTABS], [bSM_])
            tt("dve", cen3, cen3, SM_[:, 16:20, None].broadcast_to([128, 4, 128]), ALU.mult, [bCEN, bSM_], [bCEN])
            yield
            tt("dve", T1[:, :], CEN[:, :], NWSK[:, 0, :], ALU.mult, [bCEN, bNWSK], [bT1])
            tt("pool", T2[:, :], az[:, 0, :], NWSK[:, 1, :], ALU.mult, [baz, bNWSK], [bT2])
            tt("dve", T1[:, :], T1[:, :], T2[:, :], ALU.add, [bT1, bT2], [bT1])
            yield
            tt("dve", MBF[:, :], T1[:, :], az[:, 1, :], ALU.mult, [bT1, baz], [bMBF])
            if c == 0:
                dump("m0", MBF[:, :], [128, 512], [bMBF])
            yield
            for cc in range(4):
                tr(PTl[:, cc, :], MBF[:, cc * 128:(cc + 1) * 128], IDB[:, :], [bMBF, bIDB], [PB[bk[0]]])
            cp("act", MTt[:, :, :], PTl[:, 0:4, :], [PB[bk[0]]], [bMTt])
            yield
            for sg in range(2):
                for g in range(4):
                    tr(PTl[:, sg * 4 + g, :], XCS[:, g * 128:(g + 1) * 128, sg, c], IDB[:, :], [bXCS, bIDB], [PB[bk[0]]])
            cp("act", XT[:, :, :], PTl, [PB[bk[0]]], [bXT])
            yield
            for g in range(4):
                mm(PS[bk[1]][:, g * 128:(g + 1) * 128], CS128[:, 0:128], XT[:, g, :], True, False, [bCS, bXT], [PB[bk[1]]])
                mm(PS[bk[1]][:, g * 128:(g + 1) * 128], CS128[:, 256:384], XT[:, 4 + g, :], False, True, [bCS, bXT], [PB[bk[1]]])
            cp("dve", FTB[:, :, :], PS[bk[1]][:, :].rearrange("p (g t) -> p g t", t=128), [PB[bk[1]]], [bFTB])
            yield
            for g in range(4):
                mm(PS[bk[1]][:, g * 128:(g + 1) * 128], WFB[:, g, :], FTB[:, g, :], True, True, [bWFB, bFTB], [PB[bk[1]]])
            cp("act", YFT[:, :, :], PS[bk[1]][:, :].rearrange("p (g t) -> p g t", t=128), [PB[bk[1]]], [bYFT])
            yield
            for cb in range(2):
                for kc in range(4):
                    mm(PS[bk[2 + cb]][:, :], MTt[:, kc, :], WOB[:, kc, cb * 512:(cb + 1) * 512], kc == 0, False, [bMTt, bWOB], [PB[bk[2 + cb]]])
                for kc in range(4):
                    mm(PS[bk[2 + cb]][:, :], YFT[:, kc, :], WOB[:, 4 + kc, cb * 512:(cb + 1) * 512], False, kc == 3, [bYFT, bWOB], [PB[bk[2 + cb]]])
            yield
            act(JK[:, 0:512], PS[bk[2]][:, :], AF.Square, [PB[bk[2]]], [bJK, bSY], accum_out=SY[:, 0:1])
            act(JK[:, 512:1024], PS[bk[3]][:, :], AF.Square, [PB[bk[3]]], [bJK, bSY], accum_out=SY[:, 1:2])
            yield
            tt("pool", SY[:, 2:3], SY[:, 0:1], SY[:, 1:2], ALU.add, [bSY], [bSY])
            ts("pool", SY[:, 3:4], SY[:, 2:3], 1.0 / D, EPS, ALU.mult, ALU.add, [bSY], [bSY])
            tt("pool", SY[:, 4:5], SY[:, 3:4], NEGH, ALU.pow, [bSY, bTABS], [bSY])
            yield
            xr, bxr, pr_, bpr_ = XR[s_], bXR[s_], PRr[s_], bPRr[s_]
            P.dma("sp", xr[:, :], x_rot.ap()[rows, :], writes=[bxr])
            P.dma("sp", pr_[0:64, :], bc_rows(posr_d, 2 * c, 512, parts=64), reads=[bPOSRD], writes=[bpr_])
            P.dma("sp", pr_[64:128, :], bc_rows(posr_d, 2 * c + 1, 512, parts=64), reads=[bPOSRD], writes=[bpr_])
            tt("pool", xr[:, 0:512], xr[:, 0:512], pr_[:, :], ALU.add, [bxr, bpr_], [bxr])
            tt("pool", xr[:, 512:1024], xr[:, 512:1024], POSC[:, :], ALU.add, [bxr, bPOSC], [bxr])
            yield
            stt("dve", TT_[:, 0:512], PS[bk[2]][:, :], SY[:, 4:5], GP1[:, 0:512], ALU.mult, ALU.mult, [PB[bk[2]], bSY, bMOD], [bTT])
            stt("dve", TT_[:, 512:1024], PS[bk[3]][:, :], SY[:, 4:5], GP1[:, 512:1024], ALU.mult, ALU.mult, [PB[bk[3]], bSY, bMOD], [bTT])
            yield
            x1, bx1 = X1[s_], bX1[s_]
            tt("pool", x1[:, :], TT_[:, :], xr[:, :], ALU.add, [bTT, bxr], [bx1])
            P.dma("sp", out_d.ap()[rows, :], x1[:, :], reads=[bx1], writes=[bOUT])
            if c == 0:
                dump("x1_0", x1[:, :], [128, D], [bx1])
            yield
            act(JK[:, :], x1[:, :], AF.Square, [bx1], [bJK, bSY], accum_out=SY[:, 5:6])
            yield
            ts("pool", SY[:, 6:7], SY[:, 5:6], 1.0 / D, EPS, ALU.mult, ALU.add, [bSY], [bSY])
            tt("pool", SY[:, 7:8], SY[:, 6:7], NEGH, ALU.pow, [bSY, bTABS], [bSY])
            yield
            stt("dve", TT_[:, :], x1[:, :], SY[:, 7:8], G2, ALU.mult, ALU.mult, [bx1, bSY, bMOD], [bTT])
            tt("pool", H2[:, :], TT_[:, :], S2, ALU.add, [bTT, bMOD], [bH2])
            yield
            cp("act", H2H[:, :], H2[:, :], [bH2], [bH2H])
            tt("dve", H2Lw[:, :], H2[:, :], H2H[:, :], ALU.subtract, [bH2, bH2H], [bH2Lw])
            yield
            for kc in range(8):
                tr(PTl[:, kc, :], H2H[:, kc * 128:(kc + 1) * 128], IDB[:, :], [bH2H, bIDB], [PB[bk[0]]])
            cp("act", H2T[:, :, rows], PTl, [PB[bk[0]]], [bH2T])
            yield
            for kc in range(8):
                tr(PTl[:, kc, :], H2Lw[:, kc * 128:(kc + 1) * 128], IDB[:, :], [bH2Lw, bIDB], [PB[bk[0]]])
            cp("dve", H2LT[:, :, :], PTl, [PB[bk[0]]], [bH2LT])
            yield
            LGp = PS[bk[1]][:, 0:20]
            for kc in range(8):
                mm(LGp, H2T[:, kc, rows], WRH[:, kc, :], kc == 0, False, [bH2T, bWR], [PB[bk[1]]])
            for kc in range(8):
                mm(LGp, H2T[:, kc, rows], WRL[:, kc, :], False, False, [bH2T, bWR], [PB[bk[1]]])
            for kc in range(8):
                mm(LGp, H2LT[:, kc, :], WRH[:, kc, :], False, kc == 7, [bH2LT, bWR], [PB[bk[1]]])
            yield
            tt("dve", LG[:, :], LGp, BR[:, :], ALU.add, [PB[bk[1]], bBR], [bLG])
            if c == 0:
                dump("lg0", LG[:, :], [128, 20], [bLG])
            R = RT
            bR = bRT
            yield
            red("dve", R[:, 0:1], LG[:, 0:4], ALU.max, [bLG], [bR])
            ts("dve", R[:, 1:5], LG[:, 0:4], R[:, 0:1], None, ALU.is_equal, None, [bLG, bR], [bR])
            ts("dve", R[:, 5:6], R[:, 0:1], -1.0, None, ALU.mult, None, [bR], [bR])
            yield
            act(R[:, 6:10], LG[:, 0:4], AF.Exp, [bLG, bR], [bR], bias=R[:, 5:6], accum_out=R[:, 10:11])
            P.op("dve", lambda e, o=R[:, 11:12], i_=R[:, 10:11]: e.reciprocal(out=o, in_=i_), [bR], [bR])
            yield
            ts("dve", R[:, 12:16], R[:, 1:5], BIG, -BIG, ALU.mult, ALU.add, [bR], [bR])
            em = R[:, 16:32]
            tt("dve", em.rearrange("p (g j) -> p g j", j=4), LG[:, 4:20].rearrange("p (g j) -> p g j", j=4),
               R[:, 12:16, None].broadcast_to([128, 4, 4]), ALU.add, [bLG, bR], [bR])
            yield
            red("dve", R[:, 32:33], em, ALU.max, [bR], [bR])
            ts("dve", R[:, 48:64], em, R[:, 32:33], None, ALU.is_equal, None, [bR], [bR])
            stt("dve", R[:, 64:80], R[:, 48:64], -BIG, em, ALU.mult, ALU.add, [bR], [bR])
            yield
            red("dve", R[:, 33:34], R[:, 64:80], ALU.max, [bR], [bR])
            ts("dve", R[:, 80:96], R[:, 64:80], R[:, 33:34], None, ALU.is_equal, None, [bR], [bR])
            tt("dve", R[:, 34:35], R[:, 33:34], R[:, 32:33], ALU.subtract, [bR], [bR])
            yield
            act(R[:, 35:36], R[:, 34:35], AF.Exp, [bR], [bR])
            ts("dve", R[:, 36:37], R[:, 35:36], 1.0, None, ALU.add, None, [bR], [bR])
            P.op("dve", lambda e, o=R[:, 37:38], i_=R[:, 36:37]: e.reciprocal(out=o, in_=i_), [bR], [bR])
            tt("dve", R[:, 38:39], R[:, 37:38], R[:, 35:36], ALU.mult, [bR], [bR])
            yield
            tt("dve", R[:, 39:40], R[:, 37:38], R[:, 11:12], ALU.mult, [bR], [bR])
            tt("dve", R[:, 40:41], R[:, 38:39], R[:, 11:12], ALU.mult, [bR], [bR])
            ts("dve", COMB[:, c, :], R[:, 48:64], R[:, 39:40], None, ALU.mult, None, [bR], [bCOMB])
            stt("dve", COMB[:, c, :], R[:, 80:96], R[:, 40:41], COMB[:, c, :], ALU.mult, ALU.add, [bR, bCOMB], [bCOMB])
            yield

        def window6(genfs, w):
            pend = list(genfs)
            act_ = []
            while pend or act_:
                while pend and len(act_) < w:
                    act_.append(pend.pop(0)())
                for g_ in list(act_):
                    try:
                        next(g_)
                    except StopIteration:
                        act_.remove(g_)

        window6([(lambda c=c: s6_tile(c)) for c in range(OWN)], 2)
        dump("comb", COMB[:, :, :], [128, OWN, 16], [bCOMB])

        if stage <= 5:
            o_b = bOUT
            P.barrier()
            final.append((P.sem["sp"], 0))
            P.barrier()
            with nc.Block() as block:
                P.emit(block, [])
            st9.close()
            st7.close()
            return nc

        P.barrier()
        st9.close()
        st7.close()
        stE = ExitStack()

        def sbE(name, shape, dt=F32):
            return stE.enter_context(nc.sbuf_tensor(name, list(shape), dt))

        YACC = sbE("YACC", [128, OWN, D])
        bYACC = [Buf("YACC%d" % i) for i in range(OWN)]
        WG = [sbE("WG%d" % i, [128, 8, 512], BF16) for i in range(2)]
        WU = [sbE("WU%d" % i, [128, 8, 512], BF16) for i in range(2)]
        WD = [sbE("WD%d" % i, [128, 4, D], BF16) for i in range(2)]
        bWG = [Buf("WG%d" % i) for i in range(2)]
        bWU = [Buf("WU%d" % i) for i in range(2)]
        bWD = [Buf("WD%d" % i) for i in range(2)]
        ATb = [sbE("AT%d" % i, [128, 4, 512], BF16) for i in range(2)]
        bAT = [Buf("AT%d" % i) for i in range(2)]
        SG = [sbE("SG%d" % i, [128, 512]) for i in range(2)]
        bSG = [Buf("SG%d" % i) for i in range(2)]
        NEXP = 16
        gu_ctr = [0]
        dn_ctr = [0]
        for e_ in range(NEXP):
            s_ = e_ % 2
            P.dma("pool", WG[s_][:, :, :], w_gate.ap()[e_].rearrange("(k p) n -> p k n", p=128), writes=[bWG[s_]])
            P.dma("pool", WU[s_][:, :, :], w_up.ap()[e_].rearrange("(k p) n -> p k n", p=128), writes=[bWU[s_]])
            P.dma("pool", WD[s_][:, :, :], w_down.ap()[e_].rearrange("(k p) n -> p k n", p=128), writes=[bWD[s_]])
            for tb in range(OWN // 4):
                tsl = slice(tb * 512, (tb + 1) * 512)
                a_ = (e_ * 4 + tb) % 2
                for fb in range(4):
                    k_ = gu_ctr[0] % 2
                    gu_ctr[0] += 1
                    pg, pu = 2 * k_, 2 * k_ + 1
                    for kc in range(8):
                        mm(PS[pg][:, :], WG[s_][:, kc, fb * 128:(fb + 1) * 128], H2T[:, kc, tsl], kc == 0, kc == 7, [bWG[s_], bH2T], [PB[pg]])
                    for kc in range(8):
                        mm(PS[pu][:, :], WU[s_][:, kc, fb * 128:(fb + 1) * 128], H2T[:, kc, tsl], kc == 0, kc == 7, [bWU[s_], bH2T], [PB[pu]])
                    act(SG[k_][:, :], PS[pg][:, :], AF.Silu, [PB[pg]], [bSG[k_]])
                    tt("dve", ATb[a_][:, fb, :], SG[k_][:, :], PS[pu][:, :], ALU.mult, [bSG[k_], PB[pu]], [bAT[a_]])
                for t_ in range(4):
                    tile = tb * 4 + t_
                    for cb in range(2):
                        pd = 4 + dn_ctr[0] % 4
                        dn_ctr[0] += 1
                        for fb in range(4):
                            mm(PS[pd][:, :], ATb[a_][:, fb, t_ * 128:(t_ + 1) * 128], WD[s_][:, fb, cb * 512:(cb + 1) * 512], fb == 0, fb == 3, [bAT[a_], bWD[s_]], [PB[pd]])
                        ya = YACC[:, tile, cb * 512:(cb + 1) * 512]
                        if e_ == 0:
                            ts("dve", ya, PS[pd][:, :], COMB[:, tile, e_:e_ + 1], None, ALU.mult, None, [PB[pd], bCOMB], [bYACC[tile]])
                        else:
                            stt("dve", ya, PS[pd][:, :], COMB[:, tile, e_:e_ + 1], ya, ALU.mult, ALU.add, [PB[pd], bCOMB, bYACC[tile]], [bYACC[tile]])
        GP2L = sbE("GP2L", [128, D])
        bGP2L = Buf("GP2L")
        P.dma("sp", GP2L[:, :], bc_rows(mod_d, 0, D, off=5 * D), reads=[bMODD], writes=[bGP2L])
        GP2 = GP2L[:, :]
        bMOD = bGP2L
        XF = [sbE("XF%d" % i, [128, D]) for i in range(2)]
        bXF = [Buf("XF%d" % i) for i in range(2)]
        JK2 = sbE("JK2", [128, D], BF16)
        bJK2 = Buf("JK2")
        SF = sbE("SF", [128, 8])
        bSF = Buf("SF")
        OT = [sbE("OT%d" % i, [128, D]) for i in range(2)]
        bOT = [Buf("OT%d" % i) for i in range(2)]
        for c in range(OWN):
            s_ = c % 2
            rows = slice(c * 128, (c + 1) * 128)
            P.dma("sp", XF[s_][:, :], out_d.ap()[rows, :], reads=[bOUT], writes=[bXF[s_]])
            q_ = SF[:, s_ * 4:s_ * 4 + 4]
            act(JK2[:, :], YACC[:, c, :], AF.Square, [bYACC[c]], [bJK2, bSF], accum_out=q_[:, 0:1])
            ts("pool", q_[:, 1:2], q_[:, 0:1], 1.0 / D, EPS, ALU.mult, ALU.add, [bSF], [bSF])
            tt("pool", q_[:, 2:3], q_[:, 1:2], NEGH, ALU.pow, [bSF, bTABS], [bSF])
            stt("dve", OT[s_][:, :], YACC[:, c, :], q_[:, 2:3], GP2, ALU.mult, ALU.mult, [bYACC[c], bSF, bMOD], [bOT[s_]])
            tt("pool", OT[s_][:, :], OT[s_][:, :], XF[s_][:, :], ALU.add, [bOT[s_], bXF[s_]], [bOT[s_]])
            t = P.dma("sp", out_d.ap()[rows, :], OT[s_][:, :], reads=[bOT[s_], bXF[s_]], writes=[bOUT])
            final.append((t[0], t[1]))
        P.barrier()
        with nc.Block() as block:
            P.emit(block, final)
        stE.close()
    return nc


def _centered(idx, n):
    return ((idx + n // 2) % n) - n // 2


def make_inputs(core, x, c, ctx, c_ctx, w_ada, b_ada, g_pre_mix, g_post_mix, g_pre_ffn, g_post_ffn,
                w_in, conv_w, conv_b, w_q, w_k, w_v, w_if_fwd, b_if_fwd, w_if_bwd, b_if_bwd,
                mlstm_norm_w, mlstm_skip, w_fourier, w_out, w_router_group, b_router_group,
                w_router_expert, b_router_expert, w_gate, w_up, w_down, shared):
    f32 = np.float32
    j = core
    xs = x[0]
    m = {}
    m["x_rot"] = np.ascontiguousarray(np.roll(xs, -2048 * j, axis=0))
    halo = np.zeros((128, D), f32)
    hmask = np.zeros(64, f32)
    hrow = np.zeros(128, f32)
    hcol = np.zeros(128, f32)
    for g in range(NG):
        for side, tr_ in ((0, 512 * g - 1), (1, 512 * g + 512)):
            true_t = (tr_ % T + 2048 * j) % T
            own_first_true = ((512 * g) % T + 2048 * j) % T
            if side == 0:
                valid = own_first_true != 0
            else:
                valid = ((512 * g + 511) % T + 2048 * j) % T != T - 1
            halo[2 * g + side] = xs[true_t]
            hmask[2 * g + side] = 1.0 if valid else 0.0
            hrow[2 * g + side] = true_t // 64
            hcol[2 * g + side] = true_t % 64
    m["x_halo"] = halo
    tabs = np.zeros((128, 1024), f32)
    p = np.arange(128)
    for a in range(2):
        tabs[:, a] = ((2 * p + a) + 32 * j) % 256
    tabs[:, 2] = p % 64
    tabs[:, 3] = hrow
    tabs[:, 4] = hcol
    tabs[:, 5] = -0.5
    i = np.arange(128)
    true_c = (i + 16 * j) % 128
    tabs[:, 128:256] = (true_c < 16 * j).astype(f32)[None, :]
    tabs[:, 256:384] = (true_c >= 16 * j + 16).astype(f32)[None, :]
    tabs[:, 384:448] = hmask[None, :]
    tabs[:, 512:768] = np.arange(256, dtype=f32)[None, :]
    m["tabs"] = tabs
    di = np.zeros((128, 640), f32)
    n = np.arange(128)[:, None]
    k = np.arange(128)[None, :]
    di[:, 0:128] = _centered(n * k + 32, 128)
    di[:, 128:256] = _centered(n * k, 128)
    base = n * k + 2048 * j * k
    di[:, 256:384] = _centered(base + 4096, 16384)
    di[:, 384:512] = _centered(base, 16384)
    cc = (16 * j + np.arange(16))[None, :]
    di[:, 512:528] = _centered(n * cc + 32, 128)
    di[:, 528:544] = _centered(n * cc, 128)
    m["dftidx"] = di
    m.update(shared)
    return m


def make_shared(x, c, ctx, c_ctx, w_ada, b_ada, g_pre_mix, g_post_mix, g_pre_ffn, g_post_ffn,
                w_in, conv_w, conv_b, w_q, w_k, w_v, w_if_fwd, b_if_fwd, w_if_bwd, b_if_bwd,
                mlstm_norm_w, mlstm_skip, w_fourier, w_out, w_router_group, b_router_group,
                w_router_expert, b_router_expert, w_gate, w_up, w_down):
    f32 = np.float32
    s = {}
    s["ctx"] = np.ascontiguousarray(ctx[0])
    cT = np.zeros((128, 16), f32)
    cT[:, 0:8] = c[0].reshape(8, 128).T
    cT[:, 8:16] = c_ctx.reshape(8, 128).T
    s["cT"] = cT
    s["w_ada"] = np.ascontiguousarray(w_ada[0])
    s["b_ada"] = np.ascontiguousarray(b_ada[0][None, :])
    s["gains"] = np.stack([g_pre_mix[0], g_post_mix[0], g_pre_ffn[0], g_post_ffn[0]]).astype(f32)
    s["w_in"] = np.ascontiguousarray(w_in[0])
    cw = np.zeros((128, 16), f32)
    for cc in range(4):
        for k in range(3):
            cw[:, cc * 3 + k] = conv_w[0][k, cc * 128:(cc + 1) * 128]
        cw[:, 12 + cc] = conv_b[0][cc * 128:(cc + 1) * 128]
    s["convw"] = cw
    s["w_qkv"] = np.stack([w_q[0].reshape(512, 4), w_k[0].reshape(512, 4), w_v[0].reshape(512, 4)]).astype(f32)
    s["w_qkvT"] = np.stack([np.ascontiguousarray(w.transpose(0, 2, 1)).reshape(512, 4) for w in (w_q[0], w_k[0], w_v[0])]).astype(f32)
    wf, wb = w_if_fwd[0], w_if_bwd[0]
    s["w_if"] = np.ascontiguousarray(np.concatenate([wf[:, 0:4], wb[:, 0:4], wf[:, 4:8], wb[:, 4:8]], axis=1))
    bf, bb = b_if_fwd[0], b_if_bwd[0]
    s["b_if"] = np.concatenate([bf[0:4], bb[0:4], bf[4:8], bb[4:8]])[None, :].astype(f32)
    s["nrm_skip"] = np.stack([mlstm_norm_w[0], mlstm_skip[0]]).astype(f32)
    s["w_fourier"] = np.ascontiguousarray(w_fourier[0])
    s["w_out"] = np.ascontiguousarray(w_out[0])
    s["w_router"] = np.ascontiguousarray(np.concatenate([w_router_group[0], w_router_expert[0]], axis=1))
    s["b_router"] = np.concatenate([b_router_group[0], b_router_expert[0]])[None, :].astype(f32)
    s["w_gate"] = np.ascontiguousarray(w_gate[0])
    s["w_up"] = np.ascontiguousarray(w_up[0])
    s["w_down"] = np.ascontiguousarray(w_down[0])
    cst = np.zeros((128, 640), f32)
    cst[:, 0:128] = np.eye(128)
    a = np.arange(128)
    cst[:, 128:256] = (a[:, None] <= a[None, :])
    cst[:, 256:384] = (a[:, None] >= a[None, :])
    cst[:, 384:512] = 1.0
    cst[:, 512:640] = (a[:, None] // 4 == a[None, :] // 4)
    s["consts"] = cst
    return s


_CACHE = {}


def kernel(**inputs):
    inputs = {k: np.asarray(v) for k, v in inputs.items()}
    if "nc" not in _CACHE:
        _CACHE["nc"] = build()
    nc = _CACHE["nc"]
    shared = make_shared(**inputs)
    in_maps = [make_inputs(core, shared=shared, **inputs) for core in range(NCORES)]
    res = run_bass_kernel_spmd(nc, in_maps, core_ids=list(range(NCORES)))
    out = np.concatenate([res.results[i]["out"] for i in range(NCORES)], axis=0)
    return out.reshape(1, T, D).astype(np.float32)
```

```python
import math
from contextlib import ExitStack
import numpy as np
import concourse.bass as bass
import concourse.mybir as mybir
from concourse.bass_utils import run_bass_kernel_spmd

F32 = mybir.dt.float32
BF16 = mybir.dt.bfloat16
I32 = mybir.dt.int32
AF = mybir.ActivationFunctionType
ALU = mybir.AluOpType

NCORES = 8
T = 16384
D = 1024
NT = T // 128
OWN = NT // NCORES
NG = NT // 4
EPS = 1e-6
TWO_PI = 2.0 * math.pi
NHALO = 2 * NG
DBG = {}


class Buf:
    __slots__ = ("name", "w", "r")

    def __init__(self, name):
        self.name = name
        self.w = None
        self.r = {}


class Prog:
    def __init__(self, nc, stack, ndma=28):
        self.nc = nc
        self.engs = {"pe": nc.tensor, "act": nc.scalar, "dve": nc.vector, "pool": nc.gpsimd, "sp": nc.sync}
        self.ops = {k: [] for k in self.engs}
        self.sem = {k: stack.enter_context(nc.semaphore("s_" + k)) for k in self.engs}
        self.cnt = {k: 0 for k in self.engs}
        self.waited = {k: {} for k in self.engs}
        self.dsem = [stack.enter_context(nc.semaphore("d%d" % i)) for i in range(ndma)]
        self.dcnt = [0] * ndma
        self.ring = {"sp": list(range(0, ndma - 8)), "act": list(range(0, ndma - 8)), "pool": list(range(ndma - 8, ndma))}
        self.rpos = {"sp": 0, "pool": 0}

    def _collect(self, e, reads, writes, sync_same=True):
        waits = {}

        def need(tok):
            if tok is None:
                return
            sem, val, eng = tok
            if eng == e and not sync_same:
                return
            key = id(sem)
            if self.waited[e].get(key, 0) >= val:
                return
            if key not in waits or waits[key][1] < val:
                waits[key] = (sem, val)

        for b in reads:
            need(b.w)
        for b in writes:
            need(b.w)
            for t in b.r.values():
                need(t)
        for key, (sem, val) in waits.items():
            self.waited[e][key] = val
        return list(waits.values())

    def _commit(self, tok, reads, writes):
        key = id(tok[0])
        for b in reads:
            old = b.r.get(key)
            if old is None or old[1] < tok[1]:
                b.r[key] = tok
        for b in writes:
            b.w = tok
            b.r = {}

    def op(self, e, fn, reads=(), writes=(), sync_same=True):
        waits = self._collect(e, reads, writes, sync_same)
        self.cnt[e] += 1
        tok = (self.sem[e], self.cnt[e], e)
        self.ops[e].append((waits, fn, (self.sem[e], 1)))
        self._commit(tok, reads, writes)
        return tok

    def dma(self, e, out, in_, reads=(), writes=()):
        rk = "pool" if e == "pool" else "sp"
        ring = self.ring[rk]
        i = ring[self.rpos[rk] % len(ring)]
        self.rpos[rk] += 1
        sem = self.dsem[i]
        waits = self._collect(e, reads, writes)
        prev = self.dcnt[i] * 16
        if prev > 0 and self.waited[e].get(id(sem), 0) < prev:
            waits.append((sem, prev))
            self.waited[e][id(sem)] = prev
        self.dcnt[i] += 1
        tok = (sem, self.dcnt[i] * 16, "dma")
        self.ops[e].append((waits, (lambda eng, o=out, i_=in_: eng.dma_start(out=o, in_=i_)), (sem, 16)))
        self._commit(tok, reads, writes)
        return tok

    def barrier(self):
        toks = [(self.sem[k], self.cnt[k]) for k in self.engs if self.cnt[k] > 0]
        toks += [(self.dsem[i], self.dcnt[i] * 16) for i in range(len(self.dsem)) if self.dcnt[i] > 0]
        for e in self.engs:
            waits = []
            for sem, val in toks:
                if self.waited[e].get(id(sem), 0) < val:
                    waits.append((sem, val))
                    self.waited[e][id(sem)] = val
            if waits:
                self.ops[e].append((waits, None, None))

    def emit(self, block, final_waits):
        def replay(e):
            def body(eng):
                for waits, fn, inc in self.ops[e]:
                    for sem, val in waits:
                        eng.wait_ge(sem, val)
                    if fn is not None:
                        ins = fn(eng)
                        ins.then_inc(inc[0], inc[1])
                if e == "sp":
                    for sem, val in final_waits:
                        eng.wait_ge(sem, val)
            return body

        block.tensor(replay("pe"))
        block.scalar(replay("act"))
        block.vector(replay("dve"))
        block.gpsimd(replay("pool"))
        block.sync(replay("sp"))


def build(stage=99, dbg=False, ngroups=NG, cut=99):
    nc = bass.Bass("TRN2", target_bir_lowering=False)
    DBG.clear()

    def din(name, shape, dt=F32):
        return nc.dram_tensor(name, list(shape), dt, kind="ExternalInput")

    x_rot = din("x_rot", [ngroups * 512, D])
    x_halo = din("x_halo", [128, D])
    ctx_in = din("ctx", [256, D])
    cT = din("cT", [128, 16])
    w_ada = din("w_ada", [D, 6 * D])
    b_ada = din("b_ada", [1, 6 * D])
    gains = din("gains", [4, D])
    w_in = din("w_in", [D, 1536])
    convw = din("convw", [128, 16])
    w_qkv = din("w_qkv", [3, 512, 4])
    w_qkvT = din("w_qkvT", [3, 512, 4])
    w_if = din("w_if", [1536, 16])
    b_if = din("b_if", [1, 16])
    nrm_skip = din("nrm_skip", [2, 512])
    w_fourier = din("w_fourier", [4, 128, 128])
    w_out = din("w_out", [D, D])
    w_router = din("w_router", [D, 20])
    b_router = din("b_router", [1, 20])
    moe_small = stage < 8
    w_gate = din("w_gate", [16, D, 512] if not moe_small else [1, 8, 8])
    w_up = din("w_up", [16, D, 512] if not moe_small else [1, 8, 8])
    w_down = din("w_down", [16, 512, D] if not moe_small else [1, 8, 8])
    consts = din("consts", [128, 5 * 128])
    tabs = din("tabs", [128, 1024])
    dftidx = din("dftidx", [128, 5 * 128])
    out_d = nc.dram_tensor("out", [OWN * 128, D], F32, kind="ExternalOutput")

    posr_d = nc.dram_tensor("posr_d", [256, 512], F32)
    u_d = nc.dram_tensor("u_d", [512, T], BF16)
    az_d = nc.dram_tensor("az_d", [2, OWN * 128, 512], BF16)
    hs_d = nc.dram_tensor("hs_d", [OWN * 128, 512], F32)
    mod_d = nc.dram_tensor("mod_d", [1, 6 * D], F32)

    dbg_outs = {}

    def dbg_out(name, shape):
        DBG[name] = tuple(shape)
        dbg_outs[name] = nc.dram_tensor("dbg_" + name, list(shape), F32, kind="ExternalOutput")
        return dbg_outs[name]

    stack = ExitStack()
    with stack:
        P = Prog(nc, stack)
        used = [0]

        def sb(name, shape, dt=F32):
            t = stack.enter_context(nc.sbuf_tensor(name, list(shape), dt))
            return t

        def psum(name, shape, dt=F32):
            return stack.enter_context(nc.psum_tensor(name, list(shape), dt))

        def act(out, in_, func, reads, writes, eng="act", **kw):
            return P.op("act", lambda e: e.activation(out=out, in_=in_, func=func, **kw), reads, writes)

        def tt(eng, out, in0, in1, op, reads, writes):
            return P.op(eng, lambda e: e.tensor_tensor(out=out, in0=in0, in1=in1, op=op), reads, writes)

        def ts(eng, out, in0, s1, s2, op0, op1, reads, writes):
            if op1 is None:
                return P.op(eng, lambda e: e.tensor_scalar(out=out, in0=in0, scalar1=s1, scalar2=None, op0=op0), reads, writes)
            return P.op(eng, lambda e: e.tensor_scalar(out=out, in0=in0, scalar1=s1, scalar2=s2, op0=op0, op1=op1), reads, writes)

        def stt(eng, out, in0, scalar, in1, op0, op1, reads, writes):
            return P.op(eng, lambda e: e.scalar_tensor_tensor(out=out, in0=in0, scalar=scalar, in1=in1, op0=op0, op1=op1), reads, writes)

        def cp(eng, out, in_, reads, writes):
            if eng == "act":
                return P.op("act", lambda e: e.activation(out=out, in_=in_, func=AF.Copy), reads, writes)
            return P.op(eng, lambda e: e.tensor_copy(out=out, in_=in_), reads, writes)

        def mm(out, lhsT, rhs, start, stop, reads, writes):
            return P.op("pe", lambda e: e.matmul(out, lhsT=lhsT, rhs=rhs, start=start, stop=stop), reads, writes, sync_same=False)

        def tr(out, in_, ident, reads, writes):
            return P.op("pe", lambda e: e.transpose(out=out, in_=in_, identity=ident), reads, writes, sync_same=False)

        def memset(eng, ap, val, writes):
            return P.op(eng, lambda e: e.memset(ap, val), (), writes)

        def dump(name, ap, shape, reads):
            if not dbg:
                return
            dd = dbg_out(name, shape)
            P.dma("pool", dd.ap(), ap, reads=reads, writes=[Buf("dbg")])

        def bc_rows(dram_t, row, n, parts=128, off=0):
            width = dram_t.shape[-1]
            return bass.AP(dram_t, row * width + off, [[0, parts], [1, n]])

        PS = [psum("ps%d" % i, [128, 512]) for i in range(8)]
        PB = [Buf("ps%d" % i) for i in range(8)]

        CONST = sb("CONST", [128, 640])
        bCONST = Buf("CONST")
        P.dma("sp", CONST[:, :], consts.ap(), writes=[bCONST])
        IDF = CONST[:, 0:128]
        TRIU = CONST[:, 128:256]
        TRIL = CONST[:, 256:384]
        ONES = CONST[:, 384:512]
        BDM = CONST[:, 512:640]
        IDB = sb("IDB", [128, 128], BF16)
        bIDB = Buf("IDB")
        cp("dve", IDB[:, :], IDF, [bCONST], [bIDB])
        TABS = sb("TABS", [128, 1024])
        bTABS = Buf("TABS")
        P.dma("sp", TABS[:, :], tabs.ap(), writes=[bTABS])
        NEGH = TABS[:, 5:6]
        POSC = sb("POSC", [128, 512])
        bPOSC = Buf("POSC")

        final = []
        stm = ExitStack()

        def sbm(name, shape, dt=F32):
            return stm.enter_context(nc.sbuf_tensor(name, list(shape), dt))

        QTF = sbm("QTF", [128, 4 * OWN * 128], BF16)
        QT = QTF[:, :].rearrange("p (c t) -> p c t", c=4)
        bQT = Buf("QT")
        KTF = sbm("KTF", [128, 4 * OWN * 128], BF16)
        KT = KTF[:, :].rearrange("p (c t) -> p c t", c=4)
        bKT = Buf("KT")
        WINC = KTF[:, 0:4096].rearrange("p (k n) -> p k n", n=512)
        bWINC = bKT
        KTMO = sbm("KTMO", [128, OWN, 512], BF16)
        bKTMO = [Buf("KTMO%d" % i) for i in range(OWN)]
        VAO = sbm("VAO", [128, OWN, 4, 129], BF16)
        bVAO = [Buf("VAO%d" % i) for i in range(OWN)]
        OWNG = sbm("OWNG", [128, OWN, 24])
        bOWNG = Buf("OWNG")
        CF = sbm("CF", [128, 4, 129])
        bCF = Buf("CF")
        CB = sbm("CB", [128, 4, 129])
        bCB = Buf("CB")
        sts = ExitStack()

        def sbs(name, shape, dt=F32):
            return sts.enter_context(nc.sbuf_tensor(name, list(shape), dt))

        WIN = sbs("WIN", [128, 8, 1536], BF16)
        bWIN = Buf("WIN")
        BCOL = sbs("BCOL", [128, 16])
        bBCOL = Buf("BCOL")
        BZ = sbs("BZ", [128, 512])
        bBZ = Buf("BZ")
        bMODD = Buf("mod_d")

        def interleave(gens):
            gens = list(gens)
            while gens:
                for g_ in list(gens):
                    try:
                        next(g_)
                    except StopIteration:
                        gens.remove(g_)

        with ExitStack() as st0:
            def sb0(name, shape, dt=F32):
                return st0.enter_context(nc.sbuf_tensor(name, list(shape), dt))
            MOD = sb0("MOD", [128, 6 * D])
            bMOD = Buf("MOD")
            MODC = sb0("MODC", [128, 2 * D])
            bMODC = Buf("MODC")
            st0a = ExitStack()

            def sb0a(name, shape, dt=F32):
                return st0a.enter_context(nc.sbuf_tensor(name, list(shape), dt))
            CT = sb0a("CT", [128, 16])
            bCT = Buf("CT")
            P.dma("sp", CT[:, :], cT.ap(), writes=[bCT])
            SC = sb0a("SC", [128, 16])
            bSC = Buf("SC")
            act(SC[:, :], CT[:, :], AF.Silu, [bCT], [bSC])
            REP = sb0a("REP", [128, 16, 128])
            bREP = Buf("REP")
            for j in range(16):
                cp("dve", REP[:, j, :], SC[:, j:j + 1].broadcast_to([128, 128]), [bSC], [bREP])
            WA = [sb0a("WA%d" % i, [128, 8, 512]) for i in range(2)]
            bWA = [Buf("WA%d" % i) for i in range(2)]
            BA = [sb0a("BA%d" % i, [128, 512]) for i in range(2)]
            bBA = [Buf("BA%d" % i) for i in range(2)]
            w_ada_v = w_ada.ap().rearrange("(k p) n -> p k n", p=128)
            for blk in range(12):
                s = blk % 2
                P.dma("sp", WA[s][:, :, :], w_ada_v[:, :, blk * 512:(blk + 1) * 512], writes=[bWA[s]])
                P.dma("sp", BA[s][:, :], bc_rows(b_ada, 0, 512, off=blk * 512), writes=[bBA[s]])
                pb = blk % 2
                for kc in range(8):
                    mm(PS[pb][:, :], REP[:, kc, :], WA[s][:, kc, :], kc == 0, kc == 7, [bREP, bWA[s]], [PB[pb]])
                tt("dve", MOD[:, blk * 512:(blk + 1) * 512], PS[pb][:, :], BA[s][:, :], ALU.add, [PB[pb], bBA[s]], [bMOD])
                if blk < 4:
                    pc = 2 + blk % 2
                    for kc in range(8):
                        mm(PS[pc][:, :], REP[:, 8 + kc, :], WA[s][:, kc, :], kc == 0, kc == 7, [bREP, bWA[s]], [PB[pc]])
                    tt("dve", MODC[:, blk * 512:(blk + 1) * 512], PS[pc][:, :], BA[s][:, :], ALU.add, [PB[pc], bBA[s]], [bMODC])
            GB = sb0a("GB", [128, 4, D])
            bGB = Buf("GB")
            for i in range(4):
                P.dma("sp", GB[:, i, :], bc_rows(gains, i, D), writes=[bGB])
            stt("dve", MOD[:, D:2 * D], MOD[:, D:2 * D], 1.0, GB[:, 0, :], ALU.add, ALU.mult, [bMOD, bGB], [bMOD])
            tt("dve", MOD[:, 2 * D:3 * D], MOD[:, 2 * D:3 * D], GB[:, 1, :], ALU.mult, [bMOD, bGB], [bMOD])
            stt("dve", MOD[:, 4 * D:5 * D], MOD[:, 4 * D:5 * D], 1.0, GB[:, 2, :], ALU.add, ALU.mult, [bMOD, bGB], [bMOD])
            tt("dve", MOD[:, 5 * D:6 * D], MOD[:, 5 * D:6 * D], GB[:, 3, :], ALU.mult, [bMOD, bGB], [bMOD])
            stt("dve", MODC[:, D:2 * D], MODC[:, D:2 * D], 1.0, GB[:, 0, :], ALU.add, ALU.mult, [bMODC, bGB], [bMODC])
            P.dma("sp", mod_d.ap(), MOD[0:1, :], reads=[bMOD], writes=[bMODD])
            dump("mod", MOD[0:1, :], [1, 6 * D], [bMOD])
            dump("modc", MODC[0:1, :], [1, 2 * D], [bMODC])
            P.barrier()
            st0a.close()
            REPS = sb0("REPS", [128, 2, 8, 128])
            bREPS = Buf("REPS")
            GC = sb0("GC", [128, 16])
            bGC = Buf("GC")
            for kc in range(8):
                blk = slice(kc * 128, (kc + 1) * 128)
                tr(PS[0][:, 0:128], MOD[:, blk], IDF, [bMOD, bCONST], [PB[0]])
                tr(PS[0][:, 128:256], MOD[:, D + kc * 128:D + (kc + 1) * 128], IDF, [bMOD, bCONST], [PB[0]])
                tr(PS[0][:, 256:384], MODC[:, blk], IDF, [bMODC, bCONST], [PB[0]])
                tr(PS[0][:, 384:512], MODC[:, D + kc * 128:D + (kc + 1) * 128], IDF, [bMODC, bCONST], [PB[0]])
                cp("dve", REPS[:, 0, kc, :], PS[0][:, 0:128], [PB[0]], [bREPS])
                cp("dve", REPS[:, 1, kc, :], PS[0][:, 256:384], [PB[0]], [bREPS])
                cp("dve", GC[:, kc:kc + 1], PS[0][:, 128:129], [PB[0]], [bGC])
                cp("dve", GC[:, 8 + kc:9 + kc], PS[0][:, 384:385], [PB[0]], [bGC])
            WST = [sb0("WST%d" % i, [128, 1536]) for i in range(2)]
            bWST = [Buf("WST%d" % i) for i in range(2)]
            for kc in range(8):
                w_, bw_ = WST[kc % 2], bWST[kc % 2]
                P.dma("sp", w_[:, :], w_in.ap()[kc * 128:(kc + 1) * 128, :], writes=[bw_])
                ts("dve", WIN[:, kc, :], w_[:, :], GC[:, kc:kc + 1], None, ALU.mult, None, [bw_, bGC], [bWIN])
                ts("pool", WINC[:, kc, :], w_[:, 0:512], GC[:, 8 + kc:9 + kc], None, ALU.mult, None, [bw_, bGC], [bWINC])
                for blk in range(3):
                    mm(PS[2 + blk][:, :], REPS[:, 0, kc, :], w_[:, blk * 512:(blk + 1) * 512], kc == 0, kc == 7, [bREPS, bw_], [PB[2 + blk]])
                mm(PS[5][:, :], REPS[:, 1, kc, :], w_[:, 0:512], kc == 0, kc == 7, [bREPS, bw_], [PB[5]])
            BROW = sb0("BROW", [128, 4, 512])
            bBROW = Buf("BROW")
            for i in range(4):
                cp("dve", BROW[:, i, :], PS[2 + i][:, :], [PB[2 + i]], [bBROW])
            cp("dve", BZ[:, :], BROW[:, 1, :], [bBROW], [bBZ])
            for i, src in ((0, 0), (1, 2), (2, 3)):
                for j in range(4):
                    tr(PS[0][:, j * 128:(j + 1) * 128], BROW[:, src, j * 128:(j + 1) * 128], IDF, [bBROW, bCONST], [PB[0]])
                for j in range(4):
                    cp("dve", BCOL[:, i * 4 + j:i * 4 + j + 1], PS[0][:, j * 128:j * 128 + 1], [PB[0]], [bBCOL])
            P.barrier()

        BDB = sbs("BDB", [128, 3, 4, 128], BF16)
        bBDB = Buf("BDB")
        AW = sbs("AW", [128, 4, 32], BF16)
        bAW = Buf("AW")
        BIF = sbs("BIF", [128, 16])
        bBIF = Buf("BIF")
        P.dma("sp", BIF[:, :], bc_rows(b_if, 0, 16), writes=[bBIF])
        CW = sbs("CW", [128, 16])
        bCW = Buf("CW")
        P.dma("sp", CW[:, :], convw.ap(), writes=[bCW])
        XMH = sbs("XMH", [128, 4, 64])
        bXMH = Buf("XMH")
        bPOSRD = Buf("posr_d")
        NTB = 4
        XB = [sbs("XB%d" % i, [128, D]) for i in range(NTB)]
        bXB = [Buf("XB%d" % i) for i in range(NTB)]
        PRB = [sbs("PRB%d" % i, [128, 512]) for i in range(NTB)]
        bPRB = [Buf("PRB%d" % i) for i in range(NTB)]
        X0B = [sbs("X0B%d" % i, [128, D], BF16) for i in range(NTB)]
        bX0B = [Buf("X0B%d" % i) for i in range(NTB)]
        JUNK = [sbs("JUNK%d" % i, [128, D], BF16) for i in range(2)]
        bJUNK = [Buf("JUNK%d" % i) for i in range(2)]
        DG = [sbs("DG%d" % i, [128, 128], BF16) for i in range(NTB)]
        bDG = [Buf("DG%d" % i) for i in range(NTB)]
        SS = sbs("SS", [128, 4 * NTB])
        bSS = [Buf("SS%d" % i) for i in range(NTB)]
        HT = sbs("HT", [128, 8, 512], BF16)
        bHTt = [Buf("HT%d" % i) for i in range(4)]
        PT = PS[0][:, :].bitcast(BF16).rearrange("p (k t) -> p k t", t=128)

        with ExitStack() as st1:
            def sb1(name, shape, dt=F32):
                return st1.enter_context(nc.sbuf_tensor(name, list(shape), dt))
            FREQ = sb1("FREQ", [128, 256])
            bFREQ = Buf("FREQ")
            act(FREQ[:, :], TABS[:, 512:768], AF.Exp, [bTABS], [bFREQ], scale=-math.log(10000.0) / 256.0)
            WS = sb1("WS", [128, 6, 4, 4])
            bWS = Buf("WS")
            for w in range(3):
                P.dma("sp", WS[:, w, :, :], bass.AP(w_qkv, w * 2048, [[4, 128], [512, 4], [1, 4]]), writes=[bWS])
                P.dma("sp", WS[:, 3 + w, :, :], bass.AP(w_qkvT, w * 2048, [[4, 128], [512, 4], [1, 4]]), writes=[bWS])
            BDF = sb1("BDF", [128, 6, 4, 128])
            bBDF = Buf("BDF")
            BDM3 = BDM.rearrange("p (r o) -> p r o", o=4)
            for w in range(6):
                for cc in range(4):
                    tt("dve", BDF[:, w, cc, :].rearrange("p (r o) -> p r o", o=4),
                       WS[:, w, cc, None, :].broadcast_to([128, 32, 4]), BDM3, ALU.mult, [bWS, bCONST], [bBDF])
            cp("dve", BDB[:, :, :, :], BDF[:, 0:3, :, :], [bBDF], [bBDB])
            WIF = sb1("WIF", [128, 12, 16])
            bWIF = Buf("WIF")
            P.dma("sp", WIF[:, :, :], w_if.ap().rearrange("(k p) n -> p k n", p=128), writes=[bWIF])
            for cc in range(4):
                mm(PS[1][:, cc * 32:cc * 32 + 16], BDF[:, 3, cc, :], WIF[:, cc, :], True, False, [bBDF, bWIF], [PB[1]])
                mm(PS[1][:, cc * 32:cc * 32 + 16], BDF[:, 4, cc, :], WIF[:, 4 + cc, :], False, True, [bBDF, bWIF], [PB[1]])
                mm(PS[1][:, cc * 32 + 16:cc * 32 + 32], BDF[:, 5, cc, :], WIF[:, 8 + cc, :], True, True, [bBDF, bWIF], [PB[1]])
            cp("dve", AW[:, :, :], PS[1][:, 0:128].rearrange("p (c n) -> p c n", n=32), [PB[1]], [bAW])

            ANG = sb1("ANG", [128, 512])
            bANG = Buf("ANG")
            KI = sb1("KI", [128, 512], I32)
            bKI = Buf("KI")
            MSK = sb1("MSK", [128, 512])
            bMSK = Buf("MSK")

            def sincos(out, bout, idx):
                ts("dve", ANG[:, 0:256], FREQ[:, :], idx, None, ALU.mult, None, [bFREQ, bTABS], [bANG])
                ts("dve", ANG[:, 256:512], ANG[:, 0:256], math.pi / 2, None, ALU.add, None, [bANG], [bANG])
                ts("dve", KI[:, :], ANG[:, :], 1.0 / TWO_PI, None, ALU.mult, None, [bANG], [bKI])
                stt("dve", ANG[:, :], KI[:, :], -TWO_PI, ANG[:, :], ALU.mult, ALU.add, [bKI, bANG], [bANG])
                ts("dve", MSK[:, :], ANG[:, :], math.pi, TWO_PI, ALU.is_gt, ALU.mult, [bANG], [bMSK])
                tt("dve", ANG[:, :], ANG[:, :], MSK[:, :], ALU.subtract, [bANG, bMSK], [bANG])
                ts("dve", MSK[:, :], ANG[:, :], -math.pi, TWO_PI, ALU.is_lt, ALU.mult, [bANG], [bMSK])
                tt("dve", ANG[:, :], ANG[:, :], MSK[:, :], ALU.add, [bANG, bMSK], [bANG])
                ts("dve", ANG[:, :], ANG[:, :], math.pi, -math.pi, ALU.min, ALU.max, [bANG], [bANG])
                act(out, ANG[:, :], AF.Sin, [bANG], [bout])

            PR = sb1("PR", [128, 2, 512])
            bPR = Buf("PR")
            for a in range(2):
                sincos(PR[:, a, :], bPR, TABS[:, a:a + 1])
            P.dma("sp", posr_d.ap().rearrange("(p a) n -> p a n", a=2), PR[:, :, :], reads=[bPR], writes=[bPOSRD])
            sincos(POSC[:, :], bPOSC, TABS[:, 2:3])
            POSH = sb1("POSH", [128, D])
            bPOSH = Buf("POSH")
            sincos(POSH[:, 0:512], bPOSH, TABS[:, 3:4])
            sincos(POSH[:, 512:1024], bPOSH, TABS[:, 4:5])

            tile_ctr = [0]

            def tile_front(xsrc, pos_mode, ht_dst, bht):
                k = tile_ctr[0]
                tile_ctr[0] += 1
                q = k % NTB
                X, bX = XB[q], bXB[q]
                xb_, bxb_ = X0B[q], bX0B[q]
                dg, bdg = DG[q], bDG[q]
                bss = bSS[q]
                P.dma("sp", X[:, :], xsrc, writes=[bX])
                if pos_mode is not None and pos_mode[0] == "rolled":
                    i = pos_mode[1]
                    PRt, bPRt = PRB[q], bPRB[q]
                    P.dma("sp", PRt[0:64, :], bc_rows(posr_d, 2 * i, 512, parts=64), reads=[bPOSRD], writes=[bPRt])
                    P.dma("sp", PRt[64:128, :], bc_rows(posr_d, 2 * i + 1, 512, parts=64), reads=[bPOSRD], writes=[bPRt])
                    yield
                    tt("pool", xb_[:, 0:512], X[:, 0:512], PRt[:, :], ALU.add, [bX, bPRt], [bxb_])
                    tt("pool", xb_[:, 512:1024], X[:, 512:1024], POSC[:, :], ALU.add, [bX, bPOSC], [bxb_])
                elif pos_mode is not None:
                    tt("pool", xb_[:, :], X[:, :], POSH[:, :], ALU.add, [bX, bPOSH], [bxb_])
                else:
                    yield
                    cp("pool", xb_[:, :], X[:, :], [bX], [bxb_])
                yield
                sc = SS[:, q * 4:q * 4 + 4]
                act(JUNK[k % 2][:, :], xb_[:, :], AF.Square, [bxb_], [bJUNK[k % 2], bss], accum_out=sc[:, 0:1])
                yield
                ts("pool", sc[:, 1:2], sc[:, 0:1], 1.0 / D, EPS, ALU.mult, ALU.add, [bss], [bss])
                tt("pool", sc[:, 2:3], sc[:, 1:2], NEGH, ALU.pow, [bss, bTABS], [bss])
                yield
                ts("dve", dg[:, :], IDF, sc[:, 2:3], None, ALU.mult, None, [bCONST, bss], [bdg])
                yield
                for kc in range(8):
                    pb = kc // 4
                    mm(PS[pb][:, (kc % 4) * 128:(kc % 4 + 1) * 128], xb_[:, kc * 128:(kc + 1) * 128], dg[:, :], True, True, [bxb_, bdg], [PB[pb]])
                cp("act", ht_dst[:, 0:4, :], PS[0][:, :].rearrange("p (k t) -> p k t", t=128), [PB[0]], [bht])
                cp("dve", ht_dst[:, 4:8, :], PS[1][:, :].rearrange("p (k t) -> p k t", t=128), [PB[1]], [bht])
                yield

            HTH = sb1("HTH", [128, 8, 128], BF16)
            bHTH = Buf("HTH")
            interleave([tile_front(x_halo.ap(), ("halo",), HTH[:, :, :], bHTH)])
            for cc in range(4):
                for kc in range(8):
                    mm(PS[2][:, cc * 64:cc * 64 + 64], WIN[:, kc, cc * 128:(cc + 1) * 128], HTH[:, kc, 0:64], kc == 0, kc == 7, [bWIN, bHTH], [PB[2]])
            XMHF = sb1("XMHF", [128, 4, 64])
            bXMHF = Buf("XMHF")
            tt("dve", XMHF[:, :, :], PS[2][:, 0:256].rearrange("p (c n) -> p c n", n=64),
               BCOL[:, 0:4, None].broadcast_to([128, 4, 64]), ALU.add, [PB[2], bBCOL], [bXMHF])
            tt("dve", XMH[:, :, :], XMHF[:, :, :], TABS[:, None, 384:448].broadcast_to([128, 4, 64]), ALU.mult, [bXMHF, bTABS], [bXMH])
            dump("xmh", XMH[:, :, :], [128, 4, 64], [bXMH])
            P.barrier()

        XM = sbs("XM", [128, 4, 514])
        bXM = [Buf("XM%d" % i) for i in range(4)]
        ACC = [sbs("ACC%d" % i, [128, 512]) for i in range(2)]
        bACC = [Buf("ACC%d" % i) for i in range(2)]
        ACTT = [sbs("ACTT%d" % i, [128, 4, 512], BF16) for i in range(2)]
        bACTT = [[Buf("ACTT%d_%d" % (j, i)) for i in range(4)] for j in range(2)]
        XMB = [sbs("XMB%d" % i, [128, 4, 512], BF16) for i in range(2)]
        bXMB = [[Buf("XMB%d_%d" % (j, i)) for i in range(4)] for j in range(2)]
        UT = sbs("UT", [128, 4, 512], BF16)
        bUT = Buf("UT")
        bUD = Buf("u_d")
        KTMG = sbs("KTMG", [128, 4, 512], BF16)
        bKTMG = [Buf("KTMG%d" % i) for i in range(4)]
        VAG = sbs("VAG", [128, 4, 4, 129], BF16)
        bVAG = [Buf("VAG%d" % i) for i in range(4)]
        VS = [sbs("VS%d" % i, [128, 8, 129], BF16) for i in range(2)]
        bVS = [Buf("VS%d" % i) for i in range(2)]
        CBS = sbs("CBS", [128, 4, 129])
        bCBS = Buf("CBS")
        memset("pool", CBS[:, :, :], 0.0, [bCBS])
        CCB = sbs("CCB", [128, 4, 129])
        bCCB = Buf("CCB")
        GT = sbs("GT", [128, 4, 16])
        bGT = Buf("GT")
        GW = sbs("GW", [128, 4, 40])
        bGW = Buf("GW")
        EXI = sbs("EXI", [128, 4, 16])
        bEXI = Buf("EXI")
        EXO = sbs("EXO", [128, 4, 16])
        bEXO = Buf("EXO")
        PALL = sbs("PALL", [128, 5, 4])
        bPALL = Buf("PALL")
        MBT = sbs("MBT", [128, 4, 4])
        bMBT = Buf("MBT")
        WV = sbs("WV", [128, 4, 8])
        bWV = Buf("WV")
        STG = [sbs("STG%d" % i, [128, 512], BF16) for i in range(2)]
        bSTG = [Buf("STG%d" % i) for i in range(2)]
        ZT = sbs("ZT", [128, 512])
        bZT = Buf("ZT")
        bAZ = Buf("az_d")
        stg_ctr = [0]

        memset("pool", VAG[:, :, :, :], 1.0, bVAG)
        memset("pool", VAO[:, :, :, :], 1.0, bVAO)
        memset("pool", PALL[:, :, :], 0.0, [bPALL])

        PSG = PS[6][:, 384:448].rearrange("p (t g) -> p t g", g=16)
        bPSG = PB[6]
        PSB = PS[6][:, 448:512].rearrange("p (t g) -> p t g", g=16)
        bPSB = PB[6]
        DCF = [PS[5][:, 0:129], PS[5][:, 129:258], PS[5][:, 258:387], PS[6][:, 0:129]]
        bDCF = [PB[5], PB[5], PB[5], PB[6]]
        ACCB = [PS[7][:, 0:129], PS[7][:, 129:258], PS[7][:, 258:387], PS[6][:, 129:258]]
        bACCB = [PB[7], PB[7], PB[7], PB[6]]
        u_v = u_d.ap().rearrange("(c p) t -> p c t", p=128)
        LN_QS = math.log(128.0 ** -0.5)

        def front(gi, kind, par):
            n = 2 if kind == "ctx" else 4
            ntok = n * 128
            tgens = []
            for t_ in range(n):
                dst = HT[:, :, t_ * 128:(t_ + 1) * 128]
                if kind == "ctx":
                    tgens.append(tile_front(ctx_in.ap()[t_ * 128:(t_ + 1) * 128, :], None, dst, bHTt[t_]))
                else:
                    i = gi * 4 + t_
                    tgens.append(tile_front(x_rot.ap()[i * 128:(i + 1) * 128, :], ("rolled", i), dst, bHTt[t_]))
            while tgens:
                for g_ in list(tgens):
                    try:
                        next(g_)
                    except StopIteration:
                        tgens.remove(g_)
                yield
            W_, bW_, boff = (WINC, bWINC, 8) if kind == "ctx" else (WIN, bWIN, 0)
            att, batt, xmb, bxmb = ACTT[par], bACTT[par], XMB[par], bXMB[par]
            for cc in range(4):
                pb = 2 + cc % 2
                for kc in range(8):
                    mm(PS[pb][:, 0:ntok], W_[:, kc, cc * 128:(cc + 1) * 128], HT[:, kc, 0:ntok], kc == 0, kc == 7, [bW_] + bHTt, [PB[pb]])
                bias = BCOL[:, boff + cc:boff + cc + 1]
                act(XM[:, cc, 1:1 + ntok], PS[pb][:, 0:ntok], AF.Identity, [PB[pb], bBCOL], [bXM[cc]], bias=bias)
                act(xmb[:, cc, 0:ntok], PS[pb][:, 0:ntok], AF.Identity, [PB[pb], bBCOL], [bxmb[cc]], bias=bias)
                if kind == "ctx":
                    memset("pool", XM[:, cc, 0:1], 0.0, [bXM[cc]])
                    memset("pool", XM[:, cc, 1 + ntok:2 + ntok], 0.0, [bXM[cc]])
                else:
                    cp("pool", XM[:, cc, 0:1], XMH[:, cc, 2 * gi:2 * gi + 1], [bXMH], [bXM[cc]])
                    cp("pool", XM[:, cc, 513:514], XMH[:, cc, 2 * gi + 1:2 * gi + 2], [bXMH], [bXM[cc]])
                yield
                A_, bA_ = ACC[cc % 2], bACC[cc % 2]
                ts("dve", A_[:, 0:ntok], XM[:, cc, 1:1 + ntok], CW[:, cc * 3 + 1:cc * 3 + 2], CW[:, 12 + cc:13 + cc], ALU.mult, ALU.add, [bXM[cc], bCW], [bA_])
                stt("dve", A_[:, 0:ntok], XM[:, cc, 0:ntok], CW[:, cc * 3:cc * 3 + 1], A_[:, 0:ntok], ALU.mult, ALU.add, [bXM[cc], bCW, bA_], [bA_])
                stt("dve", A_[:, 0:ntok], XM[:, cc, 2:2 + ntok], CW[:, cc * 3 + 2:cc * 3 + 3], A_[:, 0:ntok], ALU.mult, ALU.add, [bXM[cc], bCW, bA_], [bA_])
                act(att[:, cc, 0:ntok], A_[:, 0:ntok], AF.Silu, [bA_], [batt[cc]])
                yield
            if kind == "own" and gi == 0:
                dump("actT", att[:, :, 0:128], [128, 4, 128], batt)
            if kind != "ctx":
                for cc in range(4):
                    pb = 2 + cc % 2
                    for kc in range(8):
                        mm(PS[pb][:, :], WIN[:, kc, 1024 + cc * 128:1024 + (cc + 1) * 128], HT[:, kc, :], kc == 0, kc == 7, [bWIN] + bHTt, [PB[pb]])
                    act(UT[:, cc, :], PS[pb][:, :], AF.Identity, [PB[pb], bBCOL], [bUT], bias=BCOL[:, 4 + cc:5 + cc])
                    yield
                P.dma("sp", u_v[:, :, gi * 512:(gi + 1) * 512], UT[:, :, :], reads=[bUT], writes=[bUD])
            if kind == "own":
                for w, dstT, bdst in ((0, QT, bQT), (1, KT, bKT)):
                    for cc in range(4):
                        pb = 2 + cc % 2
                        mm(PS[pb][:, :], BDB[:, w, cc, :], att[:, cc, :], True, True, [bBDB, batt[cc]], [PB[pb]])
                        cp("act" if cc % 2 else "dve", dstT[:, cc, gi * 512:(gi + 1) * 512], PS[pb][:, :], [PB[pb]], [bdst])
                    yield
                for t_ in range(4):
                    i = gi * 4 + t_
                    sl = slice(t_ * 128, (t_ + 1) * 128)
                    pb = 2 + t_ % 2
                    for kc in range(8):
                        mm(PS[pb][:, :], HT[:, kc, sl], WIN[:, kc, 512:1024], kc == 0, kc == 7, bHTt + [bWIN], [PB[pb]])
                    tt("dve", ZT[:, :], PS[pb][:, :], BZ[:, :], ALU.add, [PB[pb], bBZ], [bZT])
                    s_ = stg_ctr[0] % 2
                    stg_ctr[0] += 1
                    act(STG[s_][:, :], ZT[:, :], AF.Silu, [bZT], [bSTG[s_]])
                    P.dma("sp", az_d.ap()[1, i * 128:(i + 1) * 128, :], STG[s_][:, :], reads=[bSTG[s_]], writes=[bAZ])
                    for cc in range(4):
                        tr(PT[:, cc, :], att[:, cc, sl], IDB[:, :], [batt[cc], bIDB], [PB[0]])
                    s_ = stg_ctr[0] % 2
                    stg_ctr[0] += 1
                    cp("dve", STG[s_][:, :].rearrange("p (c t) -> p c t", t=128), PT[:, 0:4, :], [PB[0]], [bSTG[s_]])
                    P.dma("sp", az_d.ap()[0, i * 128:(i + 1) * 128, :], STG[s_][:, :], reads=[bSTG[s_]], writes=[bAZ])
                    yield

        def back(gi, kind, par):
            n = 2 if kind == "ctx" else 4
            att, batt, xmb, bxmb = ACTT[par], bACTT[par], XMB[par], bXMB[par]
            for t_ in range(n):
                sl = slice(t_ * 128, (t_ + 1) * 128)
                if kind == "own":
                    i = gi * 4 + t_
                    ktm, bktm, va, bva = KTMO[:, i, :], bKTMO[i], VAO[:, i, :, :], bVAO[i]
                else:
                    ktm, bktm, va, bva = KTMG[:, t_, :], bKTMG[t_], VAG[:, t_, :, :], bVAG[t_]
                for cc in range(4):
                    mm(PS[4][:, cc * 128:(cc + 1) * 128], att[:, cc, sl], BDB[:, 1, cc, :], True, True, [batt[cc], bBDB], [PB[4]])
                cp("act", ktm, PS[4][:, :], [PB[4]], [bktm])
                yield
                for cc in range(4):
                    mm(PS[4][:, cc * 128:(cc + 1) * 128], xmb[:, cc, sl], BDB[:, 2, cc, :], True, True, [bxmb[cc], bBDB], [PB[4]])
                cp("act", va[:, :, 0:128], PS[4][:, :].rearrange("p (h d) -> p h d", d=128), [PB[4]], [bva])
                for cc in range(4):
                    mm(PSG[:, t_, :], att[:, cc, sl], AW[:, cc, 0:16], cc == 0, False, [batt[cc], bAW], [bPSG])
                for cc in range(4):
                    mm(PSG[:, t_, :], xmb[:, cc, sl], AW[:, cc, 16:32], False, cc == 3, [bxmb[cc], bAW], [bPSG])
                yield
            tt("dve", GT[:, 0:n, :], PSG[:, 0:n, :], BIF[:, None, :].broadcast_to([128, n, 16]), ALU.add, [bPSG, bBIF], [bGT])
            stt("dve", GW[:, 0:n, 0:8], GT[:, 0:n, 8:16], -1.0, GT[:, 0:n, 8:16], ALU.mult, ALU.max, [bGT], [bGW])
            yield
            act(GW[:, 0:n, 8:16], GW[:, 0:n, 0:8], AF.Exp, [bGW], [bGW], scale=-1.0)
            yield
            act(GW[:, 0:n, 16:24], GW[:, 0:n, 8:16], AF.Ln, [bGW], [bGW], bias=1.0)
            ts("dve", GW[:, 0:n, 24:32], GT[:, 0:n, 8:16], 0.0, None, ALU.min, None, [bGT], [bGW])
            yield
            tt("dve", GW[:, 0:n, 32:40], GW[:, 0:n, 24:32], GW[:, 0:n, 16:24], ALU.subtract, [bGW], [bGW])
            yield
            for t_ in range(n):
                mm(PSB[:, t_, 0:4], TRIU, GW[:, t_, 32:36], True, True, [bCONST, bGW], [bPSB])
                mm(PSB[:, t_, 4:8], TRIL, GW[:, t_, 36:40], True, True, [bCONST, bGW], [bPSB])
                mm(PSB[:, t_, 8:16], ONES, GW[:, t_, 32:40], True, True, [bCONST, bGW], [bPSB])
            yield
            if kind == "own":
                i0 = gi * 4
                if gi == 0:
                    dump("gt", GT[:, :, :], [128, 4, 16], [bGT])
                    dump("lf", GW[:, :, 32:40], [128, 4, 8], [bGW])
                tt("dve", EXI[:, :, 0:8], GT[:, :, 0:8], PSB[:, :, 0:8], ALU.subtract, [bGT, bPSB], [bEXI])
                yield
                act(OWNG[:, i0:i0 + 4, 0:8], EXI[:, :, 0:8], AF.Exp, [bEXI], [bOWNG])
                act(OWNG[:, i0:i0 + 4, 8:16], PSB[:, :, 0:8], AF.Exp, [bPSB], [bOWNG], bias=LN_QS)
                act(OWNG[:, i0:i0 + 4, 16:24], PSB[:, :, 8:16], AF.Exp, [bPSB], [bOWNG])
                yield
                return
            tt("dve", EXI[:, 0:n, 0:8], GT[:, 0:n, 0:8], PSB[:, 0:n, 8:16], ALU.add, [bGT, bPSB], [bEXI])
            tt("dve", EXI[:, 0:n, 0:8], EXI[:, 0:n, 0:8], PSB[:, 0:n, 0:8], ALU.subtract, [bEXI, bPSB], [bEXI])
            yield
            if kind == "ctx":
                cp("dve", EXI[:, 0:2, 8:16], PSB[:, 0:2, 8:16], [bPSB], [bEXI])
                yield
                act(EXO[:, 0:2, :], EXI[:, 0:2, :], AF.Exp, [bEXI], [bEXO])
                yield
                tt("dve", WV[:, 0, 0:4], EXO[:, 0, 0:4], EXO[:, 1, 8:12], ALU.mult, [bEXO], [bWV])
                cp("dve", WV[:, 1, 0:4], EXO[:, 1, 0:4], [bEXO], [bWV])
                cp("dve", WV[:, 0, 4:8], EXO[:, 0, 4:8], [bEXO], [bWV])
                tt("dve", WV[:, 1, 4:8], EXO[:, 1, 4:8], EXO[:, 0, 12:16], ALU.mult, [bEXO], [bWV])
                yield
            else:
                MF = TABS[:, 128 + 4 * gi:132 + 4 * gi]
                MB = TABS[:, 256 + 4 * gi:260 + 4 * gi]
                MF3 = MF[:, :, None].broadcast_to([128, 4, 4])
                MB3 = MB[:, :, None].broadcast_to([128, 4, 4])
                tt("dve", EXI[:, :, 8:12], PSB[:, :, 8:12], MF3, ALU.mult, [bPSB, bTABS], [bEXI])
                tt("dve", MBT[:, :, :], PSB[:, :, 12:16], MB3, ALU.mult, [bPSB, bTABS], [bMBT])
                yield
                for t_ in range(4):
                    tt("dve", PALL[:, t_ + 1, :], PALL[:, t_, :], MBT[:, t_, :], ALU.add, [bPALL, bMBT], [bPALL])
                    yield
                cp("dve", EXI[:, :, 12:16], PALL[:, 0:4, :], [bPALL], [bEXI])
                yield
                act(EXO[:, :, :], EXI[:, :, :], AF.Exp, [bEXI], [bEXO])
                yield
                tt("dve", WV[:, :, 0:4], EXO[:, :, 0:4], MF3, ALU.mult, [bEXO, bTABS], [bWV])
                tt("dve", WV[:, :, 4:8], EXO[:, :, 4:8], EXO[:, :, 12:16], ALU.mult, [bEXO], [bWV])
                yield
                tt("dve", WV[:, :, 4:8], WV[:, :, 4:8], MB3, ALU.mult, [bWV, bTABS], [bWV])
                cp("dve", PALL[:, 0, :], PALL[:, 4, :], [bPALL], [bPALL])
                yield
            for t_ in range(n):
                s_ = t_ % 2
                tt("pool", VS[s_][:, 0:4, :], VAG[:, t_, :, :], WV[:, t_, 0:4, None].broadcast_to([128, 4, 129]), ALU.mult, [bVAG[t_], bWV], [bVS[s_]])
                tt("pool", VS[s_][:, 4:8, :], VAG[:, t_, :, :], WV[:, t_, 4:8, None].broadcast_to([128, 4, 129]), ALU.mult, [bVAG[t_], bWV], [bVS[s_]])
                yield
                for h in range(4):
                    klhs = KTMG[:, t_, h * 128:(h + 1) * 128]
                    mm(DCF[h], klhs, VS[s_][:, h, :], True, True, [bKTMG[t_], bVS[s_]], [bDCF[h]])
                    mm(ACCB[h], klhs, VS[s_][:, 4 + h, :], True, True, [bKTMG[t_], bVS[s_]], [bACCB[h]])
                yield
                dcf3 = PS[5][:, 0:387].rearrange("p (h d) -> p h d", d=129)
                acb3 = PS[7][:, 0:387].rearrange("p (h d) -> p h d", d=129)
                if kind == "ctx":
                    if t_ == 0:
                        cp("dve", CF[:, 0:3, :], dcf3, [PB[5]], [bCF])
                        cp("dve", CF[:, 3, :], DCF[3], [PB[6]], [bCF])
                        cp("dve", CCB[:, 0:3, :], acb3, [PB[7]], [bCCB])
                        cp("dve", CCB[:, 3, :], ACCB[3], [PB[6]], [bCCB])
                    else:
                        tt("dve", CF[:, 0:3, :], CF[:, 0:3, :], dcf3, ALU.add, [bCF, PB[5]], [bCF])
                        tt("dve", CF[:, 3, :], CF[:, 3, :], DCF[3], ALU.add, [bCF, PB[6]], [bCF])
                        tt("dve", CCB[:, 0:3, :], CCB[:, 0:3, :], acb3, ALU.add, [bCCB, PB[7]], [bCCB])
                        tt("dve", CCB[:, 3, :], CCB[:, 3, :], ACCB[3], ALU.add, [bCCB, PB[6]], [bCCB])
                else:
                    tt("dve", CF[:, :, :], CF[:, :, :], EXO[:, t_, 8:12, None].broadcast_to([128, 4, 129]), ALU.mult, [bCF, bEXO], [bCF])
                    tt("dve", CF[:, 0:3, :], CF[:, 0:3, :], dcf3, ALU.add, [bCF, PB[5]], [bCF])
                    tt("dve", CF[:, 3, :], CF[:, 3, :], DCF[3], ALU.add, [bCF, PB[6]], [bCF])
                    tt("dve", CBS[:, 0:3, :], CBS[:, 0:3, :], acb3, ALU.add, [bCBS, PB[7]], [bCBS])
                    tt("dve", CBS[:, 3, :], CBS[:, 3, :], ACCB[3], ALU.add, [bCBS, PB[6]], [bCBS])
                yield

        seq = [("ctx", 0)] + [("own", g) for g in range(min(OWN // 4, cut))]
        if cut > 4:
            seq += [("oth", g) for g in range(OWN // 4, ngroups)]
        prev = None
        def interleave_w(gw):
            gw = list(gw)
            while gw:
                for item in list(gw):
                    g_, w_ = item
                    for _ in range(w_):
                        try:
                            next(g_)
                        except StopIteration:
                            gw.remove(item)
                            break

        for idx, (kind, gi) in enumerate(seq):
            gens = [(front(gi, kind, idx % 2), 1)]
            if prev is not None:
                gens.append((back(*prev), 1))
            interleave_w(gens)
            prev = (gi, kind, idx % 2)
        interleave([back(*prev)])
        dump("cf_ctx", CF[:, :, :], [128, 4, 129], [bCF])
        act(EXO[:, 0, 0:4], PALL[:, 0, :], AF.Exp, [bPALL], [bEXO])
        for h in range(4):
            if ngroups > OWN // 4 and cut > 4:
                stt("dve", CB[:, h, :], CCB[:, h, :], EXO[:, 0, h:h + 1], CBS[:, h, :], ALU.mult, ALU.add, [bCCB, bEXO, bCBS], [bCB])
            else:
                cp("dve", CB[:, h, :], CCB[:, h, :], [bCCB], [bCB])
        dump("cf_in", CF[:, :, :], [128, 4, 129], [bCF])
        dump("cb_in", CB[:, :, :], [128, 4, 129], [bCB])
        dump("ktm0", KTMO[:, 0, :], [128, 512], [bKTMO[0]])
        dump("va0", VAO[:, 0, :, :], [128, 4, 129], [bVAO[0]])
        dump("owng", OWNG[:, :, :], [128, OWN, 24], [bOWNG])
        dump("qT", QT[:, :, 0:128], [128, 4, 128], [bQT])

        if stage <= 2:
            o_b = Buf("out")
            P.barrier()
            for i in range(OWN):
                t = P.dma("sp", out_d.ap()[i * 128:(i + 1) * 128, 0:512], POSC[:, 0:512], reads=[bPOSC], writes=[o_b])
                final.append((t[0], t[1]))
            P.barrier()
            with nc.Block() as block:
                P.emit(block, final)
            sts.close()
            stm.close()
            return nc

        P.barrier()
        sts.close()
        st6 = ExitStack()

        def sb6(name, shape, dt=F32):
            return st6.enter_context(nc.sbuf_tensor(name, list(shape), dt))

        HD = [sb6("HD%d" % i, [128, OWN, 512], BF16) for i in range(2)]
        bHD = [[Buf("HD%d_%d" % (i, c)) for c in range(OWN)] for i in range(2)]
        HS = [sb6("HS%d" % i, [128, 512]) for i in range(2)]
        bHS = [Buf("HS%d" % i) for i in range(2)]
        SM = [sb6("SM%d" % i, [128, 128], BF16) for i in range(8)]
        bSM = [Buf("SM%d" % i) for i in range(8)]
        VP = [sb6("VP%d" % i, [128, 129], BF16) for i in range(8)]
        bVP = [Buf("VP%d" % i) for i in range(8)]
        CSB = sb6("CSB", [128, 8, 129], BF16)
        bCSB = [Buf("CSB%d" % i) for i in range(8)]
        RD = [sb6("RD%d" % i, [128, 8]) for i in range(8)]
        bRD = [Buf("RD%d" % i) for i in range(8)]
        bST = [Buf("ST%d" % i) for i in range(8)]
        bHSD = Buf("hs_d")

        def chain(d, h):
            q = d * 4 + h
            ST = CF if d == 0 else CB
            bsrc = bCF if d == 0 else bCB
            hsl = slice(h * 128, (h + 1) * 128)
            cp("act", CSB[:, q, :], ST[:, h, :], [bsrc, bST[q]], [bCSB[q], bST[q]])
            yield
            order = range(OWN) if d == 0 else range(OWN - 1, -1, -1)
            for c in order:
                tsl = slice(c * 128, (c + 1) * 128)
                pS, pN, pC = PS[q][:, 0:128], PS[q][:, 128:257], PS[q][:, 257:386]
                mm(pS, KT[:, h, tsl], QT[:, h, tsl], True, True, [bKT, bQT], [PB[q]])
                ts("pool", VP[q][:, :], VAO[:, c, h, :], OWNG[:, c, q:q + 1], None, ALU.mult, None, [bVAO[c], bOWNG], [bVP[q]])
                yield
                tt("dve", SM[q][:, :], pS, TRIU if d == 0 else TRIL, ALU.mult, [PB[q], bCONST], [bSM[q]])
                yield
                mm(pN, SM[q][:, :], VP[q][:, :], True, False, [bSM[q], bVP[q]], [PB[q]])
                mm(pN, QT[:, h, tsl], CSB[:, q, :], False, True, [bQT, bCSB[q]], [PB[q]])
                mm(pC, KTMO[:, c, hsl], VP[q][:, :], True, True, [bKTMO[c], bVP[q]], [PB[q]])
                yield
                eq = OWNG[:, c, 8 + q:9 + q]
                r = RD[q]
                ts("dve", r[:, 0:1], PS[q][:, 256:257], eq, None, ALU.mult, None, [PB[q], bOWNG], [bRD[q]])
                stt("dve", r[:, 1:2], r[:, 0:1], -1.0, r[:, 0:1], ALU.mult, ALU.max, [bRD[q]], [bRD[q]])
                yield
                ts("dve", r[:, 2:3], r[:, 1:2], 1.0, None, ALU.max, None, [bRD[q]], [bRD[q]])
                P.op("dve", lambda e, o=r[:, 3:4], i_=r[:, 2:3]: e.reciprocal(out=o, in_=i_), [bRD[q]], [bRD[q]])
                yield
                tt("dve", r[:, 4:5], r[:, 3:4], eq, ALU.mult, [bRD[q], bOWNG], [bRD[q]])
                tt("dve", ST[:, h, :], ST[:, h, :], pC, ALU.add, [bST[q], PB[q]], [bST[q]])
                yield
                act(HD[d][:, c, hsl], PS[q][:, 128:256], AF.Copy, [PB[q], bRD[q]], [bHD[d][c]], scale=r[:, 4:5])
                ts("dve", ST[:, h, :], ST[:, h, :], OWNG[:, c, 16 + q:17 + q], None, ALU.mult, None, [bST[q], bOWNG], [bST[q]])
                yield
                cp("act", CSB[:, q, :], ST[:, h, :], [bST[q]], [bCSB[q]])
                yield

        interleave([chain(d, h) for d in range(2) for h in range(4)])
        for c in range(OWN):
            tt("pool", HS[c % 2][:, :], HD[0][:, c, :], HD[1][:, c, :], ALU.add, [bHD[0][c], bHD[1][c]], [bHS[c % 2]])
            P.dma("sp", hs_d.ap()[c * 128:(c + 1) * 128, :], HS[c % 2][:, :], reads=[bHS[c % 2]], writes=[bHSD])
            if c in (0, 7, 15):
                dump("hs%d" % c, HS[c % 2][:, :], [128, 512], [bHS[c % 2]])

        if stage <= 3:
            o_b = Buf("out")
            P.barrier()
            for i in range(OWN):
                t = P.dma("sp", out_d.ap()[i * 128:(i + 1) * 128, 0:512], POSC[:, 0:512], reads=[bPOSC], writes=[o_b])
                final.append((t[0], t[1]))
            P.barrier()
            with nc.Block() as block:
                P.emit(block, final)
            st6.close()
            stm.close()
            return nc

        P.barrier()
        st6.close()
        stm.close()

        H2T = sb("H2T", [128, 8, OWN * 128], BF16)
        bH2T = Buf("H2T")
        COMB = sb("COMB", [128, OWN, 16])
        bCOMB = Buf("COMB")
        st7 = ExitStack()

        def sb7(name, shape, dt=F32):
            return st7.enter_context(nc.sbuf_tensor(name, list(shape), dt))

        XCS = sb7("XCS", [128, 512, 2, 16], BF16)
        bXCS = Buf("XCS")
        CS128 = sb7("CS128", [128, 384], BF16)
        bCS = Buf("CS128")
        DI = sb7("DI", [128, 640])
        bDI = Buf("DI")
        P.dma("sp", DI[:, :], dftidx.ap(), writes=[bDI])
        CSF = sb7("CSF", [128, 384])
        bCSF = Buf("CSF")
        act(CSF[:, 0:256], DI[:, 0:256], AF.Sin, [bDI], [bCSF], scale=TWO_PI / 128.0)
        ts("dve", CSF[:, 256:384], CSF[:, 128:256], -1.0, None, ALU.mult, None, [bCSF], [bCSF])
        cp("dve", CS128[:, :], CSF[:, :], [bCSF], [bCS])
        CMY = sb7("CMY", [128, 48], BF16)
        bCMY = Buf("CMY")
        act(CSF[:, 0:32], DI[:, 512:544], AF.Sin, [bDI, bCS], [bCSF], scale=TWO_PI / 128.0)
        ts("dve", CSF[:, 32:48], CSF[:, 16:32], -1.0, None, ALU.mult, None, [bCSF], [bCSF])
        cp("dve", CMY[:, :], CSF[:, 0:48], [bCSF], [bCMY])
        with ExitStack() as st8:
            def sb8(name, shape, dt=F32):
                return st8.enter_context(nc.sbuf_tensor(name, list(shape), dt))
            TW = sb8("TW", [128, 256], BF16)
            bTW = Buf("TW")
            TWF = sb8("TWF", [128, 256])
            bTWF = Buf("TWF")
            act(TWF[:, :], DI[:, 256:512], AF.Sin, [bDI], [bTWF], scale=TWO_PI / 16384.0)
            ts("dve", TW[:, :], TWF[:, :], 1.0 / math.sqrt(16384.0 * 128.0), None, ALU.mult, None, [bTWF], [bTW])
            TC3 = TW[:, None, 0:128].broadcast_to([128, 8, 128])
            TS3 = TW[:, None, 128:256].broadcast_to([128, 8, 128])
            NW_ = 3
            UL = [sb8("UL%d" % i, [128, 16, 128], BF16) for i in range(3)]
            bUL = [Buf("UL%d" % i) for i in range(3)]
            YS = [sb8("YS%d" % i, [128, 8, 256], BF16) for i in range(NW_)]
            bYS = [Buf("YS%d" % i) for i in range(NW_)]
            MT_ = [[sb8("MTW%d_%d" % (j, i), [128, 8, 128], BF16) for i in range(4)] for j in range(NW_)]
            bMT_ = [[Buf("MTW%d_%d" % (j, i)) for i in range(4)] for j in range(NW_)]
            PQ = [sb8("PQ%d" % i, [128, 2, 8, 128], BF16) for i in range(NW_)]
            bPQ = [Buf("PQ%d" % i) for i in range(NW_)]

            def fft_half(hb):
                ub, half = hb // 2, hb % 2
                u_, bu_ = UL[ub % 3], bUL[ub % 3]
                w_ = hb % NW_
                if half == 0:
                    P.dma("sp", u_[:, :, :], bass.AP(u_d, ub * 16 * T, [[128, 128], [T, 16], [1, 128]]), reads=[bUD], writes=[bu_])
                    yield
                y_, by_ = YS[w_], bYS[w_]
                for pr in range(4):
                    pb = 1 + (hb * 4 + pr) % 4
                    for cc in range(2):
                        chl = half * 8 + pr * 2 + cc
                        mm(PS[pb][:, cc * 256:(cc + 1) * 256], u_[:, chl, :], CS128[:, 0:256], True, True, [bu_, bCS], [PB[pb]])
                    cp("act", y_[:, pr * 2:pr * 2 + 2, :], PS[pb][:, :].rearrange("p (c n) -> p c n", n=256), [PB[pb]], [by_])
                    yield
                yr = y_[:, :, 0:128]
                ys_ = y_[:, :, 128:256]
                pq, bpq = PQ[w_], bPQ[w_]
                m_, bm_ = MT_[w_], bMT_[w_]
                tt("dve", m_[0][:, :, :], yr, TC3, ALU.mult, [by_, bTW], [bm_[0]])
                tt("pool", m_[2][:, :, :], yr, TS3, ALU.mult, [by_, bTW], [bm_[2]])
                yield
                tt("dve", m_[1][:, :, :], ys_, TS3, ALU.mult, [by_, bTW], [bm_[1]])
                tt("pool", m_[3][:, :, :], ys_, TC3, ALU.mult, [by_, bTW], [bm_[3]])
                yield
                tt("dve", pq[:, 0, :, :], m_[0][:, :, :], m_[1][:, :, :], ALU.subtract, [bm_[0], bm_[1]], [bpq])
                yield
                tt("dve", pq[:, 1, :, :], m_[2][:, :, :], m_[3][:, :, :], ALU.add, [bm_[2], bm_[3]], [bpq])
                yield
                pb = 5 + hb % 3
                for cc in range(8):
                    o_c = PS[pb][:, cc * 32:cc * 32 + 16]
                    o_s = PS[pb][:, cc * 32 + 16:cc * 32 + 32]
                    mm(o_c, pq[:, 0, cc, :], CMY[:, 0:16], True, False, [bpq, bCMY], [PB[pb]])
                    mm(o_c, pq[:, 1, cc, :], CMY[:, 32:48], False, True, [bpq, bCMY], [PB[pb]])
                    mm(o_s, pq[:, 0, cc, :], CMY[:, 16:32], True, False, [bpq, bCMY], [PB[pb]])
                    mm(o_s, pq[:, 1, cc, :], CMY[:, 0:16], False, True, [bpq, bCMY], [PB[pb]])
                    if cc % 4 == 3:
                        yield
                cp("act", XCS[:, hb * 8:hb * 8 + 8, :, :], PS[pb][:, 0:256].rearrange("p (c s k) -> p c s k", s=2, k=16), [PB[pb]], [bXCS])
                yield

            def window(genfs, w):
                pend = list(genfs)
                act_ = []
                while pend or act_:
                    while pend and len(act_) < w:
                        act_.append(pend.pop(0)())
                    for g_ in list(act_):
                        try:
                            next(g_)
                        except StopIteration:
                            act_.remove(g_)

            window([(lambda hb=hb: fft_half(hb)) for hb in range(64)], NW_)
            P.barrier()
        dump("xcs", XCS[:, :, :, :], [128, 512, 2, 16], [bXCS])

        if stage <= 4:
            o_b = Buf("out")
            P.barrier()
            for i in range(OWN):
                t = P.dma("sp", out_d.ap()[i * 128:(i + 1) * 128, 0:512], POSC[:, 0:512], reads=[bPOSC], writes=[o_b])
                final.append((t[0], t[1]))
            P.barrier()
            with nc.Block() as block:
                P.emit(block, final)
            st7.close()
            return nc

        st9 = ExitStack()

        def sb9(name, shape, dt=F32):
            return st9.enter_context(nc.sbuf_tensor(name, list(shape), dt))

        WOB = sb9("WOB", [128, 8, D], BF16)
        bWOB = Buf("WOB")
        P.dma("pool", WOB[:, :, :], w_out.ap().rearrange("(k p) n -> p k n", p=128), writes=[bWOB])
        WFB = sb9("WFB", [128, 4, 128], BF16)
        bWFB = Buf("WFB")
        P.dma("pool", WFB[:, :, :], w_fourier.ap().rearrange("g c d -> c g d"), writes=[bWFB])
        NWSK = sb9("NWSK", [128, 2, 512])
        bNWSK = Buf("NWSK")
        for i in range(2):
            P.dma("sp", NWSK[:, i, :], bc_rows(nrm_skip, i, 512), writes=[bNWSK])
        WRF = sb9("WRF", [128, 8, 20])
        bWRF = Buf("WRF")
        P.dma("sp", WRF[:, :, :], w_router.ap().rearrange("(k p) n -> p k n", p=128), writes=[bWRF])
        WRH = sb9("WRH", [128, 8, 20], BF16)
        WRL = sb9("WRL", [128, 8, 20], BF16)
        bWR = Buf("WR")
        cp("dve", WRH[:, :, :], WRF[:, :, :], [bWRF], [bWR])
        tt("dve", WRL[:, :, :], WRF[:, :, :], WRH[:, :, :], ALU.subtract, [bWRF, bWR], [bWR])
        MODL = sb9("MODL", [128, 3, D])
        bMODL = Buf("MODL")
        for i_, off_ in enumerate((2 * D, 3 * D, 4 * D)):
            P.dma("sp", MODL[:, i_, :], bc_rows(mod_d, 0, D, off=off_), reads=[bMODD], writes=[bMODL])
        GP1, S2, G2 = MODL[:, 0, :], MODL[:, 1, :], MODL[:, 2, :]
        bMOD = bMODL
        BR = sb9("BR", [128, 20])
        bBR = Buf("BR")
        P.dma("sp", BR[:, :], bc_rows(b_router, 0, 20), writes=[bBR])
        HSt = [sb9("HSt%d" % i, [128, 512]) for i in range(2)]
        bHSt = [Buf("HSt%d" % i) for i in range(2)]
        AZ = [sb9("AZ%d" % i, [128, 2, 512], BF16) for i in range(2)]
        bAZ_ = [Buf("AZ%d" % i) for i in range(2)]
        SM__2 = [sb9("SM__%d" % i, [128, 32]) for i in range(2)]
        bSM__2 = [Buf("SM__%d" % i) for i in range(2)]
        CEN_2 = [sb9("CEN_%d" % i, [128, 512]) for i in range(2)]
        bCEN_2 = [Buf("CEN_%d" % i) for i in range(2)]
        SQ_2 = [sb9("SQ_%d" % i, [128, 512]) for i in range(2)]
        bSQ_2 = [Buf("SQ_%d" % i) for i in range(2)]
        T1_2 = [sb9("T1_%d" % i, [128, 512]) for i in range(2)]
        bT1_2 = [Buf("T1_%d" % i) for i in range(2)]
        T2_2 = [sb9("T2_%d" % i, [128, 512]) for i in range(2)]
        bT2_2 = [Buf("T2_%d" % i) for i in range(2)]
        MBF_2 = [sb9("MBF_%d" % i, [128, 512], BF16) for i in range(2)]
        bMBF_2 = [Buf("MBF_%d" % i) for i in range(2)]
        MTt_2 = [sb9("MTt_%d" % i, [128, 4, 128], BF16) for i in range(2)]
        bMTt_2 = [Buf("MTt_%d" % i) for i in range(2)]
        XT_2 = [sb9("XT_%d" % i, [128, 8, 128], BF16) for i in range(2)]
        bXT_2 = [Buf("XT_%d" % i) for i in range(2)]
        FTB_2 = [sb9("FTB_%d" % i, [128, 4, 128], BF16) for i in range(2)]
        bFTB_2 = [Buf("FTB_%d" % i) for i in range(2)]
        YFT_2 = [sb9("YFT_%d" % i, [128, 4, 128], BF16) for i in range(2)]
        bYFT_2 = [Buf("YFT_%d" % i) for i in range(2)]
        JK_2 = [sb9("JK_%d" % i, [128, D], BF16) for i in range(2)]
        bJK_2 = [Buf("JK_%d" % i) for i in range(2)]
        SY_2 = [sb9("SY_%d" % i, [128, 8]) for i in range(2)]
        bSY_2 = [Buf("SY_%d" % i) for i in range(2)]
        TT__2 = [sb9("TT__%d" % i, [128, D]) for i in range(2)]
        bTT_2 = [Buf("TT__%d" % i) for i in range(2)]
        H2_2 = [sb9("H2_%d" % i, [128, D]) for i in range(2)]
        bH2_2 = [Buf("H2_%d" % i) for i in range(2)]
        H2H_2 = [sb9("H2H_%d" % i, [128, D], BF16) for i in range(2)]
        bH2H_2 = [Buf("H2H_%d" % i) for i in range(2)]
        H2Lw_2 = [sb9("H2Lw_%d" % i, [128, D], BF16) for i in range(2)]
        bH2Lw_2 = [Buf("H2Lw_%d" % i) for i in range(2)]
        H2LT_2 = [sb9("H2LT_%d" % i, [128, 8, 128], BF16) for i in range(2)]
        bH2LT_2 = [Buf("H2LT_%d" % i) for i in range(2)]
        LG_2 = [sb9("LG_%d" % i, [128, 20]) for i in range(2)]
        bLG_2 = [Buf("LG_%d" % i) for i in range(2)]
        RT_2 = [sb9("RT_%d" % i, [128, 96]) for i in range(2)]
        bRT_2 = [Buf("RT_%d" % i) for i in range(2)]
        XR = [sb9("XR%d" % i, [128, D]) for i in range(2)]
        bXR = [Buf("XR%d" % i) for i in range(2)]
        PRr = [sb9("PRr%d" % i, [128, 512]) for i in range(2)]
        bPRr = [Buf("PRr%d" % i) for i in range(2)]
        X1 = [sb9("X1%d" % i, [128, D]) for i in range(2)]
        bX1 = [Buf("X1%d" % i) for i in range(2)]
        bOUT = Buf("out_d")
        BIG = 30000.0
        AX = mybir.AxisListType.X

        def red(eng, out, in_, op, reads, writes):
            return P.op(eng, lambda e: e.tensor_reduce(out=out, in_=in_, axis=AX, op=op), reads, writes)

        def s6_tile(c):
            s_ = c % 2
            rows = slice(c * 128, (c + 1) * 128)
            bk = (0, 1, 2, 3) if s_ == 0 else (4, 5, 6, 7)
            PTl = PS[bk[0]][:, :].bitcast(BF16).rearrange("p (k t) -> p k t", t=128)
            SM_, bSM_ = SM__2[s_], bSM__2[s_]
            CEN, bCEN = CEN_2[s_], bCEN_2[s_]
            SQ, bSQ = SQ_2[s_], bSQ_2[s_]
            T1, bT1 = T1_2[s_], bT1_2[s_]
            T2, bT2 = T2_2[s_], bT2_2[s_]
            MBF, bMBF = MBF_2[s_], bMBF_2[s_]
            MTt, bMTt = MTt_2[s_], bMTt_2[s_]
            XT, bXT = XT_2[s_], bXT_2[s_]
            FTB, bFTB = FTB_2[s_], bFTB_2[s_]
            YFT, bYFT = YFT_2[s_], bYFT_2[s_]
            JK, bJK = JK_2[s_], bJK_2[s_]
            SY, bSY = SY_2[s_], bSY_2[s_]
            TT_, bTT = TT__2[s_], bTT_2[s_]
            H2, bH2 = H2_2[s_], bH2_2[s_]
            H2H, bH2H = H2H_2[s_], bH2H_2[s_]
            H2Lw, bH2Lw = H2Lw_2[s_], bH2Lw_2[s_]
            H2LT, bH2LT = H2LT_2[s_], bH2LT_2[s_]
            LG, bLG = LG_2[s_], bLG_2[s_]
            RT, bRT = RT_2[s_], bRT_2[s_]
            hs, bhs, az, baz = HSt[s_], bHSt[s_], AZ[s_], bAZ_[s_]
            P.dma("sp", hs[:, :], hs_d.ap()[rows, :], reads=[bHSD], writes=[bhs])
            P.dma("sp", az[:, 0, :], az_d.ap()[0, rows, :], reads=[bAZ], writes=[baz])
            P.dma("sp", az[:, 1, :], az_d.ap()[1, rows, :], reads=[bAZ], writes=[baz])
            hs3 = hs[:, :].rearrange("p (h d) -> p h d", d=128)
            cen3 = CEN[:, :].rearrange("p (h d) -> p h d", d=128)
            sq3 = SQ[:, :].rearrange("p (h d) -> p h d", d=128)
            yield
            red("dve", SM_[:, 0:4], hs3, ALU.add, [bhs], [bSM_])
            ts("dve", SM_[:, 4:8], SM_[:, 0:4], 1.0 / 128.0, None, ALU.mult, None, [bSM_], [bSM_])
            tt("dve", cen3, hs3, SM_[:, 4:8, None].broadcast_to([128, 4, 128]), ALU.subtract, [bhs, bSM_], [bCEN])
            yield
            tt("pool", SQ[:, :], CEN[:, :], CEN[:, :], ALU.mult, [bCEN], [bSQ])
            red("dve", SM_[:, 8:12], sq3, ALU.add, [bSQ], [bSM_])
            yield
            ts("pool", SM_[:, 12:16], SM_[:, 8:12], 1.0 / 128.0, EPS, ALU.mult, ALU.add, [bSM_], [bSM_])
            tt("pool", SM_[:, 16:20], SM_[:, 12:16], NEGH.broadcast_to([128, 4]), ALU.pow, [bSM_, bTABS], [bSM_])
            tt("dve", cen3, cen3, SM_[:, 16:20, None].broadcast_to([128, 4, 128]), ALU.mult, [bCEN, bSM_], [bCEN])
            yield
            tt("dve", T1[:, :], CEN[:, :], NWSK[:, 0, :], ALU.mult, [bCEN, bNWSK], [bT1])
            tt("pool", T2[:, :], az[:, 0, :], NWSK[:, 1, :], ALU.mult, [baz, bNWSK], [bT2])
            tt("dve", T1[:, :], T1[:, :], T2[:, :], ALU.add, [bT1, bT2], [bT1])
            yield
            tt("dve", MBF[:, :], T1[:, :], az[:, 1, :], ALU.mult, [bT1, baz], [bMBF])
            if c == 0:
                dump("m0", MBF[:, :], [128, 512], [bMBF])
            yield
            for cc in range(4):
                tr(PTl[:, cc, :], MBF[:, cc * 128:(cc + 1) * 128], IDB[:, :], [bMBF, bIDB], [PB[bk[0]]])
            cp("act", MTt[:, :, :], PTl[:, 0:4, :], [PB[bk[0]]], [bMTt])
            yield
            for sg in range(2):
                for g in range(4):
                    tr(PTl[:, sg * 4 + g, :], XCS[:, g * 128:(g + 1) * 128, sg, c], IDB[:, :], [bXCS, bIDB], [PB[bk[0]]])
            cp("act", XT[:, :, :], PTl, [PB[bk[0]]], [bXT])
            yield
            for g in range(4):
                mm(PS[bk[1]][:, g * 128:(g + 1) * 128], CS128[:, 0:128], XT[:, g, :], True, False, [bCS, bXT], [PB[bk[1]]])
                mm(PS[bk[1]][:, g * 128:(g + 1) * 128], CS128[:, 256:384], XT[:, 4 + g, :], False, True, [bCS, bXT], [PB[bk[1]]])
            cp("dve", FTB[:, :, :], PS[bk[1]][:, :].rearrange("p (g t) -> p g t", t=128), [PB[bk[1]]], [bFTB])
            yield
            for g in range(4):
                mm(PS[bk[1]][:, g * 128:(g + 1) * 128], WFB[:, g, :], FTB[:, g, :], True, True, [bWFB, bFTB], [PB[bk[1]]])
            cp("act", YFT[:, :, :], PS[bk[1]][:, :].rearrange("p (g t) -> p g t", t=128), [PB[bk[1]]], [bYFT])
            yield
            for cb in range(2):
                for kc in range(4):
                    mm(PS[bk[2 + cb]][:, :], MTt[:, kc, :], WOB[:, kc, cb * 512:(cb + 1) * 512], kc == 0, False, [bMTt, bWOB], [PB[bk[2 + cb]]])
                for kc in range(4):
                    mm(PS[bk[2 + cb]][:, :], YFT[:, kc, :], WOB[:, 4 + kc, cb * 512:(cb + 1) * 512], False, kc == 3, [bYFT, bWOB], [PB[bk[2 + cb]]])
            yield
            act(JK[:, 0:512], PS[bk[2]][:, :], AF.Square, [PB[bk[2]]], [bJK, bSY], accum_out=SY[:, 0:1])
            act(JK[:, 512:1024], PS[bk[3]][:, :], AF.Square, [PB[bk[3]]], [bJK, bSY], accum_out=SY[:, 1:2])
            yield
            tt("pool", SY[:, 2:3], SY[:, 0:1], SY[:, 1:2], ALU.add, [bSY], [bSY])
            ts("pool", SY[:, 3:4], SY[:, 2:3], 1.0 / D, EPS, ALU.mult, ALU.add, [bSY], [bSY])
            tt("pool", SY[:, 4:5], SY[:, 3:4], NEGH, ALU.pow, [bSY, bTABS], [bSY])
            yield
            xr, bxr, pr_, bpr_ = XR[s_], bXR[s_], PRr[s_], bPRr[s_]
            P.dma("sp", xr[:, :], x_rot.ap()[rows, :], writes=[bxr])
            P.dma("sp", pr_[0:64, :], bc_rows(posr_d, 2 * c, 512, parts=64), reads=[bPOSRD], writes=[bpr_])
            P.dma("sp", pr_[64:128, :], bc_rows(posr_d, 2 * c + 1, 512, parts=64), reads=[bPOSRD], writes=[bpr_])
            tt("pool", xr[:, 0:512], xr[:, 0:512], pr_[:, :], ALU.add, [bxr, bpr_], [bxr])
            tt("pool", xr[:, 512:1024], xr[:, 512:1024], POSC[:, :], ALU.add, [bxr, bPOSC], [bxr])
            yield
            stt("dve", TT_[:, 0:512], PS[bk[2]][:, :], SY[:, 4:5], GP1[:, 0:512], ALU.mult, ALU.mult, [PB[bk[2]], bSY, bMOD], [bTT])
            stt("dve", TT_[:, 512:1024], PS[bk[3]][:, :], SY[:, 4:5], GP1[:, 512:1024], ALU.mult, ALU.mult, [PB[bk[3]], bSY, bMOD], [bTT])
            yield
            x1, bx1 = X1[s_], bX1[s_]
            tt("pool", x1[:, :], TT_[:, :], xr[:, :], ALU.add, [bTT, bxr], [bx1])
            P.dma("sp", out_d.ap()[rows, :], x1[:, :], reads=[bx1], writes=[bOUT])
            if c == 0:
                dump("x1_0", x1[:, :], [128, D], [bx1])
            yield
            act(JK[:, :], x1[:, :], AF.Square, [bx1], [bJK, bSY], accum_out=SY[:, 5:6])
            yield
            ts("pool", SY[:, 6:7], SY[:, 5:6], 1.0 / D, EPS, ALU.mult, ALU.add, [bSY], [bSY])
            tt("pool", SY[:, 7:8], SY[:, 6:7], NEGH, ALU.pow, [bSY, bTABS], [bSY])
            yield
            stt("dve", TT_[:, :], x1[:, :], SY[:, 7:8], G2, ALU.mult, ALU.mult, [bx1, bSY, bMOD], [bTT])
            tt("pool", H2[:, :], TT_[:, :], S2, ALU.add, [bTT, bMOD], [bH2])
            yield
            cp("act", H2H[:, :], H2[:, :], [bH2], [bH2H])
            tt("dve", H2Lw[:, :], H2[:, :], H2H[:, :], ALU.subtract, [bH2, bH2H], [bH2Lw])
            yield
            for kc in range(8):
                tr(PTl[:, kc, :], H2H[:, kc * 128:(kc + 1) * 128], IDB[:, :], [bH2H, bIDB], [PB[bk[0]]])
            cp("act", H2T[:, :, rows], PTl, [PB[bk[0]]], [bH2T])
            yield
            for kc in range(8):
                tr(PTl[:, kc, :], H2Lw[:, kc * 128:(kc + 1) * 128], IDB[:, :], [bH2Lw, bIDB], [PB[bk[0]]])
            cp("dve", H2LT[:, :, :], PTl, [PB[bk[0]]], [bH2LT])
            yield
            LGp = PS[bk[1]][:, 0:20]
            for kc in range(8):
                mm(LGp, H2T[:, kc, rows], WRH[:, kc, :], kc == 0, False, [bH2T, bWR], [PB[bk[1]]])
            for kc in range(8):
                mm(LGp, H2T[:, kc, rows], WRL[:, kc, :], False, False, [bH2T, bWR], [PB[bk[1]]])
            for kc in range(8):
                mm(LGp, H2LT[:, kc, :], WRH[:, kc, :], False, kc == 7, [bH2LT, bWR], [PB[bk[1]]])
            yield
            tt("dve", LG[:, :], LGp, BR[:, :], ALU.add, [PB[bk[1]], bBR], [bLG])
            if c == 0:
                dump("lg0", LG[:, :], [128, 20], [bLG])
            R = RT
            bR = bRT
            yield
            red("dve", R[:, 0:1], LG[:, 0:4], ALU.max, [bLG], [bR])
            ts("dve", R[:, 1:5], LG[:, 0:4], R[:, 0:1], None, ALU.is_equal, None, [bLG, bR], [bR])
            ts("dve", R[:, 5:6], R[:, 0:1], -1.0, None, ALU.mult, None, [bR], [bR])
            yield
            act(R[:, 6:10], LG[:, 0:4], AF.Exp, [bLG, bR], [bR], bias=R[:, 5:6], accum_out=R[:, 10:11])
            P.op("dve", lambda e, o=R[:, 11:12], i_=R[:, 10:11]: e.reciprocal(out=o, in_=i_), [bR], [bR])
            yield
            ts("dve", R[:, 12:16], R[:, 1:5], BIG, -BIG, ALU.mult, ALU.add, [bR], [bR])
            em = R[:, 16:32]
            tt("dve", em.rearrange("p (g j) -> p g j", j=4), LG[:, 4:20].rearrange("p (g j) -> p g j", j=4),
               R[:, 12:16, None].broadcast_to([128, 4, 4]), ALU.add, [bLG, bR], [bR])
            yield
            red("dve", R[:, 32:33], em, ALU.max, [bR], [bR])
            ts("dve", R[:, 48:64], em, R[:, 32:33], None, ALU.is_equal, None, [bR], [bR])
            stt("dve", R[:, 64:80], R[:, 48:64], -BIG, em, ALU.mult, ALU.add, [bR], [bR])
            yield
            red("dve", R[:, 33:34], R[:, 64:80], ALU.max, [bR], [bR])
            ts("dve", R[:, 80:96], R[:, 64:80], R[:, 33:34], None, ALU.is_equal, None, [bR], [bR])
            tt("dve", R[:, 34:35], R[:, 33:34], R[:, 32:33], ALU.subtract, [bR], [bR])
            yield
            act(R[:, 35:36], R[:, 34:35], AF.Exp, [bR], [bR])
            ts("dve", R[:, 36:37], R[:, 35:36], 1.0, None, ALU.add, None, [bR], [bR])
            P.op("dve", lambda e, o=R[:, 37:38], i_=R[:, 36:37]: e.reciprocal(out=o, in_=i_), [bR], [bR])
            tt("dve", R[:, 38:39], R[:, 37:38], R[:, 35:36], ALU.mult, [bR], [bR])
            yield
            tt("dve", R[:, 39:40], R[:, 37:38], R[:, 11:12], ALU.mult, [bR], [bR])
            tt("dve", R[:, 40:41], R[:, 38:39], R[:, 11:12], ALU.mult, [bR], [bR])
            ts("dve", COMB[:, c, :], R[:, 48:64], R[:, 39:40], None, ALU.mult, None, [bR], [bCOMB])
            stt("dve", COMB[:, c, :], R[:, 80:96], R[:, 40:41], COMB[:, c, :], ALU.mult, ALU.add, [bR, bCOMB], [bCOMB])
            yield

        def window6(genfs, w):
            pend = list(genfs)
            act_ = []
            while pend or act_:
                while pend and len(act_) < w:
                    act_.append(pend.pop(0)())
                for g_ in list(act_):
                    try:
                        next(g_)
                    except StopIteration:
                        act_.remove(g_)

        window6([(lambda c=c: s6_tile(c)) for c in range(OWN)], 2)
        dump("comb", COMB[:, :, :], [128, OWN, 16], [bCOMB])

        if stage <= 5:
            o_b = bOUT
            P.barrier()
            final.append((P.sem["sp"], 0))
            P.barrier()
            with nc.Block() as block:
                P.emit(block, [])
            st9.close()
            st7.close()
            return nc

        P.barrier()
        st9.close()
        st7.close()
        stE = ExitStack()

        def sbE(name, shape, dt=F32):
            return stE.enter_context(nc.sbuf_tensor(name, list(shape), dt))

        YACC = sbE("YACC", [128, OWN, D])
        bYACC = [Buf("YACC%d" % i) for i in range(OWN)]
        WG = [sbE("WG%d" % i, [128, 8, 512], BF16) for i in range(2)]
        WU = [sbE("WU%d" % i, [128, 8, 512], BF16) for i in range(2)]
        WD = [sbE("WD%d" % i, [128, 4, D], BF16) for i in range(2)]
        bWG = [Buf("WG%d" % i) for i in range(2)]
        bWU = [Buf("WU%d" % i) for i in range(2)]
        bWD = [Buf("WD%d" % i) for i in range(2)]
        ATb = [sbE("AT%d" % i, [128, 4, 512], BF16) for i in range(2)]
        bAT = [Buf("AT%d" % i) for i in range(2)]
        SG = [sbE("SG%d" % i, [128, 512]) for i in range(2)]
        bSG = [Buf("SG%d" % i) for i in range(2)]
        NEXP = 16
        gu_ctr = [0]
        dn_ctr = [0]
        for e_ in range(NEXP):
            s_ = e_ % 2
            P.dma("pool", WG[s_][:, :, :], w_gate.ap()[e_].rearrange("(k p) n -> p k n", p=128), writes=[bWG[s_]])
            P.dma("pool", WU[s_][:, :, :], w_up.ap()[e_].rearrange("(k p) n -> p k n", p=128), writes=[bWU[s_]])
            P.dma("pool", WD[s_][:, :, :], w_down.ap()[e_].rearrange("(k p) n -> p k n", p=128), writes=[bWD[s_]])
            for tb in range(OWN // 4):
                tsl = slice(tb * 512, (tb + 1) * 512)
                a_ = (e_ * 4 + tb) % 2
                for fb in range(4):
                    k_ = gu_ctr[0] % 2
                    gu_ctr[0] += 1
                    pg, pu = 2 * k_, 2 * k_ + 1
                    for kc in range(8):
                        mm(PS[pg][:, :], WG[s_][:, kc, fb * 128:(fb + 1) * 128], H2T[:, kc, tsl], kc == 0, kc == 7, [bWG[s_], bH2T], [PB[pg]])
                    for kc in range(8):
                        mm(PS[pu][:, :], WU[s_][:, kc, fb * 128:(fb + 1) * 128], H2T[:, kc, tsl], kc == 0, kc == 7, [bWU[s_], bH2T], [PB[pu]])
                    act(SG[k_][:, :], PS[pg][:, :], AF.Silu, [PB[pg]], [bSG[k_]])
                    tt("dve", ATb[a_][:, fb, :], SG[k_][:, :], PS[pu][:, :], ALU.mult, [bSG[k_], PB[pu]], [bAT[a_]])
                for t_ in range(4):
                    tile = tb * 4 + t_
                    for cb in range(2):
                        pd = 4 + dn_ctr[0] % 4
                        dn_ctr[0] += 1
                        for fb in range(4):
                            mm(PS[pd][:, :], ATb[a_][:, fb, t_ * 128:(t_ + 1) * 128], WD[s_][:, fb, cb * 512:(cb + 1) * 512], fb == 0, fb == 3, [bAT[a_], bWD[s_]], [PB[pd]])
                        ya = YACC[:, tile, cb * 512:(cb + 1) * 512]
                        if e_ == 0:
                            ts("dve", ya, PS[pd][:, :], COMB[:, tile, e_:e_ + 1], None, ALU.mult, None, [PB[pd], bCOMB], [bYACC[tile]])
                        else:
                            stt("dve", ya, PS[pd][:, :], COMB[:, tile, e_:e_ + 1], ya, ALU.mult, ALU.add, [PB[pd], bCOMB, bYACC[tile]], [bYACC[tile]])
        GP2L = sbE("GP2L", [128, D])
        bGP2L = Buf("GP2L")
        P.dma("sp", GP2L[:, :], bc_rows(mod_d, 0, D, off=5 * D), reads=[bMODD], writes=[bGP2L])
        GP2 = GP2L[:, :]
        bMOD = bGP2L
        XF = [sbE("XF%d" % i, [128, D]) for i in range(2)]
        bXF = [Buf("XF%d" % i) for i in range(2)]
        JK2 = sbE("JK2", [128, D], BF16)
        bJK2 = Buf("JK2")
        SF = sbE("SF", [128, 8])
        bSF = Buf("SF")
        OT = [sbE("OT%d" % i, [128, D]) for i in range(2)]
        bOT = [Buf("OT%d" % i) for i in range(2)]
        for c in range(OWN):
            s_ = c % 2
            rows = slice(c * 128, (c + 1) * 128)
            P.dma("sp", XF[s_][:, :], out_d.ap()[rows, :], reads=[bOUT], writes=[bXF[s_]])
            q_ = SF[:, s_ * 4:s_ * 4 + 4]
            act(JK2[:, :], YACC[:, c, :], AF.Square, [bYACC[c]], [bJK2, bSF], accum_out=q_[:, 0:1])
            ts("pool", q_[:, 1:2], q_[:, 0:1], 1.0 / D, EPS, ALU.mult, ALU.add, [bSF], [bSF])
            tt("pool", q_[:, 2:3], q_[:, 1:2], NEGH, ALU.pow, [bSF, bTABS], [bSF])
            stt("dve", OT[s_][:, :], YACC[:, c, :], q_[:, 2:3], GP2, ALU.mult, ALU.mult, [bYACC[c], bSF, bMOD], [bOT[s_]])
            tt("pool", OT[s_][:, :], OT[s_][:, :], XF[s_][:, :], ALU.add, [bOT[s_], bXF[s_]], [bOT[s_]])
            t = P.dma("sp", out_d.ap()[rows, :], OT[s_][:, :], reads=[bOT[s_], bXF[s_]], writes=[bOUT])
            final.append((t[0], t[1]))
        P.barrier()
        with nc.Block() as block:
            P.emit(block, final)
        stE.close()
    return nc


def _centered(idx, n):
    return ((idx + n // 2) % n) - n // 2


def make_inputs(core, x, c, ctx, c_ctx, w_ada, b_ada, g_pre_mix, g_post_mix, g_pre_ffn, g_post_ffn,
                w_in, conv_w, conv_b, w_q, w_k, w_v, w_if_fwd, b_if_fwd, w_if_bwd, b_if_bwd,
                mlstm_norm_w, mlstm_skip, w_fourier, w_out, w_router_group, b_router_group,
                w_router_expert, b_router_expert, w_gate, w_up, w_down, shared):
    f32 = np.float32
    j = core
    xs = x[0]
    m = {}
    m["x_rot"] = np.ascontiguousarray(np.roll(xs, -2048 * j, axis=0))
    halo = np.zeros((128, D), f32)
    hmask = np.zeros(64, f32)
    hrow = np.zeros(128, f32)
    hcol = np.zeros(128, f32)
    for g in range(NG):
        for side, tr_ in ((0, 512 * g - 1), (1, 512 * g + 512)):
            true_t = (tr_ % T + 2048 * j) % T
            own_first_true = ((512 * g) % T + 2048 * j) % T
            if side == 0:
                valid = own_first_true != 0
            else:
                valid = ((512 * g + 511) % T + 2048 * j) % T != T - 1
            halo[2 * g + side] = xs[true_t]
            hmask[2 * g + side] = 1.0 if valid else 0.0
            hrow[2 * g + side] = true_t // 64
            hcol[2 * g + side] = true_t % 64
    m["x_halo"] = halo
    tabs = np.zeros((128, 1024), f32)
    p = np.arange(128)
    for a in range(2):
        tabs[:, a] = ((2 * p + a) + 32 * j) % 256
    tabs[:, 2] = p % 64
    tabs[:, 3] = hrow
    tabs[:, 4] = hcol
    tabs[:, 5] = -0.5
    i = np.arange(128)
    true_c = (i + 16 * j) % 128
    tabs[:, 128:256] = (true_c < 16 * j).astype(f32)[None, :]
    tabs[:, 256:384] = (true_c >= 16 * j + 16).astype(f32)[None, :]
    tabs[:, 384:448] = hmask[None, :]
    tabs[:, 512:768] = np.arange(256, dtype=f32)[None, :]
    m["tabs"] = tabs
    di = np.zeros((128, 640), f32)
    n = np.arange(128)[:, None]
    k = np.arange(128)[None, :]
    di[:, 0:128] = _centered(n * k + 32, 128)
    di[:, 128:256] = _centered(n * k, 128)
    base = n * k + 2048 * j * k
    di[:, 256:384] = _centered(base + 4096, 16384)
    di[:, 384:512] = _centered(base, 16384)
    cc = (16 * j + np.arange(16))[None, :]
    di[:, 512:528] = _centered(n * cc + 32, 128)
    di[:, 528:544] = _centered(n * cc, 128)
    m["dftidx"] = di
    m.update(shared)
    return m


def make_shared(x, c, ctx, c_ctx, w_ada, b_ada, g_pre_mix, g_post_mix, g_pre_ffn, g_post_ffn,
                w_in, conv_w, conv_b, w_q, w_k, w_v, w_if_fwd, b_if_fwd, w_if_bwd, b_if_bwd,
                mlstm_norm_w, mlstm_skip, w_fourier, w_out, w_router_group, b_router_group,
                w_router_expert, b_router_expert, w_gate, w_up, w_down):
    f32 = np.float32
    s = {}
    s["ctx"] = np.ascontiguousarray(ctx[0])
    cT = np.zeros((128, 16), f32)
    cT[:, 0:8] = c[0].reshape(8, 128).T
    cT[:, 8:16] = c_ctx.reshape(8, 128).T
    s["cT"] = cT
    s["w_ada"] = np.ascontiguousarray(w_ada[0])
    s["b_ada"] = np.ascontiguousarray(b_ada[0][None, :])
    s["gains"] = np.stack([g_pre_mix[0], g_post_mix[0], g_pre_ffn[0], g_post_ffn[0]]).astype(f32)
    s["w_in"] = np.ascontiguousarray(w_in[0])
    cw = np.zeros((128, 16), f32)
    for cc in range(4):
        for k in range(3):
            cw[:, cc * 3 + k] = conv_w[0][k, cc * 128:(cc + 1) * 128]
        cw[:, 12 + cc] = conv_b[0][cc * 128:(cc + 1) * 128]
    s["convw"] = cw
    s["w_qkv"] = np.stack([w_q[0].reshape(512, 4), w_k[0].reshape(512, 4), w_v[0].reshape(512, 4)]).astype(f32)
    s["w_qkvT"] = np.stack([np.ascontiguousarray(w.transpose(0, 2, 1)).reshape(512, 4) for w in (w_q[0], w_k[0], w_v[0])]).astype(f32)
    wf, wb = w_if_fwd[0], w_if_bwd[0]
    s["w_if"] = np.ascontiguousarray(np.concatenate([wf[:, 0:4], wb[:, 0:4], wf[:, 4:8], wb[:, 4:8]], axis=1))
    bf, bb = b_if_fwd[0], b_if_bwd[0]
    s["b_if"] = np.concatenate([bf[0:4], bb[0:4], bf[4:8], bb[4:8]])[None, :].astype(f32)
    s["nrm_skip"] = np.stack([mlstm_norm_w[0], mlstm_skip[0]]).astype(f32)
    s["w_fourier"] = np.ascontiguousarray(w_fourier[0])
    s["w_out"] = np.ascontiguousarray(w_out[0])
    s["w_router"] = np.ascontiguousarray(np.concatenate([w_router_group[0], w_router_expert[0]], axis=1))
    s["b_router"] = np.concatenate([b_router_group[0], b_router_expert[0]])[None, :].astype(f32)
    s["w_gate"] = np.ascontiguousarray(w_gate[0])
    s["w_up"] = np.ascontiguousarray(w_up[0])
    s["w_down"] = np.ascontiguousarray(w_down[0])
    cst = np.zeros((128, 640), f32)
    cst[:, 0:128] = np.eye(128)
    a = np.arange(128)
    cst[:, 128:256] = (a[:, None] <= a[None, :])
    cst[:, 256:384] = (a[:, None] >= a[None, :])
    cst[:, 384:512] = 1.0
    cst[:, 512:640] = (a[:, None] // 4 == a[None, :] // 4)
    s["consts"] = cst
    return s


_CACHE = {}


def kernel(**inputs):
    inputs = {k: np.asarray(v) for k, v in inputs.items()}
    if "nc" not in _CACHE:
        _CACHE["nc"] = build()
    nc = _CACHE["nc"]
    shared = make_shared(**inputs)
    in_maps = [make_inputs(core, shared=shared, **inputs) for core in range(NCORES)]
    res = run_bass_kernel_spmd(nc, in_maps, core_ids=list(range(NCORES)))
    out = np.concatenate([res.results[i]["out"] for i in range(NCORES)], axis=0)
    return out.reshape(1, T, D).astype(np.float32)
```

```python
import math
from contextlib import ExitStack
import numpy as np
import concourse.bass as bass
import concourse.mybir as mybir
from concourse.bass_utils import run_bass_kernel_spmd

F32 = mybir.dt.float32
BF16 = mybir.dt.bfloat16
I32 = mybir.dt.int32
AF = mybir.ActivationFunctionType
ALU = mybir.AluOpType

NCORES = 8
T = 16384
D = 1024
NT = T // 128
OWN = NT // NCORES
NG = NT // 4
EPS = 1e-6
TWO_PI = 2.0 * math.pi
NHALO = 2 * NG
DBG = {}


class Buf:
    __slots__ = ("name", "w", "r")

    def __init__(self, name):
        self.name = name
        self.w = None
        self.r = {}


class Prog:
    def __init__(self, nc, stack, ndma=28):
        self.nc = nc
        self.engs = {"pe": nc.tensor, "act": nc.scalar, "dve": nc.vector, "pool": nc.gpsimd, "sp": nc.sync}
        self.ops = {k: [] for k in self.engs}
        self.sem = {k: stack.enter_context(nc.semaphore("s_" + k)) for k in self.engs}
        self.cnt = {k: 0 for k in self.engs}
        self.waited = {k: {} for k in self.engs}
        self.dsem = [stack.enter_context(nc.semaphore("d%d" % i)) for i in range(ndma)]
        self.dcnt = [0] * ndma
        self.ring = {"sp": list(range(0, ndma - 8)), "act": list(range(0, ndma - 8)), "pool": list(range(ndma - 8, ndma))}
        self.rpos = {"sp": 0, "pool": 0}

    def _collect(self, e, reads, writes, sync_same=True):
        waits = {}

        def need(tok):
            if tok is None:
                return
            sem, val, eng = tok
            if eng == e and not sync_same:
                return
            key = id(sem)
            if self.waited[e].get(key, 0) >= val:
                return
            if key not in waits or waits[key][1] < val:
                waits[key] = (sem, val)

        for b in reads:
            need(b.w)
        for b in writes:
            need(b.w)
            for t in b.r.values():
                need(t)
        for key, (sem, val) in waits.items():
            self.waited[e][key] = val
        return list(waits.values())

    def _commit(self, tok, reads, writes):
        key = id(tok[0])
        for b in reads:
            old = b.r.get(key)
            if old is None or old[1] < tok[1]:
                b.r[key] = tok
        for b in writes:
            b.w = tok
            b.r = {}

    def op(self, e, fn, reads=(), writes=(), sync_same=True):
        waits = self._collect(e, reads, writes, sync_same)
        self.cnt[e] += 1
        tok = (self.sem[e], self.cnt[e], e)
        self.ops[e].append((waits, fn, (self.sem[e], 1)))
        self._commit(tok, reads, writes)
        return tok

    def dma(self, e, out, in_, reads=(), writes=()):
        rk = "pool" if e == "pool" else "sp"
        ring = self.ring[rk]
        i = ring[self.rpos[rk] % len(ring)]
        self.rpos[rk] += 1
        sem = self.dsem[i]
        waits = self._collect(e, reads, writes)
        prev = self.dcnt[i] * 16
        if prev > 0 and self.waited[e].get(id(sem), 0) < prev:
            waits.append((sem, prev))
            self.waited[e][id(sem)] = prev
        self.dcnt[i] += 1
        tok = (sem, self.dcnt[i] * 16, "dma")
        self.ops[e].append((waits, (lambda eng, o=out, i_=in_: eng.dma_start(out=o, in_=i_)), (sem, 16)))
        self._commit(tok, reads, writes)
        return tok

    def barrier(self):
        toks = [(self.sem[k], self.cnt[k]) for k in self.engs if self.cnt[k] > 0]
        toks += [(self.dsem[i], self.dcnt[i] * 16) for i in range(len(self.dsem)) if self.dcnt[i] > 0]
        for e in self.engs:
            waits = []
            for sem, val in toks:
                if self.waited[e].get(id(sem), 0) < val:
                    waits.append((sem, val))
                    self.waited[e][id(sem)] = val
            if waits:
                self.ops[e].append((waits, None, None))

    def emit(self, block, final_waits):
        def replay(e):
            def body(eng):
                for waits, fn, inc in self.ops[e]:
                    for sem, val in waits:
                        eng.wait_ge(sem, val)
                    if fn is not None:
                        ins = fn(eng)
                        ins.then_inc(inc[0], inc[1])
                if e == "sp":
                    for sem, val in final_waits:
                        eng.wait_ge(sem, val)
            return body

        block.tensor(replay("pe"))
        block.scalar(replay("act"))
        block.vector(replay("dve"))
        block.gpsimd(replay("pool"))
        block.sync(replay("sp"))


def build(stage=99, dbg=False, ngroups=NG, cut=99):
    nc = bass.Bass("TRN2", target_bir_lowering=False)
    DBG.clear()

    def din(name, shape, dt=F32):
        return nc.dram_tensor(name, list(shape), dt, kind="ExternalInput")

    x_rot = din("x_rot", [ngroups * 512, D])
    x_halo = din("x_halo", [128, D])
    ctx_in = din("ctx", [256, D])
    cT = din("cT", [128, 16])
    w_ada = din("w_ada", [D, 6 * D])
    b_ada = din("b_ada", [1, 6 * D])
    gains = din("gains", [4, D])
    w_in = din("w_in", [D, 1536])
    convw = din("convw", [128, 16])
    w_qkv = din("w_qkv", [3, 512, 4])
    w_qkvT = din("w_qkvT", [3, 512, 4])
    w_if = din("w_if", [1536, 16])
    b_if = din("b_if", [1, 16])
    nrm_skip = din("nrm_skip", [2, 512])
    w_fourier = din("w_fourier", [4, 128, 128])
    w_out = din("w_out", [D, D])
    w_router = din("w_router", [D, 20])
    b_router = din("b_router", [1, 20])
    moe_small = stage < 8
    w_gate = din("w_gate", [16, D, 512] if not moe_small else [1, 8, 8])
    w_up = din("w_up", [16, D, 512] if not moe_small else [1, 8, 8])
    w_down = din("w_down", [16, 512, D] if not moe_small else [1, 8, 8])
    consts = din("consts", [128, 5 * 128])
    tabs = din("tabs", [128, 1024])
    dftidx = din("dftidx", [128, 5 * 128])
    out_d = nc.dram_tensor("out", [OWN * 128, D], F32, kind="ExternalOutput")

    posr_d = nc.dram_tensor("posr_d", [256, 512], F32)
    u_d = nc.dram_tensor("u_d", [512, T], BF16)
    az_d = nc.dram_tensor("az_d", [2, OWN * 128, 512], BF16)
    hs_d = nc.dram_tensor("hs_d", [OWN * 128, 512], F32)
    mod_d = nc.dram_tensor("mod_d", [1, 6 * D], F32)

    dbg_outs = {}

    def dbg_out(name, shape):
        DBG[name] = tuple(shape)
        dbg_outs[name] = nc.dram_tensor("dbg_" + name, list(shape), F32, kind="ExternalOutput")
        return dbg_outs[name]

    stack = ExitStack()
    with stack:
        P = Prog(nc, stack)
        used = [0]

        def sb(name, shape, dt=F32):
            t = stack.enter_context(nc.sbuf_tensor(name, list(shape), dt))
            return t

        def psum(name, shape, dt=F32):
            return stack.enter_context(nc.psum_tensor(name, list(shape), dt))

        def act(out, in_, func, reads, writes, eng="act", **kw):
            return P.op("act", lambda e: e.activation(out=out, in_=in_, func=func, **kw), reads, writes)

        def tt(eng, out, in0, in1, op, reads, writes):
            return P.op(eng, lambda e: e.tensor_tensor(out=out, in0=in0, in1=in1, op=op), reads, writes)

        def ts(eng, out, in0, s1, s2, op0, op1, reads, writes):
            if op1 is None:
                return P.op(eng, lambda e: e.tensor_scalar(out=out, in0=in0, scalar1=s1, scalar2=None, op0=op0), reads, writes)
            return P.op(eng, lambda e: e.tensor_scalar(out=out, in0=in0, scalar1=s1, scalar2=s2, op0=op0, op1=op1), reads, writes)

        def stt(eng, out, in0, scalar, in1, op0, op1, reads, writes):
            return P.op(eng, lambda e: e.scalar_tensor_tensor(out=out, in0=in0, scalar=scalar, in1=in1, op0=op0, op1=op1), reads, writes)

        def cp(eng, out, in_, reads, writes):
            if eng == "act":
                return P.op("act", lambda e: e.activation(out=out, in_=in_, func=AF.Copy), reads, writes)
            return P.op(eng, lambda e: e.tensor_copy(out=out, in_=in_), reads, writes)

        def mm(out, lhsT, rhs, start, stop, reads, writes):
            return P.op("pe", lambda e: e.matmul(out, lhsT=lhsT, rhs=rhs, start=start, stop=stop), reads, writes, sync_same=False)

        def tr(out, in_, ident, reads, writes):
            return P.op("pe", lambda e: e.transpose(out=out, in_=in_, identity=ident), reads, writes, sync_same=False)

        def memset(eng, ap, val, writes):
            return P.op(eng, lambda e: e.memset(ap, val), (), writes)

        def dump(name, ap, shape, reads):
            if not dbg:
                return
            dd = dbg_out(name, shape)
            P.dma("pool", dd.ap(), ap, reads=reads, writes=[Buf("dbg")])

        def bc_rows(dram_t, row, n, parts=128, off=0):
            width = dram_t.shape[-1]
            return bass.AP(dram_t, row * width + off, [[0, parts], [1, n]])

        PS = [psum("ps%d" % i, [128, 512]) for i in range(8)]
        PB = [Buf("ps%d" % i) for i in range(8)]

        CONST = sb("CONST", [128, 640])
        bCONST = Buf("CONST")
        P.dma("sp", CONST[:, :], consts.ap(), writes=[bCONST])
        IDF = CONST[:, 0:128]
        TRIU = CONST[:, 128:256]
        TRIL = CONST[:, 256:384]
        ONES = CONST[:, 384:512]
        BDM = CONST[:, 512:640]
        IDB = sb("IDB", [128, 128], BF16)
        bIDB = Buf("IDB")
        cp("dve", IDB[:, :], IDF, [bCONST], [bIDB])
        TABS = sb("TABS", [128, 1024])
        bTABS = Buf("TABS")
        P.dma("sp", TABS[:, :], tabs.ap(), writes=[bTABS])
        NEGH = TABS[:, 5:6]
        POSC = sb("POSC", [128, 512])
        bPOSC = Buf("POSC")

        final = []
        stm = ExitStack()

        def sbm(name, shape, dt=F32):
            return stm.enter_context(nc.sbuf_tensor(name, list(shape), dt))

        QTF = sbm("QTF", [128, 4 * OWN * 128], BF16)
        QT = QTF[:, :].rearrange("p (c t) -> p c t", c=4)
        bQT = Buf("QT")
        KTF = sbm("KTF", [128, 4 * OWN * 128], BF16)
        KT = KTF[:, :].rearrange("p (c t) -> p c t", c=4)
        bKT = Buf("KT")
        WINC = KTF[:, 0:4096].rearrange("p (k n) -> p k n", n=512)
        bWINC = bKT
        KTMO = sbm("KTMO", [128, OWN, 512], BF16)
        bKTMO = [Buf("KTMO%d" % i) for i in range(OWN)]
        VAO = sbm("VAO", [128, OWN, 4, 129], BF16)
        bVAO = [Buf("VAO%d" % i) for i in range(OWN)]
        OWNG = sbm("OWNG", [128, OWN, 24])
        bOWNG = Buf("OWNG")
        CF = sbm("CF", [128, 4, 129])
        bCF = Buf("CF")
        CB = sbm("CB", [128, 4, 129])
        bCB = Buf("CB")
        sts = ExitStack()

        def sbs(name, shape, dt=F32):
            return sts.enter_context(nc.sbuf_tensor(name, list(shape), dt))

        WIN = sbs("WIN", [128, 8, 1536], BF16)
        bWIN = Buf("WIN")
        BCOL = sbs("BCOL", [128, 16])
        bBCOL = Buf("BCOL")
        BZ = sbs("BZ", [128, 512])
        bBZ = Buf("BZ")
        bMODD = Buf("mod_d")

        def interleave(gens):
            gens = list(gens)
            while gens:
                for g_ in list(gens):
                    try:
                        next(g_)
                    except StopIteration:
                        gens.remove(g_)

        with ExitStack() as st0:
            def sb0(name, shape, dt=F32):
                return st0.enter_context(nc.sbuf_tensor(name, list(shape), dt))
            MOD = sb0("MOD", [128, 6 * D])
            bMOD = Buf("MOD")
            MODC = sb0("MODC", [128, 2 * D])
            bMODC = Buf("MODC")
            st0a = ExitStack()

            def sb0a(name, shape, dt=F32):
                return st0a.enter_context(nc.sbuf_tensor(name, list(shape), dt))
            CT = sb0a("CT", [128, 16])
            bCT = Buf("CT")
            P.dma("sp", CT[:, :], cT.ap(), writes=[bCT])
            SC = sb0a("SC", [128, 16])
            bSC = Buf("SC")
            act(SC[:, :], CT[:, :], AF.Silu, [bCT], [bSC])
            REP = sb0a("REP", [128, 16, 128])
            bREP = Buf("REP")
            for j in range(16):
                cp("dve", REP[:, j, :], SC[:, j:j + 1].broadcast_to([128, 128]), [bSC], [bREP])
            WA = [sb0a("WA%d" % i, [128, 8, 512]) for i in range(2)]
            bWA = [Buf("WA%d" % i) for i in range(2)]
            BA = [sb0a("BA%d" % i, [128, 512]) for i in range(2)]
            bBA = [Buf("BA%d" % i) for i in range(2)]
            w_ada_v = w_ada.ap().rearrange("(k p) n -> p k n", p=128)
            for blk in range(12):
                s = blk % 2
                P.dma("sp", WA[s][:, :, :], w_ada_v[:, :, blk * 512:(blk + 1) * 512], writes=[bWA[s]])
                P.dma("sp", BA[s][:, :], bc_rows(b_ada, 0, 512, off=blk * 512), writes=[bBA[s]])
                pb = blk % 2
                for kc in range(8):
                    mm(PS[pb][:, :], REP[:, kc, :], WA[s][:, kc, :], kc == 0, kc == 7, [bREP, bWA[s]], [PB[pb]])
                tt("dve", MOD[:, blk * 512:(blk + 1) * 512], PS[pb][:, :], BA[s][:, :], ALU.add, [PB[pb], bBA[s]], [bMOD])
                if blk < 4:
                    pc = 2 + blk % 2
                    for kc in range(8):
                        mm(PS[pc][:, :], REP[:, 8 + kc, :], WA[s][:, kc, :], kc == 0, kc == 7, [bREP, bWA[s]], [PB[pc]])
                    tt("dve", MODC[:, blk * 512:(blk + 1) * 512], PS[pc][:, :], BA[s][:, :], ALU.add, [PB[pc], bBA[s]], [bMODC])
            GB = sb0a("GB", [128, 4, D])
            bGB = Buf("GB")
            for i in range(4):
                P.dma("sp", GB[:, i, :], bc_rows(gains, i, D), writes=[bGB])
            stt("dve", MOD[:, D:2 * D], MOD[:, D:2 * D], 1.0, GB[:, 0, :], ALU.add, ALU.mult, [bMOD, bGB], [bMOD])
            tt("dve", MOD[:, 2 * D:3 * D], MOD[:, 2 * D:3 * D], GB[:, 1, :], ALU.mult, [bMOD, bGB], [bMOD])
            stt("dve", MOD[:, 4 * D:5 * D], MOD[:, 4 * D:5 * D], 1.0, GB[:, 2, :], ALU.add, ALU.mult, [bMOD, bGB], [bMOD])
            tt("dve", MOD[:, 5 * D:6 * D], MOD[:, 5 * D:6 * D], GB[:, 3, :], ALU.mult, [bMOD, bGB], [bMOD])
            stt("dve", MODC[:, D:2 * D], MODC[:, D:2 * D], 1.0, GB[:, 0, :], ALU.add, ALU.mult, [bMODC, bGB], [bMODC])
            P.dma("sp", mod_d.ap(), MOD[0:1, :], reads=[bMOD], writes=[bMODD])
            dump("mod", MOD[0:1, :], [1, 6 * D], [bMOD])
            dump("modc", MODC[0:1, :], [1, 2 * D], [bMODC])
            P.barrier()
            st0a.close()
            REPS = sb0("REPS", [128, 2, 8, 128])
            bREPS = Buf("REPS")
            GC = sb0("GC", [128, 16])
            bGC = Buf("GC")
            for kc in range(8):
                blk = slice(kc * 128, (kc + 1) * 128)
                tr(PS[0][:, 0:128], MOD[:, blk], IDF, [bMOD, bCONST], [PB[0]])
                tr(PS[0][:, 128:256], MOD[:, D + kc * 128:D + (kc + 1) * 128], IDF, [bMOD, bCONST], [PB[0]])
                tr(PS[0][:, 256:384], MODC[:, blk], IDF, [bMODC, bCONST], [PB[0]])
                tr(PS[0][:, 384:512], MODC[:, D + kc * 128:D + (kc + 1) * 128], IDF, [bMODC, bCONST], [PB[0]])
                cp("dve", REPS[:, 0, kc, :], PS[0][:, 0:128], [PB[0]], [bREPS])
                cp("dve", REPS[:, 1, kc, :], PS[0][:, 256:384], [PB[0]], [bREPS])
                cp("dve", GC[:, kc:kc + 1], PS[0][:, 128:129], [PB[0]], [bGC])
                cp("dve", GC[:, 8 + kc:9 + kc], PS[0][:, 384:385], [PB[0]], [bGC])
            WST = [sb0("WST%d" % i, [128, 1536]) for i in range(2)]
            bWST = [Buf("WST%d" % i) for i in range(2)]
            for kc in range(8):
                w_, bw_ = WST[kc % 2], bWST[kc % 2]
                P.dma("sp", w_[:, :], w_in.ap()[kc * 128:(kc + 1) * 128, :], writes=[bw_])
                ts("dve", WIN[:, kc, :], w_[:, :], GC[:, kc:kc + 1], None, ALU.mult, None, [bw_, bGC], [bWIN])
                ts("pool", WINC[:, kc, :], w_[:, 0:512], GC[:, 8 + kc:9 + kc], None, ALU.mult, None, [bw_, bGC], [bWINC])
                for blk in range(3):
                    mm(PS[2 + blk][:, :], REPS[:, 0, kc, :], w_[:, blk * 512:(blk + 1) * 512], kc == 0, kc == 7, [bREPS, bw_], [PB[2 + blk]])
                mm(PS[5][:, :], REPS[:, 1, kc, :], w_[:, 0:512], kc == 0, kc == 7, [bREPS, bw_], [PB[5]])
            BROW = sb0("BROW", [128, 4, 512])
            bBROW = Buf("BROW")
            for i in range(4):
                cp("dve", BROW[:, i, :], PS[2 + i][:, :], [PB[2 + i]], [bBROW])
            cp("dve", BZ[:, :], BROW[:, 1, :], [bBROW], [bBZ])
            for i, src in ((0, 0), (1, 2), (2, 3)):
                for j in range(4):
                    tr(PS[0][:, j * 128:(j + 1) * 128], BROW[:, src, j * 128:(j + 1) * 128], IDF, [bBROW, bCONST], [PB[0]])
                for j in range(4):
                    cp("dve", BCOL[:, i * 4 + j:i * 4 + j + 1], PS[0][:, j * 128:j * 128 + 1], [PB[0]], [bBCOL])
            P.barrier()

        BDB = sbs("BDB", [128, 3, 4, 128], BF16)
        bBDB = Buf("BDB")
        AW = sbs("AW", [128, 4, 32], BF16)
        bAW = Buf("AW")
        BIF = sbs("BIF", [128, 16])
        bBIF = Buf("BIF")
        P.dma("sp", BIF[:, :], bc_rows(b_if, 0, 16), writes=[bBIF])
        CW = sbs("CW", [128, 16])
        bCW = Buf("CW")
        P.dma("sp", CW[:, :], convw.ap(), writes=[bCW])
        XMH = sbs("XMH", [128, 4, 64])
        bXMH = Buf("XMH")
        bPOSRD = Buf("posr_d")
        NTB = 4
        XB = [sbs("XB%d" % i, [128, D]) for i in range(NTB)]
        bXB = [Buf("XB%d" % i) for i in range(NTB)]
        PRB = [sbs("PRB%d" % i, [128, 512]) for i in range(NTB)]
        bPRB = [Buf("PRB%d" % i) for i in range(NTB)]
        X0B = [sbs("X0B%d" % i, [128, D], BF16) for i in range(NTB)]
        bX0B = [Buf("X0B%d" % i) for i in range(NTB)]
        JUNK = [sbs("JUNK%d" % i, [128, D], BF16) for i in range(2)]
        bJUNK = [Buf("JUNK%d" % i) for i in range(2)]
        DG = [sbs("DG%d" % i, [128, 128], BF16) for i in range(NTB)]
        bDG = [Buf("DG%d" % i) for i in range(NTB)]
        SS = sbs("SS", [128, 4 * NTB])
        bSS = [Buf("SS%d" % i) for i in range(NTB)]
        HT = sbs("HT", [128, 8, 512], BF16)
        bHTt = [Buf("HT%d" % i) for i in range(4)]
        PT = PS[0][:, :].bitcast(BF16).rearrange("p (k t) -> p k t", t=128)

        with ExitStack() as st1:
            def sb1(name, shape, dt=F32):
                return st1.enter_context(nc.sbuf_tensor(name, list(shape), dt))
            FREQ = sb1("FREQ", [128, 256])
            bFREQ = Buf("FREQ")
            act(FREQ[:, :], TABS[:, 512:768], AF.Exp, [bTABS], [bFREQ], scale=-math.log(10000.0) / 256.0)
            WS = sb1("WS", [128, 6, 4, 4])
            bWS = Buf("WS")
            for w in range(3):
                P.dma("sp", WS[:, w, :, :], bass.AP(w_qkv, w * 2048, [[4, 128], [512, 4], [1, 4]]), writes=[bWS])
                P.dma("sp", WS[:, 3 + w, :, :], bass.AP(w_qkvT, w * 2048, [[4, 128], [512, 4], [1, 4]]), writes=[bWS])
            BDF = sb1("BDF", [128, 6, 4, 128])
            bBDF = Buf("BDF")
            BDM3 = BDM.rearrange("p (r o) -> p r o", o=4)
            for w in range(6):
                for cc in range(4):
                    tt("dve", BDF[:, w, cc, :].rearrange("p (r o) -> p r o", o=4),
                       WS[:, w, cc, None, :].broadcast_to([128, 32, 4]), BDM3, ALU.mult, [bWS, bCONST], [bBDF])
            cp("dve", BDB[:, :, :, :], BDF[:, 0:3, :, :], [bBDF], [bBDB])
            WIF = sb1("WIF", [128, 12, 16])
            bWIF = Buf("WIF")
            P.dma("sp", WIF[:, :, :], w_if.ap().rearrange("(k p) n -> p k n", p=128), writes=[bWIF])
            for cc in range(4):
                mm(PS[1][:, cc * 32:cc * 32 + 16], BDF[:, 3, cc, :], WIF[:, cc, :], True, False, [bBDF, bWIF], [PB[1]])
                mm(PS[1][:, cc * 32:cc * 32 + 16], BDF[:, 4, cc, :], WIF[:, 4 + cc, :], False, True, [bBDF, bWIF], [PB[1]])
                mm(PS[1][:, cc * 32 + 16:cc * 32 + 32], BDF[:, 5, cc, :], WIF[:, 8 + cc, :], True, True, [bBDF, bWIF], [PB[1]])
            cp("dve", AW[:, :, :], PS[1][:, 0:128].rearrange("p (c n) -> p c n", n=32), [PB[1]], [bAW])

            ANG = sb1("ANG", [128, 512])
            bANG = Buf("ANG")
            KI = sb1("KI", [128, 512], I32)
            bKI = Buf("KI")
            MSK = sb1("MSK", [128, 512])
            bMSK = Buf("MSK")

            def sincos(out, bout, idx):
                ts("dve", ANG[:, 0:256], FREQ[:, :], idx, None, ALU.mult, None, [bFREQ, bTABS], [bANG])
                ts("dve", ANG[:, 256:512], ANG[:, 0:256], math.pi / 2, None, ALU.add, None, [bANG], [bANG])
                ts("dve", KI[:, :], ANG[:, :], 1.0 / TWO_PI, None, ALU.mult, None, [bANG], [bKI])
                stt("dve", ANG[:, :], KI[:, :], -TWO_PI, ANG[:, :], ALU.mult, ALU.add, [bKI, bANG], [bANG])
                ts("dve", MSK[:, :], ANG[:, :], math.pi, TWO_PI, ALU.is_gt, ALU.mult, [bANG], [bMSK])
                tt("dve", ANG[:, :], ANG[:, :], MSK[:, :], ALU.subtract, [bANG, bMSK], [bANG])
                ts("dve", MSK[:, :], ANG[:, :], -math.pi, TWO_PI, ALU.is_lt, ALU.mult, [bANG], [bMSK])
                tt("dve", ANG[:, :], ANG[:, :], MSK[:, :], ALU.add, [bANG, bMSK], [bANG])
                ts("dve", ANG[:, :], ANG[:, :], math.pi, -math.pi, ALU.min, ALU.max, [bANG], [bANG])
                act(out, ANG[:, :], AF.Sin, [bANG], [bout])

            PR = sb1("PR", [128, 2, 512])
            bPR = Buf("PR")
            for a in range(2):
                sincos(PR[:, a, :], bPR, TABS[:, a:a + 1])
            P.dma("sp", posr_d.ap().rearrange("(p a) n -> p a n", a=2), PR[:, :, :], reads=[bPR], writes=[bPOSRD])
            sincos(POSC[:, :], bPOSC, TABS[:, 2:3])
            POSH = sb1("POSH", [128, D])
            bPOSH = Buf("POSH")
            sincos(POSH[:, 0:512], bPOSH, TABS[:, 3:4])
            sincos(POSH[:, 512:1024], bPOSH, TABS[:, 4:5])

            tile_ctr = [0]

            def tile_front(xsrc, pos_mode, ht_dst, bht):
                k = tile_ctr[0]
                tile_ctr[0] += 1
                q = k % NTB
                X, bX = XB[q], bXB[q]
                xb_, bxb_ = X0B[q], bX0B[q]
                dg, bdg = DG[q], bDG[q]
                bss = bSS[q]
                P.dma("sp", X[:, :], xsrc, writes=[bX])
                if pos_mode is not None and pos_mode[0] == "rolled":
                    i = pos_mode[1]
                    PRt, bPRt = PRB[q], bPRB[q]
                    P.dma("sp", PRt[0:64, :], bc_rows(posr_d, 2 * i, 512, parts=64), reads=[bPOSRD], writes=[bPRt])
                    P.dma("sp", PRt[64:128, :], bc_rows(posr_d, 2 * i + 1, 512, parts=64), reads=[bPOSRD], writes=[bPRt])
                    yield
                    tt("pool", xb_[:, 0:512], X[:, 0:512], PRt[:, :], ALU.add, [bX, bPRt], [bxb_])
                    tt("pool", xb_[:, 512:1024], X[:, 512:1024], POSC[:, :], ALU.add, [bX, bPOSC], [bxb_])
                elif pos_mode is not None:
                    tt("pool", xb_[:, :], X[:, :], POSH[:, :], ALU.add, [bX, bPOSH], [bxb_])
                else:
                    yield
                    cp("pool", xb_[:, :], X[:, :], [bX], [bxb_])
                yield
                sc = SS[:, q * 4:q * 4 + 4]
                act(JUNK[k % 2][:, :], xb_[:, :], AF.Square, [bxb_], [bJUNK[k % 2], bss], accum_out=sc[:, 0:1])
                yield
                ts("pool", sc[:, 1:2], sc[:, 0:1], 1.0 / D, EPS, ALU.mult, ALU.add, [bss], [bss])
                tt("pool", sc[:, 2:3], sc[:, 1:2], NEGH, ALU.pow, [bss, bTABS], [bss])
                yield
                ts("dve", dg[:, :], IDF, sc[:, 2:3], None, ALU.mult, None, [bCONST, bss], [bdg])
                yield
                for kc in range(8):
                    pb = kc // 4
                    mm(PS[pb][:, (kc % 4) * 128:(kc % 4 + 1) * 128], xb_[:, kc * 128:(kc + 1) * 128], dg[:, :], True, True, [bxb_, bdg], [PB[pb]])
                cp("act", ht_dst[:, 0:4, :], PS[0][:, :].rearrange("p (k t) -> p k t", t=128), [PB[0]], [bht])
                cp("dve", ht_dst[:, 4:8, :], PS[1][:, :].rearrange("p (k t) -> p k t", t=128), [PB[1]], [bht])
                yield

            HTH = sb1("HTH", [128, 8, 128], BF16)
            bHTH = Buf("HTH")
            interleave([tile_front(x_halo.ap(), ("halo",), HTH[:, :, :], bHTH)])
            for cc in range(4):
                for kc in range(8):
                    mm(PS[2][:, cc * 64:cc * 64 + 64], WIN[:, kc, cc * 128:(cc + 1) * 128], HTH[:, kc, 0:64], kc == 0, kc == 7, [bWIN, bHTH], [PB[2]])
            XMHF = sb1("XMHF", [128, 4, 64])
            bXMHF = Buf("XMHF")
            tt("dve", XMHF[:, :, :], PS[2][:, 0:256].rearrange("p (c n) -> p c n", n=64),
               BCOL[:, 0:4, None].broadcast_to([128, 4, 64]), ALU.add, [PB[2], bBCOL], [bXMHF])
            tt("dve", XMH[:, :, :], XMHF[:, :, :], TABS[:, None, 384:448].broadcast_to([128, 4, 64]), ALU.mult, [bXMHF, bTABS], [bXMH])
            dump("xmh", XMH[:, :, :], [128, 4, 64], [bXMH])
            P.barrier()

        XM = sbs("XM", [128, 4, 514])
        bXM = [Buf("XM%d" % i) for i in range(4)]
        ACC = [sbs("ACC%d" % i, [128, 512]) for i in range(2)]
        bACC = [Buf("ACC%d" % i) for i in range(2)]
        ACTT = [sbs("ACTT%d" % i, [128, 4, 512], BF16) for i in range(2)]
        bACTT = [[Buf("ACTT%d_%d" % (j, i)) for i in range(4)] for j in range(2)]
        XMB = [sbs("XMB%d" % i, [128, 4, 512], BF16) for i in range(2)]
        bXMB = [[Buf("XMB%d_%d" % (j, i)) for i in range(4)] for j in range(2)]
        UT = sbs("UT", [128, 4, 512], BF16)
        bUT = Buf("UT")
        bUD = Buf("u_d")
        KTMG = sbs("KTMG", [128, 4, 512], BF16)
        bKTMG = [Buf("KTMG%d" % i) for i in range(4)]
        VAG = sbs("VAG", [128, 4, 4, 129], BF16)
        bVAG = [Buf("VAG%d" % i) for i in range(4)]
        VS = [sbs("VS%d" % i, [128, 8, 129], BF16) for i in range(2)]
        bVS = [Buf("VS%d" % i) for i in range(2)]
        CBS = sbs("CBS", [128, 4, 129])
        bCBS = Buf("CBS")
        memset("pool", CBS[:, :, :], 0.0, [bCBS])
        CCB = sbs("CCB", [128, 4, 129])
        bCCB = Buf("CCB")
        GT = sbs("GT", [128, 4, 16])
        bGT = Buf("GT")
        GW = sbs("GW", [128, 4, 40])
        bGW = Buf("GW")
        EXI = sbs("EXI", [128, 4, 16])
        bEXI = Buf("EXI")
        EXO = sbs("EXO", [128, 4, 16])
        bEXO = Buf("EXO")
        PALL = sbs("PALL", [128, 5, 4])
        bPALL = Buf("PALL")
        MBT = sbs("MBT", [128, 4, 4])
        bMBT = Buf("MBT")
        WV = sbs("WV", [128, 4, 8])
        bWV = Buf("WV")
        STG = [sbs("STG%d" % i, [128, 512], BF16) for i in range(2)]
        bSTG = [Buf("STG%d" % i) for i in range(2)]
        ZT = sbs("ZT", [128, 512])
        bZT = Buf("ZT")
        bAZ = Buf("az_d")
        stg_ctr = [0]

        memset("pool", VAG[:, :, :, :], 1.0, bVAG)
        memset("pool", VAO[:, :, :, :], 1.0, bVAO)
        memset("pool", PALL[:, :, :], 0.0, [bPALL])

        PSG = PS[6][:, 384:448].rearrange("p (t g) -> p t g", g=16)
        bPSG = PB[6]
        PSB = PS[6][:, 448:512].rearrange("p (t g) -> p t g", g=16)
        bPSB = PB[6]
        DCF = [PS[5][:, 0:129], PS[5][:, 129:258], PS[5][:, 258:387], PS[6][:, 0:129]]
        bDCF = [PB[5], PB[5], PB[5], PB[6]]
        ACCB = [PS[7][:, 0:129], PS[7][:, 129:258], PS[7][:, 258:387], PS[6][:, 129:258]]
        bACCB = [PB[7], PB[7], PB[7], PB[6]]
        u_v = u_d.ap().rearrange("(c p) t -> p c t", p=128)
        LN_QS = math.log(128.0 ** -0.5)

        def front(gi, kind, par):
            n = 2 if kind == "ctx" else 4
            ntok = n * 128
            tgens = []
            for t_ in range(n):
                dst = HT[:, :, t_ * 128:(t_ + 1) * 128]
                if kind == "ctx":
                    tgens.append(tile_front(ctx_in.ap()[t_ * 128:(t_ + 1) * 128, :], None, dst, bHTt[t_]))
                else:
                    i = gi * 4 + t_
                    tgens.append(tile_front(x_rot.ap()[i * 128:(i + 1) * 128, :], ("rolled", i), dst, bHTt[t_]))
            while tgens:
                for g_ in list(tgens):
                    try:
                        next(g_)
                    except StopIteration:
                        tgens.remove(g_)
                yield
            W_, bW_, boff = (WINC, bWINC, 8) if kind == "ctx" else (WIN, bWIN, 0)
            att, batt, xmb, bxmb = ACTT[par], bACTT[par], XMB[par], bXMB[par]
            for cc in range(4):
                pb = 2 + cc % 2
                for kc in range(8):
                    mm(PS[pb][:, 0:ntok], W_[:, kc, cc * 128:(cc + 1) * 128], HT[:, kc, 0:ntok], kc == 0, kc == 7, [bW_] + bHTt, [PB[pb]])
                bias = BCOL[:, boff + cc:boff + cc + 1]
                act(XM[:, cc, 1:1 + ntok], PS[pb][:, 0:ntok], AF.Identity, [PB[pb], bBCOL], [bXM[cc]], bias=bias)
                act(xmb[:, cc, 0:ntok], PS[pb][:, 0:ntok], AF.Identity, [PB[pb], bBCOL], [bxmb[cc]], bias=bias)
                if kind == "ctx":
                    memset("pool", XM[:, cc, 0:1], 0.0, [bXM[cc]])
                    memset("pool", XM[:, cc, 1 + ntok:2 + ntok], 0.0, [bXM[cc]])
                else:
                    cp("pool", XM[:, cc, 0:1], XMH[:, cc, 2 * gi:2 * gi + 1], [bXMH], [bXM[cc]])
                    cp("pool", XM[:, cc, 513:514], XMH[:, cc, 2 * gi + 1:2 * gi + 2], [bXMH], [bXM[cc]])
                yield
                A_, bA_ = ACC[cc % 2], bACC[cc % 2]
                ts("dve", A_[:, 0:ntok], XM[:, cc, 1:1 + ntok], CW[:, cc * 3 + 1:cc * 3 + 2], CW[:, 12 + cc:13 + cc], ALU.mult, ALU.add, [bXM[cc], bCW], [bA_])
                stt("dve", A_[:, 0:ntok], XM[:, cc, 0:ntok], CW[:, cc * 3:cc * 3 + 1], A_[:, 0:ntok], ALU.mult, ALU.add, [bXM[cc], bCW, bA_], [bA_])
                stt("dve", A_[:, 0:ntok], XM[:, cc, 2:2 + ntok], CW[:, cc * 3 + 2:cc * 3 + 3], A_[:, 0:ntok], ALU.mult, ALU.add, [bXM[cc], bCW, bA_], [bA_])
                act(att[:, cc, 0:ntok], A_[:, 0:ntok], AF.Silu, [bA_], [batt[cc]])
                yield
            if kind == "own" and gi == 0:
                dump("actT", att[:, :, 0:128], [128, 4, 128], batt)
            if kind != "ctx":
                for cc in range(4):
                    pb = 2 + cc % 2
                    for kc in range(8):
                        mm(PS[pb][:, :], WIN[:, kc, 1024 + cc * 128:1024 + (cc + 1) * 128], HT[:, kc, :], kc == 0, kc == 7, [bWIN] + bHTt, [PB[pb]])
                    act(UT[:, cc, :], PS[pb][:, :], AF.Identity, [PB[pb], bBCOL], [bUT], bias=BCOL[:, 4 + cc:5 + cc])
                    yield
                P.dma("act", u_v[:, :, gi * 512:(gi + 1) * 512], UT[:, :, :], reads=[bUT], writes=[bUD])
            if kind == "own":
                for w, dstT, bdst in ((0, QT, bQT), (1, KT, bKT)):
                    for cc in range(4):
                        pb = 2 + cc % 2
                        mm(PS[pb][:, :], BDB[:, w, cc, :], att[:, cc, :], True, True, [bBDB, batt[cc]], [PB[pb]])
                        cp("act" if cc % 2 else "dve", dstT[:, cc, gi * 512:(gi + 1) * 512], PS[pb][:, :], [PB[pb]], [bdst])
                    yield
                for t_ in range(4):
                    i = gi * 4 + t_
                    sl = slice(t_ * 128, (t_ + 1) * 128)
                    pb = 2 + t_ % 2
                    for kc in range(8):
                        mm(PS[pb][:, :], HT[:, kc, sl], WIN[:, kc, 512:1024], kc == 0, kc == 7, bHTt + [bWIN], [PB[pb]])
                    tt("dve", ZT[:, :], PS[pb][:, :], BZ[:, :], ALU.add, [PB[pb], bBZ], [bZT])
                    s_ = stg_ctr[0] % 2
                    stg_ctr[0] += 1
                    act(STG[s_][:, :], ZT[:, :], AF.Silu, [bZT], [bSTG[s_]])
                    P.dma("act", az_d.ap()[1, i * 128:(i + 1) * 128, :], STG[s_][:, :], reads=[bSTG[s_]], writes=[bAZ])
                    for cc in range(4):
                        tr(PT[:, cc, :], att[:, cc, sl], IDB[:, :], [batt[cc], bIDB], [PB[0]])
                    s_ = stg_ctr[0] % 2
                    stg_ctr[0] += 1
                    cp("dve", STG[s_][:, :].rearrange("p (c t) -> p c t", t=128), PT[:, 0:4, :], [PB[0]], [bSTG[s_]])
                    P.dma("act", az_d.ap()[0, i * 128:(i + 1) * 128, :], STG[s_][:, :], reads=[bSTG[s_]], writes=[bAZ])
                    yield

        def back(gi, kind, par):
            n = 2 if kind == "ctx" else 4
            att, batt, xmb, bxmb = ACTT[par], bACTT[par], XMB[par], bXMB[par]
            for t_ in range(n):
                sl = slice(t_ * 128, (t_ + 1) * 128)
                if kind == "own":
                    i = gi * 4 + t_
                    ktm, bktm, va, bva = KTMO[:, i, :], bKTMO[i], VAO[:, i, :, :], bVAO[i]
                else:
                    ktm, bktm, va, bva = KTMG[:, t_, :], bKTMG[t_], VAG[:, t_, :, :], bVAG[t_]
                for cc in range(4):
                    mm(PS[4][:, cc * 128:(cc + 1) * 128], att[:, cc, sl], BDB[:, 1, cc, :], True, True, [batt[cc], bBDB], [PB[4]])
                cp("act", ktm, PS[4][:, :], [PB[4]], [bktm])
                yield
                for cc in range(4):
                    mm(PS[4][:, cc * 128:(cc + 1) * 128], xmb[:, cc, sl], BDB[:, 2, cc, :], True, True, [bxmb[cc], bBDB], [PB[4]])
                cp("act", va[:, :, 0:128], PS[4][:, :].rearrange("p (h d) -> p h d", d=128), [PB[4]], [bva])
                for cc in range(4):
                    mm(PSG[:, t_, :], att[:, cc, sl], AW[:, cc, 0:16], cc == 0, False, [batt[cc], bAW], [bPSG])
                for cc in range(4):
                    mm(PSG[:, t_, :], xmb[:, cc, sl], AW[:, cc, 16:32], False, cc == 3, [bxmb[cc], bAW], [bPSG])
                yield
            tt("dve", GT[:, 0:n, :], PSG[:, 0:n, :], BIF[:, None, :].broadcast_to([128, n, 16]), ALU.add, [bPSG, bBIF], [bGT])
            stt("dve", GW[:, 0:n, 0:8], GT[:, 0:n, 8:16], -1.0, GT[:, 0:n, 8:16], ALU.mult, ALU.max, [bGT], [bGW])
            yield
            act(GW[:, 0:n, 8:16], GW[:, 0:n, 0:8], AF.Exp, [bGW], [bGW], scale=-1.0)
            yield
            act(GW[:, 0:n, 16:24], GW[:, 0:n, 8:16], AF.Ln, [bGW], [bGW], bias=1.0)
            ts("dve", GW[:, 0:n, 24:32], GT[:, 0:n, 8:16], 0.0, None, ALU.min, None, [bGT], [bGW])
            yield
            tt("dve", GW[:, 0:n, 32:40], GW[:, 0:n, 24:32], GW[:, 0:n, 16:24], ALU.subtract, [bGW], [bGW])
            yield
            for t_ in range(n):
                mm(PSB[:, t_, 0:4], TRIU, GW[:, t_, 32:36], True, True, [bCONST, bGW], [bPSB])
                mm(PSB[:, t_, 4:8], TRIL, GW[:, t_, 36:40], True, True, [bCONST, bGW], [bPSB])
                mm(PSB[:, t_, 8:16], ONES, GW[:, t_, 32:40], True, True, [bCONST, bGW], [bPSB])
            yield
            if kind == "own":
                i0 = gi * 4
                if gi == 0:
                    dump("gt", GT[:, :, :], [128, 4, 16], [bGT])
                    dump("lf", GW[:, :, 32:40], [128, 4, 8], [bGW])
                tt("dve", EXI[:, :, 0:8], GT[:, :, 0:8], PSB[:, :, 0:8], ALU.subtract, [bGT, bPSB], [bEXI])
                yield
                act(OWNG[:, i0:i0 + 4, 0:8], EXI[:, :, 0:8], AF.Exp, [bEXI], [bOWNG])
                act(OWNG[:, i0:i0 + 4, 8:16], PSB[:, :, 0:8], AF.Exp, [bPSB], [bOWNG], bias=LN_QS)
                act(OWNG[:, i0:i0 + 4, 16:24], PSB[:, :, 8:16], AF.Exp, [bPSB], [bOWNG])
                yield
                return
            tt("dve", EXI[:, 0:n, 0:8], GT[:, 0:n, 0:8], PSB[:, 0:n, 8:16], ALU.add, [bGT, bPSB], [bEXI])
            tt("dve", EXI[:, 0:n, 0:8], EXI[:, 0:n, 0:8], PSB[:, 0:n, 0:8], ALU.subtract, [bEXI, bPSB], [bEXI])
            yield
            if kind == "ctx":
                cp("dve", EXI[:, 0:2, 8:16], PSB[:, 0:2, 8:16], [bPSB], [bEXI])
                yield
                act(EXO[:, 0:2, :], EXI[:, 0:2, :], AF.Exp, [bEXI], [bEXO])
                yield
                tt("dve", WV[:, 0, 0:4], EXO[:, 0, 0:4], EXO[:, 1, 8:12], ALU.mult, [bEXO], [bWV])
                cp("dve", WV[:, 1, 0:4], EXO[:, 1, 0:4], [bEXO], [bWV])
                cp("dve", WV[:, 0, 4:8], EXO[:, 0, 4:8], [bEXO], [bWV])
                tt("dve", WV[:, 1, 4:8], EXO[:, 1, 4:8], EXO[:, 0, 12:16], ALU.mult, [bEXO], [bWV])
                yield
            else:
                MF = TABS[:, 128 + 4 * gi:132 + 4 * gi]
                MB = TABS[:, 256 + 4 * gi:260 + 4 * gi]
                MF3 = MF[:, :, None].broadcast_to([128, 4, 4])
                MB3 = MB[:, :, None].broadcast_to([128, 4, 4])
                tt("dve", EXI[:, :, 8:12], PSB[:, :, 8:12], MF3, ALU.mult, [bPSB, bTABS], [bEXI])
                tt("dve", MBT[:, :, :], PSB[:, :, 12:16], MB3, ALU.mult, [bPSB, bTABS], [bMBT])
                yield
                for t_ in range(4):
                    tt("dve", PALL[:, t_ + 1, :], PALL[:, t_, :], MBT[:, t_, :], ALU.add, [bPALL, bMBT], [bPALL])
                    yield
                cp("dve", EXI[:, :, 12:16], PALL[:, 0:4, :], [bPALL], [bEXI])
                yield
                act(EXO[:, :, :], EXI[:, :, :], AF.Exp, [bEXI], [bEXO])
                yield
                tt("dve", WV[:, :, 0:4], EXO[:, :, 0:4], MF3, ALU.mult, [bEXO, bTABS], [bWV])
                tt("dve", WV[:, :, 4:8], EXO[:, :, 4:8], EXO[:, :, 12:16], ALU.mult, [bEXO], [bWV])
                yield
                tt("dve", WV[:, :, 4:8], WV[:, :, 4:8], MB3, ALU.mult, [bWV, bTABS], [bWV])
                cp("dve", PALL[:, 0, :], PALL[:, 4, :], [bPALL], [bPALL])
                yield
            for t_ in range(n):
                s_ = t_ % 2
                tt("pool", VS[s_][:, 0:4, :], VAG[:, t_, :, :], WV[:, t_, 0:4, None].broadcast_to([128, 4, 129]), ALU.mult, [bVAG[t_], bWV], [bVS[s_]])
                tt("pool", VS[s_][:, 4:8, :], VAG[:, t_, :, :], WV[:, t_, 4:8, None].broadcast_to([128, 4, 129]), ALU.mult, [bVAG[t_], bWV], [bVS[s_]])
                yield
                for h in range(4):
                    klhs = KTMG[:, t_, h * 128:(h + 1) * 128]
                    mm(DCF[h], klhs, VS[s_][:, h, :], True, True, [bKTMG[t_], bVS[s_]], [bDCF[h]])
                    mm(ACCB[h], klhs, VS[s_][:, 4 + h, :], True, True, [bKTMG[t_], bVS[s_]], [bACCB[h]])
                yield
                dcf3 = PS[5][:, 0:387].rearrange("p (h d) -> p h d", d=129)
                acb3 = PS[7][:, 0:387].rearrange("p (h d) -> p h d", d=129)
                if kind == "ctx":
                    if t_ == 0:
                        cp("dve", CF[:, 0:3, :], dcf3, [PB[5]], [bCF])
                        cp("dve", CF[:, 3, :], DCF[3], [PB[6]], [bCF])
                        cp("dve", CCB[:, 0:3, :], acb3, [PB[7]], [bCCB])
                        cp("dve", CCB[:, 3, :], ACCB[3], [PB[6]], [bCCB])
                    else:
                        tt("dve", CF[:, 0:3, :], CF[:, 0:3, :], dcf3, ALU.add, [bCF, PB[5]], [bCF])
                        tt("dve", CF[:, 3, :], CF[:, 3, :], DCF[3], ALU.add, [bCF, PB[6]], [bCF])
                        tt("dve", CCB[:, 0:3, :], CCB[:, 0:3, :], acb3, ALU.add, [bCCB, PB[7]], [bCCB])
                        tt("dve", CCB[:, 3, :], CCB[:, 3, :], ACCB[3], ALU.add, [bCCB, PB[6]], [bCCB])
                else:
                    tt("dve", CF[:, :, :], CF[:, :, :], EXO[:, t_, 8:12, None].broadcast_to([128, 4, 129]), ALU.mult, [bCF, bEXO], [bCF])
                    tt("dve", CF[:, 0:3, :], CF[:, 0:3, :], dcf3, ALU.add, [bCF, PB[5]], [bCF])
                    tt("dve", CF[:, 3, :], CF[:, 3, :], DCF[3], ALU.add, [bCF, PB[6]], [bCF])
                    tt("dve", CBS[:, 0:3, :], CBS[:, 0:3, :], acb3, ALU.add, [bCBS, PB[7]], [bCBS])
                    tt("dve", CBS[:, 3, :], CBS[:, 3, :], ACCB[3], ALU.add, [bCBS, PB[6]], [bCBS])
                yield

        seq = [("ctx", 0)] + [("own", g) for g in range(min(OWN // 4, cut))]
        if cut > 4:
            seq += [("oth", g) for g in range(OWN // 4, ngroups)]
        prev = None
        for idx, (kind, gi) in enumerate(seq):
            gens = [front(gi, kind, idx % 2)]
            if prev is not None:
                gens.append(back(*prev))
            interleave(gens)
            prev = (gi, kind, idx % 2)
        interleave([back(*prev)])
        dump("cf_ctx", CF[:, :, :], [128, 4, 129], [bCF])
        act(EXO[:, 0, 0:4], PALL[:, 0, :], AF.Exp, [bPALL], [bEXO])
        for h in range(4):
            if ngroups > OWN // 4 and cut > 4:
                stt("dve", CB[:, h, :], CCB[:, h, :], EXO[:, 0, h:h + 1], CBS[:, h, :], ALU.mult, ALU.add, [bCCB, bEXO, bCBS], [bCB])
            else:
                cp("dve", CB[:, h, :], CCB[:, h, :], [bCCB], [bCB])
        dump("cf_in", CF[:, :, :], [128, 4, 129], [bCF])
        dump("cb_in", CB[:, :, :], [128, 4, 129], [bCB])
        dump("ktm0", KTMO[:, 0, :], [128, 512], [bKTMO[0]])
        dump("va0", VAO[:, 0, :, :], [128, 4, 129], [bVAO[0]])
        dump("owng", OWNG[:, :, :], [128, OWN, 24], [bOWNG])
        dump("qT", QT[:, :, 0:128], [128, 4, 128], [bQT])

        if stage <= 2:
            o_b = Buf("out")
            P.barrier()
            for i in range(OWN):
                t = P.dma("sp", out_d.ap()[i * 128:(i + 1) * 128, 0:512], POSC[:, 0:512], reads=[bPOSC], writes=[o_b])
                final.append((t[0], t[1]))
            P.barrier()
            with nc.Block() as block:
                P.emit(block, final)
            sts.close()
            stm.close()
            return nc

        P.barrier()
        sts.close()
        st6 = ExitStack()

        def sb6(name, shape, dt=F32):
            return st6.enter_context(nc.sbuf_tensor(name, list(shape), dt))

        HD = [sb6("HD%d" % i, [128, OWN, 512], BF16) for i in range(2)]
        bHD = [[Buf("HD%d_%d" % (i, c)) for c in range(OWN)] for i in range(2)]
        HS = [sb6("HS%d" % i, [128, 512]) for i in range(2)]
        bHS = [Buf("HS%d" % i) for i in range(2)]
        SM = [sb6("SM%d" % i, [128, 128], BF16) for i in range(8)]
        bSM = [Buf("SM%d" % i) for i in range(8)]
        VP = [sb6("VP%d" % i, [128, 129], BF16) for i in range(8)]
        bVP = [Buf("VP%d" % i) for i in range(8)]
        CSB = sb6("CSB", [128, 8, 129], BF16)
        bCSB = [Buf("CSB%d" % i) for i in range(8)]
        RD = [sb6("RD%d" % i, [128, 8]) for i in range(8)]
        bRD = [Buf("RD%d" % i) for i in range(8)]
        bST = [Buf("ST%d" % i) for i in range(8)]
        bHSD = Buf("hs_d")

        def chain(d, h):
            q = d * 4 + h
            ST = CF if d == 0 else CB
            bsrc = bCF if d == 0 else bCB
            hsl = slice(h * 128, (h + 1) * 128)
            cp("act", CSB[:, q, :], ST[:, h, :], [bsrc, bST[q]], [bCSB[q], bST[q]])
            yield
            order = range(OWN) if d == 0 else range(OWN - 1, -1, -1)
            for c in order:
                tsl = slice(c * 128, (c + 1) * 128)
                pS, pN, pC = PS[q][:, 0:128], PS[q][:, 128:257], PS[q][:, 257:386]
                mm(pS, KT[:, h, tsl], QT[:, h, tsl], True, True, [bKT, bQT], [PB[q]])
                ts("pool", VP[q][:, :], VAO[:, c, h, :], OWNG[:, c, q:q + 1], None, ALU.mult, None, [bVAO[c], bOWNG], [bVP[q]])
                yield
                tt("dve", SM[q][:, :], pS, TRIU if d == 0 else TRIL, ALU.mult, [PB[q], bCONST], [bSM[q]])
                yield
                mm(pN, SM[q][:, :], VP[q][:, :], True, False, [bSM[q], bVP[q]], [PB[q]])
                mm(pN, QT[:, h, tsl], CSB[:, q, :], False, True, [bQT, bCSB[q]], [PB[q]])
                mm(pC, KTMO[:, c, hsl], VP[q][:, :], True, True, [bKTMO[c], bVP[q]], [PB[q]])
                yield
                eq = OWNG[:, c, 8 + q:9 + q]
                r = RD[q]
                ts("dve", r[:, 0:1], PS[q][:, 256:257], eq, None, ALU.mult, None, [PB[q], bOWNG], [bRD[q]])
                stt("dve", r[:, 1:2], r[:, 0:1], -1.0, r[:, 0:1], ALU.mult, ALU.max, [bRD[q]], [bRD[q]])
                yield
                ts("dve", r[:, 2:3], r[:, 1:2], 1.0, None, ALU.max, None, [bRD[q]], [bRD[q]])
                P.op("dve", lambda e, o=r[:, 3:4], i_=r[:, 2:3]: e.reciprocal(out=o, in_=i_), [bRD[q]], [bRD[q]])
                yield
                tt("dve", r[:, 4:5], r[:, 3:4], eq, ALU.mult, [bRD[q], bOWNG], [bRD[q]])
                tt("dve", ST[:, h, :], ST[:, h, :], pC, ALU.add, [bST[q], PB[q]], [bST[q]])
                yield
                act(HD[d][:, c, hsl], PS[q][:, 128:256], AF.Copy, [PB[q], bRD[q]], [bHD[d][c]], scale=r[:, 4:5])
                ts("dve", ST[:, h, :], ST[:, h, :], OWNG[:, c, 16 + q:17 + q], None, ALU.mult, None, [bST[q], bOWNG], [bST[q]])
                yield
                cp("act", CSB[:, q, :], ST[:, h, :], [bST[q]], [bCSB[q]])
                yield

        interleave([chain(d, h) for d in range(2) for h in range(4)])
        for c in range(OWN):
            tt("pool", HS[c % 2][:, :], HD[0][:, c, :], HD[1][:, c, :], ALU.add, [bHD[0][c], bHD[1][c]], [bHS[c % 2]])
            P.dma("sp", hs_d.ap()[c * 128:(c + 1) * 128, :], HS[c % 2][:, :], reads=[bHS[c % 2]], writes=[bHSD])
            if c in (0, 7, 15):
                dump("hs%d" % c, HS[c % 2][:, :], [128, 512], [bHS[c % 2]])

        if stage <= 3:
            o_b = Buf("out")
            P.barrier()
            for i in range(OWN):
                t = P.dma("sp", out_d.ap()[i * 128:(i + 1) * 128, 0:512], POSC[:, 0:512], reads=[bPOSC], writes=[o_b])
                final.append((t[0], t[1]))
            P.barrier()
            with nc.Block() as block:
                P.emit(block, final)
            st6.close()
            stm.close()
            return nc

        P.barrier()
        st6.close()
        stm.close()

        H2T = sb("H2T", [128, 8, OWN * 128], BF16)
        bH2T = Buf("H2T")
        COMB = sb("COMB", [128, OWN, 16])
        bCOMB = Buf("COMB")
        st7 = ExitStack()

        def sb7(name, shape, dt=F32):
            return st7.enter_context(nc.sbuf_tensor(name, list(shape), dt))

        XCS = sb7("XCS", [128, 512, 2, 16], BF16)
        bXCS = Buf("XCS")
        CS128 = sb7("CS128", [128, 384], BF16)
        bCS = Buf("CS128")
        DI = sb7("DI", [128, 640])
        bDI = Buf("DI")
        P.dma("sp", DI[:, :], dftidx.ap(), writes=[bDI])
        CSF = sb7("CSF", [128, 384])
        bCSF = Buf("CSF")
        act(CSF[:, 0:256], DI[:, 0:256], AF.Sin, [bDI], [bCSF], scale=TWO_PI / 128.0)
        ts("dve", CSF[:, 256:384], CSF[:, 128:256], -1.0, None, ALU.mult, None, [bCSF], [bCSF])
        cp("dve", CS128[:, :], CSF[:, :], [bCSF], [bCS])
        CMY = sb7("CMY", [128, 48], BF16)
        bCMY = Buf("CMY")
        act(CSF[:, 0:32], DI[:, 512:544], AF.Sin, [bDI, bCS], [bCSF], scale=TWO_PI / 128.0)
        ts("dve", CSF[:, 32:48], CSF[:, 16:32], -1.0, None, ALU.mult, None, [bCSF], [bCSF])
        cp("dve", CMY[:, :], CSF[:, 0:48], [bCSF], [bCMY])
        with ExitStack() as st8:
            def sb8(name, shape, dt=F32):
                return st8.enter_context(nc.sbuf_tensor(name, list(shape), dt))
            TW = sb8("TW", [128, 256], BF16)
            bTW = Buf("TW")
            TWF = sb8("TWF", [128, 256])
            bTWF = Buf("TWF")
            act(TWF[:, :], DI[:, 256:512], AF.Sin, [bDI], [bTWF], scale=TWO_PI / 16384.0)
            ts("dve", TW[:, :], TWF[:, :], 1.0 / math.sqrt(16384.0 * 128.0), None, ALU.mult, None, [bTWF], [bTW])
            TC3 = TW[:, None, 0:128].broadcast_to([128, 8, 128])
            TS3 = TW[:, None, 128:256].broadcast_to([128, 8, 128])
            NW_ = 3
            UL = [sb8("UL%d" % i, [128, 16, 128], BF16) for i in range(3)]
            bUL = [Buf("UL%d" % i) for i in range(3)]
            YS = [sb8("YS%d" % i, [128, 8, 256], BF16) for i in range(NW_)]
            bYS = [Buf("YS%d" % i) for i in range(NW_)]
            MT_ = [[sb8("MTW%d_%d" % (j, i), [128, 8, 128], BF16) for i in range(4)] for j in range(NW_)]
            bMT_ = [[Buf("MTW%d_%d" % (j, i)) for i in range(4)] for j in range(NW_)]
            PQ = [sb8("PQ%d" % i, [128, 2, 8, 128], BF16) for i in range(NW_)]
            bPQ = [Buf("PQ%d" % i) for i in range(NW_)]

            def fft_half(hb):
                ub, half = hb // 2, hb % 2
                u_, bu_ = UL[ub % 3], bUL[ub % 3]
                w_ = hb % NW_
                if half == 0:
                    P.dma("sp", u_[:, :, :], bass.AP(u_d, ub * 16 * T, [[128, 128], [T, 16], [1, 128]]), reads=[bUD], writes=[bu_])
                    yield
                y_, by_ = YS[w_], bYS[w_]
                for pr in range(4):
                    pb = 1 + (hb * 4 + pr) % 4
                    for cc in range(2):
                        chl = half * 8 + pr * 2 + cc
                        mm(PS[pb][:, cc * 256:(cc + 1) * 256], u_[:, chl, :], CS128[:, 0:256], True, True, [bu_, bCS], [PB[pb]])
                    cp("act", y_[:, pr * 2:pr * 2 + 2, :], PS[pb][:, :].rearrange("p (c n) -> p c n", n=256), [PB[pb]], [by_])
                    yield
                yr = y_[:, :, 0:128]
                ys_ = y_[:, :, 128:256]
                pq, bpq = PQ[w_], bPQ[w_]
                m_, bm_ = MT_[w_], bMT_[w_]
                tt("dve", m_[0][:, :, :], yr, TC3, ALU.mult, [by_, bTW], [bm_[0]])
                tt("pool", m_[2][:, :, :], yr, TS3, ALU.mult, [by_, bTW], [bm_[2]])
                yield
                tt("dve", m_[1][:, :, :], ys_, TS3, ALU.mult, [by_, bTW], [bm_[1]])
                tt("pool", m_[3][:, :, :], ys_, TC3, ALU.mult, [by_, bTW], [bm_[3]])
                yield
                tt("dve", pq[:, 0, :, :], m_[0][:, :, :], m_[1][:, :, :], ALU.subtract, [bm_[0], bm_[1]], [bpq])
                tt("pool", pq[:, 1, :, :], m_[2][:, :, :], m_[3][:, :, :], ALU.add, [bm_[2], bm_[3]], [bpq])
                yield
                pb = 5 + hb % 3
                for cc in range(8):
                    o_c = PS[pb][:, cc * 32:cc * 32 + 16]
                    o_s = PS[pb][:, cc * 32 + 16:cc * 32 + 32]
                    mm(o_c, pq[:, 0, cc, :], CMY[:, 0:16], True, False, [bpq, bCMY], [PB[pb]])
                    mm(o_c, pq[:, 1, cc, :], CMY[:, 32:48], False, True, [bpq, bCMY], [PB[pb]])
                    mm(o_s, pq[:, 0, cc, :], CMY[:, 16:32], True, False, [bpq, bCMY], [PB[pb]])
                    mm(o_s, pq[:, 1, cc, :], CMY[:, 0:16], False, True, [bpq, bCMY], [PB[pb]])
                    if cc % 4 == 3:
                        yield
                cp("act", XCS[:, hb * 8:hb * 8 + 8, :, :], PS[pb][:, 0:256].rearrange("p (c s k) -> p c s k", s=2, k=16), [PB[pb]], [bXCS])
                yield

            def window(genfs, w):
                pend = list(genfs)
                act_ = []
                while pend or act_:
                    while pend and len(act_) < w:
                        act_.append(pend.pop(0)())
                    for g_ in list(act_):
                        try:
                            next(g_)
                        except StopIteration:
                            act_.remove(g_)

            window([(lambda hb=hb: fft_half(hb)) for hb in range(64)], NW_)
            P.barrier()
        dump("xcs", XCS[:, :, :, :], [128, 512, 2, 16], [bXCS])

        if stage <= 4:
            o_b = Buf("out")
            P.barrier()
            for i in range(OWN):
                t = P.dma("sp", out_d.ap()[i * 128:(i + 1) * 128, 0:512], POSC[:, 0:512], reads=[bPOSC], writes=[o_b])
                final.append((t[0], t[1]))
            P.barrier()
            with nc.Block() as block:
                P.emit(block, final)
            st7.close()
            return nc

        st9 = ExitStack()

        def sb9(name, shape, dt=F32):
            return st9.enter_context(nc.sbuf_tensor(name, list(shape), dt))

        WOB = sb9("WOB", [128, 8, D], BF16)
        bWOB = Buf("WOB")
        P.dma("pool", WOB[:, :, :], w_out.ap().rearrange("(k p) n -> p k n", p=128), writes=[bWOB])
        WFB = sb9("WFB", [128, 4, 128], BF16)
        bWFB = Buf("WFB")
        P.dma("pool", WFB[:, :, :], w_fourier.ap().rearrange("g c d -> c g d"), writes=[bWFB])
        NWSK = sb9("NWSK", [128, 2, 512])
        bNWSK = Buf("NWSK")
        for i in range(2):
            P.dma("sp", NWSK[:, i, :], bc_rows(nrm_skip, i, 512), writes=[bNWSK])
        WRF = sb9("WRF", [128, 8, 20])
        bWRF = Buf("WRF")
        P.dma("sp", WRF[:, :, :], w_router.ap().rearrange("(k p) n -> p k n", p=128), writes=[bWRF])
        WRH = sb9("WRH", [128, 8, 20], BF16)
        WRL = sb9("WRL", [128, 8, 20], BF16)
        bWR = Buf("WR")
        cp("dve", WRH[:, :, :], WRF[:, :, :], [bWRF], [bWR])
        tt("dve", WRL[:, :, :], WRF[:, :, :], WRH[:, :, :], ALU.subtract, [bWRF, bWR], [bWR])
        MODL = sb9("MODL", [128, 3, D])
        bMODL = Buf("MODL")
        for i_, off_ in enumerate((2 * D, 3 * D, 4 * D)):
            P.dma("sp", MODL[:, i_, :], bc_rows(mod_d, 0, D, off=off_), reads=[bMODD], writes=[bMODL])
        GP1, S2, G2 = MODL[:, 0, :], MODL[:, 1, :], MODL[:, 2, :]
        bMOD = bMODL
        BR = sb9("BR", [128, 20])
        bBR = Buf("BR")
        P.dma("sp", BR[:, :], bc_rows(b_router, 0, 20), writes=[bBR])
        HSt = [sb9("HSt%d" % i, [128, 512]) for i in range(2)]
        bHSt = [Buf("HSt%d" % i) for i in range(2)]
        AZ = [sb9("AZ%d" % i, [128, 2, 512], BF16) for i in range(2)]
        bAZ_ = [Buf("AZ%d" % i) for i in range(2)]
        SM__2 = [sb9("SM__%d" % i, [128, 32]) for i in range(2)]
        bSM__2 = [Buf("SM__%d" % i) for i in range(2)]
        CEN_2 = [sb9("CEN_%d" % i, [128, 512]) for i in range(2)]
        bCEN_2 = [Buf("CEN_%d" % i) for i in range(2)]
        SQ_2 = [sb9("SQ_%d" % i, [128, 512]) for i in range(2)]
        bSQ_2 = [Buf("SQ_%d" % i) for i in range(2)]
        T1_2 = [sb9("T1_%d" % i, [128, 512]) for i in range(2)]
        bT1_2 = [Buf("T1_%d" % i) for i in range(2)]
        T2_2 = [sb9("T2_%d" % i, [128, 512]) for i in range(2)]
        bT2_2 = [Buf("T2_%d" % i) for i in range(2)]
        MBF_2 = [sb9("MBF_%d" % i, [128, 512], BF16) for i in range(2)]
        bMBF_2 = [Buf("MBF_%d" % i) for i in range(2)]
        MTt_2 = [sb9("MTt_%d" % i, [128, 4, 128], BF16) for i in range(2)]
        bMTt_2 = [Buf("MTt_%d" % i) for i in range(2)]
        XT_2 = [sb9("XT_%d" % i, [128, 8, 128], BF16) for i in range(2)]
        bXT_2 = [Buf("XT_%d" % i) for i in range(2)]
        FTB_2 = [sb9("FTB_%d" % i, [128, 4, 128], BF16) for i in range(2)]
        bFTB_2 = [Buf("FTB_%d" % i) for i in range(2)]
        YFT_2 = [sb9("YFT_%d" % i, [128, 4, 128], BF16) for i in range(2)]
        bYFT_2 = [Buf("YFT_%d" % i) for i in range(2)]
        JK_2 = [sb9("JK_%d" % i, [128, D], BF16) for i in range(2)]
        bJK_2 = [Buf("JK_%d" % i) for i in range(2)]
        SY_2 = [sb9("SY_%d" % i, [128, 8]) for i in range(2)]
        bSY_2 = [Buf("SY_%d" % i) for i in range(2)]
        TT__2 = [sb9("TT__%d" % i, [128, D]) for i in range(2)]
        bTT_2 = [Buf("TT__%d" % i) for i in range(2)]
        H2_2 = [sb9("H2_%d" % i, [128, D]) for i in range(2)]
        bH2_2 = [Buf("H2_%d" % i) for i in range(2)]
        H2H_2 = [sb9("H2H_%d" % i, [128, D], BF16) for i in range(2)]
        bH2H_2 = [Buf("H2H_%d" % i) for i in range(2)]
        H2Lw_2 = [sb9("H2Lw_%d" % i, [128, D], BF16) for i in range(2)]
        bH2Lw_2 = [Buf("H2Lw_%d" % i) for i in range(2)]
        H2LT_2 = [sb9("H2LT_%d" % i, [128, 8, 128], BF16) for i in range(2)]
        bH2LT_2 = [Buf("H2LT_%d" % i) for i in range(2)]
        LG_2 = [sb9("LG_%d" % i, [128, 20]) for i in range(2)]
        bLG_2 = [Buf("LG_%d" % i) for i in range(2)]
        RT_2 = [sb9("RT_%d" % i, [128, 96]) for i in range(2)]
        bRT_2 = [Buf("RT_%d" % i) for i in range(2)]
        XR = [sb9("XR%d" % i, [128, D]) for i in range(2)]
        bXR = [Buf("XR%d" % i) for i in range(2)]
        PRr = [sb9("PRr%d" % i, [128, 512]) for i in range(2)]
        bPRr = [Buf("PRr%d" % i) for i in range(2)]
        X1 = [sb9("X1%d" % i, [128, D]) for i in range(2)]
        bX1 = [Buf("X1%d" % i) for i in range(2)]
        bOUT = Buf("out_d")
        BIG = 30000.0
        AX = mybir.AxisListType.X

        def red(eng, out, in_, op, reads, writes):
            return P.op(eng, lambda e: e.tensor_reduce(out=out, in_=in_, axis=AX, op=op), reads, writes)

        def s6_tile(c):
            s_ = c % 2
            rows = slice(c * 128, (c + 1) * 128)
            bk = (0, 1, 2, 3) if s_ == 0 else (4, 5, 6, 7)
            PTl = PS[bk[0]][:, :].bitcast(BF16).rearrange("p (k t) -> p k t", t=128)
            SM_, bSM_ = SM__2[s_], bSM__2[s_]
            CEN, bCEN = CEN_2[s_], bCEN_2[s_]
            SQ, bSQ = SQ_2[s_], bSQ_2[s_]
            T1, bT1 = T1_2[s_], bT1_2[s_]
            T2, bT2 = T2_2[s_], bT2_2[s_]
            MBF, bMBF = MBF_2[s_], bMBF_2[s_]
            MTt, bMTt = MTt_2[s_], bMTt_2[s_]
            XT, bXT = XT_2[s_], bXT_2[s_]
            FTB, bFTB = FTB_2[s_], bFTB_2[s_]
            YFT, bYFT = YFT_2[s_], bYFT_2[s_]
            JK, bJK = JK_2[s_], bJK_2[s_]
            SY, bSY = SY_2[s_], bSY_2[s_]
            TT_, bTT = TT__2[s_], bTT_2[s_]
            H2, bH2 = H2_2[s_], bH2_2[s_]
            H2H, bH2H = H2H_2[s_], bH2H_2[s_]
            H2Lw, bH2Lw = H2Lw_2[s_], bH2Lw_2[s_]
            H2LT, bH2LT = H2LT_2[s_], bH2LT_2[s_]
            LG, bLG = LG_2[s_], bLG_2[s_]
            RT, bRT = RT_2[s_], bRT_2[s_]
            hs, bhs, az, baz = HSt[s_], bHSt[s_], AZ[s_], bAZ_[s_]
            P.dma("sp", hs[:, :], hs_d.ap()[rows, :], reads=[bHSD], writes=[bhs])
            P.dma("sp", az[:, 0, :], az_d.ap()[0, rows, :], reads=[bAZ], writes=[baz])
            P.dma("sp", az[:, 1, :], az_d.ap()[1, rows, :], reads=[bAZ], writes=[baz])
            hs3 = hs[:, :].rearrange("p (h d) -> p h d", d=128)
            cen3 = CEN[:, :].rearrange("p (h d) -> p h d", d=128)
            sq3 = SQ[:, :].rearrange("p (h d) -> p h d", d=128)
            yield
            red("dve", SM_[:, 0:4], hs3, ALU.add, [bhs], [bSM_])
            ts("dve", SM_[:, 4:8], SM_[:, 0:4], 1.0 / 128.0, None, ALU.mult, None, [bSM_], [bSM_])
            tt("dve", cen3, hs3, SM_[:, 4:8, None].broadcast_to([128, 4, 128]), ALU.subtract, [bhs, bSM_], [bCEN])
            yield
            tt("pool", SQ[:, :], CEN[:, :], CEN[:, :], ALU.mult, [bCEN], [bSQ])
            red("dve", SM_[:, 8:12], sq3, ALU.add, [bSQ], [bSM_])
            yield
            ts("pool", SM_[:, 12:16], SM_[:, 8:12], 1.0 / 128.0, EPS, ALU.mult, ALU.add, [bSM_], [bSM_])
            tt("pool", SM_[:, 16:20], SM_[:, 12:16], NEGH.broadcast_to([128, 4]), ALU.pow, [bSM_, bTABS], [bSM_])
            tt("dve", cen3, cen3, SM_[:, 16:20, None].broadcast_to([128, 4, 128]), ALU.mult, [bCEN, bSM_], [bCEN])
            yield
            tt("dve", T1[:, :], CEN[:, :], NWSK[:, 0, :], ALU.mult, [bCEN, bNWSK], [bT1])
            tt("pool", T2[:, :], az[:, 0, :], NWSK[:, 1, :], ALU.mult, [baz, bNWSK], [bT2])
            tt("dve", T1[:, :], T1[:, :], T2[:, :], ALU.add, [bT1, bT2], [bT1])
            yield
            tt("dve", MBF[:, :], T1[:, :], az[:, 1, :], ALU.mult, [bT1, baz], [bMBF])
            if c == 0:
                dump("m0", MBF[:, :], [128, 512], [bMBF])
            yield
            for cc in range(4):
                tr(PTl[:, cc, :], MBF[:, cc * 128:(cc + 1) * 128], IDB[:, :], [bMBF, bIDB], [PB[bk[0]]])
            cp("act", MTt[:, :, :], PTl[:, 0:4, :], [PB[bk[0]]], [bMTt])
            yield
            for sg in range(2):
                for g in range(4):
                    tr(PTl[:, sg * 4 + g, :], XCS[:, g * 128:(g + 1) * 128, sg, c], IDB[:, :], [bXCS, bIDB], [PB[bk[0]]])
            cp("act", XT[:, :, :], PTl, [PB[bk[0]]], [bXT])
            yield
            for g in range(4):
                mm(PS[bk[1]][:, g * 128:(g + 1) * 128], CS128[:, 0:128], XT[:, g, :], True, False, [bCS, bXT], [PB[bk[1]]])
                mm(PS[bk[1]][:, g * 128:(g + 1) * 128], CS128[:, 256:384], XT[:, 4 + g, :], False, True, [bCS, bXT], [PB[bk[1]]])
            cp("dve", FTB[:, :, :], PS[bk[1]][:, :].rearrange("p (g t) -> p g t", t=128), [PB[bk[1]]], [bFTB])
            yield
            for g in range(4):
                mm(PS[bk[1]][:, g * 128:(g + 1) * 128], WFB[:, g, :], FTB[:, g, :], True, True, [bWFB, bFTB], [PB[bk[1]]])
            cp("act", YFT[:, :, :], PS[bk[1]][:, :].rearrange("p (g t) -> p g t", t=128), [PB[bk[1]]], [bYFT])
            yield
            for cb in range(2):
                for kc in range(4):
                    mm(PS[bk[2 + cb]][:, :], MTt[:, kc, :], WOB[:, kc, cb * 512:(cb + 1) * 512], kc == 0, False, [bMTt, bWOB], [PB[bk[2 + cb]]])
                for kc in range(4):
                    mm(PS[bk[2 + cb]][:, :], YFT[:, kc, :], WOB[:, 4 + kc, cb * 512:(cb + 1) * 512], False, kc == 3, [bYFT, bWOB], [PB[bk[2 + cb]]])
            yield
            act(JK[:, 0:512], PS[bk[2]][:, :], AF.Square, [PB[bk[2]]], [bJK, bSY], accum_out=SY[:, 0:1])
            act(JK[:, 512:1024], PS[bk[3]][:, :], AF.Square, [PB[bk[3]]], [bJK, bSY], accum_out=SY[:, 1:2])
            yield
            tt("pool", SY[:, 2:3], SY[:, 0:1], SY[:, 1:2], ALU.add, [bSY], [bSY])
            ts("pool", SY[:, 3:4], SY[:, 2:3], 1.0 / D, EPS, ALU.mult, ALU.add, [bSY], [bSY])
            tt("pool", SY[:, 4:5], SY[:, 3:4], NEGH, ALU.pow, [bSY, bTABS], [bSY])
            yield
            xr, bxr, pr_, bpr_ = XR[s_], bXR[s_], PRr[s_], bPRr[s_]
            P.dma("sp", xr[:, :], x_rot.ap()[rows, :], writes=[bxr])
            P.dma("sp", pr_[0:64, :], bc_rows(posr_d, 2 * c, 512, parts=64), reads=[bPOSRD], writes=[bpr_])
            P.dma("sp", pr_[64:128, :], bc_rows(posr_d, 2 * c + 1, 512, parts=64), reads=[bPOSRD], writes=[bpr_])
            tt("pool", xr[:, 0:512], xr[:, 0:512], pr_[:, :], ALU.add, [bxr, bpr_], [bxr])
            tt("pool", xr[:, 512:1024], xr[:, 512:1024], POSC[:, :], ALU.add, [bxr, bPOSC], [bxr])
            yield
            stt("dve", TT_[:, 0:512], PS[bk[2]][:, :], SY[:, 4:5], GP1[:, 0:512], ALU.mult, ALU.mult, [PB[bk[2]], bSY, bMOD], [bTT])
            stt("dve", TT_[:, 512:1024], PS[bk[3]][:, :], SY[:, 4:5], GP1[:, 512:1024], ALU.mult, ALU.mult, [PB[bk[3]], bSY, bMOD], [bTT])
            yield
            x1, bx1 = X1[s_], bX1[s_]
            tt("pool", x1[:, :], TT_[:, :], xr[:, :], ALU.add, [bTT, bxr], [bx1])
            P.dma("sp", out_d.ap()[rows, :], x1[:, :], reads=[bx1], writes=[bOUT])
            if c == 0:
                dump("x1_0", x1[:, :], [128, D], [bx1])
            yield
            act(JK[:, :], x1[:, :], AF.Square, [bx1], [bJK, bSY], accum_out=SY[:, 5:6])
            yield
            ts("pool", SY[:, 6:7], SY[:, 5:6], 1.0 / D, EPS, ALU.mult, ALU.add, [bSY], [bSY])
            tt("pool", SY[:, 7:8], SY[:, 6:7], NEGH, ALU.pow, [bSY, bTABS], [bSY])
            yield
            stt("dve", TT_[:, :], x1[:, :], SY[:, 7:8], G2, ALU.mult, ALU.mult, [bx1, bSY, bMOD], [bTT])
            tt("pool", H2[:, :], TT_[:, :], S2, ALU.add, [bTT, bMOD], [bH2])
            yield
            cp("act", H2H[:, :], H2[:, :], [bH2], [bH2H])
            tt("dve", H2Lw[:, :], H2[:, :], H2H[:, :], ALU.subtract, [bH2, bH2H], [bH2Lw])
            yield
            for kc in range(8):
                tr(PTl[:, kc, :], H2H[:, kc * 128:(kc + 1) * 128], IDB[:, :], [bH2H, bIDB], [PB[bk[0]]])
            cp("act", H2T[:, :, rows], PTl, [PB[bk[0]]], [bH2T])
            yield
            for kc in range(8):
                tr(PTl[:, kc, :], H2Lw[:, kc * 128:(kc + 1) * 128], IDB[:, :], [bH2Lw, bIDB], [PB[bk[0]]])
            cp("dve", H2LT[:, :, :], PTl, [PB[bk[0]]], [bH2LT])
            yield
            LGp = PS[bk[1]][:, 0:20]
            for kc in range(8):
                mm(LGp, H2T[:, kc, rows], WRH[:, kc, :], kc == 0, False, [bH2T, bWR], [PB[bk[1]]])
            for kc in range(8):
                mm(LGp, H2T[:, kc, rows], WRL[:, kc, :], False, False, [bH2T, bWR], [PB[bk[1]]])
            for kc in range(8):
                mm(LGp, H2LT[:, kc, :], WRH[:, kc, :], False, kc == 7, [bH2LT, bWR], [PB[bk[1]]])
            yield
            tt("dve", LG[:, :], LGp, BR[:, :], ALU.add, [PB[bk[1]], bBR], [bLG])
            if c == 0:
                dump("lg0", LG[:, :], [128, 20], [bLG])
            R = RT
            bR = bRT
            yield
            red("dve", R[:, 0:1], LG[:, 0:4], ALU.max, [bLG], [bR])
            ts("dve", R[:, 1:5], LG[:, 0:4], R[:, 0:1], None, ALU.is_equal, None, [bLG, bR], [bR])
            ts("dve", R[:, 5:6], R[:, 0:1], -1.0, None, ALU.mult, None, [bR], [bR])
            yield
            act(R[:, 6:10], LG[:, 0:4], AF.Exp, [bLG, bR], [bR], bias=R[:, 5:6], accum_out=R[:, 10:11])
            P.op("dve", lambda e, o=R[:, 11:12], i_=R[:, 10:11]: e.reciprocal(out=o, in_=i_), [bR], [bR])
            yield
            ts("dve", R[:, 12:16], R[:, 1:5], BIG, -BIG, ALU.mult, ALU.add, [bR], [bR])
            em = R[:, 16:32]
            tt("dve", em.rearrange("p (g j) -> p g j", j=4), LG[:, 4:20].rearrange("p (g j) -> p g j", j=4),
               R[:, 12:16, None].broadcast_to([128, 4, 4]), ALU.add, [bLG, bR], [bR])
            yield
            red("dve", R[:, 32:33], em, ALU.max, [bR], [bR])
            ts("dve", R[:, 48:64], em, R[:, 32:33], None, ALU.is_equal, None, [bR], [bR])
            stt("dve", R[:, 64:80], R[:, 48:64], -BIG, em, ALU.mult, ALU.add, [bR], [bR])
            yield
            red("dve", R[:, 33:34], R[:, 64:80], ALU.max, [bR], [bR])
            ts("dve", R[:, 80:96], R[:, 64:80], R[:, 33:34], None, ALU.is_equal, None, [bR], [bR])
            tt("dve", R[:, 34:35], R[:, 33:34], R[:, 32:33], ALU.subtract, [bR], [bR])
            yield
            act(R[:, 35:36], R[:, 34:35], AF.Exp, [bR], [bR])
            ts("dve", R[:, 36:37], R[:, 35:36], 1.0, None, ALU.add, None, [bR], [bR])
            P.op("dve", lambda e, o=R[:, 37:38], i_=R[:, 36:37]: e.reciprocal(out=o, in_=i_), [bR], [bR])
            tt("dve", R[:, 38:39], R[:, 37:38], R[:, 35:36], ALU.mult, [bR], [bR])
            yield
            tt("dve", R[:, 39:40], R[:, 37:38], R[:, 11:12], ALU.mult, [bR], [bR])
            tt("dve", R[:, 40:41], R[:, 38:39], R[:, 11:12], ALU.mult, [bR], [bR])
            ts("dve", COMB[:, c, :], R[:, 48:64], R[:, 39:40], None, ALU.mult, None, [bR], [bCOMB])
            stt("dve", COMB[:, c, :], R[:, 80:96], R[:, 40:41], COMB[:, c, :], ALU.mult, ALU.add, [bR, bCOMB], [bCOMB])
            yield

        def window6(genfs, w):
            pend = list(genfs)
            act_ = []
            while pend or act_:
                while pend and len(act_) < w:
                    act_.append(pend.pop(0)())
                for g_ in list(act_):
                    try:
                        next(g_)
                    except StopIteration:
                        act_.remove(g_)

        window6([(lambda c=c: s6_tile(c)) for c in range(OWN)], 2)
        dump("comb", COMB[:, :, :], [128, OWN, 16], [bCOMB])

        if stage <= 5:
            o_b = bOUT
            P.barrier()
            final.append((P.sem["sp"], 0))
            P.barrier()
            with nc.Block() as block:
                P.emit(block, [])
            st9.close()
            st7.close()
            return nc

        P.barrier()
        st9.close()
        st7.close()
        stE = ExitStack()

        def sbE(name, shape, dt=F32):
            return stE.enter_context(nc.sbuf_tensor(name, list(shape), dt))

        YACC = sbE("YACC", [128, OWN, D])
        bYACC = [Buf("YACC%d" % i) for i in range(OWN)]
        WG = [sbE("WG%d" % i, [128, 8, 512], BF16) for i in range(2)]
        WU = [sbE("WU%d" % i, [128, 8, 512], BF16) for i in range(2)]
        WD = [sbE("WD%d" % i, [128, 4, D], BF16) for i in range(2)]
        bWG = [Buf("WG%d" % i) for i in range(2)]
        bWU = [Buf("WU%d" % i) for i in range(2)]
        bWD = [Buf("WD%d" % i) for i in range(2)]
        ATb = [sbE("AT%d" % i, [128, 4, 512], BF16) for i in range(2)]
        bAT = [Buf("AT%d" % i) for i in range(2)]
        SG = [sbE("SG%d" % i, [128, 512]) for i in range(2)]
        bSG = [Buf("SG%d" % i) for i in range(2)]
        NEXP = 16
        gu_ctr = [0]
        dn_ctr = [0]
        for e_ in range(NEXP):
            s_ = e_ % 2
            P.dma("pool", WG[s_][:, :, :], w_gate.ap()[e_].rearrange("(k p) n -> p k n", p=128), writes=[bWG[s_]])
            P.dma("pool", WU[s_][:, :, :], w_up.ap()[e_].rearrange("(k p) n -> p k n", p=128), writes=[bWU[s_]])
            P.dma("pool", WD[s_][:, :, :], w_down.ap()[e_].rearrange("(k p) n -> p k n", p=128), writes=[bWD[s_]])
            for tb in range(OWN // 4):
                tsl = slice(tb * 512, (tb + 1) * 512)
                a_ = (e_ * 4 + tb) % 2
                for fb in range(4):
                    k_ = gu_ctr[0] % 2
                    gu_ctr[0] += 1
                    pg, pu = 2 * k_, 2 * k_ + 1
                    for kc in range(8):
                        mm(PS[pg][:, :], WG[s_][:, kc, fb * 128:(fb + 1) * 128], H2T[:, kc, tsl], kc == 0, kc == 7, [bWG[s_], bH2T], [PB[pg]])
                    for kc in range(8):
                        mm(PS[pu][:, :], WU[s_][:, kc, fb * 128:(fb + 1) * 128], H2T[:, kc, tsl], kc == 0, kc == 7, [bWU[s_], bH2T], [PB[pu]])
                    act(SG[k_][:, :], PS[pg][:, :], AF.Silu, [PB[pg]], [bSG[k_]])
                    tt("dve", ATb[a_][:, fb, :], SG[k_][:, :], PS[pu][:, :], ALU.mult, [bSG[k_], PB[pu]], [bAT[a_]])
                for t_ in range(4):
                    tile = tb * 4 + t_
                    for cb in range(2):
                        pd = 4 + dn_ctr[0] % 4
                        dn_ctr[0] += 1
                        for fb in range(4):
                            mm(PS[pd][:, :], ATb[a_][:, fb, t_ * 128:(t_ + 1) * 128], WD[s_][:, fb, cb * 512:(cb + 1) * 512], fb == 0, fb == 3, [bAT[a_], bWD[s_]], [PB[pd]])
                        ya = YACC[:, tile, cb * 512:(cb + 1) * 512]
                        if e_ == 0:
                            ts("dve", ya, PS[pd][:, :], COMB[:, tile, e_:e_ + 1], None, ALU.mult, None, [PB[pd], bCOMB], [bYACC[tile]])
                        else:
                            stt("dve", ya, PS[pd][:, :], COMB[:, tile, e_:e_ + 1], ya, ALU.mult, ALU.add, [PB[pd], bCOMB, bYACC[tile]], [bYACC[tile]])
        GP2L = sbE("GP2L", [128, D])
        bGP2L = Buf("GP2L")
        P.dma("sp", GP2L[:, :], bc_rows(mod_d, 0, D, off=5 * D), reads=[bMODD], writes=[bGP2L])
        GP2 = GP2L[:, :]
        bMOD = bGP2L
        XF = [sbE("XF%d" % i, [128, D]) for i in range(2)]
        bXF = [Buf("XF%d" % i) for i in range(2)]
        JK2 = sbE("JK2", [128, D], BF16)
        bJK2 = Buf("JK2")
        SF = sbE("SF", [128, 8])
        bSF = Buf("SF")
        OT = [sbE("OT%d" % i, [128, D]) for i in range(2)]
        bOT = [Buf("OT%d" % i) for i in range(2)]
        for c in range(OWN):
            s_ = c % 2
            rows = slice(c * 128, (c + 1) * 128)
            P.dma("sp", XF[s_][:, :], out_d.ap()[rows, :], reads=[bOUT], writes=[bXF[s_]])
            q_ = SF[:, s_ * 4:s_ * 4 + 4]
            act(JK2[:, :], YACC[:, c, :], AF.Square, [bYACC[c]], [bJK2, bSF], accum_out=q_[:, 0:1])
            ts("pool", q_[:, 1:2], q_[:, 0:1], 1.0 / D, EPS, ALU.mult, ALU.add, [bSF], [bSF])
            tt("pool", q_[:, 2:3], q_[:, 1:2], NEGH, ALU.pow, [bSF, bTABS], [bSF])
            stt("dve", OT[s_][:, :], YACC[:, c, :], q_[:, 2:3], GP2, ALU.mult, ALU.mult, [bYACC[c], bSF, bMOD], [bOT[s_]])
            tt("pool", OT[s_][:, :], OT[s_][:, :], XF[s_][:, :], ALU.add, [bOT[s_], bXF[s_]], [bOT[s_]])
            t = P.dma("sp", out_d.ap()[rows, :], OT[s_][:, :], reads=[bOT[s_], bXF[s_]], writes=[bOUT])
            final.append((t[0], t[1]))
        P.barrier()
        with nc.Block() as block:
            P.emit(block, final)
        stE.close()
    return nc


def _centered(idx, n):
    return ((idx + n // 2) % n) - n // 2


def make_inputs(core, x, c, ctx, c_ctx, w_ada, b_ada, g_pre_mix, g_post_mix, g_pre_ffn, g_post_ffn,
                w_in, conv_w, conv_b, w_q, w_k, w_v, w_if_fwd, b_if_fwd, w_if_bwd, b_if_bwd,
                mlstm_norm_w, mlstm_skip, w_fourier, w_out, w_router_group, b_router_group,
                w_router_expert, b_router_expert, w_gate, w_up, w_down, shared):
    f32 = np.float32
    j = core
    xs = x[0]
    m = {}
    m["x_rot"] = np.ascontiguousarray(np.roll(xs, -2048 * j, axis=0))
    halo = np.zeros((128, D), f32)
    hmask = np.zeros(64, f32)
    hrow = np.zeros(128, f32)
    hcol = np.zeros(128, f32)
    for g in range(NG):
        for side, tr_ in ((0, 512 * g - 1), (1, 512 * g + 512)):
            true_t = (tr_ % T + 2048 * j) % T
            own_first_true = ((512 * g) % T + 2048 * j) % T
            if side == 0:
                valid = own_first_true != 0
            else:
                valid = ((512 * g + 511) % T + 2048 * j) % T != T - 1
            halo[2 * g + side] = xs[true_t]
            hmask[2 * g + side] = 1.0 if valid else 0.0
            hrow[2 * g + side] = true_t // 64
            hcol[2 * g + side] = true_t % 64
    m["x_halo"] = halo
    tabs = np.zeros((128, 1024), f32)
    p = np.arange(128)
    for a in range(2):
        tabs[:, a] = ((2 * p + a) + 32 * j) % 256
    tabs[:, 2] = p % 64
    tabs[:, 3] = hrow
    tabs[:, 4] = hcol
    tabs[:, 5] = -0.5
    i = np.arange(128)
    true_c = (i + 16 * j) % 128
    tabs[:, 128:256] = (true_c < 16 * j).astype(f32)[None, :]
    tabs[:, 256:384] = (true_c >= 16 * j + 16).astype(f32)[None, :]
    tabs[:, 384:448] = hmask[None, :]
    tabs[:, 512:768] = np.arange(256, dtype=f32)[None, :]
    m["tabs"] = tabs
    di = np.zeros((128, 640), f32)
    n = np.arange(128)[:, None]
    k = np.arange(128)[None, :]
    di[:, 0:128] = _centered(n * k + 32, 128)
    di[:, 128:256] = _centered(n * k, 128)
    base = n * k + 2048 * j * k
    di[:, 256:384] = _centered(base + 4096, 16384)
    di[:, 384:512] = _centered(base, 16384)
    cc = (16 * j + np.arange(16))[None, :]
    di[:, 512:528] = _centered(n * cc + 32, 128)
    di[:, 528:544] = _centered(n * cc, 128)
    m["dftidx"] = di
    m.update(shared)
    return m


def make_shared(x, c, ctx, c_ctx, w_ada, b_ada, g_pre_mix, g_post_mix, g_pre_ffn, g_post_ffn,
                w_in, conv_w, conv_b, w_q, w_k, w_v, w_if_fwd, b_if_fwd, w_if_bwd, b_if_bwd,
                mlstm_norm_w, mlstm_skip, w_fourier, w_out, w_router_group, b_router_group,
                w_router_expert, b_router_expert, w_gate, w_up, w_down):
    f32 = np.float32
    s = {}
    s["ctx"] = np.ascontiguousarray(ctx[0])
    cT = np.zeros((128, 16), f32)
    cT[:, 0:8] = c[0].reshape(8, 128).T
    cT[:, 8:16] = c_ctx.reshape(8, 128).T
    s["cT"] = cT
    s["w_ada"] = np.ascontiguousarray(w_ada[0])
    s["b_ada"] = np.ascontiguousarray(b_ada[0][None, :])
    s["gains"] = np.stack([g_pre_mix[0], g_post_mix[0], g_pre_ffn[0], g_post_ffn[0]]).astype(f32)
    s["w_in"] = np.ascontiguousarray(w_in[0])
    cw = np.zeros((128, 16), f32)
    for cc in range(4):
        for k in range(3):
            cw[:, cc * 3 + k] = conv_w[0][k, cc * 128:(cc + 1) * 128]
        cw[:, 12 + cc] = conv_b[0][cc * 128:(cc + 1) * 128]
    s["convw"] = cw
    s["w_qkv"] = np.stack([w_q[0].reshape(512, 4), w_k[0].reshape(512, 4), w_v[0].reshape(512, 4)]).astype(f32)
    s["w_qkvT"] = np.stack([np.ascontiguousarray(w.transpose(0, 2, 1)).reshape(512, 4) for w in (w_q[0], w_k[0], w_v[0])]).astype(f32)
    wf, wb = w_if_fwd[0], w_if_bwd[0]
    s["w_if"] = np.ascontiguousarray(np.concatenate([wf[:, 0:4], wb[:, 0:4], wf[:, 4:8], wb[:, 4:8]], axis=1))
    bf, bb = b_if_fwd[0], b_if_bwd[0]
    s["b_if"] = np.concatenate([bf[0:4], bb[0:4], bf[4:8], bb[4:8]])[None, :].astype(f32)
    s["nrm_skip"] = np.stack([mlstm_norm_w[0], mlstm_skip[0]]).astype(f32)
    s["w_fourier"] = np.ascontiguousarray(w_fourier[0])
    s["w_out"] = np.ascontiguousarray(w_out[0])
    s["w_router"] = np.ascontiguousarray(np.concatenate([w_router_group[0], w_router_expert[0]], axis=1))
    s["b_router"] = np.concatenate([b_router_group[0], b_router_expert[0]])[None, :].astype(f32)
    s["w_gate"] = np.ascontiguousarray(w_gate[0])
    s["w_up"] = np.ascontiguousarray(w_up[0])
    s["w_down"] = np.ascontiguousarray(w_down[0])
    cst = np.zeros((128, 640), f32)
    cst[:, 0:128] = np.eye(128)
    a = np.arange(128)
    cst[:, 128:256] = (a[:, None] <= a[None, :])
    cst[:, 256:384] = (a[:, None] >= a[None, :])
    cst[:, 384:512] = 1.0
    cst[:, 512:640] = (a[:, None] // 4 == a[None, :] // 4)
    s["consts"] = cst
    return s


_CACHE = {}


def kernel(**inputs):
    inputs = {k: np.asarray(v) for k, v in inputs.items()}
    if "nc" not in _CACHE:
        _CACHE["nc"] = build()
    nc = _CACHE["nc"]
    shared = make_shared(**inputs)
    in_maps = [make_inputs(core, shared=shared, **inputs) for core in range(NCORES)]
    res = run_bass_kernel_spmd(nc, in_maps, core_ids=list(range(NCORES)))
    out = np.concatenate([res.results[i]["out"] for i in range(NCORES)], axis=0)
    return out.reshape(1, T, D).astype(np.float32)
```

```python
import math
from contextlib import ExitStack
import numpy as np
import concourse.bass as bass
import concourse.mybir as mybir
from concourse.bass_utils import run_bass_kernel_spmd

F32 = mybir.dt.float32
BF16 = mybir.dt.bfloat16
I32 = mybir.dt.int32
AF = mybir.ActivationFunctionType
ALU = mybir.AluOpType

NCORES = 8
T = 16384
D = 1024
NT = T // 128
OWN = NT // NCORES
NG = NT // 4
EPS = 1e-6
TWO_PI = 2.0 * math.pi
NHALO = 2 * NG
DBG = {}


class Buf:
    __slots__ = ("name", "w", "r")

    def __init__(self, name):
        self.name = name
        self.w = None
        self.r = {}


class Prog:
    def __init__(self, nc, stack, ndma=28):
        self.nc = nc
        self.engs = {"pe": nc.tensor, "act": nc.scalar, "dve": nc.vector, "pool": nc.gpsimd, "sp": nc.sync}
        self.ops = {k: [] for k in self.engs}
        self.sem = {k: stack.enter_context(nc.semaphore("s_" + k)) for k in self.engs}
        self.cnt = {k: 0 for k in self.engs}
        self.waited = {k: {} for k in self.engs}
        self.dsem = [stack.enter_context(nc.semaphore("d%d" % i)) for i in range(ndma)]
        self.dcnt = [0] * ndma
        self.ring = {"sp": list(range(0, ndma - 8)), "act": list(range(0, ndma - 8)), "pool": list(range(ndma - 8, ndma))}
        self.rpos = {"sp": 0, "pool": 0}

    def _collect(self, e, reads, writes, sync_same=True):
        waits = {}

        def need(tok):
            if tok is None:
                return
            sem, val, eng = tok
            if eng == e and not sync_same:
                return
            key = id(sem)
            if self.waited[e].get(key, 0) >= val:
                return
            if key not in waits or waits[key][1] < val:
                waits[key] = (sem, val)

        for b in reads:
            need(b.w)
        for b in writes:
            need(b.w)
            for t in b.r.values():
                need(t)
        for key, (sem, val) in waits.items():
            self.waited[e][key] = val
        return list(waits.values())

    def _commit(self, tok, reads, writes):
        key = id(tok[0])
        for b in reads:
            old = b.r.get(key)
            if old is None or old[1] < tok[1]:
                b.r[key] = tok
        for b in writes:
            b.w = tok
            b.r = {}

    def op(self, e, fn, reads=(), writes=(), sync_same=True):
        waits = self._collect(e, reads, writes, sync_same)
        self.cnt[e] += 1
        tok = (self.sem[e], self.cnt[e], e)
        self.ops[e].append((waits, fn, (self.sem[e], 1)))
        self._commit(tok, reads, writes)
        return tok

    def dma(self, e, out, in_, reads=(), writes=()):
        rk = "pool" if e == "pool" else "sp"
        ring = self.ring[rk]
        i = ring[self.rpos[rk] % len(ring)]
        self.rpos[rk] += 1
        sem = self.dsem[i]
        waits = self._collect(e, reads, writes)
        prev = self.dcnt[i] * 16
        if prev > 0 and self.waited[e].get(id(sem), 0) < prev:
            waits.append((sem, prev))
            self.waited[e][id(sem)] = prev
        self.dcnt[i] += 1
        tok = (sem, self.dcnt[i] * 16, "dma")
        self.ops[e].append((waits, (lambda eng, o=out, i_=in_: eng.dma_start(out=o, in_=i_)), (sem, 16)))
        self._commit(tok, reads, writes)
        return tok

    def barrier(self):
        toks = [(self.sem[k], self.cnt[k]) for k in self.engs if self.cnt[k] > 0]
        toks += [(self.dsem[i], self.dcnt[i] * 16) for i in range(len(self.dsem)) if self.dcnt[i] > 0]
        for e in self.engs:
            waits = []
            for sem, val in toks:
                if self.waited[e].get(id(sem), 0) < val:
                    waits.append((sem, val))
                    self.waited[e][id(sem)] = val
            if waits:
                self.ops[e].append((waits, None, None))

    def emit(self, block, final_waits):
        def replay(e):
            def body(eng):
                for waits, fn, inc in self.ops[e]:
                    for sem, val in waits:
                        eng.wait_ge(sem, val)
                    if fn is not None:
                        ins = fn(eng)
                        ins.then_inc(inc[0], inc[1])
                if e == "sp":
                    for sem, val in final_waits:
                        eng.wait_ge(sem, val)
            return body

        block.tensor(replay("pe"))
        block.scalar(replay("act"))
        block.vector(replay("dve"))
        block.gpsimd(replay("pool"))
        block.sync(replay("sp"))


def build(stage=99, dbg=False, ngroups=NG, cut=99):
    nc = bass.Bass("TRN2", target_bir_lowering=False)
    DBG.clear()

    def din(name, shape, dt=F32):
        return nc.dram_tensor(name, list(shape), dt, kind="ExternalInput")

    x_rot = din("x_rot", [ngroups * 512, D])
    x_halo = din("x_halo", [128, D])
    ctx_in = din("ctx", [256, D])
    cT = din("cT", [128, 16])
    w_ada = din("w_ada", [D, 6 * D])
    b_ada = din("b_ada", [1, 6 * D])
    gains = din("gains", [4, D])
    w_in = din("w_in", [D, 1536])
    convw = din("convw", [128, 16])
    w_qkv = din("w_qkv", [3, 512, 4])
    w_qkvT = din("w_qkvT", [3, 512, 4])
    w_if = din("w_if", [1536, 16])
    b_if = din("b_if", [1, 16])
    nrm_skip = din("nrm_skip", [2, 512])
    w_fourier = din("w_fourier", [4, 128, 128])
    w_out = din("w_out", [D, D])
    w_router = din("w_router", [D, 20])
    b_router = din("b_router", [1, 20])
    moe_small = stage < 8
    w_gate = din("w_gate", [16, D, 512] if not moe_small else [1, 8, 8])
    w_up = din("w_up", [16, D, 512] if not moe_small else [1, 8, 8])
    w_down = din("w_down", [16, 512, D] if not moe_small else [1, 8, 8])
    consts = din("consts", [128, 5 * 128])
    tabs = din("tabs", [128, 1024])
    dftidx = din("dftidx", [128, 5 * 128])
    out_d = nc.dram_tensor("out", [OWN * 128, D], F32, kind="ExternalOutput")

    posr_d = nc.dram_tensor("posr_d", [256, 512], F32)
    u_d = nc.dram_tensor("u_d", [512, T], BF16)
    az_d = nc.dram_tensor("az_d", [2, OWN * 128, 512], BF16)
    hs_d = nc.dram_tensor("hs_d", [OWN * 128, 512], F32)
    mod_d = nc.dram_tensor("mod_d", [1, 6 * D], F32)

    dbg_outs = {}

    def dbg_out(name, shape):
        DBG[name] = tuple(shape)
        dbg_outs[name] = nc.dram_tensor("dbg_" + name, list(shape), F32, kind="ExternalOutput")
        return dbg_outs[name]

    stack = ExitStack()
    with stack:
        P = Prog(nc, stack)
        used = [0]

        def sb(name, shape, dt=F32):
            t = stack.enter_context(nc.sbuf_tensor(name, list(shape), dt))
            return t

        def psum(name, shape, dt=F32):
            return stack.enter_context(nc.psum_tensor(name, list(shape), dt))

        def act(out, in_, func, reads, writes, eng="act", **kw):
            return P.op("act", lambda e: e.activation(out=out, in_=in_, func=func, **kw), reads, writes)

        def tt(eng, out, in0, in1, op, reads, writes):
            return P.op(eng, lambda e: e.tensor_tensor(out=out, in0=in0, in1=in1, op=op), reads, writes)

        def ts(eng, out, in0, s1, s2, op0, op1, reads, writes):
            if op1 is None:
                return P.op(eng, lambda e: e.tensor_scalar(out=out, in0=in0, scalar1=s1, scalar2=None, op0=op0), reads, writes)
            return P.op(eng, lambda e: e.tensor_scalar(out=out, in0=in0, scalar1=s1, scalar2=s2, op0=op0, op1=op1), reads, writes)

        def stt(eng, out, in0, scalar, in1, op0, op1, reads, writes):
            return P.op(eng, lambda e: e.scalar_tensor_tensor(out=out, in0=in0, scalar=scalar, in1=in1, op0=op0, op1=op1), reads, writes)

        def cp(eng, out, in_, reads, writes):
            if eng == "act":
                return P.op("act", lambda e: e.activation(out=out, in_=in_, func=AF.Copy), reads, writes)
            return P.op(eng, lambda e: e.tensor_copy(out=out, in_=in_), reads, writes)

        def mm(out, lhsT, rhs, start, stop, reads, writes):
            return P.op("pe", lambda e: e.matmul(out, lhsT=lhsT, rhs=rhs, start=start, stop=stop), reads, writes, sync_same=False)

        def tr(out, in_, ident, reads, writes):
            return P.op("pe", lambda e: e.transpose(out=out, in_=in_, identity=ident), reads, writes, sync_same=False)

        def memset(eng, ap, val, writes):
            return P.op(eng, lambda e: e.memset(ap, val), (), writes)

        def dump(name, ap, shape, reads):
            if not dbg:
                return
            dd = dbg_out(name, shape)
            P.dma("pool", dd.ap(), ap, reads=reads, writes=[Buf("dbg")])

        def bc_rows(dram_t, row, n, parts=128, off=0):
            width = dram_t.shape[-1]
            return bass.AP(dram_t, row * width + off, [[0, parts], [1, n]])

        PS = [psum("ps%d" % i, [128, 512]) for i in range(8)]
        PB = [Buf("ps%d" % i) for i in range(8)]

        CONST = sb("CONST", [128, 640])
        bCONST = Buf("CONST")
        P.dma("sp", CONST[:, :], consts.ap(), writes=[bCONST])
        IDF = CONST[:, 0:128]
        TRIU = CONST[:, 128:256]
        TRIL = CONST[:, 256:384]
        ONES = CONST[:, 384:512]
        BDM = CONST[:, 512:640]
        IDB = sb("IDB", [128, 128], BF16)
        bIDB = Buf("IDB")
        cp("dve", IDB[:, :], IDF, [bCONST], [bIDB])
        TABS = sb("TABS", [128, 1024])
        bTABS = Buf("TABS")
        P.dma("sp", TABS[:, :], tabs.ap(), writes=[bTABS])
        NEGH = TABS[:, 5:6]
        POSC = sb("POSC", [128, 512])
        bPOSC = Buf("POSC")

        final = []
        stm = ExitStack()

        def sbm(name, shape, dt=F32):
            return stm.enter_context(nc.sbuf_tensor(name, list(shape), dt))

        QTF = sbm("QTF", [128, 4 * OWN * 128], BF16)
        QT = QTF[:, :].rearrange("p (c t) -> p c t", c=4)
        bQT = Buf("QT")
        KTF = sbm("KTF", [128, 4 * OWN * 128], BF16)
        KT = KTF[:, :].rearrange("p (c t) -> p c t", c=4)
        bKT = Buf("KT")
        WINC = KTF[:, 0:4096].rearrange("p (k n) -> p k n", n=512)
        bWINC = bKT
        KTMO = sbm("KTMO", [128, OWN, 512], BF16)
        bKTMO = [Buf("KTMO%d" % i) for i in range(OWN)]
        VAO = sbm("VAO", [128, OWN, 4, 129], BF16)
        bVAO = [Buf("VAO%d" % i) for i in range(OWN)]
        OWNG = sbm("OWNG", [128, OWN, 24])
        bOWNG = Buf("OWNG")
        CF = sbm("CF", [128, 4, 129])
        bCF = Buf("CF")
        CB = sbm("CB", [128, 4, 129])
        bCB = Buf("CB")
        sts = ExitStack()

        def sbs(name, shape, dt=F32):
            return sts.enter_context(nc.sbuf_tensor(name, list(shape), dt))

        WIN = sbs("WIN", [128, 8, 1536], BF16)
        bWIN = Buf("WIN")
        BCOL = sbs("BCOL", [128, 16])
        bBCOL = Buf("BCOL")
        BZ = sbs("BZ", [128, 512])
        bBZ = Buf("BZ")
        bMODD = Buf("mod_d")

        def interleave(gens):
            gens = list(gens)
            while gens:
                for g_ in list(gens):
                    try:
                        next(g_)
                    except StopIteration:
                        gens.remove(g_)

        with ExitStack() as st0:
            def sb0(name, shape, dt=F32):
                return st0.enter_context(nc.sbuf_tensor(name, list(shape), dt))
            MOD = sb0("MOD", [128, 6 * D])
            bMOD = Buf("MOD")
            MODC = sb0("MODC", [128, 2 * D])
            bMODC = Buf("MODC")
            st0a = ExitStack()

            def sb0a(name, shape, dt=F32):
                return st0a.enter_context(nc.sbuf_tensor(name, list(shape), dt))
            CT = sb0a("CT", [128, 16])
            bCT = Buf("CT")
            P.dma("sp", CT[:, :], cT.ap(), writes=[bCT])
            SC = sb0a("SC", [128, 16])
            bSC = Buf("SC")
            act(SC[:, :], CT[:, :], AF.Silu, [bCT], [bSC])
            REP = sb0a("REP", [128, 16, 128])
            bREP = Buf("REP")
            for j in range(16):
                cp("dve", REP[:, j, :], SC[:, j:j + 1].broadcast_to([128, 128]), [bSC], [bREP])
            WA = [sb0a("WA%d" % i, [128, 8, 512]) for i in range(2)]
            bWA = [Buf("WA%d" % i) for i in range(2)]
            BA = [sb0a("BA%d" % i, [128, 512]) for i in range(2)]
            bBA = [Buf("BA%d" % i) for i in range(2)]
            w_ada_v = w_ada.ap().rearrange("(k p) n -> p k n", p=128)
            for blk in range(12):
                s = blk % 2
                P.dma("sp", WA[s][:, :, :], w_ada_v[:, :, blk * 512:(blk + 1) * 512], writes=[bWA[s]])
                P.dma("sp", BA[s][:, :], bc_rows(b_ada, 0, 512, off=blk * 512), writes=[bBA[s]])
                pb = blk % 2
                for kc in range(8):
                    mm(PS[pb][:, :], REP[:, kc, :], WA[s][:, kc, :], kc == 0, kc == 7, [bREP, bWA[s]], [PB[pb]])
                tt("dve", MOD[:, blk * 512:(blk + 1) * 512], PS[pb][:, :], BA[s][:, :], ALU.add, [PB[pb], bBA[s]], [bMOD])
                if blk < 4:
                    pc = 2 + blk % 2
                    for kc in range(8):
                        mm(PS[pc][:, :], REP[:, 8 + kc, :], WA[s][:, kc, :], kc == 0, kc == 7, [bREP, bWA[s]], [PB[pc]])
                    tt("dve", MODC[:, blk * 512:(blk + 1) * 512], PS[pc][:, :], BA[s][:, :], ALU.add, [PB[pc], bBA[s]], [bMODC])
            GB = sb0a("GB", [128, 4, D])
            bGB = Buf("GB")
            for i in range(4):
                P.dma("sp", GB[:, i, :], bc_rows(gains, i, D), writes=[bGB])
            stt("dve", MOD[:, D:2 * D], MOD[:, D:2 * D], 1.0, GB[:, 0, :], ALU.add, ALU.mult, [bMOD, bGB], [bMOD])
            tt("dve", MOD[:, 2 * D:3 * D], MOD[:, 2 * D:3 * D], GB[:, 1, :], ALU.mult, [bMOD, bGB], [bMOD])
            stt("dve", MOD[:, 4 * D:5 * D], MOD[:, 4 * D:5 * D], 1.0, GB[:, 2, :], ALU.add, ALU.mult, [bMOD, bGB], [bMOD])
            tt("dve", MOD[:, 5 * D:6 * D], MOD[:, 5 * D:6 * D], GB[:, 3, :], ALU.mult, [bMOD, bGB], [bMOD])
            stt("dve", MODC[:, D:2 * D], MODC[:, D:2 * D], 1.0, GB[:, 0, :], ALU.add, ALU.mult, [bMODC, bGB], [bMODC])
            P.dma("sp", mod_d.ap(), MOD[0:1, :], reads=[bMOD], writes=[bMODD])
            dump("mod", MOD[0:1, :], [1, 6 * D], [bMOD])
            dump("modc", MODC[0:1, :], [1, 2 * D], [bMODC])
            P.barrier()
            st0a.close()
            REPS = sb0("REPS", [128, 2, 8, 128])
            bREPS = Buf("REPS")
            GC = sb0("GC", [128, 16])
            bGC = Buf("GC")
            for kc in range(8):
                blk = slice(kc * 128, (kc + 1) * 128)
                tr(PS[0][:, 0:128], MOD[:, blk], IDF, [bMOD, bCONST], [PB[0]])
                tr(PS[0][:, 128:256], MOD[:, D + kc * 128:D + (kc + 1) * 128], IDF, [bMOD, bCONST], [PB[0]])
                tr(PS[0][:, 256:384], MODC[:, blk], IDF, [bMODC, bCONST], [PB[0]])
                tr(PS[0][:, 384:512], MODC[:, D + kc * 128:D + (kc + 1) * 128], IDF, [bMODC, bCONST], [PB[0]])
                cp("dve", REPS[:, 0, kc, :], PS[0][:, 0:128], [PB[0]], [bREPS])
                cp("dve", REPS[:, 1, kc, :], PS[0][:, 256:384], [PB[0]], [bREPS])
                cp("dve", GC[:, kc:kc + 1], PS[0][:, 128:129], [PB[0]], [bGC])
                cp("dve", GC[:, 8 + kc:9 + kc], PS[0][:, 384:385], [PB[0]], [bGC])
            WST = [sb0("WST%d" % i, [128, 1536]) for i in range(2)]
            bWST = [Buf("WST%d" % i) for i in range(2)]
            for kc in range(8):
                w_, bw_ = WST[kc % 2], bWST[kc % 2]
                P.dma("sp", w_[:, :], w_in.ap()[kc * 128:(kc + 1) * 128, :], writes=[bw_])
                ts("dve", WIN[:, kc, :], w_[:, :], GC[:, kc:kc + 1], None, ALU.mult, None, [bw_, bGC], [bWIN])
                ts("pool", WINC[:, kc, :], w_[:, 0:512], GC[:, 8 + kc:9 + kc], None, ALU.mult, None, [bw_, bGC], [bWINC])
                for blk in range(3):
                    mm(PS[2 + blk][:, :], REPS[:, 0, kc, :], w_[:, blk * 512:(blk + 1) * 512], kc == 0, kc == 7, [bREPS, bw_], [PB[2 + blk]])
                mm(PS[5][:, :], REPS[:, 1, kc, :], w_[:, 0:512], kc == 0, kc == 7, [bREPS, bw_], [PB[5]])
            BROW = sb0("BROW", [128, 4, 512])
            bBROW = Buf("BROW")
            for i in range(4):
                cp("dve", BROW[:, i, :], PS[2 + i][:, :], [PB[2 + i]], [bBROW])
            cp("dve", BZ[:, :], BROW[:, 1, :], [bBROW], [bBZ])
            for i, src in ((0, 0), (1, 2), (2, 3)):
                for j in range(4):
                    tr(PS[0][:, j * 128:(j + 1) * 128], BROW[:, src, j * 128:(j + 1) * 128], IDF, [bBROW, bCONST], [PB[0]])
                for j in range(4):
                    cp("dve", BCOL[:, i * 4 + j:i * 4 + j + 1], PS[0][:, j * 128:j * 128 + 1], [PB[0]], [bBCOL])
            P.barrier()

        BDB = sbs("BDB", [128, 3, 4, 128], BF16)
        bBDB = Buf("BDB")
        AW = sbs("AW", [128, 4, 32], BF16)
        bAW = Buf("AW")
        BIF = sbs("BIF", [128, 16])
        bBIF = Buf("BIF")
        P.dma("sp", BIF[:, :], bc_rows(b_if, 0, 16), writes=[bBIF])
        CW = sbs("CW", [128, 16])
        bCW = Buf("CW")
        P.dma("sp", CW[:, :], convw.ap(), writes=[bCW])
        XMH = sbs("XMH", [128, 4, 64])
        bXMH = Buf("XMH")
        bPOSRD = Buf("posr_d")
        NTB = 4
        XB = [sbs("XB%d" % i, [128, D]) for i in range(NTB)]
        bXB = [Buf("XB%d" % i) for i in range(NTB)]
        PRB = [sbs("PRB%d" % i, [128, 512]) for i in range(NTB)]
        bPRB = [Buf("PRB%d" % i) for i in range(NTB)]
        X0B = [sbs("X0B%d" % i, [128, D], BF16) for i in range(NTB)]
        bX0B = [Buf("X0B%d" % i) for i in range(NTB)]
        JUNK = [sbs("JUNK%d" % i, [128, D], BF16) for i in range(2)]
        bJUNK = [Buf("JUNK%d" % i) for i in range(2)]
        DG = [sbs("DG%d" % i, [128, 128], BF16) for i in range(NTB)]
        bDG = [Buf("DG%d" % i) for i in range(NTB)]
        SS = sbs("SS", [128, 4 * NTB])
        bSS = [Buf("SS%d" % i) for i in range(NTB)]
        HT = sbs("HT", [128, 8, 512], BF16)
        bHTt = [Buf("HT%d" % i) for i in range(4)]
        PT = PS[0][:, :].bitcast(BF16).rearrange("p (k t) -> p k t", t=128)

        with ExitStack() as st1:
            def sb1(name, shape, dt=F32):
                return st1.enter_context(nc.sbuf_tensor(name, list(shape), dt))
            FREQ = sb1("FREQ", [128, 256])
            bFREQ = Buf("FREQ")
            act(FREQ[:, :], TABS[:, 512:768], AF.Exp, [bTABS], [bFREQ], scale=-math.log(10000.0) / 256.0)
            WS = sb1("WS", [128, 6, 4, 4])
            bWS = Buf("WS")
            for w in range(3):
                P.dma("sp", WS[:, w, :, :], bass.AP(w_qkv, w * 2048, [[4, 128], [512, 4], [1, 4]]), writes=[bWS])
                P.dma("sp", WS[:, 3 + w, :, :], bass.AP(w_qkvT, w * 2048, [[4, 128], [512, 4], [1, 4]]), writes=[bWS])
            BDF = sb1("BDF", [128, 6, 4, 128])
            bBDF = Buf("BDF")
            BDM3 = BDM.rearrange("p (r o) -> p r o", o=4)
            for w in range(6):
                for cc in range(4):
                    tt("dve", BDF[:, w, cc, :].rearrange("p (r o) -> p r o", o=4),
                       WS[:, w, cc, None, :].broadcast_to([128, 32, 4]), BDM3, ALU.mult, [bWS, bCONST], [bBDF])
            cp("dve", BDB[:, :, :, :], BDF[:, 0:3, :, :], [bBDF], [bBDB])
            WIF = sb1("WIF", [128, 12, 16])
            bWIF = Buf("WIF")
            P.dma("sp", WIF[:, :, :], w_if.ap().rearrange("(k p) n -> p k n", p=128), writes=[bWIF])
            for cc in range(4):
                mm(PS[1][:, cc * 32:cc * 32 + 16], BDF[:, 3, cc, :], WIF[:, cc, :], True, False, [bBDF, bWIF], [PB[1]])
                mm(PS[1][:, cc * 32:cc * 32 + 16], BDF[:, 4, cc, :], WIF[:, 4 + cc, :], False, True, [bBDF, bWIF], [PB[1]])
                mm(PS[1][:, cc * 32 + 16:cc * 32 + 32], BDF[:, 5, cc, :], WIF[:, 8 + cc, :], True, True, [bBDF, bWIF], [PB[1]])
            cp("dve", AW[:, :, :], PS[1][:, 0:128].rearrange("p (c n) -> p c n", n=32), [PB[1]], [bAW])

            ANG = sb1("ANG", [128, 512])
            bANG = Buf("ANG")
            KI = sb1("KI", [128, 512], I32)
            bKI = Buf("KI")
            MSK = sb1("MSK", [128, 512])
            bMSK = Buf("MSK")

            def sincos(out, bout, idx):
                ts("dve", ANG[:, 0:256], FREQ[:, :], idx, None, ALU.mult, None, [bFREQ, bTABS], [bANG])
                ts("dve", ANG[:, 256:512], ANG[:, 0:256], math.pi / 2, None, ALU.add, None, [bANG], [bANG])
                ts("dve", KI[:, :], ANG[:, :], 1.0 / TWO_PI, None, ALU.mult, None, [bANG], [bKI])
                stt("dve", ANG[:, :], KI[:, :], -TWO_PI, ANG[:, :], ALU.mult, ALU.add, [bKI, bANG], [bANG])
                ts("dve", MSK[:, :], ANG[:, :], math.pi, TWO_PI, ALU.is_gt, ALU.mult, [bANG], [bMSK])
                tt("dve", ANG[:, :], ANG[:, :], MSK[:, :], ALU.subtract, [bANG, bMSK], [bANG])
                ts("dve", MSK[:, :], ANG[:, :], -math.pi, TWO_PI, ALU.is_lt, ALU.mult, [bANG], [bMSK])
                tt("dve", ANG[:, :], ANG[:, :], MSK[:, :], ALU.add, [bANG, bMSK], [bANG])
                ts("dve", ANG[:, :], ANG[:, :], math.pi, -math.pi, ALU.min, ALU.max, [bANG], [bANG])
                act(out, ANG[:, :], AF.Sin, [bANG], [bout])

            PR = sb1("PR", [128, 2, 512])
            bPR = Buf("PR")
            for a in range(2):
                sincos(PR[:, a, :], bPR, TABS[:, a:a + 1])
            P.dma("sp", posr_d.ap().rearrange("(p a) n -> p a n", a=2), PR[:, :, :], reads=[bPR], writes=[bPOSRD])
            sincos(POSC[:, :], bPOSC, TABS[:, 2:3])
            POSH = sb1("POSH", [128, D])
            bPOSH = Buf("POSH")
            sincos(POSH[:, 0:512], bPOSH, TABS[:, 3:4])
            sincos(POSH[:, 512:1024], bPOSH, TABS[:, 4:5])

            tile_ctr = [0]

            def tile_front(xsrc, pos_mode, ht_dst, bht):
                k = tile_ctr[0]
                tile_ctr[0] += 1
                q = k % NTB
                X, bX = XB[q], bXB[q]
                xb_, bxb_ = X0B[q], bX0B[q]
                dg, bdg = DG[q], bDG[q]
                bss = bSS[q]
                P.dma("sp", X[:, :], xsrc, writes=[bX])
                if pos_mode is not None and pos_mode[0] == "rolled":
                    i = pos_mode[1]
                    PRt, bPRt = PRB[q], bPRB[q]
                    P.dma("sp", PRt[0:64, :], bc_rows(posr_d, 2 * i, 512, parts=64), reads=[bPOSRD], writes=[bPRt])
                    P.dma("sp", PRt[64:128, :], bc_rows(posr_d, 2 * i + 1, 512, parts=64), reads=[bPOSRD], writes=[bPRt])
                    yield
                    tt("pool", xb_[:, 0:512], X[:, 0:512], PRt[:, :], ALU.add, [bX, bPRt], [bxb_])
                    tt("pool", xb_[:, 512:1024], X[:, 512:1024], POSC[:, :], ALU.add, [bX, bPOSC], [bxb_])
                elif pos_mode is not None:
                    tt("pool", xb_[:, :], X[:, :], POSH[:, :], ALU.add, [bX, bPOSH], [bxb_])
                else:
                    yield
                    cp("pool", xb_[:, :], X[:, :], [bX], [bxb_])
                yield
                sc = SS[:, q * 4:q * 4 + 4]
                act(JUNK[k % 2][:, :], xb_[:, :], AF.Square, [bxb_], [bJUNK[k % 2], bss], accum_out=sc[:, 0:1])
                yield
                ts("pool", sc[:, 1:2], sc[:, 0:1], 1.0 / D, EPS, ALU.mult, ALU.add, [bss], [bss])
                tt("pool", sc[:, 2:3], sc[:, 1:2], NEGH, ALU.pow, [bss, bTABS], [bss])
                yield
                ts("dve", dg[:, :], IDF, sc[:, 2:3], None, ALU.mult, None, [bCONST, bss], [bdg])
                yield
                for kc in range(8):
                    pb = kc // 4
                    mm(PS[pb][:, (kc % 4) * 128:(kc % 4 + 1) * 128], xb_[:, kc * 128:(kc + 1) * 128], dg[:, :], True, True, [bxb_, bdg], [PB[pb]])
                cp("act", ht_dst[:, 0:4, :], PS[0][:, :].rearrange("p (k t) -> p k t", t=128), [PB[0]], [bht])
                cp("dve", ht_dst[:, 4:8, :], PS[1][:, :].rearrange("p (k t) -> p k t", t=128), [PB[1]], [bht])
                yield

            HTH = sb1("HTH", [128, 8, 128], BF16)
            bHTH = Buf("HTH")
            interleave([tile_front(x_halo.ap(), ("halo",), HTH[:, :, :], bHTH)])
            for cc in range(4):
                for kc in range(8):
                    mm(PS[2][:, cc * 64:cc * 64 + 64], WIN[:, kc, cc * 128:(cc + 1) * 128], HTH[:, kc, 0:64], kc == 0, kc == 7, [bWIN, bHTH], [PB[2]])
            XMHF = sb1("XMHF", [128, 4, 64])
            bXMHF = Buf("XMHF")
            tt("dve", XMHF[:, :, :], PS[2][:, 0:256].rearrange("p (c n) -> p c n", n=64),
               BCOL[:, 0:4, None].broadcast_to([128, 4, 64]), ALU.add, [PB[2], bBCOL], [bXMHF])
            tt("dve", XMH[:, :, :], XMHF[:, :, :], TABS[:, None, 384:448].broadcast_to([128, 4, 64]), ALU.mult, [bXMHF, bTABS], [bXMH])
            dump("xmh", XMH[:, :, :], [128, 4, 64], [bXMH])
            P.barrier()

        XM = sbs("XM", [128, 4, 514])
        bXM = [Buf("XM%d" % i) for i in range(4)]
        ACC = [sbs("ACC%d" % i, [128, 512]) for i in range(2)]
        bACC = [Buf("ACC%d" % i) for i in range(2)]
        ACTT = [sbs("ACTT%d" % i, [128, 4, 512], BF16) for i in range(2)]
        bACTT = [[Buf("ACTT%d_%d" % (j, i)) for i in range(4)] for j in range(2)]
        XMB = [sbs("XMB%d" % i, [128, 4, 512], BF16) for i in range(2)]
        bXMB = [[Buf("XMB%d_%d" % (j, i)) for i in range(4)] for j in range(2)]
        UT = sbs("UT", [128, 4, 512], BF16)
        bUT = Buf("UT")
        bUD = Buf("u_d")
        KTMG = sbs("KTMG", [128, 4, 512], BF16)
        bKTMG = [Buf("KTMG%d" % i) for i in range(4)]
        VAG = sbs("VAG", [128, 4, 4, 129], BF16)
        bVAG = [Buf("VAG%d" % i) for i in range(4)]
        VS = [sbs("VS%d" % i, [128, 8, 129], BF16) for i in range(2)]
        bVS = [Buf("VS%d" % i) for i in range(2)]
        CBS = sbs("CBS", [128, 4, 129])
        bCBS = Buf("CBS")
        memset("pool", CBS[:, :, :], 0.0, [bCBS])
        CCB = sbs("CCB", [128, 4, 129])
        bCCB = Buf("CCB")
        GT = sbs("GT", [128, 4, 16])
        bGT = Buf("GT")
        GW = sbs("GW", [128, 4, 40])
        bGW = Buf("GW")
        EXI = sbs("EXI", [128, 4, 16])
        bEXI = Buf("EXI")
        EXO = sbs("EXO", [128, 4, 16])
        bEXO = Buf("EXO")
        PALL = sbs("PALL", [128, 5, 4])
        bPALL = Buf("PALL")
        MBT = sbs("MBT", [128, 4, 4])
        bMBT = Buf("MBT")
        WV = sbs("WV", [128, 4, 8])
        bWV = Buf("WV")
        STG = [sbs("STG%d" % i, [128, 512], BF16) for i in range(2)]
        bSTG = [Buf("STG%d" % i) for i in range(2)]
        ZT = sbs("ZT", [128, 512])
        bZT = Buf("ZT")
        bAZ = Buf("az_d")
        stg_ctr = [0]

        memset("pool", VAG[:, :, :, :], 1.0, bVAG)
        memset("pool", VAO[:, :, :, :], 1.0, bVAO)
        memset("pool", PALL[:, :, :], 0.0, [bPALL])

        PSG = PS[6][:, 384:448].rearrange("p (t g) -> p t g", g=16)
        bPSG = PB[6]
        PSB = PS[6][:, 448:512].rearrange("p (t g) -> p t g", g=16)
        bPSB = PB[6]
        DCF = [PS[5][:, 0:129], PS[5][:, 129:258], PS[5][:, 258:387], PS[6][:, 0:129]]
        bDCF = [PB[5], PB[5], PB[5], PB[6]]
        ACCB = [PS[7][:, 0:129], PS[7][:, 129:258], PS[7][:, 258:387], PS[6][:, 129:258]]
        bACCB = [PB[7], PB[7], PB[7], PB[6]]
        u_v = u_d.ap().rearrange("(c p) t -> p c t", p=128)
        LN_QS = math.log(128.0 ** -0.5)

        def front(gi, kind, par):
            n = 2 if kind == "ctx" else 4
            ntok = n * 128
            tgens = []
            for t_ in range(n):
                dst = HT[:, :, t_ * 128:(t_ + 1) * 128]
                if kind == "ctx":
                    tgens.append(tile_front(ctx_in.ap()[t_ * 128:(t_ + 1) * 128, :], None, dst, bHTt[t_]))
                else:
                    i = gi * 4 + t_
                    tgens.append(tile_front(x_rot.ap()[i * 128:(i + 1) * 128, :], ("rolled", i), dst, bHTt[t_]))
            while tgens:
                for g_ in list(tgens):
                    try:
                        next(g_)
                    except StopIteration:
                        tgens.remove(g_)
                yield
            W_, bW_, boff = (WINC, bWINC, 8) if kind == "ctx" else (WIN, bWIN, 0)
            att, batt, xmb, bxmb = ACTT[par], bACTT[par], XMB[par], bXMB[par]
            for cc in range(4):
                pb = 2 + cc % 2
                for kc in range(8):
                    mm(PS[pb][:, 0:ntok], W_[:, kc, cc * 128:(cc + 1) * 128], HT[:, kc, 0:ntok], kc == 0, kc == 7, [bW_] + bHTt, [PB[pb]])
                bias = BCOL[:, boff + cc:boff + cc + 1]
                act(XM[:, cc, 1:1 + ntok], PS[pb][:, 0:ntok], AF.Identity, [PB[pb], bBCOL], [bXM[cc]], bias=bias)
                act(xmb[:, cc, 0:ntok], PS[pb][:, 0:ntok], AF.Identity, [PB[pb], bBCOL], [bxmb[cc]], bias=bias)
                if kind == "ctx":
                    memset("pool", XM[:, cc, 0:1], 0.0, [bXM[cc]])
                    memset("pool", XM[:, cc, 1 + ntok:2 + ntok], 0.0, [bXM[cc]])
                else:
                    cp("pool", XM[:, cc, 0:1], XMH[:, cc, 2 * gi:2 * gi + 1], [bXMH], [bXM[cc]])
                    cp("pool", XM[:, cc, 513:514], XMH[:, cc, 2 * gi + 1:2 * gi + 2], [bXMH], [bXM[cc]])
                yield
                A_, bA_ = ACC[cc % 2], bACC[cc % 2]
                ts("dve", A_[:, 0:ntok], XM[:, cc, 1:1 + ntok], CW[:, cc * 3 + 1:cc * 3 + 2], CW[:, 12 + cc:13 + cc], ALU.mult, ALU.add, [bXM[cc], bCW], [bA_])
                stt("dve", A_[:, 0:ntok], XM[:, cc, 0:ntok], CW[:, cc * 3:cc * 3 + 1], A_[:, 0:ntok], ALU.mult, ALU.add, [bXM[cc], bCW, bA_], [bA_])
                stt("dve", A_[:, 0:ntok], XM[:, cc, 2:2 + ntok], CW[:, cc * 3 + 2:cc * 3 + 3], A_[:, 0:ntok], ALU.mult, ALU.add, [bXM[cc], bCW, bA_], [bA_])
                act(att[:, cc, 0:ntok], A_[:, 0:ntok], AF.Silu, [bA_], [batt[cc]])
                yield
            if kind == "own" and gi == 0:
                dump("actT", att[:, :, 0:128], [128, 4, 128], batt)
            if kind != "ctx":
                for cc in range(4):
                    pb = 2 + cc % 2
                    for kc in range(8):
                        mm(PS[pb][:, :], WIN[:, kc, 1024 + cc * 128:1024 + (cc + 1) * 128], HT[:, kc, :], kc == 0, kc == 7, [bWIN] + bHTt, [PB[pb]])
                    act(UT[:, cc, :], PS[pb][:, :], AF.Identity, [PB[pb], bBCOL], [bUT], bias=BCOL[:, 4 + cc:5 + cc])
                    yield
                P.dma("act", u_v[:, :, gi * 512:(gi + 1) * 512], UT[:, :, :], reads=[bUT], writes=[bUD])
            if kind == "own":
                for w, dstT, bdst in ((0, QT, bQT), (1, KT, bKT)):
                    for cc in range(4):
                        pb = 2 + cc % 2
                        mm(PS[pb][:, :], BDB[:, w, cc, :], att[:, cc, :], True, True, [bBDB, batt[cc]], [PB[pb]])
                        cp("act" if cc % 2 else "dve", dstT[:, cc, gi * 512:(gi + 1) * 512], PS[pb][:, :], [PB[pb]], [bdst])
                    yield
                for t_ in range(4):
                    i = gi * 4 + t_
                    sl = slice(t_ * 128, (t_ + 1) * 128)
                    pb = 2 + t_ % 2
                    for kc in range(8):
                        mm(PS[pb][:, :], HT[:, kc, sl], WIN[:, kc, 512:1024], kc == 0, kc == 7, bHTt + [bWIN], [PB[pb]])
                    tt("dve", ZT[:, :], PS[pb][:, :], BZ[:, :], ALU.add, [PB[pb], bBZ], [bZT])
                    s_ = stg_ctr[0] % 2
                    stg_ctr[0] += 1
                    act(STG[s_][:, :], ZT[:, :], AF.Silu, [bZT], [bSTG[s_]])
                    P.dma("act", az_d.ap()[1, i * 128:(i + 1) * 128, :], STG[s_][:, :], reads=[bSTG[s_]], writes=[bAZ])
                    for cc in range(4):
                        tr(PT[:, cc, :], att[:, cc, sl], IDB[:, :], [batt[cc], bIDB], [PB[0]])
                    s_ = stg_ctr[0] % 2
                    stg_ctr[0] += 1
                    cp("dve", STG[s_][:, :].rearrange("p (c t) -> p c t", t=128), PT[:, 0:4, :], [PB[0]], [bSTG[s_]])
                    P.dma("act", az_d.ap()[0, i * 128:(i + 1) * 128, :], STG[s_][:, :], reads=[bSTG[s_]], writes=[bAZ])
                    yield

        def back(gi, kind, par):
            n = 2 if kind == "ctx" else 4
            att, batt, xmb, bxmb = ACTT[par], bACTT[par], XMB[par], bXMB[par]
            for t_ in range(n):
                sl = slice(t_ * 128, (t_ + 1) * 128)
                if kind == "own":
                    i = gi * 4 + t_
                    ktm, bktm, va, bva = KTMO[:, i, :], bKTMO[i], VAO[:, i, :, :], bVAO[i]
                else:
                    ktm, bktm, va, bva = KTMG[:, t_, :], bKTMG[t_], VAG[:, t_, :, :], bVAG[t_]
                for cc in range(4):
                    mm(PS[4][:, cc * 128:(cc + 1) * 128], att[:, cc, sl], BDB[:, 1, cc, :], True, True, [batt[cc], bBDB], [PB[4]])
                cp("act", ktm, PS[4][:, :], [PB[4]], [bktm])
                yield
                for cc in range(4):
                    mm(PS[4][:, cc * 128:(cc + 1) * 128], xmb[:, cc, sl], BDB[:, 2, cc, :], True, True, [bxmb[cc], bBDB], [PB[4]])
                cp("act", va[:, :, 0:128], PS[4][:, :].rearrange("p (h d) -> p h d", d=128), [PB[4]], [bva])
                for cc in range(4):
                    mm(PSG[:, t_, :], att[:, cc, sl], AW[:, cc, 0:16], cc == 0, False, [batt[cc], bAW], [bPSG])
                for cc in range(4):
                    mm(PSG[:, t_, :], xmb[:, cc, sl], AW[:, cc, 16:32], False, cc == 3, [bxmb[cc], bAW], [bPSG])
                yield
            tt("dve", GT[:, 0:n, :], PSG[:, 0:n, :], BIF[:, None, :].broadcast_to([128, n, 16]), ALU.add, [bPSG, bBIF], [bGT])
            stt("dve", GW[:, 0:n, 0:8], GT[:, 0:n, 8:16], -1.0, GT[:, 0:n, 8:16], ALU.mult, ALU.max, [bGT], [bGW])
            yield
            act(GW[:, 0:n, 8:16], GW[:, 0:n, 0:8], AF.Exp, [bGW], [bGW], scale=-1.0)
            yield
            act(GW[:, 0:n, 16:24], GW[:, 0:n, 8:16], AF.Ln, [bGW], [bGW], bias=1.0)
            ts("dve", GW[:, 0:n, 24:32], GT[:, 0:n, 8:16], 0.0, None, ALU.min, None, [bGT], [bGW])
            yield
            tt("dve", GW[:, 0:n, 32:40], GW[:, 0:n, 24:32], GW[:, 0:n, 16:24], ALU.subtract, [bGW], [bGW])
            yield
            for t_ in range(n):
                mm(PSB[:, t_, 0:4], TRIU, GW[:, t_, 32:36], True, True, [bCONST, bGW], [bPSB])
                mm(PSB[:, t_, 4:8], TRIL, GW[:, t_, 36:40], True, True, [bCONST, bGW], [bPSB])
                mm(PSB[:, t_, 8:16], ONES, GW[:, t_, 32:40], True, True, [bCONST, bGW], [bPSB])
            yield
            if kind == "own":
                i0 = gi * 4
                if gi == 0:
                    dump("gt", GT[:, :, :], [128, 4, 16], [bGT])
                    dump("lf", GW[:, :, 32:40], [128, 4, 8], [bGW])
                tt("dve", EXI[:, :, 0:8], GT[:, :, 0:8], PSB[:, :, 0:8], ALU.subtract, [bGT, bPSB], [bEXI])
                yield
                act(OWNG[:, i0:i0 + 4, 0:8], EXI[:, :, 0:8], AF.Exp, [bEXI], [bOWNG])
                act(OWNG[:, i0:i0 + 4, 8:16], PSB[:, :, 0:8], AF.Exp, [bPSB], [bOWNG], bias=LN_QS)
                act(OWNG[:, i0:i0 + 4, 16:24], PSB[:, :, 8:16], AF.Exp, [bPSB], [bOWNG])
                yield
                return
            tt("dve", EXI[:, 0:n, 0:8], GT[:, 0:n, 0:8], PSB[:, 0:n, 8:16], ALU.add, [bGT, bPSB], [bEXI])
            tt("dve", EXI[:, 0:n, 0:8], EXI[:, 0:n, 0:8], PSB[:, 0:n, 0:8], ALU.subtract, [bEXI, bPSB], [bEXI])
            yield
            if kind == "ctx":
                cp("dve", EXI[:, 0:2, 8:16], PSB[:, 0:2, 8:16], [bPSB], [bEXI])
                yield
                act(EXO[:, 0:2, :], EXI[:, 0:2, :], AF.Exp, [bEXI], [bEXO])
                yield
                tt("dve", WV[:, 0, 0:4], EXO[:, 0, 0:4], EXO[:, 1, 8:12], ALU.mult, [bEXO], [bWV])
                cp("dve", WV[:, 1, 0:4], EXO[:, 1, 0:4], [bEXO], [bWV])
                cp("dve", WV[:, 0, 4:8], EXO[:, 0, 4:8], [bEXO], [bWV])
                tt("dve", WV[:, 1, 4:8], EXO[:, 1, 4:8], EXO[:, 0, 12:16], ALU.mult, [bEXO], [bWV])
                yield
            else:
                MF = TABS[:, 128 + 4 * gi:132 + 4 * gi]
                MB = TABS[:, 256 + 4 * gi:260 + 4 * gi]
                MF3 = MF[:, :, None].broadcast_to([128, 4, 4])
                MB3 = MB[:, :, None].broadcast_to([128, 4, 4])
                tt("dve", EXI[:, :, 8:12], PSB[:, :, 8:12], MF3, ALU.mult, [bPSB, bTABS], [bEXI])
                tt("dve", MBT[:, :, :], PSB[:, :, 12:16], MB3, ALU.mult, [bPSB, bTABS], [bMBT])
                yield
                for t_ in range(4):
                    tt("dve", PALL[:, t_ + 1, :], PALL[:, t_, :], MBT[:, t_, :], ALU.add, [bPALL, bMBT], [bPALL])
                    yield
                cp("dve", EXI[:, :, 12:16], PALL[:, 0:4, :], [bPALL], [bEXI])
                yield
                act(EXO[:, :, :], EXI[:, :, :], AF.Exp, [bEXI], [bEXO])
                yield
                tt("dve", WV[:, :, 0:4], EXO[:, :, 0:4], MF3, ALU.mult, [bEXO, bTABS], [bWV])
                tt("dve", WV[:, :, 4:8], EXO[:, :, 4:8], EXO[:, :, 12:16], ALU.mult, [bEXO], [bWV])
                yield
                tt("dve", WV[:, :, 4:8], WV[:, :, 4:8], MB3, ALU.mult, [bWV, bTABS], [bWV])
                cp("dve", PALL[:, 0, :], PALL[:, 4, :], [bPALL], [bPALL])
                yield
            for t_ in range(n):
                s_ = t_ % 2
                tt("pool", VS[s_][:, 0:4, :], VAG[:, t_, :, :], WV[:, t_, 0:4, None].broadcast_to([128, 4, 129]), ALU.mult, [bVAG[t_], bWV], [bVS[s_]])
                tt("pool", VS[s_][:, 4:8, :], VAG[:, t_, :, :], WV[:, t_, 4:8, None].broadcast_to([128, 4, 129]), ALU.mult, [bVAG[t_], bWV], [bVS[s_]])
                yield
                for h in range(4):
                    klhs = KTMG[:, t_, h * 128:(h + 1) * 128]
                    mm(DCF[h], klhs, VS[s_][:, h, :], True, True, [bKTMG[t_], bVS[s_]], [bDCF[h]])
                    mm(ACCB[h], klhs, VS[s_][:, 4 + h, :], True, True, [bKTMG[t_], bVS[s_]], [bACCB[h]])
                yield
                dcf3 = PS[5][:, 0:387].rearrange("p (h d) -> p h d", d=129)
                acb3 = PS[7][:, 0:387].rearrange("p (h d) -> p h d", d=129)
                if kind == "ctx":
                    if t_ == 0:
                        cp("dve", CF[:, 0:3, :], dcf3, [PB[5]], [bCF])
                        cp("dve", CF[:, 3, :], DCF[3], [PB[6]], [bCF])
                        cp("dve", CCB[:, 0:3, :], acb3, [PB[7]], [bCCB])
                        cp("dve", CCB[:, 3, :], ACCB[3], [PB[6]], [bCCB])
                    else:
                        tt("dve", CF[:, 0:3, :], CF[:, 0:3, :], dcf3, ALU.add, [bCF, PB[5]], [bCF])
                        tt("dve", CF[:, 3, :], CF[:, 3, :], DCF[3], ALU.add, [bCF, PB[6]], [bCF])
                        tt("dve", CCB[:, 0:3, :], CCB[:, 0:3, :], acb3, ALU.add, [bCCB, PB[7]], [bCCB])
                        tt("dve", CCB[:, 3, :], CCB[:, 3, :], ACCB[3], ALU.add, [bCCB, PB[6]], [bCCB])
                else:
                    tt("dve", CF[:, :, :], CF[:, :, :], EXO[:, t_, 8:12, None].broadcast_to([128, 4, 129]), ALU.mult, [bCF, bEXO], [bCF])
                    tt("dve", CF[:, 0:3, :], CF[:, 0:3, :], dcf3, ALU.add, [bCF, PB[5]], [bCF])
                    tt("dve", CF[:, 3, :], CF[:, 3, :], DCF[3], ALU.add, [bCF, PB[6]], [bCF])
                    tt("dve", CBS[:, 0:3, :], CBS[:, 0:3, :], acb3, ALU.add, [bCBS, PB[7]], [bCBS])
                    tt("dve", CBS[:, 3, :], CBS[:, 3, :], ACCB[3], ALU.add, [bCBS, PB[6]], [bCBS])
                yield

        seq = [("ctx", 0)] + [("own", g) for g in range(min(OWN // 4, cut))]
        if cut > 4:
            seq += [("oth", g) for g in range(OWN // 4, ngroups)]
        prev = None
        for idx, (kind, gi) in enumerate(seq):
            gens = [front(gi, kind, idx % 2)]
            if prev is not None:
                gens.append(back(*prev))
            interleave(gens)
            prev = (gi, kind, idx % 2)
        interleave([back(*prev)])
        dump("cf_ctx", CF[:, :, :], [128, 4, 129], [bCF])
        act(EXO[:, 0, 0:4], PALL[:, 0, :], AF.Exp, [bPALL], [bEXO])
        for h in range(4):
            if ngroups > OWN // 4 and cut > 4:
                stt("dve", CB[:, h, :], CCB[:, h, :], EXO[:, 0, h:h + 1], CBS[:, h, :], ALU.mult, ALU.add, [bCCB, bEXO, bCBS], [bCB])
            else:
                cp("dve", CB[:, h, :], CCB[:, h, :], [bCCB], [bCB])
        dump("cf_in", CF[:, :, :], [128, 4, 129], [bCF])
        dump("cb_in", CB[:, :, :], [128, 4, 129], [bCB])
        dump("ktm0", KTMO[:, 0, :], [128, 512], [bKTMO[0]])
        dump("va0", VAO[:, 0, :, :], [128, 4, 129], [bVAO[0]])
        dump("owng", OWNG[:, :, :], [128, OWN, 24], [bOWNG])
        dump("qT", QT[:, :, 0:128], [128, 4, 128], [bQT])

        if stage <= 2:
            o_b = Buf("out")
            P.barrier()
            for i in range(OWN):
                t = P.dma("sp", out_d.ap()[i * 128:(i + 1) * 128, 0:512], POSC[:, 0:512], reads=[bPOSC], writes=[o_b])
                final.append((t[0], t[1]))
            P.barrier()
            with nc.Block() as block:
                P.emit(block, final)
            sts.close()
            stm.close()
            return nc

        P.barrier()
        sts.close()
        st6 = ExitStack()

        def sb6(name, shape, dt=F32):
            return st6.enter_context(nc.sbuf_tensor(name, list(shape), dt))

        HD = [sb6("HD%d" % i, [128, OWN, 512], BF16) for i in range(2)]
        bHD = [[Buf("HD%d_%d" % (i, c)) for c in range(OWN)] for i in range(2)]
        HS = [sb6("HS%d" % i, [128, 512]) for i in range(2)]
        bHS = [Buf("HS%d" % i) for i in range(2)]
        SM = [sb6("SM%d" % i, [128, 128], BF16) for i in range(8)]
        bSM = [Buf("SM%d" % i) for i in range(8)]
        VP = [sb6("VP%d" % i, [128, 129], BF16) for i in range(8)]
        bVP = [Buf("VP%d" % i) for i in range(8)]
        CSB = sb6("CSB", [128, 8, 129], BF16)
        bCSB = [Buf("CSB%d" % i) for i in range(8)]
        RD = [sb6("RD%d" % i, [128, 8]) for i in range(8)]
        bRD = [Buf("RD%d" % i) for i in range(8)]
        bST = [Buf("ST%d" % i) for i in range(8)]
        bHSD = Buf("hs_d")

        def chain(d, h):
            q = d * 4 + h
            ST = CF if d == 0 else CB
            bsrc = bCF if d == 0 else bCB
            hsl = slice(h * 128, (h + 1) * 128)
            cp("act", CSB[:, q, :], ST[:, h, :], [bsrc, bST[q]], [bCSB[q], bST[q]])
            yield
            order = range(OWN) if d == 0 else range(OWN - 1, -1, -1)
            for c in order:
                tsl = slice(c * 128, (c + 1) * 128)
                pS, pN, pC = PS[q][:, 0:128], PS[q][:, 128:257], PS[q][:, 257:386]
                mm(pS, KT[:, h, tsl], QT[:, h, tsl], True, True, [bKT, bQT], [PB[q]])
                ts("pool", VP[q][:, :], VAO[:, c, h, :], OWNG[:, c, q:q + 1], None, ALU.mult, None, [bVAO[c], bOWNG], [bVP[q]])
                yield
                tt("dve", SM[q][:, :], pS, TRIU if d == 0 else TRIL, ALU.mult, [PB[q], bCONST], [bSM[q]])
                yield
                mm(pN, SM[q][:, :], VP[q][:, :], True, False, [bSM[q], bVP[q]], [PB[q]])
                mm(pN, QT[:, h, tsl], CSB[:, q, :], False, True, [bQT, bCSB[q]], [PB[q]])
                mm(pC, KTMO[:, c, hsl], VP[q][:, :], True, True, [bKTMO[c], bVP[q]], [PB[q]])
                yield
                eq = OWNG[:, c, 8 + q:9 + q]
                r = RD[q]
                ts("dve", r[:, 0:1], PS[q][:, 256:257], eq, None, ALU.mult, None, [PB[q], bOWNG], [bRD[q]])
                stt("dve", r[:, 1:2], r[:, 0:1], -1.0, r[:, 0:1], ALU.mult, ALU.max, [bRD[q]], [bRD[q]])
                yield
                ts("dve", r[:, 2:3], r[:, 1:2], 1.0, None, ALU.max, None, [bRD[q]], [bRD[q]])
                P.op("dve", lambda e, o=r[:, 3:4], i_=r[:, 2:3]: e.reciprocal(out=o, in_=i_), [bRD[q]], [bRD[q]])
                yield
                tt("dve", r[:, 4:5], r[:, 3:4], eq, ALU.mult, [bRD[q], bOWNG], [bRD[q]])
                tt("dve", ST[:, h, :], ST[:, h, :], pC, ALU.add, [bST[q], PB[q]], [bST[q]])
                yield
                act(HD[d][:, c, hsl], PS[q][:, 128:256], AF.Copy, [PB[q], bRD[q]], [bHD[d][c]], scale=r[:, 4:5])
                ts("dve", ST[:, h, :], ST[:, h, :], OWNG[:, c, 16 + q:17 + q], None, ALU.mult, None, [bST[q], bOWNG], [bST[q]])
                yield
                cp("act", CSB[:, q, :], ST[:, h, :], [bST[q]], [bCSB[q]])
                yield

        interleave([chain(d, h) for d in range(2) for h in range(4)])
        for c in range(OWN):
            tt("pool", HS[c % 2][:, :], HD[0][:, c, :], HD[1][:, c, :], ALU.add, [bHD[0][c], bHD[1][c]], [bHS[c % 2]])
            P.dma("sp", hs_d.ap()[c * 128:(c + 1) * 128, :], HS[c % 2][:, :], reads=[bHS[c % 2]], writes=[bHSD])
            if c in (0, 7, 15):
                dump("hs%d" % c, HS[c % 2][:, :], [128, 512], [bHS[c % 2]])

        if stage <= 3:
            o_b = Buf("out")
            P.barrier()
            for i in range(OWN):
                t = P.dma("sp", out_d.ap()[i * 128:(i + 1) * 128, 0:512], POSC[:, 0:512], reads=[bPOSC], writes=[o_b])
                final.append((t[0], t[1]))
            P.barrier()
            with nc.Block() as block:
                P.emit(block, final)
            st6.close()
            stm.close()
            return nc

        P.barrier()
        st6.close()
        stm.close()

        H2T = sb("H2T", [128, 8, OWN * 128], BF16)
        bH2T = Buf("H2T")
        COMB = sb("COMB", [128, OWN, 16])
        bCOMB = Buf("COMB")
        st7 = ExitStack()

        def sb7(name, shape, dt=F32):
            return st7.enter_context(nc.sbuf_tensor(name, list(shape), dt))

        XCS = sb7("XCS", [128, 512, 2, 16], BF16)
        bXCS = Buf("XCS")
        CS128 = sb7("CS128", [128, 384], BF16)
        bCS = Buf("CS128")
        DI = sb7("DI", [128, 640])
        bDI = Buf("DI")
        P.dma("sp", DI[:, :], dftidx.ap(), writes=[bDI])
        CSF = sb7("CSF", [128, 384])
        bCSF = Buf("CSF")
        act(CSF[:, 0:256], DI[:, 0:256], AF.Sin, [bDI], [bCSF], scale=TWO_PI / 128.0)
        ts("dve", CSF[:, 256:384], CSF[:, 128:256], -1.0, None, ALU.mult, None, [bCSF], [bCSF])
        cp("dve", CS128[:, :], CSF[:, :], [bCSF], [bCS])
        CMY = sb7("CMY", [128, 48], BF16)
        bCMY = Buf("CMY")
        act(CSF[:, 0:32], DI[:, 512:544], AF.Sin, [bDI, bCS], [bCSF], scale=TWO_PI / 128.0)
        ts("dve", CSF[:, 32:48], CSF[:, 16:32], -1.0, None, ALU.mult, None, [bCSF], [bCSF])
        cp("dve", CMY[:, :], CSF[:, 0:48], [bCSF], [bCMY])
        with ExitStack() as st8:
            def sb8(name, shape, dt=F32):
                return st8.enter_context(nc.sbuf_tensor(name, list(shape), dt))
            TW = sb8("TW", [128, 256], BF16)
            bTW = Buf("TW")
            TWF = sb8("TWF", [128, 256])
            bTWF = Buf("TWF")
            act(TWF[:, :], DI[:, 256:512], AF.Sin, [bDI], [bTWF], scale=TWO_PI / 16384.0)
            ts("dve", TW[:, :], TWF[:, :], 1.0 / math.sqrt(16384.0 * 128.0), None, ALU.mult, None, [bTWF], [bTW])
            TC3 = TW[:, None, 0:128].broadcast_to([128, 8, 128])
            TS3 = TW[:, None, 128:256].broadcast_to([128, 8, 128])
            NW_ = 3
            UL = [sb8("UL%d" % i, [128, 16, 128], BF16) for i in range(3)]
            bUL = [Buf("UL%d" % i) for i in range(3)]
            YS = [sb8("YS%d" % i, [128, 8, 256], BF16) for i in range(NW_)]
            bYS = [Buf("YS%d" % i) for i in range(NW_)]
            MT_ = [[sb8("MTW%d_%d" % (j, i), [128, 8, 128], BF16) for i in range(4)] for j in range(NW_)]
            bMT_ = [[Buf("MTW%d_%d" % (j, i)) for i in range(4)] for j in range(NW_)]
            PQ = [sb8("PQ%d" % i, [128, 2, 8, 128], BF16) for i in range(NW_)]
            bPQ = [Buf("PQ%d" % i) for i in range(NW_)]

            def fft_half(hb):
                ub, half = hb // 2, hb % 2
                u_, bu_ = UL[ub % 3], bUL[ub % 3]
                w_ = hb % NW_
                if half == 0:
                    P.dma("sp", u_[:, :, :], bass.AP(u_d, ub * 16 * T, [[128, 128], [T, 16], [1, 128]]), reads=[bUD], writes=[bu_])
                    yield
                y_, by_ = YS[w_], bYS[w_]
                for pr in range(4):
                    pb = 1 + (hb * 4 + pr) % 4
                    for cc in range(2):
                        chl = half * 8 + pr * 2 + cc
                        mm(PS[pb][:, cc * 256:(cc + 1) * 256], u_[:, chl, :], CS128[:, 0:256], True, True, [bu_, bCS], [PB[pb]])
                    cp("act", y_[:, pr * 2:pr * 2 + 2, :], PS[pb][:, :].rearrange("p (c n) -> p c n", n=256), [PB[pb]], [by_])
                    yield
                yr = y_[:, :, 0:128]
                ys_ = y_[:, :, 128:256]
                pq, bpq = PQ[w_], bPQ[w_]
                m_, bm_ = MT_[w_], bMT_[w_]
                tt("dve", m_[0][:, :, :], yr, TC3, ALU.mult, [by_, bTW], [bm_[0]])
                tt("pool", m_[2][:, :, :], yr, TS3, ALU.mult, [by_, bTW], [bm_[2]])
                yield
                tt("dve", m_[1][:, :, :], ys_, TS3, ALU.mult, [by_, bTW], [bm_[1]])
                tt("pool", m_[3][:, :, :], ys_, TC3, ALU.mult, [by_, bTW], [bm_[3]])
                yield
                tt("dve", pq[:, 0, :, :], m_[0][:, :, :], m_[1][:, :, :], ALU.subtract, [bm_[0], bm_[1]], [bpq])
                yield
                tt("dve", pq[:, 1, :, :], m_[2][:, :, :], m_[3][:, :, :], ALU.add, [bm_[2], bm_[3]], [bpq])
                yield
                pb = 5 + hb % 3
                for cc in range(8):
                    o_c = PS[pb][:, cc * 32:cc * 32 + 16]
                    o_s = PS[pb][:, cc * 32 + 16:cc * 32 + 32]
                    mm(o_c, pq[:, 0, cc, :], CMY[:, 0:16], True, False, [bpq, bCMY], [PB[pb]])
                    mm(o_c, pq[:, 1, cc, :], CMY[:, 32:48], False, True, [bpq, bCMY], [PB[pb]])
                    mm(o_s, pq[:, 0, cc, :], CMY[:, 16:32], True, False, [bpq, bCMY], [PB[pb]])
                    mm(o_s, pq[:, 1, cc, :], CMY[:, 0:16], False, True, [bpq, bCMY], [PB[pb]])
                    if cc % 4 == 3:
                        yield
                cp("act", XCS[:, hb * 8:hb * 8 + 8, :, :], PS[pb][:, 0:256].rearrange("p (c s k) -> p c s k", s=2, k=16), [PB[pb]], [bXCS])
                yield

            def window(genfs, w):
                pend = list(genfs)
                act_ = []
                while pend or act_:
                    while pend and len(act_) < w:
                        act_.append(pend.pop(0)())
                    for g_ in list(act_):
                        try:
                            next(g_)
                        except StopIteration:
                            act_.remove(g_)

            window([(lambda hb=hb: fft_half(hb)) for hb in range(64)], NW_)
            P.barrier()
        dump("xcs", XCS[:, :, :, :], [128, 512, 2, 16], [bXCS])

        if stage <= 4:
            o_b = Buf("out")
            P.barrier()
            for i in range(OWN):
                t = P.dma("sp", out_d.ap()[i * 128:(i + 1) * 128, 0:512], POSC[:, 0:512], reads=[bPOSC], writes=[o_b])
                final.append((t[0], t[1]))
            P.barrier()
            with nc.Block() as block:
                P.emit(block, final)
            st7.close()
            return nc

        st9 = ExitStack()

        def sb9(name, shape, dt=F32):
            return st9.enter_context(nc.sbuf_tensor(name, list(shape), dt))

        WOB = sb9("WOB", [128, 8, D], BF16)
        bWOB = Buf("WOB")
        P.dma("pool", WOB[:, :, :], w_out.ap().rearrange("(k p) n -> p k n", p=128), writes=[bWOB])
        WFB = sb9("WFB", [128, 4, 128], BF16)
        bWFB = Buf("WFB")
        P.dma("pool", WFB[:, :, :], w_fourier.ap().rearrange("g c d -> c g d"), writes=[bWFB])
        NWSK = sb9("NWSK", [128, 2, 512])
        bNWSK = Buf("NWSK")
        for i in range(2):
            P.dma("sp", NWSK[:, i, :], bc_rows(nrm_skip, i, 512), writes=[bNWSK])
        WRF = sb9("WRF", [128, 8, 20])
        bWRF = Buf("WRF")
        P.dma("sp", WRF[:, :, :], w_router.ap().rearrange("(k p) n -> p k n", p=128), writes=[bWRF])
        WRH = sb9("WRH", [128, 8, 20], BF16)
        WRL = sb9("WRL", [128, 8, 20], BF16)
        bWR = Buf("WR")
        cp("dve", WRH[:, :, :], WRF[:, :, :], [bWRF], [bWR])
        tt("dve", WRL[:, :, :], WRF[:, :, :], WRH[:, :, :], ALU.subtract, [bWRF, bWR], [bWR])
        MODL = sb9("MODL", [128, 3, D])
        bMODL = Buf("MODL")
        for i_, off_ in enumerate((2 * D, 3 * D, 4 * D)):
            P.dma("sp", MODL[:, i_, :], bc_rows(mod_d, 0, D, off=off_), reads=[bMODD], writes=[bMODL])
        GP1, S2, G2 = MODL[:, 0, :], MODL[:, 1, :], MODL[:, 2, :]
        bMOD = bMODL
        BR = sb9("BR", [128, 20])
        bBR = Buf("BR")
        P.dma("sp", BR[:, :], bc_rows(b_router, 0, 20), writes=[bBR])
        HSt = [sb9("HSt%d" % i, [128, 512]) for i in range(2)]
        bHSt = [Buf("HSt%d" % i) for i in range(2)]
        AZ = [sb9("AZ%d" % i, [128, 2, 512], BF16) for i in range(2)]
        bAZ_ = [Buf("AZ%d" % i) for i in range(2)]
        SM__2 = [sb9("SM__%d" % i, [128, 32]) for i in range(2)]
        bSM__2 = [Buf("SM__%d" % i) for i in range(2)]
        CEN_2 = [sb9("CEN_%d" % i, [128, 512]) for i in range(2)]
        bCEN_2 = [Buf("CEN_%d" % i) for i in range(2)]
        SQ_2 = [sb9("SQ_%d" % i, [128, 512]) for i in range(2)]
        bSQ_2 = [Buf("SQ_%d" % i) for i in range(2)]
        T1_2 = [sb9("T1_%d" % i, [128, 512]) for i in range(2)]
        bT1_2 = [Buf("T1_%d" % i) for i in range(2)]
        T2_2 = [sb9("T2_%d" % i, [128, 512]) for i in range(2)]
        bT2_2 = [Buf("T2_%d" % i) for i in range(2)]
        MBF_2 = [sb9("MBF_%d" % i, [128, 512], BF16) for i in range(2)]
        bMBF_2 = [Buf("MBF_%d" % i) for i in range(2)]
        MTt_2 = [sb9("MTt_%d" % i, [128, 4, 128], BF16) for i in range(2)]
        bMTt_2 = [Buf("MTt_%d" % i) for i in range(2)]
        XT_2 = [sb9("XT_%d" % i, [128, 8, 128], BF16) for i in range(2)]
        bXT_2 = [Buf("XT_%d" % i) for i in range(2)]
        FTB_2 = [sb9("FTB_%d" % i, [128, 4, 128], BF16) for i in range(2)]
        bFTB_2 = [Buf("FTB_%d" % i) for i in range(2)]
        YFT_2 = [sb9("YFT_%d" % i, [128, 4, 128], BF16) for i in range(2)]
        bYFT_2 = [Buf("YFT_%d" % i) for i in range(2)]
        JK_2 = [sb9("JK_%d" % i, [128, D], BF16) for i in range(2)]
        bJK_2 = [Buf("JK_%d" % i) for i in range(2)]
        SY_2 = [sb9("SY_%d" % i, [128, 8]) for i in range(2)]
        bSY_2 = [Buf("SY_%d" % i) for i in range(2)]
        TT__2 = [sb9("TT__%d" % i, [128, D]) for i in range(2)]
        bTT_2 = [Buf("TT__%d" % i) for i in range(2)]
        H2_2 = [sb9("H2_%d" % i, [128, D]) for i in range(2)]
        bH2_2 = [Buf("H2_%d" % i) for i in range(2)]
        H2H_2 = [sb9("H2H_%d" % i, [128, D], BF16) for i in range(2)]
        bH2H_2 = [Buf("H2H_%d" % i) for i in range(2)]
        H2Lw_2 = [sb9("H2Lw_%d" % i, [128, D], BF16) for i in range(2)]
        bH2Lw_2 = [Buf("H2Lw_%d" % i) for i in range(2)]
        H2LT_2 = [sb9("H2LT_%d" % i, [128, 8, 128], BF16) for i in range(2)]
        bH2LT_2 = [Buf("H2LT_%d" % i) for i in range(2)]
        LG_2 = [sb9("LG_%d" % i, [128, 20]) for i in range(2)]
        bLG_2 = [Buf("LG_%d" % i) for i in range(2)]
        RT_2 = [sb9("RT_%d" % i, [128, 96]) for i in range(2)]
        bRT_2 = [Buf("RT_%d" % i) for i in range(2)]
        XR = [sb9("XR%d" % i, [128, D]) for i in range(2)]
        bXR = [Buf("XR%d" % i) for i in range(2)]
        PRr = [sb9("PRr%d" % i, [128, 512]) for i in range(2)]
        bPRr = [Buf("PRr%d" % i) for i in range(2)]
        X1 = [sb9("X1%d" % i, [128, D]) for i in range(2)]
        bX1 = [Buf("X1%d" % i) for i in range(2)]
        bOUT = Buf("out_d")
        BIG = 30000.0
        AX = mybir.AxisListType.X

        def red(eng, out, in_, op, reads, writes):
            return P.op(eng, lambda e: e.tensor_reduce(out=out, in_=in_, axis=AX, op=op), reads, writes)

        def s6_tile(c):
            s_ = c % 2
            rows = slice(c * 128, (c + 1) * 128)
            bk = (0, 1, 2, 3) if s_ == 0 else (4, 5, 6, 7)
            PTl = PS[bk[0]][:, :].bitcast(BF16).rearrange("p (k t) -> p k t", t=128)
            SM_, bSM_ = SM__2[s_], bSM__2[s_]
            CEN, bCEN = CEN_2[s_], bCEN_2[s_]
            SQ, bSQ = SQ_2[s_], bSQ_2[s_]
            T1, bT1 = T1_2[s_], bT1_2[s_]
            T2, bT2 = T2_2[s_], bT2_2[s_]
            MBF, bMBF = MBF_2[s_], bMBF_2[s_]
            MTt, bMTt = MTt_2[s_], bMTt_2[s_]
            XT, bXT = XT_2[s_], bXT_2[s_]
            FTB, bFTB = FTB_2[s_], bFTB_2[s_]
            YFT, bYFT = YFT_2[s_], bYFT_2[s_]
            JK, bJK = JK_2[s_], bJK_2[s_]
            SY, bSY = SY_2[s_], bSY_2[s_]
            TT_, bTT = TT__2[s_], bTT_2[s_]
            H2, bH2 = H2_2[s_], bH2_2[s_]
            H2H, bH2H = H2H_2[s_], bH2H_2[s_]
            H2Lw, bH2Lw = H2Lw_2[s_], bH2Lw_2[s_]
            H2LT, bH2LT = H2LT_2[s_], bH2LT_2[s_]
            LG, bLG = LG_2[s_], bLG_2[s_]
            RT, bRT = RT_2[s_], bRT_2[s_]
            hs, bhs, az, baz = HSt[s_], bHSt[s_], AZ[s_], bAZ_[s_]
            P.dma("sp", hs[:, :], hs_d.ap()[rows, :], reads=[bHSD], writes=[bhs])
            P.dma("sp", az[:, 0, :], az_d.ap()[0, rows, :], reads=[bAZ], writes=[baz])
            P.dma("sp", az[:, 1, :], az_d.ap()[1, rows, :], reads=[bAZ], writes=[baz])
            hs3 = hs[:, :].rearrange("p (h d) -> p h d", d=128)
            cen3 = CEN[:, :].rearrange("p (h d) -> p h d", d=128)
            sq3 = SQ[:, :].rearrange("p (h d) -> p h d", d=128)
            yield
            red("dve", SM_[:, 0:4], hs3, ALU.add, [bhs], [bSM_])
            ts("dve", SM_[:, 4:8], SM_[:, 0:4], 1.0 / 128.0, None, ALU.mult, None, [bSM_], [bSM_])
            tt("dve", cen3, hs3, SM_[:, 4:8, None].broadcast_to([128, 4, 128]), ALU.subtract, [bhs, bSM_], [bCEN])
            yield
            tt("pool", SQ[:, :], CEN[:, :], CEN[:, :], ALU.mult, [bCEN], [bSQ])
            red("dve", SM_[:, 8:12], sq3, ALU.add, [bSQ], [bSM_])
            yield
            ts("pool", SM_[:, 12:16], SM_[:, 8:12], 1.0 / 128.0, EPS, ALU.mult, ALU.add, [bSM_], [bSM_])
            tt("pool", SM_[:, 16:20], SM_[:, 12:16], NEGH.broadcast_to([128, 4]), ALU.pow, [bSM_, bTABS], [bSM_])
            tt("dve", cen3, cen3, SM_[:, 16:20, None].broadcast_to([128, 4, 128]), ALU.mult, [bCEN, bSM_], [bCEN])
            yield
            tt("dve", T1[:, :], CEN[:, :], NWSK[:, 0, :], ALU.mult, [bCEN, bNWSK], [bT1])
            tt("pool", T2[:, :], az[:, 0, :], NWSK[:, 1, :], ALU.mult, [baz, bNWSK], [bT2])
            tt("dve", T1[:, :], T1[:, :], T2[:, :], ALU.add, [bT1, bT2], [bT1])
            yield
            tt("dve", MBF[:, :], T1[:, :], az[:, 1, :], ALU.mult, [bT1, baz], [bMBF])
            if c == 0:
                dump("m0", MBF[:, :], [128, 512], [bMBF])
            yield
            for cc in range(4):
                tr(PTl[:, cc, :], MBF[:, cc * 128:(cc + 1) * 128], IDB[:, :], [bMBF, bIDB], [PB[bk[0]]])
            cp("act", MTt[:, :, :], PTl[:, 0:4, :], [PB[bk[0]]], [bMTt])
            yield
            for sg in range(2):
                for g in range(4):
                    tr(PTl[:, sg * 4 + g, :], XCS[:, g * 128:(g + 1) * 128, sg, c], IDB[:, :], [bXCS, bIDB], [PB[bk[0]]])
            cp("act", XT[:, :, :], PTl, [PB[bk[0]]], [bXT])
            yield
            for g in range(4):
                mm(PS[bk[1]][:, g * 128:(g + 1) * 128], CS128[:, 0:128], XT[:, g, :], True, False, [bCS, bXT], [PB[bk[1]]])
                mm(PS[bk[1]][:, g * 128:(g + 1) * 128], CS128[:, 256:384], XT[:, 4 + g, :], False, True, [bCS, bXT], [PB[bk[1]]])
            cp("dve", FTB[:, :, :], PS[bk[1]][:, :].rearrange("p (g t) -> p g t", t=128), [PB[bk[1]]], [bFTB])
            yield
            for g in range(4):
                mm(PS[bk[1]][:, g * 128:(g + 1) * 128], WFB[:, g, :], FTB[:, g, :], True, True, [bWFB, bFTB], [PB[bk[1]]])
            cp("act", YFT[:, :, :], PS[bk[1]][:, :].rearrange("p (g t) -> p g t", t=128), [PB[bk[1]]], [bYFT])
            yield
            for cb in range(2):
                for kc in range(4):
                    mm(PS[bk[2 + cb]][:, :], MTt[:, kc, :], WOB[:, kc, cb * 512:(cb + 1) * 512], kc == 0, False, [bMTt, bWOB], [PB[bk[2 + cb]]])
                for kc in range(4):
                    mm(PS[bk[2 + cb]][:, :], YFT[:, kc, :], WOB[:, 4 + kc, cb * 512:(cb + 1) * 512], False, kc == 3, [bYFT, bWOB], [PB[bk[2 + cb]]])
            yield
            act(JK[:, 0:512], PS[bk[2]][:, :], AF.Square, [PB[bk[2]]], [bJK, bSY], accum_out=SY[:, 0:1])
            act(JK[:, 512:1024], PS[bk[3]][:, :], AF.Square, [PB[bk[3]]], [bJK, bSY], accum_out=SY[:, 1:2])
            yield
            tt("pool", SY[:, 2:3], SY[:, 0:1], SY[:, 1:2], ALU.add, [bSY], [bSY])
            ts("pool", SY[:, 3:4], SY[:, 2:3], 1.0 / D, EPS, ALU.mult, ALU.add, [bSY], [bSY])
            tt("pool", SY[:, 4:5], SY[:, 3:4], NEGH, ALU.pow, [bSY, bTABS], [bSY])
            yield
            xr, bxr, pr_, bpr_ = XR[s_], bXR[s_], PRr[s_], bPRr[s_]
            P.dma("sp", xr[:, :], x_rot.ap()[rows, :], writes=[bxr])
            P.dma("sp", pr_[0:64, :], bc_rows(posr_d, 2 * c, 512, parts=64), reads=[bPOSRD], writes=[bpr_])
            P.dma("sp", pr_[64:128, :], bc_rows(posr_d, 2 * c + 1, 512, parts=64), reads=[bPOSRD], writes=[bpr_])
            tt("pool", xr[:, 0:512], xr[:, 0:512], pr_[:, :], ALU.add, [bxr, bpr_], [bxr])
            tt("pool", xr[:, 512:1024], xr[:, 512:1024], POSC[:, :], ALU.add, [bxr, bPOSC], [bxr])
            yield
            stt("dve", TT_[:, 0:512], PS[bk[2]][:, :], SY[:, 4:5], GP1[:, 0:512], ALU.mult, ALU.mult, [PB[bk[2]], bSY, bMOD], [bTT])
            stt("dve", TT_[:, 512:1024], PS[bk[3]][:, :], SY[:, 4:5], GP1[:, 512:1024], ALU.mult, ALU.mult, [PB[bk[3]], bSY, bMOD], [bTT])
            yield
            x1, bx1 = X1[s_], bX1[s_]
            tt("pool", x1[:, :], TT_[:, :], xr[:, :], ALU.add, [bTT, bxr], [bx1])
            P.dma("pool", out_d.ap()[rows, :], x1[:, :], reads=[bx1], writes=[bOUT])
            if c == 0:
                dump("x1_0", x1[:, :], [128, D], [bx1])
            yield
            act(JK[:, :], x1[:, :], AF.Square, [bx1], [bJK, bSY], accum_out=SY[:, 5:6])
            yield
            ts("pool", SY[:, 6:7], SY[:, 5:6], 1.0 / D, EPS, ALU.mult, ALU.add, [bSY], [bSY])
            tt("pool", SY[:, 7:8], SY[:, 6:7], NEGH, ALU.pow, [bSY, bTABS], [bSY])
            yield
            stt("dve", TT_[:, :], x1[:, :], SY[:, 7:8], G2, ALU.mult, ALU.mult, [bx1, bSY, bMOD], [bTT])
            tt("pool", H2[:, :], TT_[:, :], S2, ALU.add, [bTT, bMOD], [bH2])
            yield
            cp("act", H2H[:, :], H2[:, :], [bH2], [bH2H])
            tt("dve", H2Lw[:, :], H2[:, :], H2H[:, :], ALU.subtract, [bH2, bH2H], [bH2Lw])
            yield
            for kc in range(8):
                tr(PTl[:, kc, :], H2H[:, kc * 128:(kc + 1) * 128], IDB[:, :], [bH2H, bIDB], [PB[bk[0]]])
            cp("act", H2T[:, :, rows], PTl, [PB[bk[0]]], [bH2T])
            yield
            for kc in range(8):
                tr(PTl[:, kc, :], H2Lw[:, kc * 128:(kc + 1) * 128], IDB[:, :], [bH2Lw, bIDB], [PB[bk[0]]])
            cp("dve", H2LT[:, :, :], PTl, [PB[bk[0]]], [bH2LT])
            yield
            LGp = PS[bk[1]][:, 0:20]
            for kc in range(8):
                mm(LGp, H2T[:, kc, rows], WRH[:, kc, :], kc == 0, False, [bH2T, bWR], [PB[bk[1]]])
            for kc in range(8):
                mm(LGp, H2T[:, kc, rows], WRL[:, kc, :], False, False, [bH2T, bWR], [PB[bk[1]]])
            for kc in range(8):
                mm(LGp, H2LT[:, kc, :], WRH[:, kc, :], False, kc == 7, [bH2LT, bWR], [PB[bk[1]]])
            yield
            tt("dve", LG[:, :], LGp, BR[:, :], ALU.add, [PB[bk[1]], bBR], [bLG])
            if c == 0:
                dump("lg0", LG[:, :], [128, 20], [bLG])
            R = RT
            bR = bRT
            yield
            red("dve", R[:, 0:1], LG[:, 0:4], ALU.max, [bLG], [bR])
            ts("dve", R[:, 1:5], LG[:, 0:4], R[:, 0:1], None, ALU.is_equal, None, [bLG, bR], [bR])
            ts("dve", R[:, 5:6], R[:, 0:1], -1.0, None, ALU.mult, None, [bR], [bR])
            yield
            act(R[:, 6:10], LG[:, 0:4], AF.Exp, [bLG, bR], [bR], bias=R[:, 5:6], accum_out=R[:, 10:11])
            P.op("dve", lambda e, o=R[:, 11:12], i_=R[:, 10:11]: e.reciprocal(out=o, in_=i_), [bR], [bR])
            yield
            ts("dve", R[:, 12:16], R[:, 1:5], BIG, -BIG, ALU.mult, ALU.add, [bR], [bR])
            em = R[:, 16:32]
            tt("dve", em.rearrange("p (g j) -> p g j", j=4), LG[:, 4:20].rearrange("p (g j) -> p g j", j=4),
               R[:, 12:16, None].broadcast_to([128, 4, 4]), ALU.add, [bLG, bR], [bR])
            yield
            red("dve", R[:, 32:33], em, ALU.max, [bR], [bR])
            ts("dve", R[:, 48:64], em, R[:, 32:33], None, ALU.is_equal, None, [bR], [bR])
            stt("dve", R[:, 64:80], R[:, 48:64], -BIG, em, ALU.mult, ALU.add, [bR], [bR])
            yield
            red("dve", R[:, 33:34], R[:, 64:80], ALU.max, [bR], [bR])
            ts("dve", R[:, 80:96], R[:, 64:80], R[:, 33:34], None, ALU.is_equal, None, [bR], [bR])
            tt("dve", R[:, 34:35], R[:, 33:34], R[:, 32:33], ALU.subtract, [bR], [bR])
            yield
            act(R[:, 35:36], R[:, 34:35], AF.Exp, [bR], [bR])
            ts("dve", R[:, 36:37], R[:, 35:36], 1.0, None, ALU.add, None, [bR], [bR])
            P.op("dve", lambda e, o=R[:, 37:38], i_=R[:, 36:37]: e.reciprocal(out=o, in_=i_), [bR], [bR])
            tt("dve", R[:, 38:39], R[:, 37:38], R[:, 35:36], ALU.mult, [bR], [bR])
            yield
            tt("dve", R[:, 39:40], R[:, 37:38], R[:, 11:12], ALU.mult, [bR], [bR])
            tt("dve", R[:, 40:41], R[:, 38:39], R[:, 11:12], ALU.mult, [bR], [bR])
            ts("dve", COMB[:, c, :], R[:, 48:64], R[:, 39:40], None, ALU.mult, None, [bR], [bCOMB])
            stt("dve", COMB[:, c, :], R[:, 80:96], R[:, 40:41], COMB[:, c, :], ALU.mult, ALU.add, [bR, bCOMB], [bCOMB])
            yield

        def window6(genfs, w):
            pend = list(genfs)
            act_ = []
            while pend or act_:
                while pend and len(act_) < w:
                    act_.append(pend.pop(0)())
                for g_ in list(act_):
                    try:
                        next(g_)
                    except StopIteration:
                        act_.remove(g_)

        window6([(lambda c=c: s6_tile(c)) for c in range(OWN)], 2)
        dump("comb", COMB[:, :, :], [128, OWN, 16], [bCOMB])

        if stage <= 5:
            o_b = bOUT
            P.barrier()
            final.append((P.sem["sp"], 0))
            P.barrier()
            with nc.Block() as block:
                P.emit(block, [])
            st9.close()
            st7.close()
            return nc

        P.barrier()
        st9.close()
        st7.close()
        stE = ExitStack()

        def sbE(name, shape, dt=F32):
            return stE.enter_context(nc.sbuf_tensor(name, list(shape), dt))

        YACC = sbE("YACC", [128, OWN, D])
        bYACC = [Buf("YACC%d" % i) for i in range(OWN)]
        WG = [sbE("WG%d" % i, [128, 8, 512], BF16) for i in range(2)]
        WU = [sbE("WU%d" % i, [128, 8, 512], BF16) for i in range(2)]
        WD = [sbE("WD%d" % i, [128, 4, D], BF16) for i in range(2)]
        bWG = [Buf("WG%d" % i) for i in range(2)]
        bWU = [Buf("WU%d" % i) for i in range(2)]
        bWD = [Buf("WD%d" % i) for i in range(2)]
        ATb = [sbE("AT%d" % i, [128, 4, 512], BF16) for i in range(2)]
        bAT = [Buf("AT%d" % i) for i in range(2)]
        SG = [sbE("SG%d" % i, [128, 512]) for i in range(2)]
        bSG = [Buf("SG%d" % i) for i in range(2)]
        NEXP = 16
        gu_ctr = [0]
        dn_ctr = [0]
        for e_ in range(NEXP):
            s_ = e_ % 2
            P.dma("pool", WG[s_][:, :, :], w_gate.ap()[e_].rearrange("(k p) n -> p k n", p=128), writes=[bWG[s_]])
            P.dma("pool", WU[s_][:, :, :], w_up.ap()[e_].rearrange("(k p) n -> p k n", p=128), writes=[bWU[s_]])
            P.dma("pool", WD[s_][:, :, :], w_down.ap()[e_].rearrange("(k p) n -> p k n", p=128), writes=[bWD[s_]])
            for tb in range(OWN // 4):
                tsl = slice(tb * 512, (tb + 1) * 512)
                a_ = (e_ * 4 + tb) % 2
                for fb in range(4):
                    k_ = gu_ctr[0] % 2
                    gu_ctr[0] += 1
                    pg, pu = 2 * k_, 2 * k_ + 1
                    for kc in range(8):
                        mm(PS[pg][:, :], WG[s_][:, kc, fb * 128:(fb + 1) * 128], H2T[:, kc, tsl], kc == 0, kc == 7, [bWG[s_], bH2T], [PB[pg]])
                    for kc in range(8):
                        mm(PS[pu][:, :], WU[s_][:, kc, fb * 128:(fb + 1) * 128], H2T[:, kc, tsl], kc == 0, kc == 7, [bWU[s_], bH2T], [PB[pu]])
                    act(SG[k_][:, :], PS[pg][:, :], AF.Silu, [PB[pg]], [bSG[k_]])
                    tt("dve", ATb[a_][:, fb, :], SG[k_][:, :], PS[pu][:, :], ALU.mult, [bSG[k_], PB[pu]], [bAT[a_]])
                for t_ in range(4):
                    tile = tb * 4 + t_
                    for cb in range(2):
                        pd = 4 + dn_ctr[0] % 4
                        dn_ctr[0] += 1
                        for fb in range(4):
                            mm(PS[pd][:, :], ATb[a_][:, fb, t_ * 128:(t_ + 1) * 128], WD[s_][:, fb, cb * 512:(cb + 1) * 512], fb == 0, fb == 3, [bAT[a_], bWD[s_]], [PB[pd]])
                        ya = YACC[:, tile, cb * 512:(cb + 1) * 512]
                        if e_ == 0:
                            ts("dve", ya, PS[pd][:, :], COMB[:, tile, e_:e_ + 1], None, ALU.mult, None, [PB[pd], bCOMB], [bYACC[tile]])
                        else:
                            stt("dve", ya, PS[pd][:, :], COMB[:, tile, e_:e_ + 1], ya, ALU.mult, ALU.add, [PB[pd], bCOMB, bYACC[tile]], [bYACC[tile]])
        GP2L = sbE("GP2L", [128, D])
        bGP2L = Buf("GP2L")
        P.dma("sp", GP2L[:, :], bc_rows(mod_d, 0, D, off=5 * D), reads=[bMODD], writes=[bGP2L])
        GP2 = GP2L[:, :]
        bMOD = bGP2L
        XF = [sbE("XF%d" % i, [128, D]) for i in range(2)]
        bXF = [Buf("XF%d" % i) for i in range(2)]
        JK2 = sbE("JK2", [128, D], BF16)
        bJK2 = Buf("JK2")
        SF = sbE("SF", [128, 8])
        bSF = Buf("SF")
        OT = [sbE("OT%d" % i, [128, D]) for i in range(2)]
        bOT = [Buf("OT%d" % i) for i in range(2)]
        for c in range(OWN):
            s_ = c % 2
            rows = slice(c * 128, (c + 1) * 128)
            P.dma("sp", XF[s_][:, :], out_d.ap()[rows, :], reads=[bOUT], writes=[bXF[s_]])
            q_ = SF[:, s_ * 4:s_ * 4 + 4]
            act(JK2[:, :], YACC[:, c, :], AF.Square, [bYACC[c]], [bJK2, bSF], accum_out=q_[:, 0:1])
            ts("pool", q_[:, 1:2], q_[:, 0:1], 1.0 / D, EPS, ALU.mult, ALU.add, [bSF], [bSF])
            tt("pool", q_[:, 2:3], q_[:, 1:2], NEGH, ALU.pow, [bSF, bTABS], [bSF])
            stt("dve", OT[s_][:, :], YACC[:, c, :], q_[:, 2:3], GP2, ALU.mult, ALU.mult, [bYACC[c], bSF, bMOD], [bOT[s_]])
            tt("pool", OT[s_][:, :], OT[s_][:, :], XF[s_][:, :], ALU.add, [bOT[s_], bXF[s_]], [bOT[s_]])
            t = P.dma("sp", out_d.ap()[rows, :], OT[s_][:, :], reads=[bOT[s_], bXF[s_]], writes=[bOUT])
            final.append((t[0], t[1]))
        P.barrier()
        with nc.Block() as block:
            P.emit(block, final)
        stE.close()
    return nc


def _centered(idx, n):
    return ((idx + n // 2) % n) - n // 2


def make_inputs(core, x, c, ctx, c_ctx, w_ada, b_ada, g_pre_mix, g_post_mix, g_pre_ffn, g_post_ffn,
                w_in, conv_w, conv_b, w_q, w_k, w_v, w_if_fwd, b_if_fwd, w_if_bwd, b_if_bwd,
                mlstm_norm_w, mlstm_skip, w_fourier, w_out, w_router_group, b_router_group,
                w_router_expert, b_router_expert, w_gate, w_up, w_down, shared):
    f32 = np.float32
    j = core
    xs = x[0]
    m = {}
    m["x_rot"] = np.ascontiguousarray(np.roll(xs, -2048 * j, axis=0))
    halo = np.zeros((128, D), f32)
    hmask = np.zeros(64, f32)
    hrow = np.zeros(128, f32)
    hcol = np.zeros(128, f32)
    for g in range(NG):
        for side, tr_ in ((0, 512 * g - 1), (1, 512 * g + 512)):
            true_t = (tr_ % T + 2048 * j) % T
            own_first_true = ((512 * g) % T + 2048 * j) % T
            if side == 0:
                valid = own_first_true != 0
            else:
                valid = ((512 * g + 511) % T + 2048 * j) % T != T - 1
            halo[2 * g + side] = xs[true_t]
            hmask[2 * g + side] = 1.0 if valid else 0.0
            hrow[2 * g + side] = true_t // 64
            hcol[2 * g + side] = true_t % 64
    m["x_halo"] = halo
    tabs = np.zeros((128, 1024), f32)
    p = np.arange(128)
    for a in range(2):
        tabs[:, a] = ((2 * p + a) + 32 * j) % 256
    tabs[:, 2] = p % 64
    tabs[:, 3] = hrow
    tabs[:, 4] = hcol
    tabs[:, 5] = -0.5
    i = np.arange(128)
    true_c = (i + 16 * j) % 128
    tabs[:, 128:256] = (true_c < 16 * j).astype(f32)[None, :]
    tabs[:, 256:384] = (true_c >= 16 * j + 16).astype(f32)[None, :]
    tabs[:, 384:448] = hmask[None, :]
    tabs[:, 512:768] = np.arange(256, dtype=f32)[None, :]
    m["tabs"] = tabs
    di = np.zeros((128, 640), f32)
    n = np.arange(128)[:, None]
    k = np.arange(128)[None, :]
    di[:, 0:128] = _centered(n * k + 32, 128)
    di[:, 128:256] = _centered(n * k, 128)
    base = n * k + 2048 * j * k
    di[:, 256:384] = _centered(base + 4096, 16384)
    di[:, 384:512] = _centered(base, 16384)
    cc = (16 * j + np.arange(16))[None, :]
    di[:, 512:528] = _centered(n * cc + 32, 128)
    di[:, 528:544] = _centered(n * cc, 128)
    m["dftidx"] = di
    m.update(shared)
    return m


def make_shared(x, c, ctx, c_ctx, w_ada, b_ada, g_pre_mix, g_post_mix, g_pre_ffn, g_post_ffn,
                w_in, conv_w, conv_b, w_q, w_k, w_v, w_if_fwd, b_if_fwd, w_if_bwd, b_if_bwd,
                mlstm_norm_w, mlstm_skip, w_fourier, w_out, w_router_group, b_router_group,
                w_router_expert, b_router_expert, w_gate, w_up, w_down):
    f32 = np.float32
    s = {}
    s["ctx"] = np.ascontiguousarray(ctx[0])
    cT = np.zeros((128, 16), f32)
    cT[:, 0:8] = c[0].reshape(8, 128).T
    cT[:, 8:16] = c_ctx.reshape(8, 128).T
    s["cT"] = cT
    s["w_ada"] = np.ascontiguousarray(w_ada[0])
    s["b_ada"] = np.ascontiguousarray(b_ada[0][None, :])
    s["gains"] = np.stack([g_pre_mix[0], g_post_mix[0], g_pre_ffn[0], g_post_ffn[0]]).astype(f32)
    s["w_in"] = np.ascontiguousarray(w_in[0])
    cw = np.zeros((128, 16), f32)
    for cc in range(4):
        for k in range(3):
            cw[:, cc * 3 + k] = conv_w[0][k, cc * 128:(cc + 1) * 128]
        cw[:, 12 + cc] = conv_b[0][cc * 128:(cc + 1) * 128]
    s["convw"] = cw
    s["w_qkv"] = np.stack([w_q[0].reshape(512, 4), w_k[0].reshape(512, 4), w_v[0].reshape(512, 4)]).astype(f32)
    s["w_qkvT"] = np.stack([np.ascontiguousarray(w.transpose(0, 2, 1)).reshape(512, 4) for w in (w_q[0], w_k[0], w_v[0])]).astype(f32)
    wf, wb = w_if_fwd[0], w_if_bwd[0]
    s["w_if"] = np.ascontiguousarray(np.concatenate([wf[:, 0:4], wb[:, 0:4], wf[:, 4:8], wb[:, 4:8]], axis=1))
    bf, bb = b_if_fwd[0], b_if_bwd[0]
    s["b_if"] = np.concatenate([bf[0:4], bb[0:4], bf[4:8], bb[4:8]])[None, :].astype(f32)
    s["nrm_skip"] = np.stack([mlstm_norm_w[0], mlstm_skip[0]]).astype(f32)
    s["w_fourier"] = np.ascontiguousarray(w_fourier[0])
    s["w_out"] = np.ascontiguousarray(w_out[0])
    s["w_router"] = np.ascontiguousarray(np.concatenate([w_router_group[0], w_router_expert[0]], axis=1))
    s["b_router"] = np.concatenate([b_router_group[0], b_router_expert[0]])[None, :].astype(f32)
    s["w_gate"] = np.ascontiguousarray(w_gate[0])
    s["w_up"] = np.ascontiguousarray(w_up[0])
    s["w_down"] = np.ascontiguousarray(w_down[0])
    cst = np.zeros((128, 640), f32)
    cst[:, 0:128] = np.eye(128)
    a = np.arange(128)
    cst[:, 128:256] = (a[:, None] <= a[None, :])
    cst[:, 256:384] = (a[:, None] >= a[None, :])
    cst[:, 384:512] = 1.0
    cst[:, 512:640] = (a[:, None] // 4 == a[None, :] // 4)
    s["consts"] = cst
    return s


_CACHE = {}


def kernel(**inputs):
    inputs = {k: np.asarray(v) for k, v in inputs.items()}
    if "nc" not in _CACHE:
        _CACHE["nc"] = build()
    nc = _CACHE["nc"]
    shared = make_shared(**inputs)
    in_maps = [make_inputs(core, shared=shared, **inputs) for core in range(NCORES)]
    res = run_bass_kernel_spmd(nc, in_maps, core_ids=list(range(NCORES)))
    out = np.concatenate([res.results[i]["out"] for i in range(NCORES)], axis=0)
    return out.reshape(1, T, D).astype(np.float32)
```

```python
import math
from contextlib import ExitStack
import numpy as np
import concourse.bass as bass
import concourse.mybir as mybir
from concourse.bass_utils import run_bass_kernel_spmd

F32 = mybir.dt.float32
BF16 = mybir.dt.bfloat16
I32 = mybir.dt.int32
AF = mybir.ActivationFunctionType
ALU = mybir.AluOpType

NCORES = 8
T = 16384
D = 1024
NT = T // 128
OWN = NT // NCORES
NG = NT // 4
EPS = 1e-6
TWO_PI = 2.0 * math.pi
NHALO = 2 * NG
DBG = {}


class Buf:
    __slots__ = ("name", "w", "r")

    def __init__(self, name):
        self.name = name
        self.w = None
        self.r = {}


class Prog:
    def __init__(self, nc, stack, ndma=28):
        self.nc = nc
        self.engs = {"pe": nc.tensor, "act": nc.scalar, "dve": nc.vector, "pool": nc.gpsimd, "sp": nc.sync}
        self.ops = {k: [] for k in self.engs}
        self.sem = {k: stack.enter_context(nc.semaphore("s_" + k)) for k in self.engs}
        self.cnt = {k: 0 for k in self.engs}
        self.waited = {k: {} for k in self.engs}
        self.dsem = [stack.enter_context(nc.semaphore("d%d" % i)) for i in range(ndma)]
        self.dcnt = [0] * ndma
        self.ring = {"sp": list(range(0, ndma - 8)), "act": list(range(0, ndma - 8)), "pool": list(range(ndma - 8, ndma))}
        self.rpos = {"sp": 0, "pool": 0}

    def _collect(self, e, reads, writes, sync_same=True):
        waits = {}

        def need(tok):
            if tok is None:
                return
            sem, val, eng = tok
            if eng == e and not sync_same:
                return
            key = id(sem)
            if self.waited[e].get(key, 0) >= val:
                return
            if key not in waits or waits[key][1] < val:
                waits[key] = (sem, val)

        for b in reads:
            need(b.w)
        for b in writes:
            need(b.w)
            for t in b.r.values():
                need(t)
        for key, (sem, val) in waits.items():
            self.waited[e][key] = val
        return list(waits.values())

    def _commit(self, tok, reads, writes):
        key = id(tok[0])
        for b in reads:
            old = b.r.get(key)
            if old is None or old[1] < tok[1]:
                b.r[key] = tok
        for b in writes:
            b.w = tok
            b.r = {}

    def op(self, e, fn, reads=(), writes=(), sync_same=True):
        waits = self._collect(e, reads, writes, sync_same)
        self.cnt[e] += 1
        tok = (self.sem[e], self.cnt[e], e)
        self.ops[e].append((waits, fn, (self.sem[e], 1)))
        self._commit(tok, reads, writes)
        return tok

    def dma(self, e, out, in_, reads=(), writes=()):
        rk = "pool" if e == "pool" else "sp"
        ring = self.ring[rk]
        i = ring[self.rpos[rk] % len(ring)]
        self.rpos[rk] += 1
        sem = self.dsem[i]
        waits = self._collect(e, reads, writes)
        prev = self.dcnt[i] * 16
        if prev > 0 and self.waited[e].get(id(sem), 0) < prev:
            waits.append((sem, prev))
            self.waited[e][id(sem)] = prev
        self.dcnt[i] += 1
        tok = (sem, self.dcnt[i] * 16, "dma")
        self.ops[e].append((waits, (lambda eng, o=out, i_=in_: eng.dma_start(out=o, in_=i_)), (sem, 16)))
        self._commit(tok, reads, writes)
        return tok

    def barrier(self):
        toks = [(self.sem[k], self.cnt[k]) for k in self.engs if self.cnt[k] > 0]
        toks += [(self.dsem[i], self.dcnt[i] * 16) for i in range(len(self.dsem)) if self.dcnt[i] > 0]
        for e in self.engs:
            waits = []
            for sem, val in toks:
                if self.waited[e].get(id(sem), 0) < val:
                    waits.append((sem, val))
                    self.waited[e][id(sem)] = val
            if waits:
                self.ops[e].append((waits, None, None))

    def emit(self, block, final_waits):
        def replay(e):
            def body(eng):
                for waits, fn, inc in self.ops[e]:
                    for sem, val in waits:
                        eng.wait_ge(sem, val)
                    if fn is not None:
                        ins = fn(eng)
                        ins.then_inc(inc[0], inc[1])
                if e == "sp":
                    for sem, val in final_waits:
                        eng.wait_ge(sem, val)
            return body

        block.tensor(replay("pe"))
        block.scalar(replay("act"))
        block.vector(replay("dve"))
        block.gpsimd(replay("pool"))
        block.sync(replay("sp"))


def build(stage=99, dbg=False, ngroups=NG, cut=99):
    nc = bass.Bass("TRN2", target_bir_lowering=False)
    DBG.clear()

    def din(name, shape, dt=F32):
        return nc.dram_tensor(name, list(shape), dt, kind="ExternalInput")

    x_rot = din("x_rot", [ngroups * 512, D])
    x_halo = din("x_halo", [128, D])
    ctx_in = din("ctx", [256, D])
    cT = din("cT", [128, 16])
    w_ada = din("w_ada", [D, 6 * D])
    b_ada = din("b_ada", [1, 6 * D])
    gains = din("gains", [4, D])
    w_in = din("w_in", [D, 1536])
    convw = din("convw", [128, 16])
    w_qkv = din("w_qkv", [3, 512, 4])
    w_qkvT = din("w_qkvT", [3, 512, 4])
    w_if = din("w_if", [1536, 16])
    b_if = din("b_if", [1, 16])
    nrm_skip = din("nrm_skip", [2, 512])
    w_fourier = din("w_fourier", [4, 128, 128])
    w_out = din("w_out", [D, D])
    w_router = din("w_router", [D, 20])
    b_router = din("b_router", [1, 20])
    moe_small = stage < 8
    w_gate = din("w_gate", [16, D, 512] if not moe_small else [1, 8, 8])
    w_up = din("w_up", [16, D, 512] if not moe_small else [1, 8, 8])
    w_down = din("w_down", [16, 512, D] if not moe_small else [1, 8, 8])
    consts = din("consts", [128, 5 * 128])
    tabs = din("tabs", [128, 1024])
    dftidx = din("dftidx", [128, 5 * 128])
    out_d = nc.dram_tensor("out", [OWN * 128, D], F32, kind="ExternalOutput")

    posr_d = nc.dram_tensor("posr_d", [256, 512], F32)
    u_d = nc.dram_tensor("u_d", [512, T], BF16)
    az_d = nc.dram_tensor("az_d", [2, OWN * 128, 512], BF16)
    hs_d = nc.dram_tensor("hs_d", [OWN * 128, 512], F32)
    mod_d = nc.dram_tensor("mod_d", [1, 6 * D], F32)

    dbg_outs = {}

    def dbg_out(name, shape):
        DBG[name] = tuple(shape)
        dbg_outs[name] = nc.dram_tensor("dbg_" + name, list(shape), F32, kind="ExternalOutput")
        return dbg_outs[name]

    stack = ExitStack()
    with stack:
        P = Prog(nc, stack)
        used = [0]

        def sb(name, shape, dt=F32):
            t = stack.enter_context(nc.sbuf_tensor(name, list(shape), dt))
            return t

        def psum(name, shape, dt=F32):
            return stack.enter_context(nc.psum_tensor(name, list(shape), dt))

        def act(out, in_, func, reads, writes, eng="act", **kw):
            return P.op("act", lambda e: e.activation(out=out, in_=in_, func=func, **kw), reads, writes)

        def tt(eng, out, in0, in1, op, reads, writes):
            return P.op(eng, lambda e: e.tensor_tensor(out=out, in0=in0, in1=in1, op=op), reads, writes)

        def ts(eng, out, in0, s1, s2, op0, op1, reads, writes):
            if op1 is None:
                return P.op(eng, lambda e: e.tensor_scalar(out=out, in0=in0, scalar1=s1, scalar2=None, op0=op0), reads, writes)
            return P.op(eng, lambda e: e.tensor_scalar(out=out, in0=in0, scalar1=s1, scalar2=s2, op0=op0, op1=op1), reads, writes)

        def stt(eng, out, in0, scalar, in1, op0, op1, reads, writes):
            return P.op(eng, lambda e: e.scalar_tensor_tensor(out=out, in0=in0, scalar=scalar, in1=in1, op0=op0, op1=op1), reads, writes)

        def cp(eng, out, in_, reads, writes):
            if eng == "act":
                return P.op("act", lambda e: e.activation(out=out, in_=in_, func=AF.Copy), reads, writes)
            return P.op(eng, lambda e: e.tensor_copy(out=out, in_=in_), reads, writes)

        def mm(out, lhsT, rhs, start, stop, reads, writes):
            return P.op("pe", lambda e: e.matmul(out, lhsT=lhsT, rhs=rhs, start=start, stop=stop), reads, writes, sync_same=False)

        def tr(out, in_, ident, reads, writes):
            return P.op("pe", lambda e: e.transpose(out=out, in_=in_, identity=ident), reads, writes, sync_same=False)

        def memset(eng, ap, val, writes):
            return P.op(eng, lambda e: e.memset(ap, val), (), writes)

        def dump(name, ap, shape, reads):
            if not dbg:
                return
            dd = dbg_out(name, shape)
            P.dma("pool", dd.ap(), ap, reads=reads, writes=[Buf("dbg")])

        def bc_rows(dram_t, row, n, parts=128, off=0):
            width = dram_t.shape[-1]
            return bass.AP(dram_t, row * width + off, [[0, parts], [1, n]])

        PS = [psum("ps%d" % i, [128, 512]) for i in range(8)]
        PB = [Buf("ps%d" % i) for i in range(8)]

        CONST = sb("CONST", [128, 640])
        bCONST = Buf("CONST")
        P.dma("sp", CONST[:, :], consts.ap(), writes=[bCONST])
        IDF = CONST[:, 0:128]
        TRIU = CONST[:, 128:256]
        TRIL = CONST[:, 256:384]
        ONES = CONST[:, 384:512]
        BDM = CONST[:, 512:640]
        IDB = sb("IDB", [128, 128], BF16)
        bIDB = Buf("IDB")
        cp("dve", IDB[:, :], IDF, [bCONST], [bIDB])
        TABS = sb("TABS", [128, 1024])
        bTABS = Buf("TABS")
        P.dma("sp", TABS[:, :], tabs.ap(), writes=[bTABS])
        NEGH = TABS[:, 5:6]
        POSC = sb("POSC", [128, 512])
        bPOSC = Buf("POSC")

        final = []
        stm = ExitStack()

        def sbm(name, shape, dt=F32):
            return stm.enter_context(nc.sbuf_tensor(name, list(shape), dt))

        QTF = sbm("QTF", [128, 4 * OWN * 128], BF16)
        QT = QTF[:, :].rearrange("p (c t) -> p c t", c=4)
        bQT = Buf("QT")
        KTF = sbm("KTF", [128, 4 * OWN * 128], BF16)
        KT = KTF[:, :].rearrange("p (c t) -> p c t", c=4)
        bKT = Buf("KT")
        WINC = KTF[:, 0:4096].rearrange("p (k n) -> p k n", n=512)
        bWINC = bKT
        KTMO = sbm("KTMO", [128, OWN, 512], BF16)
        bKTMO = [Buf("KTMO%d" % i) for i in range(OWN)]
        VAO = sbm("VAO", [128, OWN, 4, 129], BF16)
        bVAO = [Buf("VAO%d" % i) for i in range(OWN)]
        OWNG = sbm("OWNG", [128, OWN, 24])
        bOWNG = Buf("OWNG")
        CF = sbm("CF", [128, 4, 129])
        bCF = Buf("CF")
        CB = sbm("CB", [128, 4, 129])
        bCB = Buf("CB")
        sts = ExitStack()

        def sbs(name, shape, dt=F32):
            return sts.enter_context(nc.sbuf_tensor(name, list(shape), dt))

        WIN = sbs("WIN", [128, 8, 1536], BF16)
        bWIN = Buf("WIN")
        BCOL = sbs("BCOL", [128, 16])
        bBCOL = Buf("BCOL")
        BZ = sbs("BZ", [128, 512])
        bBZ = Buf("BZ")
        bMODD = Buf("mod_d")

        def interleave(gens):
            gens = list(gens)
            while gens:
                for g_ in list(gens):
                    try:
                        next(g_)
                    except StopIteration:
                        gens.remove(g_)

        with ExitStack() as st0:
            def sb0(name, shape, dt=F32):
                return st0.enter_context(nc.sbuf_tensor(name, list(shape), dt))
            MOD = sb0("MOD", [128, 6 * D])
            bMOD = Buf("MOD")
            MODC = sb0("MODC", [128, 2 * D])
            bMODC = Buf("MODC")
            st0a = ExitStack()

            def sb0a(name, shape, dt=F32):
                return st0a.enter_context(nc.sbuf_tensor(name, list(shape), dt))
            CT = sb0a("CT", [128, 16])
            bCT = Buf("CT")
            P.dma("sp", CT[:, :], cT.ap(), writes=[bCT])
            SC = sb0a("SC", [128, 16])
            bSC = Buf("SC")
            act(SC[:, :], CT[:, :], AF.Silu, [bCT], [bSC])
            REP = sb0a("REP", [128, 16, 128])
            bREP = Buf("REP")
            for j in range(16):
                cp("dve", REP[:, j, :], SC[:, j:j + 1].broadcast_to([128, 128]), [bSC], [bREP])
            WA = [sb0a("WA%d" % i, [128, 8, 512]) for i in range(2)]
            bWA = [Buf("WA%d" % i) for i in range(2)]
            BA = [sb0a("BA%d" % i, [128, 512]) for i in range(2)]
            bBA = [Buf("BA%d" % i) for i in range(2)]
            w_ada_v = w_ada.ap().rearrange("(k p) n -> p k n", p=128)
            for blk in range(12):
                s = blk % 2
                P.dma("sp", WA[s][:, :, :], w_ada_v[:, :, blk * 512:(blk + 1) * 512], writes=[bWA[s]])
                P.dma("sp", BA[s][:, :], bc_rows(b_ada, 0, 512, off=blk * 512), writes=[bBA[s]])
                pb = blk % 2
                for kc in range(8):
                    mm(PS[pb][:, :], REP[:, kc, :], WA[s][:, kc, :], kc == 0, kc == 7, [bREP, bWA[s]], [PB[pb]])
                tt("dve", MOD[:, blk * 512:(blk + 1) * 512], PS[pb][:, :], BA[s][:, :], ALU.add, [PB[pb], bBA[s]], [bMOD])
                if blk < 4:
                    pc = 2 + blk % 2
                    for kc in range(8):
                        mm(PS[pc][:, :], REP[:, 8 + kc, :], WA[s][:, kc, :], kc == 0, kc == 7, [bREP, bWA[s]], [PB[pc]])
                    tt("dve", MODC[:, blk * 512:(blk + 1) * 512], PS[pc][:, :], BA[s][:, :], ALU.add, [PB[pc], bBA[s]], [bMODC])
            GB = sb0a("GB", [128, 4, D])
            bGB = Buf("GB")
            for i in range(4):
                P.dma("sp", GB[:, i, :], bc_rows(gains, i, D), writes=[bGB])
            stt("dve", MOD[:, D:2 * D], MOD[:, D:2 * D], 1.0, GB[:, 0, :], ALU.add, ALU.mult, [bMOD, bGB], [bMOD])
            tt("dve", MOD[:, 2 * D:3 * D], MOD[:, 2 * D:3 * D], GB[:, 1, :], ALU.mult, [bMOD, bGB], [bMOD])
            stt("dve", MOD[:, 4 * D:5 * D], MOD[:, 4 * D:5 * D], 1.0, GB[:, 2, :], ALU.add, ALU.mult, [bMOD, bGB], [bMOD])
            tt("dve", MOD[:, 5 * D:6 * D], MOD[:, 5 * D:6 * D], GB[:, 3, :], ALU.mult, [bMOD, bGB], [bMOD])
            stt("dve", MODC[:, D:2 * D], MODC[:, D:2 * D], 1.0, GB[:, 0, :], ALU.add, ALU.mult, [bMODC, bGB], [bMODC])
            P.dma("sp", mod_d.ap(), MOD[0:1, :], reads=[bMOD], writes=[bMODD])
            dump("mod", MOD[0:1, :], [1, 6 * D], [bMOD])
            dump("modc", MODC[0:1, :], [1, 2 * D], [bMODC])
            P.barrier()
            st0a.close()
            REPS = sb0("REPS", [128, 2, 8, 128])
            bREPS = Buf("REPS")
            GC = sb0("GC", [128, 16])
            bGC = Buf("GC")
            for kc in range(8):
                blk = slice(kc * 128, (kc + 1) * 128)
                tr(PS[0][:, 0:128], MOD[:, blk], IDF, [bMOD, bCONST], [PB[0]])
                tr(PS[0][:, 128:256], MOD[:, D + kc * 128:D + (kc + 1) * 128], IDF, [bMOD, bCONST], [PB[0]])
                tr(PS[0][:, 256:384], MODC[:, blk], IDF, [bMODC, bCONST], [PB[0]])
                tr(PS[0][:, 384:512], MODC[:, D + kc * 128:D + (kc + 1) * 128], IDF, [bMODC, bCONST], [PB[0]])
                cp("dve", REPS[:, 0, kc, :], PS[0][:, 0:128], [PB[0]], [bREPS])
                cp("dve", REPS[:, 1, kc, :], PS[0][:, 256:384], [PB[0]], [bREPS])
                cp("dve", GC[:, kc:kc + 1], PS[0][:, 128:129], [PB[0]], [bGC])
                cp("dve", GC[:, 8 + kc:9 + kc], PS[0][:, 384:385], [PB[0]], [bGC])
            WST = [sb0("WST%d" % i, [128, 1536]) for i in range(2)]
            bWST = [Buf("WST%d" % i) for i in range(2)]
            for kc in range(8):
                w_, bw_ = WST[kc % 2], bWST[kc % 2]
                P.dma("sp", w_[:, :], w_in.ap()[kc * 128:(kc + 1) * 128, :], writes=[bw_])
                ts("dve", WIN[:, kc, :], w_[:, :], GC[:, kc:kc + 1], None, ALU.mult, None, [bw_, bGC], [bWIN])
                ts("pool", WINC[:, kc, :], w_[:, 0:512], GC[:, 8 + kc:9 + kc], None, ALU.mult, None, [bw_, bGC], [bWINC])
                for blk in range(3):
                    mm(PS[2 + blk][:, :], REPS[:, 0, kc, :], w_[:, blk * 512:(blk + 1) * 512], kc == 0, kc == 7, [bREPS, bw_], [PB[2 + blk]])
                mm(PS[5][:, :], REPS[:, 1, kc, :], w_[:, 0:512], kc == 0, kc == 7, [bREPS, bw_], [PB[5]])
            BROW = sb0("BROW", [128, 4, 512])
            bBROW = Buf("BROW")
            for i in range(4):
                cp("dve", BROW[:, i, :], PS[2 + i][:, :], [PB[2 + i]], [bBROW])
            cp("dve", BZ[:, :], BROW[:, 1, :], [bBROW], [bBZ])
            for i, src in ((0, 0), (1, 2), (2, 3)):
                for j in range(4):
                    tr(PS[0][:, j * 128:(j + 1) * 128], BROW[:, src, j * 128:(j + 1) * 128], IDF, [bBROW, bCONST], [PB[0]])
                for j in range(4):
                    cp("dve", BCOL[:, i * 4 + j:i * 4 + j + 1], PS[0][:, j * 128:j * 128 + 1], [PB[0]], [bBCOL])
            P.barrier()

        BDB = sbs("BDB", [128, 3, 4, 128], BF16)
        bBDB = Buf("BDB")
        AW = sbs("AW", [128, 4, 32], BF16)
        bAW = Buf("AW")
        BIF = sbs("BIF", [128, 16])
        bBIF = Buf("BIF")
        P.dma("sp", BIF[:, :], bc_rows(b_if, 0, 16), writes=[bBIF])
        CW = sbs("CW", [128, 16])
        bCW = Buf("CW")
        P.dma("sp", CW[:, :], convw.ap(), writes=[bCW])
        XMH = sbs("XMH", [128, 4, 64])
        bXMH = Buf("XMH")
        bPOSRD = Buf("posr_d")
        NTB = 4
        XB = [sbs("XB%d" % i, [128, D]) for i in range(NTB)]
        bXB = [Buf("XB%d" % i) for i in range(NTB)]
        PRB = [sbs("PRB%d" % i, [128, 512]) for i in range(NTB)]
        bPRB = [Buf("PRB%d" % i) for i in range(NTB)]
        X0B = [sbs("X0B%d" % i, [128, D], BF16) for i in range(NTB)]
        bX0B = [Buf("X0B%d" % i) for i in range(NTB)]
        JUNK = [sbs("JUNK%d" % i, [128, D], BF16) for i in range(2)]
        bJUNK = [Buf("JUNK%d" % i) for i in range(2)]
        DG = [sbs("DG%d" % i, [128, 128], BF16) for i in range(NTB)]
        bDG = [Buf("DG%d" % i) for i in range(NTB)]
        SS = sbs("SS", [128, 4 * NTB])
        bSS = [Buf("SS%d" % i) for i in range(NTB)]
        HT = sbs("HT", [128, 8, 512], BF16)
        bHTt = [Buf("HT%d" % i) for i in range(4)]
        PT = PS[0][:, :].bitcast(BF16).rearrange("p (k t) -> p k t", t=128)

        with ExitStack() as st1:
            def sb1(name, shape, dt=F32):
                return st1.enter_context(nc.sbuf_tensor(name, list(shape), dt))
            FREQ = sb1("FREQ", [128, 256])
            bFREQ = Buf("FREQ")
            act(FREQ[:, :], TABS[:, 512:768], AF.Exp, [bTABS], [bFREQ], scale=-math.log(10000.0) / 256.0)
            WS = sb1("WS", [128, 6, 4, 4])
            bWS = Buf("WS")
            for w in range(3):
                P.dma("sp", WS[:, w, :, :], bass.AP(w_qkv, w * 2048, [[4, 128], [512, 4], [1, 4]]), writes=[bWS])
                P.dma("sp", WS[:, 3 + w, :, :], bass.AP(w_qkvT, w * 2048, [[4, 128], [512, 4], [1, 4]]), writes=[bWS])
            BDF = sb1("BDF", [128, 6, 4, 128])
            bBDF = Buf("BDF")
            BDM3 = BDM.rearrange("p (r o) -> p r o", o=4)
            for w in range(6):
                for cc in range(4):
                    tt("dve", BDF[:, w, cc, :].rearrange("p (r o) -> p r o", o=4),
                       WS[:, w, cc, None, :].broadcast_to([128, 32, 4]), BDM3, ALU.mult, [bWS, bCONST], [bBDF])
            cp("dve", BDB[:, :, :, :], BDF[:, 0:3, :, :], [bBDF], [bBDB])
            WIF = sb1("WIF", [128, 12, 16])
            bWIF = Buf("WIF")
            P.dma("sp", WIF[:, :, :], w_if.ap().rearrange("(k p) n -> p k n", p=128), writes=[bWIF])
            for cc in range(4):
                mm(PS[1][:, cc * 32:cc * 32 + 16], BDF[:, 3, cc, :], WIF[:, cc, :], True, False, [bBDF, bWIF], [PB[1]])
                mm(PS[1][:, cc * 32:cc * 32 + 16], BDF[:, 4, cc, :], WIF[:, 4 + cc, :], False, True, [bBDF, bWIF], [PB[1]])
                mm(PS[1][:, cc * 32 + 16:cc * 32 + 32], BDF[:, 5, cc, :], WIF[:, 8 + cc, :], True, True, [bBDF, bWIF], [PB[1]])
            cp("dve", AW[:, :, :], PS[1][:, 0:128].rearrange("p (c n) -> p c n", n=32), [PB[1]], [bAW])

            ANG = sb1("ANG", [128, 512])
            bANG = Buf("ANG")
            KI = sb1("KI", [128, 512], I32)
            bKI = Buf("KI")
            MSK = sb1("MSK", [128, 512])
            bMSK = Buf("MSK")

            def sincos(out, bout, idx):
                ts("dve", ANG[:, 0:256], FREQ[:, :], idx, None, ALU.mult, None, [bFREQ, bTABS], [bANG])
                ts("dve", ANG[:, 256:512], ANG[:, 0:256], math.pi / 2, None, ALU.add, None, [bANG], [bANG])
                ts("dve", KI[:, :], ANG[:, :], 1.0 / TWO_PI, None, ALU.mult, None, [bANG], [bKI])
                stt("dve", ANG[:, :], KI[:, :], -TWO_PI, ANG[:, :], ALU.mult, ALU.add, [bKI, bANG], [bANG])
                ts("dve", MSK[:, :], ANG[:, :], math.pi, TWO_PI, ALU.is_gt, ALU.mult, [bANG], [bMSK])
                tt("dve", ANG[:, :], ANG[:, :], MSK[:, :], ALU.subtract, [bANG, bMSK], [bANG])
                ts("dve", MSK[:, :], ANG[:, :], -math.pi, TWO_PI, ALU.is_lt, ALU.mult, [bANG], [bMSK])
                tt("dve", ANG[:, :], ANG[:, :], MSK[:, :], ALU.add, [bANG, bMSK], [bANG])
                ts("dve", ANG[:, :], ANG[:, :], math.pi, -math.pi, ALU.min, ALU.max, [bANG], [bANG])
                act(out, ANG[:, :], AF.Sin, [bANG], [bout])

            PR = sb1("PR", [128, 2, 512])
            bPR = Buf("PR")
            for a in range(2):
                sincos(PR[:, a, :], bPR, TABS[:, a:a + 1])
            P.dma("sp", posr_d.ap().rearrange("(p a) n -> p a n", a=2), PR[:, :, :], reads=[bPR], writes=[bPOSRD])
            sincos(POSC[:, :], bPOSC, TABS[:, 2:3])
            POSH = sb1("POSH", [128, D])
            bPOSH = Buf("POSH")
            sincos(POSH[:, 0:512], bPOSH, TABS[:, 3:4])
            sincos(POSH[:, 512:1024], bPOSH, TABS[:, 4:5])

            tile_ctr = [0]

            def tile_front(xsrc, pos_mode, ht_dst, bht):
                k = tile_ctr[0]
                tile_ctr[0] += 1
                q = k % NTB
                X, bX = XB[q], bXB[q]
                xb_, bxb_ = X0B[q], bX0B[q]
                dg, bdg = DG[q], bDG[q]
                bss = bSS[q]
                P.dma("sp", X[:, :], xsrc, writes=[bX])
                if pos_mode is not None and pos_mode[0] == "rolled":
                    i = pos_mode[1]
                    PRt, bPRt = PRB[q], bPRB[q]
                    P.dma("sp", PRt[0:64, :], bc_rows(posr_d, 2 * i, 512, parts=64), reads=[bPOSRD], writes=[bPRt])
                    P.dma("sp", PRt[64:128, :], bc_rows(posr_d, 2 * i + 1, 512, parts=64), reads=[bPOSRD], writes=[bPRt])
                    yield
                    tt("pool", xb_[:, 0:512], X[:, 0:512], PRt[:, :], ALU.add, [bX, bPRt], [bxb_])
                    tt("pool", xb_[:, 512:1024], X[:, 512:1024], POSC[:, :], ALU.add, [bX, bPOSC], [bxb_])
                elif pos_mode is not None:
                    tt("pool", xb_[:, :], X[:, :], POSH[:, :], ALU.add, [bX, bPOSH], [bxb_])
                else:
                    yield
                    cp("pool", xb_[:, :], X[:, :], [bX], [bxb_])
                yield
                sc = SS[:, q * 4:q * 4 + 4]
                act(JUNK[k % 2][:, :], xb_[:, :], AF.Square, [bxb_], [bJUNK[k % 2], bss], accum_out=sc[:, 0:1])
                yield
                ts("pool", sc[:, 1:2], sc[:, 0:1], 1.0 / D, EPS, ALU.mult, ALU.add, [bss], [bss])
                tt("pool", sc[:, 2:3], sc[:, 1:2], NEGH, ALU.pow, [bss, bTABS], [bss])
                yield
                ts("dve", dg[:, :], IDF, sc[:, 2:3], None, ALU.mult, None, [bCONST, bss], [bdg])
                yield
                for kc in range(8):
                    pb = kc // 4
                    mm(PS[pb][:, (kc % 4) * 128:(kc % 4 + 1) * 128], xb_[:, kc * 128:(kc + 1) * 128], dg[:, :], True, True, [bxb_, bdg], [PB[pb]])
                cp("act", ht_dst[:, 0:4, :], PS[0][:, :].rearrange("p (k t) -> p k t", t=128), [PB[0]], [bht])
                cp("dve", ht_dst[:, 4:8, :], PS[1][:, :].rearrange("p (k t) -> p k t", t=128), [PB[1]], [bht])
                yield

            HTH = sb1("HTH", [128, 8, 128], BF16)
            bHTH = Buf("HTH")
            interleave([tile_front(x_halo.ap(), ("halo",), HTH[:, :, :], bHTH)])
            for cc in range(4):
                for kc in range(8):
                    mm(PS[2][:, cc * 64:cc * 64 + 64], WIN[:, kc, cc * 128:(cc + 1) * 128], HTH[:, kc, 0:64], kc == 0, kc == 7, [bWIN, bHTH], [PB[2]])
            XMHF = sb1("XMHF", [128, 4, 64])
            bXMHF = Buf("XMHF")
            tt("dve", XMHF[:, :, :], PS[2][:, 0:256].rearrange("p (c n) -> p c n", n=64),
               BCOL[:, 0:4, None].broadcast_to([128, 4, 64]), ALU.add, [PB[2], bBCOL], [bXMHF])
            tt("dve", XMH[:, :, :], XMHF[:, :, :], TABS[:, None, 384:448].broadcast_to([128, 4, 64]), ALU.mult, [bXMHF, bTABS], [bXMH])
            dump("xmh", XMH[:, :, :], [128, 4, 64], [bXMH])
            P.barrier()

        XM = sbs("XM", [128, 4, 514])
        bXM = [Buf("XM%d" % i) for i in range(4)]
        ACC = [sbs("ACC%d" % i, [128, 512]) for i in range(2)]
        bACC = [Buf("ACC%d" % i) for i in range(2)]
        ACTT = [sbs("ACTT%d" % i, [128, 4, 512], BF16) for i in range(2)]
        bACTT = [[Buf("ACTT%d_%d" % (j, i)) for i in range(4)] for j in range(2)]
        XMB = [sbs("XMB%d" % i, [128, 4, 512], BF16) for i in range(2)]
        bXMB = [[Buf("XMB%d_%d" % (j, i)) for i in range(4)] for j in range(2)]
        UT = sbs("UT", [128, 4, 512], BF16)
        bUT = Buf("UT")
        bUD = Buf("u_d")
        KTMG = sbs("KTMG", [128, 4, 512], BF16)
        bKTMG = [Buf("KTMG%d" % i) for i in range(4)]
        VAG = sbs("VAG", [128, 4, 4, 129], BF16)
        bVAG = [Buf("VAG%d" % i) for i in range(4)]
        VS = [sbs("VS%d" % i, [128, 8, 129], BF16) for i in range(2)]
        bVS = [Buf("VS%d" % i) for i in range(2)]
        CBS = sbs("CBS", [128, 4, 129])
        bCBS = Buf("CBS")
        memset("pool", CBS[:, :, :], 0.0, [bCBS])
        CCB = sbs("CCB", [128, 4, 129])
        bCCB = Buf("CCB")
        GT = sbs("GT", [128, 4, 16])
        bGT = Buf("GT")
        GW = sbs("GW", [128, 4, 40])
        bGW = Buf("GW")
        EXI = sbs("EXI", [128, 4, 16])
        bEXI = Buf("EXI")
        EXO = sbs("EXO", [128, 4, 16])
        bEXO = Buf("EXO")
        PALL = sbs("PALL", [128, 5, 4])
        bPALL = Buf("PALL")
        MBT = sbs("MBT", [128, 4, 4])
        bMBT = Buf("MBT")
        WV = sbs("WV", [128, 4, 8])
        bWV = Buf("WV")
        STG = [sbs("STG%d" % i, [128, 512], BF16) for i in range(2)]
        bSTG = [Buf("STG%d" % i) for i in range(2)]
        ZT = sbs("ZT", [128, 512])
        bZT = Buf("ZT")
        bAZ = Buf("az_d")
        stg_ctr = [0]

        memset("pool", VAG[:, :, :, :], 1.0, bVAG)
        memset("pool", VAO[:, :, :, :], 1.0, bVAO)
        memset("pool", PALL[:, :, :], 0.0, [bPALL])

        PSG = PS[6][:, 384:448].rearrange("p (t g) -> p t g", g=16)
        bPSG = PB[6]
        PSB = PS[6][:, 448:512].rearrange("p (t g) -> p t g", g=16)
        bPSB = PB[6]
        DCF = [PS[5][:, 0:129], PS[5][:, 129:258], PS[5][:, 258:387], PS[6][:, 0:129]]
        bDCF = [PB[5], PB[5], PB[5], PB[6]]
        ACCB = [PS[7][:, 0:129], PS[7][:, 129:258], PS[7][:, 258:387], PS[6][:, 129:258]]
        bACCB = [PB[7], PB[7], PB[7], PB[6]]
        u_v = u_d.ap().rearrange("(c p) t -> p c t", p=128)
        LN_QS = math.log(128.0 ** -0.5)

        def front(gi, kind, par):
            n = 2 if kind == "ctx" else 4
            ntok = n * 128
            tgens = []
            for t_ in range(n):
                dst = HT[:, :, t_ * 128:(t_ + 1) * 128]
                if kind == "ctx":
                    tgens.append(tile_front(ctx_in.ap()[t_ * 128:(t_ + 1) * 128, :], None, dst, bHTt[t_]))
                else:
                    i = gi * 4 + t_
                    tgens.append(tile_front(x_rot.ap()[i * 128:(i + 1) * 128, :], ("rolled", i), dst, bHTt[t_]))
            while tgens:
                for g_ in list(tgens):
                    try:
                        next(g_)
                    except StopIteration:
                        tgens.remove(g_)
                yield
            W_, bW_, boff = (WINC, bWINC, 8) if kind == "ctx" else (WIN, bWIN, 0)
            att, batt, xmb, bxmb = ACTT[par], bACTT[par], XMB[par], bXMB[par]
            for cc in range(4):
                pb = 2 + cc % 2
                for kc in range(8):
                    mm(PS[pb][:, 0:ntok], W_[:, kc, cc * 128:(cc + 1) * 128], HT[:, kc, 0:ntok], kc == 0, kc == 7, [bW_] + bHTt, [PB[pb]])
                bias = BCOL[:, boff + cc:boff + cc + 1]
                act(XM[:, cc, 1:1 + ntok], PS[pb][:, 0:ntok], AF.Identity, [PB[pb], bBCOL], [bXM[cc]], bias=bias)
                act(xmb[:, cc, 0:ntok], PS[pb][:, 0:ntok], AF.Identity, [PB[pb], bBCOL], [bxmb[cc]], bias=bias)
                if kind == "ctx":
                    memset("pool", XM[:, cc, 0:1], 0.0, [bXM[cc]])
                    memset("pool", XM[:, cc, 1 + ntok:2 + ntok], 0.0, [bXM[cc]])
                else:
                    cp("pool", XM[:, cc, 0:1], XMH[:, cc, 2 * gi:2 * gi + 1], [bXMH], [bXM[cc]])
                    cp("pool", XM[:, cc, 513:514], XMH[:, cc, 2 * gi + 1:2 * gi + 2], [bXMH], [bXM[cc]])
                yield
                A_, bA_ = ACC[cc % 2], bACC[cc % 2]
                ts("dve", A_[:, 0:ntok], XM[:, cc, 1:1 + ntok], CW[:, cc * 3 + 1:cc * 3 + 2], CW[:, 12 + cc:13 + cc], ALU.mult, ALU.add, [bXM[cc], bCW], [bA_])
                stt("dve", A_[:, 0:ntok], XM[:, cc, 0:ntok], CW[:, cc * 3:cc * 3 + 1], A_[:, 0:ntok], ALU.mult, ALU.add, [bXM[cc], bCW, bA_], [bA_])
                stt("dve", A_[:, 0:ntok], XM[:, cc, 2:2 + ntok], CW[:, cc * 3 + 2:cc * 3 + 3], A_[:, 0:ntok], ALU.mult, ALU.add, [bXM[cc], bCW, bA_], [bA_])
                act(att[:, cc, 0:ntok], A_[:, 0:ntok], AF.Silu, [bA_], [batt[cc]])
                yield
            if kind == "own" and gi == 0:
                dump("actT", att[:, :, 0:128], [128, 4, 128], batt)
            if kind != "ctx":
                for cc in range(4):
                    pb = 2 + cc % 2
                    for kc in range(8):
                        mm(PS[pb][:, :], WIN[:, kc, 1024 + cc * 128:1024 + (cc + 1) * 128], HT[:, kc, :], kc == 0, kc == 7, [bWIN] + bHTt, [PB[pb]])
                    act(UT[:, cc, :], PS[pb][:, :], AF.Identity, [PB[pb], bBCOL], [bUT], bias=BCOL[:, 4 + cc:5 + cc])
                    yield
                P.dma("act", u_v[:, :, gi * 512:(gi + 1) * 512], UT[:, :, :], reads=[bUT], writes=[bUD])
            if kind == "own":
                for w, dstT, bdst in ((0, QT, bQT), (1, KT, bKT)):
                    for cc in range(4):
                        pb = 2 + cc % 2
                        mm(PS[pb][:, :], BDB[:, w, cc, :], att[:, cc, :], True, True, [bBDB, batt[cc]], [PB[pb]])
                        cp("act" if cc % 2 else "dve", dstT[:, cc, gi * 512:(gi + 1) * 512], PS[pb][:, :], [PB[pb]], [bdst])
                    yield
                for t_ in range(4):
                    i = gi * 4 + t_
                    sl = slice(t_ * 128, (t_ + 1) * 128)
                    pb = 2 + t_ % 2
                    for kc in range(8):
                        mm(PS[pb][:, :], HT[:, kc, sl], WIN[:, kc, 512:1024], kc == 0, kc == 7, bHTt + [bWIN], [PB[pb]])
                    tt("dve", ZT[:, :], PS[pb][:, :], BZ[:, :], ALU.add, [PB[pb], bBZ], [bZT])
                    s_ = stg_ctr[0] % 2
                    stg_ctr[0] += 1
                    act(STG[s_][:, :], ZT[:, :], AF.Silu, [bZT], [bSTG[s_]])
                    P.dma("act", az_d.ap()[1, i * 128:(i + 1) * 128, :], STG[s_][:, :], reads=[bSTG[s_]], writes=[bAZ])
                    for cc in range(4):
                        tr(PT[:, cc, :], att[:, cc, sl], IDB[:, :], [batt[cc], bIDB], [PB[0]])
                    s_ = stg_ctr[0] % 2
                    stg_ctr[0] += 1
                    cp("dve", STG[s_][:, :].rearrange("p (c t) -> p c t", t=128), PT[:, 0:4, :], [PB[0]], [bSTG[s_]])
                    P.dma("act", az_d.ap()[0, i * 128:(i + 1) * 128, :], STG[s_][:, :], reads=[bSTG[s_]], writes=[bAZ])
                    yield

        def back(gi, kind, par):
            n = 2 if kind == "ctx" else 4
            att, batt, xmb, bxmb = ACTT[par], bACTT[par], XMB[par], bXMB[par]
            for t_ in range(n):
                sl = slice(t_ * 128, (t_ + 1) * 128)
                if kind == "own":
                    i = gi * 4 + t_
                    ktm, bktm, va, bva = KTMO[:, i, :], bKTMO[i], VAO[:, i, :, :], bVAO[i]
                else:
                    ktm, bktm, va, bva = KTMG[:, t_, :], bKTMG[t_], VAG[:, t_, :, :], bVAG[t_]
                for cc in range(4):
                    mm(PS[4][:, cc * 128:(cc + 1) * 128], att[:, cc, sl], BDB[:, 1, cc, :], True, True, [batt[cc], bBDB], [PB[4]])
                cp("act", ktm, PS[4][:, :], [PB[4]], [bktm])
                yield
                for cc in range(4):
                    mm(PS[4][:, cc * 128:(cc + 1) * 128], xmb[:, cc, sl], BDB[:, 2, cc, :], True, True, [bxmb[cc], bBDB], [PB[4]])
                cp("act", va[:, :, 0:128], PS[4][:, :].rearrange("p (h d) -> p h d", d=128), [PB[4]], [bva])
                for cc in range(4):
                    mm(PSG[:, t_, :], att[:, cc, sl], AW[:, cc, 0:16], cc == 0, False, [batt[cc], bAW], [bPSG])
                for cc in range(4):
                    mm(PSG[:, t_, :], xmb[:, cc, sl], AW[:, cc, 16:32], False, cc == 3, [bxmb[cc], bAW], [bPSG])
                yield
            tt("dve", GT[:, 0:n, :], PSG[:, 0:n, :], BIF[:, None, :].broadcast_to([128, n, 16]), ALU.add, [bPSG, bBIF], [bGT])
            stt("dve", GW[:, 0:n, 0:8], GT[:, 0:n, 8:16], -1.0, GT[:, 0:n, 8:16], ALU.mult, ALU.max, [bGT], [bGW])
            yield
            ts("dve", GW[:, 0:n, 24:32], GT[:, 0:n, 8:16], 0.0, None, ALU.min, None, [bGT], [bGW])
            act(GW[:, 0:n, 8:16], GW[:, 0:n, 0:8], AF.Exp, [bGW], [bGW], scale=-1.0)
            act(GW[:, 0:n, 16:24], GW[:, 0:n, 8:16], AF.Ln, [bGW], [bGW], bias=1.0)
            yield
            tt("dve", GW[:, 0:n, 32:40], GW[:, 0:n, 24:32], GW[:, 0:n, 16:24], ALU.subtract, [bGW], [bGW])
            yield
            for t_ in range(n):
                mm(PSB[:, t_, 0:4], TRIU, GW[:, t_, 32:36], True, True, [bCONST, bGW], [bPSB])
                mm(PSB[:, t_, 4:8], TRIL, GW[:, t_, 36:40], True, True, [bCONST, bGW], [bPSB])
                mm(PSB[:, t_, 8:16], ONES, GW[:, t_, 32:40], True, True, [bCONST, bGW], [bPSB])
            yield
            if kind == "own":
                i0 = gi * 4
                if gi == 0:
                    dump("gt", GT[:, :, :], [128, 4, 16], [bGT])
                    dump("lf", GW[:, :, 32:40], [128, 4, 8], [bGW])
                tt("dve", EXI[:, :, 0:8], GT[:, :, 0:8], PSB[:, :, 0:8], ALU.subtract, [bGT, bPSB], [bEXI])
                yield
                act(OWNG[:, i0:i0 + 4, 0:8], EXI[:, :, 0:8], AF.Exp, [bEXI], [bOWNG])
                act(OWNG[:, i0:i0 + 4, 8:16], PSB[:, :, 0:8], AF.Exp, [bPSB], [bOWNG], bias=LN_QS)
                act(OWNG[:, i0:i0 + 4, 16:24], PSB[:, :, 8:16], AF.Exp, [bPSB], [bOWNG])
                yield
                return
            tt("dve", EXI[:, 0:n, 0:8], GT[:, 0:n, 0:8], PSB[:, 0:n, 8:16], ALU.add, [bGT, bPSB], [bEXI])
            tt("dve", EXI[:, 0:n, 0:8], EXI[:, 0:n, 0:8], PSB[:, 0:n, 0:8], ALU.subtract, [bEXI, bPSB], [bEXI])
            yield
            if kind == "ctx":
                cp("dve", EXI[:, 0:2, 8:16], PSB[:, 0:2, 8:16], [bPSB], [bEXI])
                yield
                act(EXO[:, 0:2, :], EXI[:, 0:2, :], AF.Exp, [bEXI], [bEXO])
                yield
                tt("dve", WV[:, 0, 0:4], EXO[:, 0, 0:4], EXO[:, 1, 8:12], ALU.mult, [bEXO], [bWV])
                cp("dve", WV[:, 1, 0:4], EXO[:, 1, 0:4], [bEXO], [bWV])
                cp("dve", WV[:, 0, 4:8], EXO[:, 0, 4:8], [bEXO], [bWV])
                tt("dve", WV[:, 1, 4:8], EXO[:, 1, 4:8], EXO[:, 0, 12:16], ALU.mult, [bEXO], [bWV])
                yield
            else:
                MF = TABS[:, 128 + 4 * gi:132 + 4 * gi]
                MB = TABS[:, 256 + 4 * gi:260 + 4 * gi]
                MF3 = MF[:, :, None].broadcast_to([128, 4, 4])
                MB3 = MB[:, :, None].broadcast_to([128, 4, 4])
                tt("dve", EXI[:, :, 8:12], PSB[:, :, 8:12], MF3, ALU.mult, [bPSB, bTABS], [bEXI])
                tt("dve", MBT[:, :, :], PSB[:, :, 12:16], MB3, ALU.mult, [bPSB, bTABS], [bMBT])
                yield
                for t_ in range(4):
                    tt("dve", PALL[:, t_ + 1, :], PALL[:, t_, :], MBT[:, t_, :], ALU.add, [bPALL, bMBT], [bPALL])
                    yield
                cp("dve", EXI[:, :, 12:16], PALL[:, 0:4, :], [bPALL], [bEXI])
                yield
                act(EXO[:, :, :], EXI[:, :, :], AF.Exp, [bEXI], [bEXO])
                yield
                tt("dve", WV[:, :, 0:4], EXO[:, :, 0:4], MF3, ALU.mult, [bEXO, bTABS], [bWV])
                tt("dve", WV[:, :, 4:8], EXO[:, :, 4:8], EXO[:, :, 12:16], ALU.mult, [bEXO], [bWV])
                yield
                tt("dve", WV[:, :, 4:8], WV[:, :, 4:8], MB3, ALU.mult, [bWV, bTABS], [bWV])
                cp("dve", PALL[:, 0, :], PALL[:, 4, :], [bPALL], [bPALL])
                yield
            for t_ in range(n):
                s_ = t_ % 2
                tt("pool", VS[s_][:, 0:4, :], VAG[:, t_, :, :], WV[:, t_, 0:4, None].broadcast_to([128, 4, 129]), ALU.mult, [bVAG[t_], bWV], [bVS[s_]])
                tt("pool", VS[s_][:, 4:8, :], VAG[:, t_, :, :], WV[:, t_, 4:8, None].broadcast_to([128, 4, 129]), ALU.mult, [bVAG[t_], bWV], [bVS[s_]])
                yield
                for h in range(4):
                    klhs = KTMG[:, t_, h * 128:(h + 1) * 128]
                    mm(DCF[h], klhs, VS[s_][:, h, :], True, True, [bKTMG[t_], bVS[s_]], [bDCF[h]])
                    mm(ACCB[h], klhs, VS[s_][:, 4 + h, :], True, True, [bKTMG[t_], bVS[s_]], [bACCB[h]])
                yield
                dcf3 = PS[5][:, 0:387].rearrange("p (h d) -> p h d", d=129)
                acb3 = PS[7][:, 0:387].rearrange("p (h d) -> p h d", d=129)
                if kind == "ctx":
                    if t_ == 0:
                        cp("dve", CF[:, 0:3, :], dcf3, [PB[5]], [bCF])
                        cp("dve", CF[:, 3, :], DCF[3], [PB[6]], [bCF])
                        cp("dve", CCB[:, 0:3, :], acb3, [PB[7]], [bCCB])
                        cp("dve", CCB[:, 3, :], ACCB[3], [PB[6]], [bCCB])
                    else:
                        tt("dve", CF[:, 0:3, :], CF[:, 0:3, :], dcf3, ALU.add, [bCF, PB[5]], [bCF])
                        tt("dve", CF[:, 3, :], CF[:, 3, :], DCF[3], ALU.add, [bCF, PB[6]], [bCF])
                        tt("dve", CCB[:, 0:3, :], CCB[:, 0:3, :], acb3, ALU.add, [bCCB, PB[7]], [bCCB])
                        tt("dve", CCB[:, 3, :], CCB[:, 3, :], ACCB[3], ALU.add, [bCCB, PB[6]], [bCCB])
                else:
                    tt("dve", CF[:, :, :], CF[:, :, :], EXO[:, t_, 8:12, None].broadcast_to([128, 4, 129]), ALU.mult, [bCF, bEXO], [bCF])
                    tt("dve", CF[:, 0:3, :], CF[:, 0:3, :], dcf3, ALU.add, [bCF, PB[5]], [bCF])
                    tt("dve", CF[:, 3, :], CF[:, 3, :], DCF[3], ALU.add, [bCF, PB[6]], [bCF])
                    tt("dve", CBS[:, 0:3, :], CBS[:, 0:3, :], acb3, ALU.add, [bCBS, PB[7]], [bCBS])
                    tt("dve", CBS[:, 3, :], CBS[:, 3, :], ACCB[3], ALU.add, [bCBS, PB[6]], [bCBS])
                yield

        seq = [("ctx", 0)] + [("own", g) for g in range(min(OWN // 4, cut))]
        if cut > 4:
            seq += [("oth", g) for g in range(OWN // 4, ngroups)]
        prev = None
        for idx, (kind, gi) in enumerate(seq):
            gens = [front(gi, kind, idx % 2)]
            if prev is not None:
                gens.append(back(*prev))
            interleave(gens)
            prev = (gi, kind, idx % 2)
        interleave([back(*prev)])
        dump("cf_ctx", CF[:, :, :], [128, 4, 129], [bCF])
        act(EXO[:, 0, 0:4], PALL[:, 0, :], AF.Exp, [bPALL], [bEXO])
        for h in range(4):
            if ngroups > OWN // 4 and cut > 4:
                stt("dve", CB[:, h, :], CCB[:, h, :], EXO[:, 0, h:h + 1], CBS[:, h, :], ALU.mult, ALU.add, [bCCB, bEXO, bCBS], [bCB])
            else:
                cp("dve", CB[:, h, :], CCB[:, h, :], [bCCB], [bCB])
        dump("cf_in", CF[:, :, :], [128, 4, 129], [bCF])
        dump("cb_in", CB[:, :, :], [128, 4, 129], [bCB])
        dump("ktm0", KTMO[:, 0, :], [128, 512], [bKTMO[0]])
        dump("va0", VAO[:, 0, :, :], [128, 4, 129], [bVAO[0]])
        dump("owng", OWNG[:, :, :], [128, OWN, 24], [bOWNG])
        dump("qT", QT[:, :, 0:128], [128, 4, 128], [bQT])

        if stage <= 2:
            o_b = Buf("out")
            P.barrier()
            for i in range(OWN):
                t = P.dma("sp", out_d.ap()[i * 128:(i + 1) * 128, 0:512], POSC[:, 0:512], reads=[bPOSC], writes=[o_b])
                final.append((t[0], t[1]))
            P.barrier()
            with nc.Block() as block:
                P.emit(block, final)
            sts.close()
            stm.close()
            return nc

        P.barrier()
        sts.close()
        st6 = ExitStack()

        def sb6(name, shape, dt=F32):
            return st6.enter_context(nc.sbuf_tensor(name, list(shape), dt))

        HD = [sb6("HD%d" % i, [128, OWN, 512], BF16) for i in range(2)]
        bHD = [[Buf("HD%d_%d" % (i, c)) for c in range(OWN)] for i in range(2)]
        HS = [sb6("HS%d" % i, [128, 512]) for i in range(2)]
        bHS = [Buf("HS%d" % i) for i in range(2)]
        SM = [sb6("SM%d" % i, [128, 128], BF16) for i in range(8)]
        bSM = [Buf("SM%d" % i) for i in range(8)]
        VP = [sb6("VP%d" % i, [128, 129], BF16) for i in range(8)]
        bVP = [Buf("VP%d" % i) for i in range(8)]
        CSB = sb6("CSB", [128, 8, 129], BF16)
        bCSB = [Buf("CSB%d" % i) for i in range(8)]
        RD = [sb6("RD%d" % i, [128, 8]) for i in range(8)]
        bRD = [Buf("RD%d" % i) for i in range(8)]
        bST = [Buf("ST%d" % i) for i in range(8)]
        bHSD = Buf("hs_d")

        def chain(d, h):
            q = d * 4 + h
            ST = CF if d == 0 else CB
            bsrc = bCF if d == 0 else bCB
            hsl = slice(h * 128, (h + 1) * 128)
            cp("act", CSB[:, q, :], ST[:, h, :], [bsrc, bST[q]], [bCSB[q], bST[q]])
            yield
            order = range(OWN) if d == 0 else range(OWN - 1, -1, -1)
            for c in order:
                tsl = slice(c * 128, (c + 1) * 128)
                pS, pN, pC = PS[q][:, 0:128], PS[q][:, 128:257], PS[q][:, 257:386]
                mm(pS, KT[:, h, tsl], QT[:, h, tsl], True, True, [bKT, bQT], [PB[q]])
                ts("pool", VP[q][:, :], VAO[:, c, h, :], OWNG[:, c, q:q + 1], None, ALU.mult, None, [bVAO[c], bOWNG], [bVP[q]])
                yield
                tt("dve", SM[q][:, :], pS, TRIU if d == 0 else TRIL, ALU.mult, [PB[q], bCONST], [bSM[q]])
                yield
                mm(pN, SM[q][:, :], VP[q][:, :], True, False, [bSM[q], bVP[q]], [PB[q]])
                mm(pN, QT[:, h, tsl], CSB[:, q, :], False, True, [bQT, bCSB[q]], [PB[q]])
                mm(pC, KTMO[:, c, hsl], VP[q][:, :], True, True, [bKTMO[c], bVP[q]], [PB[q]])
                yield
                eq = OWNG[:, c, 8 + q:9 + q]
                r = RD[q]
                ts("dve", r[:, 0:1], PS[q][:, 256:257], eq, None, ALU.mult, None, [PB[q], bOWNG], [bRD[q]])
                stt("dve", r[:, 1:2], r[:, 0:1], -1.0, r[:, 0:1], ALU.mult, ALU.max, [bRD[q]], [bRD[q]])
                yield
                ts("dve", r[:, 2:3], r[:, 1:2], 1.0, None, ALU.max, None, [bRD[q]], [bRD[q]])
                P.op("dve", lambda e, o=r[:, 3:4], i_=r[:, 2:3]: e.reciprocal(out=o, in_=i_), [bRD[q]], [bRD[q]])
                yield
                tt("dve", r[:, 4:5], r[:, 3:4], eq, ALU.mult, [bRD[q], bOWNG], [bRD[q]])
                tt("dve", ST[:, h, :], ST[:, h, :], pC, ALU.add, [bST[q], PB[q]], [bST[q]])
                yield
                act(HD[d][:, c, hsl], PS[q][:, 128:256], AF.Copy, [PB[q], bRD[q]], [bHD[d][c]], scale=r[:, 4:5])
                ts("dve", ST[:, h, :], ST[:, h, :], OWNG[:, c, 16 + q:17 + q], None, ALU.mult, None, [bST[q], bOWNG], [bST[q]])
                yield
                cp("act", CSB[:, q, :], ST[:, h, :], [bST[q]], [bCSB[q]])
                yield

        interleave([chain(d, h) for d in range(2) for h in range(4)])
        for c in range(OWN):
            tt("pool", HS[c % 2][:, :], HD[0][:, c, :], HD[1][:, c, :], ALU.add, [bHD[0][c], bHD[1][c]], [bHS[c % 2]])
            P.dma("sp", hs_d.ap()[c * 128:(c + 1) * 128, :], HS[c % 2][:, :], reads=[bHS[c % 2]], writes=[bHSD])
            if c in (0, 7, 15):
                dump("hs%d" % c, HS[c % 2][:, :], [128, 512], [bHS[c % 2]])

        if stage <= 3:
            o_b = Buf("out")
            P.barrier()
            for i in range(OWN):
                t = P.dma("sp", out_d.ap()[i * 128:(i + 1) * 128, 0:512], POSC[:, 0:512], reads=[bPOSC], writes=[o_b])
                final.append((t[0], t[1]))
            P.barrier()
            with nc.Block() as block:
                P.emit(block, final)
            st6.close()
            stm.close()
            return nc

        P.barrier()
        st6.close()
        stm.close()

        H2T = sb("H2T", [128, 8, OWN * 128], BF16)
        bH2T = Buf("H2T")
        COMB = sb("COMB", [128, OWN, 16])
        bCOMB = Buf("COMB")
        st7 = ExitStack()

        def sb7(name, shape, dt=F32):
            return st7.enter_context(nc.sbuf_tensor(name, list(shape), dt))

        XCS = sb7("XCS", [128, 512, 2, 16], BF16)
        bXCS = Buf("XCS")
        CS128 = sb7("CS128", [128, 384], BF16)
        bCS = Buf("CS128")
        DI = sb7("DI", [128, 640])
        bDI = Buf("DI")
        P.dma("sp", DI[:, :], dftidx.ap(), writes=[bDI])
        CSF = sb7("CSF", [128, 384])
        bCSF = Buf("CSF")
        act(CSF[:, 0:256], DI[:, 0:256], AF.Sin, [bDI], [bCSF], scale=TWO_PI / 128.0)
        ts("dve", CSF[:, 256:384], CSF[:, 128:256], -1.0, None, ALU.mult, None, [bCSF], [bCSF])
        cp("dve", CS128[:, :], CSF[:, :], [bCSF], [bCS])
        CMY = sb7("CMY", [128, 48], BF16)
        bCMY = Buf("CMY")
        act(CSF[:, 0:32], DI[:, 512:544], AF.Sin, [bDI, bCS], [bCSF], scale=TWO_PI / 128.0)
        ts("dve", CSF[:, 32:48], CSF[:, 16:32], -1.0, None, ALU.mult, None, [bCSF], [bCSF])
        cp("dve", CMY[:, :], CSF[:, 0:48], [bCSF], [bCMY])
        with ExitStack() as st8:
            def sb8(name, shape, dt=F32):
                return st8.enter_context(nc.sbuf_tensor(name, list(shape), dt))
            TW = sb8("TW", [128, 256], BF16)
            bTW = Buf("TW")
            TWF = sb8("TWF", [128, 256])
            bTWF = Buf("TWF")
            act(TWF[:, :], DI[:, 256:512], AF.Sin, [bDI], [bTWF], scale=TWO_PI / 16384.0)
            ts("dve", TW[:, :], TWF[:, :], 1.0 / math.sqrt(16384.0 * 128.0), None, ALU.mult, None, [bTWF], [bTW])
            TC3 = TW[:, None, 0:128].broadcast_to([128, 8, 128])
            TS3 = TW[:, None, 128:256].broadcast_to([128, 8, 128])
            NW_ = 3
            UL = [sb8("UL%d" % i, [128, 16, 128], BF16) for i in range(3)]
            bUL = [Buf("UL%d" % i) for i in range(3)]
            YS = [sb8("YS%d" % i, [128, 8, 256], BF16) for i in range(NW_)]
            bYS = [Buf("YS%d" % i) for i in range(NW_)]
            MT_ = [[sb8("MTW%d_%d" % (j, i), [128, 8, 128], BF16) for i in range(4)] for j in range(NW_)]
            bMT_ = [[Buf("MTW%d_%d" % (j, i)) for i in range(4)] for j in range(NW_)]
            PQ = [sb8("PQ%d" % i, [128, 2, 8, 128], BF16) for i in range(NW_)]
            bPQ = [Buf("PQ%d" % i) for i in range(NW_)]

            def fft_half(hb):
                ub, half = hb // 2, hb % 2
                u_, bu_ = UL[ub % 3], bUL[ub % 3]
                w_ = hb % NW_
                if half == 0:
                    P.dma("sp", u_[:, :, :], bass.AP(u_d, ub * 16 * T, [[128, 128], [T, 16], [1, 128]]), reads=[bUD], writes=[bu_])
                    yield
                y_, by_ = YS[w_], bYS[w_]
                for pr in range(4):
                    pb = 1 + (hb * 4 + pr) % 4
                    for cc in range(2):
                        chl = half * 8 + pr * 2 + cc
                        mm(PS[pb][:, cc * 256:(cc + 1) * 256], u_[:, chl, :], CS128[:, 0:256], True, True, [bu_, bCS], [PB[pb]])
                    cp("act", y_[:, pr * 2:pr * 2 + 2, :], PS[pb][:, :].rearrange("p (c n) -> p c n", n=256), [PB[pb]], [by_])
                    yield
                yr = y_[:, :, 0:128]
                ys_ = y_[:, :, 128:256]
                pq, bpq = PQ[w_], bPQ[w_]
                m_, bm_ = MT_[w_], bMT_[w_]
                tt("dve", m_[0][:, :, :], yr, TC3, ALU.mult, [by_, bTW], [bm_[0]])
                tt("pool", m_[2][:, :, :], yr, TS3, ALU.mult, [by_, bTW], [bm_[2]])
                yield
                tt("dve", m_[1][:, :, :], ys_, TS3, ALU.mult, [by_, bTW], [bm_[1]])
                tt("pool", m_[3][:, :, :], ys_, TC3, ALU.mult, [by_, bTW], [bm_[3]])
                yield
                tt("dve", pq[:, 0, :, :], m_[0][:, :, :], m_[1][:, :, :], ALU.subtract, [bm_[0], bm_[1]], [bpq])
                yield
                tt("dve", pq[:, 1, :, :], m_[2][:, :, :], m_[3][:, :, :], ALU.add, [bm_[2], bm_[3]], [bpq])
                yield
                pb = 5 + hb % 3
                for cc in range(8):
                    o_c = PS[pb][:, cc * 32:cc * 32 + 16]
                    o_s = PS[pb][:, cc * 32 + 16:cc * 32 + 32]
                    mm(o_c, pq[:, 0, cc, :], CMY[:, 0:16], True, False, [bpq, bCMY], [PB[pb]])
                    mm(o_c, pq[:, 1, cc, :], CMY[:, 32:48], False, True, [bpq, bCMY], [PB[pb]])
                    mm(o_s, pq[:, 0, cc, :], CMY[:, 16:32], True, False, [bpq, bCMY], [PB[pb]])
                    mm(o_s, pq[:, 1, cc, :], CMY[:, 0:16], False, True, [bpq, bCMY], [PB[pb]])
                    if cc % 4 == 3:
                        yield
                cp("act", XCS[:, hb * 8:hb * 8 + 8, :, :], PS[pb][:, 0:256].rearrange("p (c s k) -> p c s k", s=2, k=16), [PB[pb]], [bXCS])
                yield

            def window(genfs, w):
                pend = list(genfs)
                act_ = []
                while pend or act_:
                    while pend and len(act_) < w:
                        act_.append(pend.pop(0)())
                    for g_ in list(act_):
                        try:
                            next(g_)
                        except StopIteration:
                            act_.remove(g_)

            window([(lambda hb=hb: fft_half(hb)) for hb in range(64)], NW_)
            P.barrier()
        dump("xcs", XCS[:, :, :, :], [128, 512, 2, 16], [bXCS])

        if stage <= 4:
            o_b = Buf("out")
            P.barrier()
            for i in range(OWN):
                t = P.dma("sp", out_d.ap()[i * 128:(i + 1) * 128, 0:512], POSC[:, 0:512], reads=[bPOSC], writes=[o_b])
                final.append((t[0], t[1]))
            P.barrier()
            with nc.Block() as block:
                P.emit(block, final)
            st7.close()
            return nc

        st9 = ExitStack()

        def sb9(name, shape, dt=F32):
            return st9.enter_context(nc.sbuf_tensor(name, list(shape), dt))

        WOB = sb9("WOB", [128, 8, D], BF16)
        bWOB = Buf("WOB")
        P.dma("pool", WOB[:, :, :], w_out.ap().rearrange("(k p) n -> p k n", p=128), writes=[bWOB])
        WFB = sb9("WFB", [128, 4, 128], BF16)
        bWFB = Buf("WFB")
        P.dma("pool", WFB[:, :, :], w_fourier.ap().rearrange("g c d -> c g d"), writes=[bWFB])
        NWSK = sb9("NWSK", [128, 2, 512])
        bNWSK = Buf("NWSK")
        for i in range(2):
            P.dma("sp", NWSK[:, i, :], bc_rows(nrm_skip, i, 512), writes=[bNWSK])
        WRF = sb9("WRF", [128, 8, 20])
        bWRF = Buf("WRF")
        P.dma("sp", WRF[:, :, :], w_router.ap().rearrange("(k p) n -> p k n", p=128), writes=[bWRF])
        WRH = sb9("WRH", [128, 8, 20], BF16)
        WRL = sb9("WRL", [128, 8, 20], BF16)
        bWR = Buf("WR")
        cp("dve", WRH[:, :, :], WRF[:, :, :], [bWRF], [bWR])
        tt("dve", WRL[:, :, :], WRF[:, :, :], WRH[:, :, :], ALU.subtract, [bWRF, bWR], [bWR])
        MODL = sb9("MODL", [128, 3, D])
        bMODL = Buf("MODL")
        for i_, off_ in enumerate((2 * D, 3 * D, 4 * D)):
            P.dma("sp", MODL[:, i_, :], bc_rows(mod_d, 0, D, off=off_), reads=[bMODD], writes=[bMODL])
        GP1, S2, G2 = MODL[:, 0, :], MODL[:, 1, :], MODL[:, 2, :]
        bMOD = bMODL
        BR = sb9("BR", [128, 20])
        bBR = Buf("BR")
        P.dma("sp", BR[:, :], bc_rows(b_router, 0, 20), writes=[bBR])
        HSt = [sb9("HSt%d" % i, [128, 512]) for i in range(2)]
        bHSt = [Buf("HSt%d" % i) for i in range(2)]
        AZ = [sb9("AZ%d" % i, [128, 2, 512], BF16) for i in range(2)]
        bAZ_ = [Buf("AZ%d" % i) for i in range(2)]
        SM__2 = [sb9("SM__%d" % i, [128, 32]) for i in range(2)]
        bSM__2 = [Buf("SM__%d" % i) for i in range(2)]
        CEN_2 = [sb9("CEN_%d" % i, [128, 512]) for i in range(2)]
        bCEN_2 = [Buf("CEN_%d" % i) for i in range(2)]
        SQ_2 = [sb9("SQ_%d" % i, [128, 512]) for i in range(2)]
        bSQ_2 = [Buf("SQ_%d" % i) for i in range(2)]
        T1_2 = [sb9("T1_%d" % i, [128, 512]) for i in range(2)]
        bT1_2 = [Buf("T1_%d" % i) for i in range(2)]
        T2_2 = [sb9("T2_%d" % i, [128, 512]) for i in range(2)]
        bT2_2 = [Buf("T2_%d" % i) for i in range(2)]
        MBF_2 = [sb9("MBF_%d" % i, [128, 512], BF16) for i in range(2)]
        bMBF_2 = [Buf("MBF_%d" % i) for i in range(2)]
        MTt_2 = [sb9("MTt_%d" % i, [128, 4, 128], BF16) for i in range(2)]
        bMTt_2 = [Buf("MTt_%d" % i) for i in range(2)]
        XT_2 = [sb9("XT_%d" % i, [128, 8, 128], BF16) for i in range(2)]
        bXT_2 = [Buf("XT_%d" % i) for i in range(2)]
        FTB_2 = [sb9("FTB_%d" % i, [128, 4, 128], BF16) for i in range(2)]
        bFTB_2 = [Buf("FTB_%d" % i) for i in range(2)]
        YFT_2 = [sb9("YFT_%d" % i, [128, 4, 128], BF16) for i in range(2)]
        bYFT_2 = [Buf("YFT_%d" % i) for i in range(2)]
        JK_2 = [sb9("JK_%d" % i, [128, D], BF16) for i in range(2)]
        bJK_2 = [Buf("JK_%d" % i) for i in range(2)]
        SY_2 = [sb9("SY_%d" % i, [128, 8]) for i in range(2)]
        bSY_2 = [Buf("SY_%d" % i) for i in range(2)]
        TT__2 = [sb9("TT__%d" % i, [128, D]) for i in range(2)]
        bTT_2 = [Buf("TT__%d" % i) for i in range(2)]
        H2_2 = [sb9("H2_%d" % i, [128, D]) for i in range(2)]
        bH2_2 = [Buf("H2_%d" % i) for i in range(2)]
        H2H_2 = [sb9("H2H_%d" % i, [128, D], BF16) for i in range(2)]
        bH2H_2 = [Buf("H2H_%d" % i) for i in range(2)]
        H2Lw_2 = [sb9("H2Lw_%d" % i, [128, D], BF16) for i in range(2)]
        bH2Lw_2 = [Buf("H2Lw_%d" % i) for i in range(2)]
        H2LT_2 = [sb9("H2LT_%d" % i, [128, 8, 128], BF16) for i in range(2)]
        bH2LT_2 = [Buf("H2LT_%d" % i) for i in range(2)]
        LG_2 = [sb9("LG_%d" % i, [128, 20]) for i in range(2)]
        bLG_2 = [Buf("LG_%d" % i) for i in range(2)]
        RT_2 = [sb9("RT_%d" % i, [128, 96]) for i in range(2)]
        bRT_2 = [Buf("RT_%d" % i) for i in range(2)]
        XR = [sb9("XR%d" % i, [128, D]) for i in range(2)]
        bXR = [Buf("XR%d" % i) for i in range(2)]
        PRr = [sb9("PRr%d" % i, [128, 512]) for i in range(2)]
        bPRr = [Buf("PRr%d" % i) for i in range(2)]
        X1 = [sb9("X1%d" % i, [128, D]) for i in range(2)]
        bX1 = [Buf("X1%d" % i) for i in range(2)]
        bOUT = Buf("out_d")
        BIG = 30000.0
        AX = mybir.AxisListType.X

        def red(eng, out, in_, op, reads, writes):
            return P.op(eng, lambda e: e.tensor_reduce(out=out, in_=in_, axis=AX, op=op), reads, writes)

        def s6_tile(c):
            s_ = c % 2
            rows = slice(c * 128, (c + 1) * 128)
            bk = (0, 1, 2, 3) if s_ == 0 else (4, 5, 6, 7)
            PTl = PS[bk[0]][:, :].bitcast(BF16).rearrange("p (k t) -> p k t", t=128)
            SM_, bSM_ = SM__2[s_], bSM__2[s_]
            CEN, bCEN = CEN_2[s_], bCEN_2[s_]
            SQ, bSQ = SQ_2[s_], bSQ_2[s_]
            T1, bT1 = T1_2[s_], bT1_2[s_]
            T2, bT2 = T2_2[s_], bT2_2[s_]
            MBF, bMBF = MBF_2[s_], bMBF_2[s_]
            MTt, bMTt = MTt_2[s_], bMTt_2[s_]
            XT, bXT = XT_2[s_], bXT_2[s_]
            FTB, bFTB = FTB_2[s_], bFTB_2[s_]
            YFT, bYFT = YFT_2[s_], bYFT_2[s_]
            JK, bJK = JK_2[s_], bJK_2[s_]
            SY, bSY = SY_2[s_], bSY_2[s_]
            TT_, bTT = TT__2[s_], bTT_2[s_]
            H2, bH2 = H2_2[s_], bH2_2[s_]
            H2H, bH2H = H2H_2[s_], bH2H_2[s_]
            H2Lw, bH2Lw = H2Lw_2[s_], bH2Lw_2[s_]
            H2LT, bH2LT = H2LT_2[s_], bH2LT_2[s_]
            LG, bLG = LG_2[s_], bLG_2[s_]
            RT, bRT = RT_2[s_], bRT_2[s_]
            hs, bhs, az, baz = HSt[s_], bHSt[s_], AZ[s_], bAZ_[s_]
            P.dma("sp", hs[:, :], hs_d.ap()[rows, :], reads=[bHSD], writes=[bhs])
            P.dma("sp", az[:, 0, :], az_d.ap()[0, rows, :], reads=[bAZ], writes=[baz])
            P.dma("sp", az[:, 1, :], az_d.ap()[1, rows, :], reads=[bAZ], writes=[baz])
            hs3 = hs[:, :].rearrange("p (h d) -> p h d", d=128)
            cen3 = CEN[:, :].rearrange("p (h d) -> p h d", d=128)
            sq3 = SQ[:, :].rearrange("p (h d) -> p h d", d=128)
            yield
            red("dve", SM_[:, 0:4], hs3, ALU.add, [bhs], [bSM_])
            ts("dve", SM_[:, 4:8], SM_[:, 0:4], 1.0 / 128.0, None, ALU.mult, None, [bSM_], [bSM_])
            tt("dve", cen3, hs3, SM_[:, 4:8, None].broadcast_to([128, 4, 128]), ALU.subtract, [bhs, bSM_], [bCEN])
            yield
            tt("pool", SQ[:, :], CEN[:, :], CEN[:, :], ALU.mult, [bCEN], [bSQ])
            red("dve", SM_[:, 8:12], sq3, ALU.add, [bSQ], [bSM_])
            yield
            ts("pool", SM_[:, 12:16], SM_[:, 8:12], 1.0 / 128.0, EPS, ALU.mult, ALU.add, [bSM_], [bSM_])
            tt("pool", SM_[:, 16:20], SM_[:, 12:16], NEGH.broadcast_to([128, 4]), ALU.pow, [bSM_, bTABS], [bSM_])
            tt("dve", cen3, cen3, SM_[:, 16:20, None].broadcast_to([128, 4, 128]), ALU.mult, [bCEN, bSM_], [bCEN])
            yield
            tt("dve", T1[:, :], CEN[:, :], NWSK[:, 0, :], ALU.mult, [bCEN, bNWSK], [bT1])
            tt("pool", T2[:, :], az[:, 0, :], NWSK[:, 1, :], ALU.mult, [baz, bNWSK], [bT2])
            tt("dve", T1[:, :], T1[:, :], T2[:, :], ALU.add, [bT1, bT2], [bT1])
            yield
            tt("dve", MBF[:, :], T1[:, :], az[:, 1, :], ALU.mult, [bT1, baz], [bMBF])
            if c == 0:
                dump("m0", MBF[:, :], [128, 512], [bMBF])
            yield
            for cc in range(4):
                tr(PTl[:, cc, :], MBF[:, cc * 128:(cc + 1) * 128], IDB[:, :], [bMBF, bIDB], [PB[bk[0]]])
            cp("act", MTt[:, :, :], PTl[:, 0:4, :], [PB[bk[0]]], [bMTt])
            yield
            for sg in range(2):
                for g in range(4):
                    tr(PTl[:, sg * 4 + g, :], XCS[:, g * 128:(g + 1) * 128, sg, c], IDB[:, :], [bXCS, bIDB], [PB[bk[0]]])
            cp("act", XT[:, :, :], PTl, [PB[bk[0]]], [bXT])
            yield
            for g in range(4):
                mm(PS[bk[1]][:, g * 128:(g + 1) * 128], CS128[:, 0:128], XT[:, g, :], True, False, [bCS, bXT], [PB[bk[1]]])
                mm(PS[bk[1]][:, g * 128:(g + 1) * 128], CS128[:, 256:384], XT[:, 4 + g, :], False, True, [bCS, bXT], [PB[bk[1]]])
            cp("dve", FTB[:, :, :], PS[bk[1]][:, :].rearrange("p (g t) -> p g t", t=128), [PB[bk[1]]], [bFTB])
            yield
            for g in range(4):
                mm(PS[bk[1]][:, g * 128:(g + 1) * 128], WFB[:, g, :], FTB[:, g, :], True, True, [bWFB, bFTB], [PB[bk[1]]])
            cp("act", YFT[:, :, :], PS[bk[1]][:, :].rearrange("p (g t) -> p g t", t=128), [PB[bk[1]]], [bYFT])
            yield
            for cb in range(2):
                for kc in range(4):
                    mm(PS[bk[2 + cb]][:, :], MTt[:, kc, :], WOB[:, kc, cb * 512:(cb + 1) * 512], kc == 0, False, [bMTt, bWOB], [PB[bk[2 + cb]]])
                for kc in range(4):
                    mm(PS[bk[2 + cb]][:, :], YFT[:, kc, :], WOB[:, 4 + kc, cb * 512:(cb + 1) * 512], False, kc == 3, [bYFT, bWOB], [PB[bk[2 + cb]]])
            yield
            act(JK[:, 0:512], PS[bk[2]][:, :], AF.Square, [PB[bk[2]]], [bJK, bSY], accum_out=SY[:, 0:1])
            act(JK[:, 512:1024], PS[bk[3]][:, :], AF.Square, [PB[bk[3]]], [bJK, bSY], accum_out=SY[:, 1:2])
            yield
            tt("pool", SY[:, 2:3], SY[:, 0:1], SY[:, 1:2], ALU.add, [bSY], [bSY])
            ts("pool", SY[:, 3:4], SY[:, 2:3], 1.0 / D, EPS, ALU.mult, ALU.add, [bSY], [bSY])
            tt("pool", SY[:, 4:5], SY[:, 3:4], NEGH, ALU.pow, [bSY, bTABS], [bSY])
            yield
            xr, bxr, pr_, bpr_ = XR[s_], bXR[s_], PRr[s_], bPRr[s_]
            P.dma("sp", xr[:, :], x_rot.ap()[rows, :], writes=[bxr])
            P.dma("sp", pr_[0:64, :], bc_rows(posr_d, 2 * c, 512, parts=64), reads=[bPOSRD], writes=[bpr_])
            P.dma("sp", pr_[64:128, :], bc_rows(posr_d, 2 * c + 1, 512, parts=64), reads=[bPOSRD], writes=[bpr_])
            tt("pool", xr[:, 0:512], xr[:, 0:512], pr_[:, :], ALU.add, [bxr, bpr_], [bxr])
            tt("pool", xr[:, 512:1024], xr[:, 512:1024], POSC[:, :], ALU.add, [bxr, bPOSC], [bxr])
            yield
            stt("dve", TT_[:, 0:512], PS[bk[2]][:, :], SY[:, 4:5], GP1[:, 0:512], ALU.mult, ALU.mult, [PB[bk[2]], bSY, bMOD], [bTT])
            stt("dve", TT_[:, 512:1024], PS[bk[3]][:, :], SY[:, 4:5], GP1[:, 512:1024], ALU.mult, ALU.mult, [PB[bk[3]], bSY, bMOD], [bTT])
            yield
            x1, bx1 = X1[s_], bX1[s_]
            tt("pool", x1[:, :], TT_[:, :], xr[:, :], ALU.add, [bTT, bxr], [bx1])
            P.dma("sp", out_d.ap()[rows, :], x1[:, :], reads=[bx1], writes=[bOUT])
            if c == 0:
                dump("x1_0", x1[:, :], [128, D], [bx1])
            yield
            act(JK[:, :], x1[:, :], AF.Square, [bx1], [bJK, bSY], accum_out=SY[:, 5:6])
            yield
            ts("pool", SY[:, 6:7], SY[:, 5:6], 1.0 / D, EPS, ALU.mult, ALU.add, [bSY], [bSY])
            tt("pool", SY[:, 7:8], SY[:, 6:7], NEGH, ALU.pow, [bSY, bTABS], [bSY])
            yield
            stt("dve", TT_[:, :], x1[:, :], SY[:, 7:8], G2, ALU.mult, ALU.mult, [bx1, bSY, bMOD], [bTT])
            tt("pool", H2[:, :], TT_[:, :], S2, ALU.add, [bTT, bMOD], [bH2])
            yield
            cp("act", H2H[:, :], H2[:, :], [bH2], [bH2H])
            tt("dve", H2Lw[:, :], H2[:, :], H2H[:, :], ALU.subtract, [bH2, bH2H], [bH2Lw])
            yield
            for kc in range(8):
                tr(PTl[:, kc, :], H2H[:, kc * 128:(kc + 1) * 128], IDB[:, :], [bH2H, bIDB], [PB[bk[0]]])
            cp("act", H2T[:, :, rows], PTl, [PB[bk[0]]], [bH2T])
            yield
            for kc in range(8):
                tr(PTl[:, kc, :], H2Lw[:, kc * 128:(kc + 1) * 128], IDB[:, :], [bH2Lw, bIDB], [PB[bk[0]]])
            cp("dve", H2LT[:, :, :], PTl, [PB[bk[0]]], [bH2LT])
            yield
            LGp = PS[bk[1]][:, 0:20]
            for kc in range(8):
                mm(LGp, H2T[:, kc, rows], WRH[:, kc, :], kc == 0, False, [bH2T, bWR], [PB[bk[1]]])
            for kc in range(8):
                mm(LGp, H2T[:, kc, rows], WRL[:, kc, :], False, False, [bH2T, bWR], [PB[bk[1]]])
            for kc in range(8):
                mm(LGp, H2LT[:, kc, :], WRH[:, kc, :], False, kc == 7, [bH2LT, bWR], [PB[bk[1]]])
            yield
            tt("dve", LG[:, :], LGp, BR[:, :], ALU.add, [PB[bk[1]], bBR], [bLG])
            if c == 0:
                dump("lg0", LG[:, :], [128, 20], [bLG])
            R = RT
            bR = bRT
            yield
            red("dve", R[:, 0:1], LG[:, 0:4], ALU.max, [bLG], [bR])
            ts("dve", R[:, 1:5], LG[:, 0:4], R[:, 0:1], None, ALU.is_equal, None, [bLG, bR], [bR])
            ts("dve", R[:, 5:6], R[:, 0:1], -1.0, None, ALU.mult, None, [bR], [bR])
            yield
            act(R[:, 6:10], LG[:, 0:4], AF.Exp, [bLG, bR], [bR], bias=R[:, 5:6], accum_out=R[:, 10:11])
            P.op("dve", lambda e, o=R[:, 11:12], i_=R[:, 10:11]: e.reciprocal(out=o, in_=i_), [bR], [bR])
            yield
            ts("dve", R[:, 12:16], R[:, 1:5], BIG, -BIG, ALU.mult, ALU.add, [bR], [bR])
            em = R[:, 16:32]
            tt("dve", em.rearrange("p (g j) -> p g j", j=4), LG[:, 4:20].rearrange("p (g j) -> p g j", j=4),
               R[:, 12:16, None].broadcast_to([128, 4, 4]), ALU.add, [bLG, bR], [bR])
            yield
            red("dve", R[:, 32:33], em, ALU.max, [bR], [bR])
            ts("dve", R[:, 48:64], em, R[:, 32:33], None, ALU.is_equal, None, [bR], [bR])
            stt("dve", R[:, 64:80], R[:, 48:64], -BIG, em, ALU.mult, ALU.add, [bR], [bR])
            yield
            red("dve", R[:, 33:34], R[:, 64:80], ALU.max, [bR], [bR])
            ts("dve", R[:, 80:96], R[:, 64:80], R[:, 33:34], None, ALU.is_equal, None, [bR], [bR])
            tt("dve", R[:, 34:35], R[:, 33:34], R[:, 32:33], ALU.subtract, [bR], [bR])
            yield
            act(R[:, 35:36], R[:, 34:35], AF.Exp, [bR], [bR])
            ts("dve", R[:, 36:37], R[:, 35:36], 1.0, None, ALU.add, None, [bR], [bR])
            P.op("dve", lambda e, o=R[:, 37:38], i_=R[:, 36:37]: e.reciprocal(out=o, in_=i_), [bR], [bR])
            tt("dve", R[:, 38:39], R[:, 37:38], R[:, 35:36], ALU.mult, [bR], [bR])
            yield
            tt("dve", R[:, 39:40], R[:, 37:38], R[:, 11:12], ALU.mult, [bR], [bR])
            tt("dve", R[:, 40:41], R[:, 38:39], R[:, 11:12], ALU.mult, [bR], [bR])
            ts("dve", COMB[:, c, :], R[:, 48:64], R[:, 39:40], None, ALU.mult, None, [bR], [bCOMB])
            stt("dve", COMB[:, c, :], R[:, 80:96], R[:, 40:41], COMB[:, c, :], ALU.mult, ALU.add, [bR, bCOMB], [bCOMB])
            yield

        def window6(genfs, w):
            pend = list(genfs)
            act_ = []
            while pend or act_:
                while pend and len(act_) < w:
                    act_.append(pend.pop(0)())
                for g_ in list(act_):
                    try:
                        next(g_)
                    except StopIteration:
                        act_.remove(g_)

        window6([(lambda c=c: s6_tile(c)) for c in range(OWN)], 2)
        dump("comb", COMB[:, :, :], [128, OWN, 16], [bCOMB])

        if stage <= 5:
            o_b = bOUT
            P.barrier()
            final.append((P.sem["sp"], 0))
            P.barrier()
            with nc.Block() as block:
                P.emit(block, [])
            st9.close()
            st7.close()
            return nc

        P.barrier()
        st9.close()
        st7.close()
        stE = ExitStack()

        def sbE(name, shape, dt=F32):
            return stE.enter_context(nc.sbuf_tensor(name, list(shape), dt))

        YACC = sbE("YACC", [128, OWN, D])
        bYACC = [Buf("YACC%d" % i) for i in range(OWN)]
        WG = [sbE("WG%d" % i, [128, 8, 512], BF16) for i in range(2)]
        WU = [sbE("WU%d" % i, [128, 8, 512], BF16) for i in range(2)]
        WD = [sbE("WD%d" % i, [128, 4, D], BF16) for i in range(2)]
        bWG = [Buf("WG%d" % i) for i in range(2)]
        bWU = [Buf("WU%d" % i) for i in range(2)]
        bWD = [Buf("WD%d" % i) for i in range(2)]
        ATb = [sbE("AT%d" % i, [128, 4, 512], BF16) for i in range(2)]
        bAT = [Buf("AT%d" % i) for i in range(2)]
        SG = [sbE("SG%d" % i, [128, 512]) for i in range(2)]
        bSG = [Buf("SG%d" % i) for i in range(2)]
        NEXP = 16
        gu_ctr = [0]
        dn_ctr = [0]
        for e_ in range(NEXP):
            s_ = e_ % 2
            P.dma("pool", WG[s_][:, :, :], w_gate.ap()[e_].rearrange("(k p) n -> p k n", p=128), writes=[bWG[s_]])
            P.dma("pool", WU[s_][:, :, :], w_up.ap()[e_].rearrange("(k p) n -> p k n", p=128), writes=[bWU[s_]])
            P.dma("pool", WD[s_][:, :, :], w_down.ap()[e_].rearrange("(k p) n -> p k n", p=128), writes=[bWD[s_]])
            for tb in range(OWN // 4):
                tsl = slice(tb * 512, (tb + 1) * 512)
                a_ = (e_ * 4 + tb) % 2
                for fb in range(4):
                    k_ = gu_ctr[0] % 2
                    gu_ctr[0] += 1
                    pg, pu = 2 * k_, 2 * k_ + 1
                    for kc in range(8):
                        mm(PS[pg][:, :], WG[s_][:, kc, fb * 128:(fb + 1) * 128], H2T[:, kc, tsl], kc == 0, kc == 7, [bWG[s_], bH2T], [PB[pg]])
                    for kc in range(8):
                        mm(PS[pu][:, :], WU[s_][:, kc, fb * 128:(fb + 1) * 128], H2T[:, kc, tsl], kc == 0, kc == 7, [bWU[s_], bH2T], [PB[pu]])
                    act(SG[k_][:, :], PS[pg][:, :], AF.Silu, [PB[pg]], [bSG[k_]])
                    tt("dve", ATb[a_][:, fb, :], SG[k_][:, :], PS[pu][:, :], ALU.mult, [bSG[k_], PB[pu]], [bAT[a_]])
                for t_ in range(4):
                    tile = tb * 4 + t_
                    for cb in range(2):
                        pd = 4 + dn_ctr[0] % 4
                        dn_ctr[0] += 1
                        for fb in range(4):
                            mm(PS[pd][:, :], ATb[a_][:, fb, t_ * 128:(t_ + 1) * 128], WD[s_][:, fb, cb * 512:(cb + 1) * 512], fb == 0, fb == 3, [bAT[a_], bWD[s_]], [PB[pd]])
                        ya = YACC[:, tile, cb * 512:(cb + 1) * 512]
                        if e_ == 0:
                            ts("dve", ya, PS[pd][:, :], COMB[:, tile, e_:e_ + 1], None, ALU.mult, None, [PB[pd], bCOMB], [bYACC[tile]])
                        else:
                            stt("dve", ya, PS[pd][:, :], COMB[:, tile, e_:e_ + 1], ya, ALU.mult, ALU.add, [PB[pd], bCOMB, bYACC[tile]], [bYACC[tile]])
        GP2L = sbE("GP2L", [128, D])
        bGP2L = Buf("GP2L")
        P.dma("sp", GP2L[:, :], bc_rows(mod_d, 0, D, off=5 * D), reads=[bMODD], writes=[bGP2L])
        GP2 = GP2L[:, :]
        bMOD = bGP2L
        XF = [sbE("XF%d" % i, [128, D]) for i in range(2)]
        bXF = [Buf("XF%d" % i) for i in range(2)]
        JK2 = sbE("JK2", [128, D], BF16)
        bJK2 = Buf("JK2")
        SF = sbE("SF", [128, 8])
        bSF = Buf("SF")
        OT = [sbE("OT%d" % i, [128, D]) for i in range(2)]
        bOT = [Buf("OT%d" % i) for i in range(2)]
        for c in range(OWN):
            s_ = c % 2
            rows = slice(c * 128, (c + 1) * 128)
            P.dma("sp", XF[s_][:, :], out_d.ap()[rows, :], reads=[bOUT], writes=[bXF[s_]])
            q_ = SF[:, s_ * 4:s_ * 4 + 4]
            act(JK2[:, :], YACC[:, c, :], AF.Square, [bYACC[c]], [bJK2, bSF], accum_out=q_[:, 0:1])
            ts("pool", q_[:, 1:2], q_[:, 0:1], 1.0 / D, EPS, ALU.mult, ALU.add, [bSF], [bSF])
            tt("pool", q_[:, 2:3], q_[:, 1:2], NEGH, ALU.pow, [bSF, bTABS], [bSF])
            stt("dve", OT[s_][:, :], YACC[:, c, :], q_[:, 2:3], GP2, ALU.mult, ALU.mult, [bYACC[c], bSF, bMOD], [bOT[s_]])
            tt("pool", OT[s_][:, :], OT[s_][:, :], XF[s_][:, :], ALU.add, [bOT[s_], bXF[s_]], [bOT[s_]])
            t = P.dma("sp", out_d.ap()[rows, :], OT[s_][:, :], reads=[bOT[s_], bXF[s_]], writes=[bOUT])
            final.append((t[0], t[1]))
        P.barrier()
        with nc.Block() as block:
            P.emit(block, final)
        stE.close()
    return nc


def _centered(idx, n):
    return ((idx + n // 2) % n) - n // 2


def make_inputs(core, x, c, ctx, c_ctx, w_ada, b_ada, g_pre_mix, g_post_mix, g_pre_ffn, g_post_ffn,
                w_in, conv_w, conv_b, w_q, w_k, w_v, w_if_fwd, b_if_fwd, w_if_bwd, b_if_bwd,
                mlstm_norm_w, mlstm_skip, w_fourier, w_out, w_router_group, b_router_group,
                w_router_expert, b_router_expert, w_gate, w_up, w_down, shared):
    f32 = np.float32
    j = core
    xs = x[0]
    m = {}
    m["x_rot"] = np.ascontiguousarray(np.roll(xs, -2048 * j, axis=0))
    halo = np.zeros((128, D), f32)
    hmask = np.zeros(64, f32)
    hrow = np.zeros(128, f32)
    hcol = np.zeros(128, f32)
    for g in range(NG):
        for side, tr_ in ((0, 512 * g - 1), (1, 512 * g + 512)):
            true_t = (tr_ % T + 2048 * j) % T
            own_first_true = ((512 * g) % T + 2048 * j) % T
            if side == 0:
                valid = own_first_true != 0
            else:
                valid = ((512 * g + 511) % T + 2048 * j) % T != T - 1
            halo[2 * g + side] = xs[true_t]
            hmask[2 * g + side] = 1.0 if valid else 0.0
            hrow[2 * g + side] = true_t // 64
            hcol[2 * g + side] = true_t % 64
    m["x_halo"] = halo
    tabs = np.zeros((128, 1024), f32)
    p = np.arange(128)
    for a in range(2):
        tabs[:, a] = ((2 * p + a) + 32 * j) % 256
    tabs[:, 2] = p % 64
    tabs[:, 3] = hrow
    tabs[:, 4] = hcol
    tabs[:, 5] = -0.5
    i = np.arange(128)
    true_c = (i + 16 * j) % 128
    tabs[:, 128:256] = (true_c < 16 * j).astype(f32)[None, :]
    tabs[:, 256:384] = (true_c >= 16 * j + 16).astype(f32)[None, :]
    tabs[:, 384:448] = hmask[None, :]
    tabs[:, 512:768] = np.arange(256, dtype=f32)[None, :]
    m["tabs"] = tabs
    di = np.zeros((128, 640), f32)
    n = np.arange(128)[:, None]
    k = np.arange(128)[None, :]
    di[:, 0:128] = _centered(n * k + 32, 128)
    di[:, 128:256] = _centered(n * k, 128)
    base = n * k + 2048 * j * k
    di[:, 256:384] = _centered(base + 4096, 16384)
    di[:, 384:512] = _centered(base, 16384)
    cc = (16 * j + np.arange(16))[None, :]
    di[:, 512:528] = _centered(n * cc + 32, 128)
    di[:, 528:544] = _centered(n * cc, 128)
    m["dftidx"] = di
    m.update(shared)
    return m


def make_shared(x, c, ctx, c_ctx, w_ada, b_ada, g_pre_mix, g_post_mix, g_pre_ffn, g_post_ffn,
                w_in, conv_w, conv_b, w_q, w_k, w_v, w_if_fwd, b_if_fwd, w_if_bwd, b_if_bwd,
                mlstm_norm_w, mlstm_skip, w_fourier, w_out, w_router_group, b_router_group,
                w_router_expert, b_router_expert, w_gate, w_up, w_down):
    f32 = np.float32
    s = {}
    s["ctx"] = np.ascontiguousarray(ctx[0])
    cT = np.zeros((128, 16), f32)
    cT[:, 0:8] = c[0].reshape(8, 128).T
    cT[:, 8:16] = c_ctx.reshape(8, 128).T
    s["cT"] = cT
    s["w_ada"] = np.ascontiguousarray(w_ada[0])
    s["b_ada"] = np.ascontiguousarray(b_ada[0][None, :])
    s["gains"] = np.stack([g_pre_mix[0], g_post_mix[0], g_pre_ffn[0], g_post_ffn[0]]).astype(f32)
    s["w_in"] = np.ascontiguousarray(w_in[0])
    cw = np.zeros((128, 16), f32)
    for cc in range(4):
        for k in range(3):
            cw[:, cc * 3 + k] = conv_w[0][k, cc * 128:(cc + 1) * 128]
        cw[:, 12 + cc] = conv_b[0][cc * 128:(cc + 1) * 128]
    s["convw"] = cw
    s["w_qkv"] = np.stack([w_q[0].reshape(512, 4), w_k[0].reshape(512, 4), w_v[0].reshape(512, 4)]).astype(f32)
    s["w_qkvT"] = np.stack([np.ascontiguousarray(w.transpose(0, 2, 1)).reshape(512, 4) for w in (w_q[0], w_k[0], w_v[0])]).astype(f32)
    wf, wb = w_if_fwd[0], w_if_bwd[0]
    s["w_if"] = np.ascontiguousarray(np.concatenate([wf[:, 0:4], wb[:, 0:4], wf[:, 4:8], wb[:, 4:8]], axis=1))
    bf, bb = b_if_fwd[0], b_if_bwd[0]
    s["b_if"] = np.concatenate([bf[0:4], bb[0:4], bf[4:8], bb[4:8]])[None, :].astype(f32)
    s["nrm_skip"] = np.stack([mlstm_norm_w[0], mlstm_skip[0]]).astype(f32)
    s["w_fourier"] = np.ascontiguousarray(w_fourier[0])
    s["w_out"] = np.ascontiguousarray(w_out[0])
    s["w_router"] = np.ascontiguousarray(np.concatenate([w_router_group[0], w_router_expert[0]], axis=1))
    s["b_router"] = np.concatenate([b_router_group[0], b_router_expert[0]])[None, :].astype(f32)
    s["w_gate"] = np.ascontiguousarray(w_gate[0])
    s["w_up"] = np.ascontiguousarray(w_up[0])
    s["w_down"] = np.ascontiguousarray(w_down[0])
    cst = np.zeros((128, 640), f32)
    cst[:, 0:128] = np.eye(128)
    a = np.arange(128)
    cst[:, 128:256] = (a[:, None] <= a[None, :])
    cst[:, 256:384] = (a[:, None] >= a[None, :])
    cst[:, 384:512] = 1.0
    cst[:, 512:640] = (a[:, None] // 4 == a[None, :] // 4)
    s["consts"] = cst
    return s


_CACHE = {}


def kernel(**inputs):
    inputs = {k: np.asarray(v) for k, v in inputs.items()}
    if "nc" not in _CACHE:
        _CACHE["nc"] = build()
    nc = _CACHE["nc"]
    shared = make_shared(**inputs)
    in_maps = [make_inputs(core, shared=shared, **inputs) for core in range(NCORES)]
    res = run_bass_kernel_spmd(nc, in_maps, core_ids=list(range(NCORES)))
    out = np.concatenate([res.results[i]["out"] for i in range(NCORES)], axis=0)
    return out.reshape(1, T, D).astype(np.float32)
```

```python
import math
from contextlib import ExitStack
import numpy as np
import concourse.bass as bass
import concourse.mybir as mybir
from concourse.bass_utils import run_bass_kernel_spmd

F32 = mybir.dt.float32
BF16 = mybir.dt.bfloat16
I32 = mybir.dt.int32
AF = mybir.ActivationFunctionType
ALU = mybir.AluOpType

NCORES = 8
T = 16384
D = 1024
NT = T // 128
OWN = NT // NCORES
NG = NT // 4
EPS = 1e-6
TWO_PI = 2.0 * math.pi
NHALO = 2 * NG
DBG = {}


class Buf:
    __slots__ = ("name", "w", "r")

    def __init__(self, name):
        self.name = name
        self.w = None
        self.r = {}


class Prog:
    def __init__(self, nc, stack, ndma=28):
        self.nc = nc
        self.engs = {"pe": nc.tensor, "act": nc.scalar, "dve": nc.vector, "pool": nc.gpsimd, "sp": nc.sync}
        self.ops = {k: [] for k in self.engs}
        self.sem = {k: stack.enter_context(nc.semaphore("s_" + k)) for k in self.engs}
        self.cnt = {k: 0 for k in self.engs}
        self.waited = {k: {} for k in self.engs}
        self.dsem = [stack.enter_context(nc.semaphore("d%d" % i)) for i in range(ndma)]
        self.dcnt = [0] * ndma
        self.ring = {"sp": list(range(0, ndma - 8)), "act": list(range(0, ndma - 8)), "pool": list(range(ndma - 8, ndma))}
        self.rpos = {"sp": 0, "pool": 0}

    def _collect(self, e, reads, writes, sync_same=True):
        waits = {}

        def need(tok):
            if tok is None:
                return
            sem, val, eng = tok
            if eng == e and not sync_same:
                return
            key = id(sem)
            if self.waited[e].get(key, 0) >= val:
                return
            if key not in waits or waits[key][1] < val:
                waits[key] = (sem, val)

        for b in reads:
            need(b.w)
        for b in writes:
            need(b.w)
            for t in b.r.values():
                need(t)
        for key, (sem, val) in waits.items():
            self.waited[e][key] = val
        return list(waits.values())

    def _commit(self, tok, reads, writes):
        key = id(tok[0])
        for b in reads:
            old = b.r.get(key)
            if old is None or old[1] < tok[1]:
                b.r[key] = tok
        for b in writes:
            b.w = tok
            b.r = {}

    def op(self, e, fn, reads=(), writes=(), sync_same=True):
        waits = self._collect(e, reads, writes, sync_same)
        self.cnt[e] += 1
        tok = (self.sem[e], self.cnt[e], e)
        self.ops[e].append((waits, fn, (self.sem[e], 1)))
        self._commit(tok, reads, writes)
        return tok

    def dma(self, e, out, in_, reads=(), writes=()):
        rk = "pool" if e == "pool" else "sp"
        ring = self.ring[rk]
        i = ring[self.rpos[rk] % len(ring)]
        self.rpos[rk] += 1
        sem = self.dsem[i]
        waits = self._collect(e, reads, writes)
        prev = self.dcnt[i] * 16
        if prev > 0 and self.waited[e].get(id(sem), 0) < prev:
            waits.append((sem, prev))
            self.waited[e][id(sem)] = prev
        self.dcnt[i] += 1
        tok = (sem, self.dcnt[i] * 16, "dma")
        self.ops[e].append((waits, (lambda eng, o=out, i_=in_: eng.dma_start(out=o, in_=i_)), (sem, 16)))
        self._commit(tok, reads, writes)
        return tok

    def barrier(self):
        toks = [(self.sem[k], self.cnt[k]) for k in self.engs if self.cnt[k] > 0]
        toks += [(self.dsem[i], self.dcnt[i] * 16) for i in range(len(self.dsem)) if self.dcnt[i] > 0]
        for e in self.engs:
            waits = []
            for sem, val in toks:
                if self.waited[e].get(id(sem), 0) < val:
                    waits.append((sem, val))
                    self.waited[e][id(sem)] = val
            if waits:
                self.ops[e].append((waits, None, None))

    def emit(self, block, final_waits):
        def replay(e):
            def body(eng):
                for waits, fn, inc in self.ops[e]:
                    for sem, val in waits:
                        eng.wait_ge(sem, val)
                    if fn is not None:
                        ins = fn(eng)
                        ins.then_inc(inc[0], inc[1])
                if e == "sp":
                    for sem, val in final_waits:
                        eng.wait_ge(sem, val)
            return body

        block.tensor(replay("pe"))
        block.scalar(replay("act"))
        block.vector(replay("dve"))
        block.gpsimd(replay("pool"))
        block.sync(replay("sp"))


def build(stage=99, dbg=False, ngroups=NG, cut=99):
    nc = bass.Bass("TRN2", target_bir_lowering=False)
    DBG.clear()

    def din(name, shape, dt=F32):
        return nc.dram_tensor(name, list(shape), dt, kind="ExternalInput")

    x_rot = din("x_rot", [ngroups * 512, D])
    x_halo = din("x_halo", [128, D])
    ctx_in = din("ctx", [256, D])
    cT = din("cT", [128, 16])
    w_ada = din("w_ada", [D, 6 * D])
    b_ada = din("b_ada", [1, 6 * D])
    gains = din("gains", [4, D])
    w_in = din("w_in", [D, 1536])
    convw = din("convw", [128, 16])
    w_qkv = din("w_qkv", [3, 512, 4])
    w_qkvT = din("w_qkvT", [3, 512, 4])
    w_if = din("w_if", [1536, 16])
    b_if = din("b_if", [1, 16])
    nrm_skip = din("nrm_skip", [2, 512])
    w_fourier = din("w_fourier", [4, 128, 128])
    w_out = din("w_out", [D, D])
    w_router = din("w_router", [D, 20])
    b_router = din("b_router", [1, 20])
    moe_small = stage < 8
    w_gate = din("w_gate", [16, D, 512] if not moe_small else [1, 8, 8])
    w_up = din("w_up", [16, D, 512] if not moe_small else [1, 8, 8])
    w_down = din("w_down", [16, 512, D] if not moe_small else [1, 8, 8])
    consts = din("consts", [128, 5 * 128])
    tabs = din("tabs", [128, 1024])
    dftidx = din("dftidx", [128, 5 * 128])
    out_d = nc.dram_tensor("out", [OWN * 128, D], F32, kind="ExternalOutput")

    posr_d = nc.dram_tensor("posr_d", [256, 512], F32)
    u_d = nc.dram_tensor("u_d", [512, T], BF16)
    az_d = nc.dram_tensor("az_d", [2, OWN * 128, 512], BF16)
    hs_d = nc.dram_tensor("hs_d", [OWN * 128, 512], F32)
    mod_d = nc.dram_tensor("mod_d", [1, 6 * D], F32)

    dbg_outs = {}

    def dbg_out(name, shape):
        DBG[name] = tuple(shape)
        dbg_outs[name] = nc.dram_tensor("dbg_" + name, list(shape), F32, kind="ExternalOutput")
        return dbg_outs[name]

    stack = ExitStack()
    with stack:
        P = Prog(nc, stack)
        used = [0]

        def sb(name, shape, dt=F32):
            t = stack.enter_context(nc.sbuf_tensor(name, list(shape), dt))
            return t

        def psum(name, shape, dt=F32):
            return stack.enter_context(nc.psum_tensor(name, list(shape), dt))

        def act(out, in_, func, reads, writes, eng="act", **kw):
            return P.op("act", lambda e: e.activation(out=out, in_=in_, func=func, **kw), reads, writes)

        def tt(eng, out, in0, in1, op, reads, writes):
            return P.op(eng, lambda e: e.tensor_tensor(out=out, in0=in0, in1=in1, op=op), reads, writes)

        def ts(eng, out, in0, s1, s2, op0, op1, reads, writes):
            if op1 is None:
                return P.op(eng, lambda e: e.tensor_scalar(out=out, in0=in0, scalar1=s1, scalar2=None, op0=op0), reads, writes)
            return P.op(eng, lambda e: e.tensor_scalar(out=out, in0=in0, scalar1=s1, scalar2=s2, op0=op0, op1=op1), reads, writes)

        def stt(eng, out, in0, scalar, in1, op0, op1, reads, writes):
            return P.op(eng, lambda e: e.scalar_tensor_tensor(out=out, in0=in0, scalar=scalar, in1=in1, op0=op0, op1=op1), reads, writes)

        def cp(eng, out, in_, reads, writes):
            if eng == "act":
                return P.op("act", lambda e: e.activation(out=out, in_=in_, func=AF.Copy), reads, writes)
            return P.op(eng, lambda e: e.tensor_copy(out=out, in_=in_), reads, writes)

        def mm(out, lhsT, rhs, start, stop, reads, writes):
            return P.op("pe", lambda e: e.matmul(out, lhsT=lhsT, rhs=rhs, start=start, stop=stop), reads, writes, sync_same=False)

        def tr(out, in_, ident, reads, writes):
            return P.op("pe", lambda e: e.transpose(out=out, in_=in_, identity=ident), reads, writes, sync_same=False)

        def memset(eng, ap, val, writes):
            return P.op(eng, lambda e: e.memset(ap, val), (), writes)

        def dump(name, ap, shape, reads):
            if not dbg:
                return
            dd = dbg_out(name, shape)
            P.dma("pool", dd.ap(), ap, reads=reads, writes=[Buf("dbg")])

        def bc_rows(dram_t, row, n, parts=128, off=0):
            width = dram_t.shape[-1]
            return bass.AP(dram_t, row * width + off, [[0, parts], [1, n]])

        PS = [psum("ps%d" % i, [128, 512]) for i in range(8)]
        PB = [Buf("ps%d" % i) for i in range(8)]

        CONST = sb("CONST", [128, 640])
        bCONST = Buf("CONST")
        P.dma("sp", CONST[:, :], consts.ap(), writes=[bCONST])
        IDF = CONST[:, 0:128]
        TRIU = CONST[:, 128:256]
        TRIL = CONST[:, 256:384]
        ONES = CONST[:, 384:512]
        BDM = CONST[:, 512:640]
        IDB = sb("IDB", [128, 128], BF16)
        bIDB = Buf("IDB")
        cp("dve", IDB[:, :], IDF, [bCONST], [bIDB])
        TABS = sb("TABS", [128, 1024])
        bTABS = Buf("TABS")
        P.dma("sp", TABS[:, :], tabs.ap(), writes=[bTABS])
        NEGH = TABS[:, 5:6]
        POSC = sb("POSC", [128, 512])
        bPOSC = Buf("POSC")

        final = []
        stm = ExitStack()

        def sbm(name, shape, dt=F32):
            return stm.enter_context(nc.sbuf_tensor(name, list(shape), dt))

        QTF = sbm("QTF", [128, 4 * OWN * 128], BF16)
        QT = QTF[:, :].rearrange("p (c t) -> p c t", c=4)
        bQT = Buf("QT")
        KTF = sbm("KTF", [128, 4 * OWN * 128], BF16)
        KT = KTF[:, :].rearrange("p (c t) -> p c t", c=4)
        bKT = Buf("KT")
        WINC = KTF[:, 0:4096].rearrange("p (k n) -> p k n", n=512)
        bWINC = bKT
        KTMO = sbm("KTMO", [128, OWN, 512], BF16)
        bKTMO = [Buf("KTMO%d" % i) for i in range(OWN)]
        VAO = sbm("VAO", [128, OWN, 4, 129], BF16)
        bVAO = [Buf("VAO%d" % i) for i in range(OWN)]
        OWNG = sbm("OWNG", [128, OWN, 24])
        bOWNG = Buf("OWNG")
        CF = sbm("CF", [128, 4, 129])
        bCF = Buf("CF")
        CB = sbm("CB", [128, 4, 129])
        bCB = Buf("CB")
        sts = ExitStack()

        def sbs(name, shape, dt=F32):
            return sts.enter_context(nc.sbuf_tensor(name, list(shape), dt))

        WIN = sbs("WIN", [128, 8, 1536], BF16)
        bWIN = Buf("WIN")
        BCOL = sbs("BCOL", [128, 16])
        bBCOL = Buf("BCOL")
        BZ = sbs("BZ", [128, 512])
        bBZ = Buf("BZ")
        bMODD = Buf("mod_d")

        def interleave(gens):
            gens = list(gens)
            while gens:
                for g_ in list(gens):
                    try:
                        next(g_)
                    except StopIteration:
                        gens.remove(g_)

        with ExitStack() as st0:
            def sb0(name, shape, dt=F32):
                return st0.enter_context(nc.sbuf_tensor(name, list(shape), dt))
            MOD = sb0("MOD", [128, 6 * D])
            bMOD = Buf("MOD")
            MODC = sb0("MODC", [128, 2 * D])
            bMODC = Buf("MODC")
            st0a = ExitStack()

            def sb0a(name, shape, dt=F32):
                return st0a.enter_context(nc.sbuf_tensor(name, list(shape), dt))
            CT = sb0a("CT", [128, 16])
            bCT = Buf("CT")
            P.dma("sp", CT[:, :], cT.ap(), writes=[bCT])
            SC = sb0a("SC", [128, 16])
            bSC = Buf("SC")
            act(SC[:, :], CT[:, :], AF.Silu, [bCT], [bSC])
            REP = sb0a("REP", [128, 16, 128])
            bREP = Buf("REP")
            for j in range(16):
                cp("dve", REP[:, j, :], SC[:, j:j + 1].broadcast_to([128, 128]), [bSC], [bREP])
            WA = [sb0a("WA%d" % i, [128, 8, 512]) for i in range(2)]
            bWA = [Buf("WA%d" % i) for i in range(2)]
            BA = [sb0a("BA%d" % i, [128, 512]) for i in range(2)]
            bBA = [Buf("BA%d" % i) for i in range(2)]
            w_ada_v = w_ada.ap().rearrange("(k p) n -> p k n", p=128)
            for blk in range(12):
                s = blk % 2
                P.dma("sp", WA[s][:, :, :], w_ada_v[:, :, blk * 512:(blk + 1) * 512], writes=[bWA[s]])
                P.dma("sp", BA[s][:, :], bc_rows(b_ada, 0, 512, off=blk * 512), writes=[bBA[s]])
                pb = blk % 2
                for kc in range(8):
                    mm(PS[pb][:, :], REP[:, kc, :], WA[s][:, kc, :], kc == 0, kc == 7, [bREP, bWA[s]], [PB[pb]])
                tt("dve", MOD[:, blk * 512:(blk + 1) * 512], PS[pb][:, :], BA[s][:, :], ALU.add, [PB[pb], bBA[s]], [bMOD])
                if blk < 4:
                    pc = 2 + blk % 2
                    for kc in range(8):
                        mm(PS[pc][:, :], REP[:, 8 + kc, :], WA[s][:, kc, :], kc == 0, kc == 7, [bREP, bWA[s]], [PB[pc]])
                    tt("dve", MODC[:, blk * 512:(blk + 1) * 512], PS[pc][:, :], BA[s][:, :], ALU.add, [PB[pc], bBA[s]], [bMODC])
            GB = sb0a("GB", [128, 4, D])
            bGB = Buf("GB")
            for i in range(4):
                P.dma("sp", GB[:, i, :], bc_rows(gains, i, D), writes=[bGB])
            stt("dve", MOD[:, D:2 * D], MOD[:, D:2 * D], 1.0, GB[:, 0, :], ALU.add, ALU.mult, [bMOD, bGB], [bMOD])
            tt("dve", MOD[:, 2 * D:3 * D], MOD[:, 2 * D:3 * D], GB[:, 1, :], ALU.mult, [bMOD, bGB], [bMOD])
            stt("dve", MOD[:, 4 * D:5 * D], MOD[:, 4 * D:5 * D], 1.0, GB[:, 2, :], ALU.add, ALU.mult, [bMOD, bGB], [bMOD])
            tt("dve", MOD[:, 5 * D:6 * D], MOD[:, 5 * D:6 * D], GB[:, 3, :], ALU.mult, [bMOD, bGB], [bMOD])
            stt("dve", MODC[:, D:2 * D], MODC[:, D:2 * D], 1.0, GB[:, 0, :], ALU.add, ALU.mult, [bMODC, bGB], [bMODC])
            P.dma("sp", mod_d.ap(), MOD[0:1, :], reads=[bMOD], writes=[bMODD])
            dump("mod", MOD[0:1, :], [1, 6 * D], [bMOD])
            dump("modc", MODC[0:1, :], [1, 2 * D], [bMODC])
            P.barrier()
            st0a.close()
            REPS = sb0("REPS", [128, 2, 8, 128])
            bREPS = Buf("REPS")
            GC = sb0("GC", [128, 16])
            bGC = Buf("GC")
            for kc in range(8):
                blk = slice(kc * 128, (kc + 1) * 128)
                tr(PS[0][:, 0:128], MOD[:, blk], IDF, [bMOD, bCONST], [PB[0]])
                tr(PS[0][:, 128:256], MOD[:, D + kc * 128:D + (kc + 1) * 128], IDF, [bMOD, bCONST], [PB[0]])
                tr(PS[0][:, 256:384], MODC[:, blk], IDF, [bMODC, bCONST], [PB[0]])
                tr(PS[0][:, 384:512], MODC[:, D + kc * 128:D + (kc + 1) * 128], IDF, [bMODC, bCONST], [PB[0]])
                cp("dve", REPS[:, 0, kc, :], PS[0][:, 0:128], [PB[0]], [bREPS])
                cp("dve", REPS[:, 1, kc, :], PS[0][:, 256:384], [PB[0]], [bREPS])
                cp("dve", GC[:, kc:kc + 1], PS[0][:, 128:129], [PB[0]], [bGC])
                cp("dve", GC[:, 8 + kc:9 + kc], PS[0][:, 384:385], [PB[0]], [bGC])
            WST = [sb0("WST%d" % i, [128, 1536]) for i in range(2)]
            bWST = [Buf("WST%d" % i) for i in range(2)]
            for kc in range(8):
                w_, bw_ = WST[kc % 2], bWST[kc % 2]
                P.dma("sp", w_[:, :], w_in.ap()[kc * 128:(kc + 1) * 128, :], writes=[bw_])
                ts("dve", WIN[:, kc, :], w_[:, :], GC[:, kc:kc + 1], None, ALU.mult, None, [bw_, bGC], [bWIN])
                ts("pool", WINC[:, kc, :], w_[:, 0:512], GC[:, 8 + kc:9 + kc], None, ALU.mult, None, [bw_, bGC], [bWINC])
                for blk in range(3):
                    mm(PS[2 + blk][:, :], REPS[:, 0, kc, :], w_[:, blk * 512:(blk + 1) * 512], kc == 0, kc == 7, [bREPS, bw_], [PB[2 + blk]])
                mm(PS[5][:, :], REPS[:, 1, kc, :], w_[:, 0:512], kc == 0, kc == 7, [bREPS, bw_], [PB[5]])
            BROW = sb0("BROW", [128, 4, 512])
            bBROW = Buf("BROW")
            for i in range(4):
                cp("dve", BROW[:, i, :], PS[2 + i][:, :], [PB[2 + i]], [bBROW])
            cp("dve", BZ[:, :], BROW[:, 1, :], [bBROW], [bBZ])
            for i, src in ((0, 0), (1, 2), (2, 3)):
                for j in range(4):
                    tr(PS[0][:, j * 128:(j + 1) * 128], BROW[:, src, j * 128:(j + 1) * 128], IDF, [bBROW, bCONST], [PB[0]])
                for j in range(4):
                    cp("dve", BCOL[:, i * 4 + j:i * 4 + j + 1], PS[0][:, j * 128:j * 128 + 1], [PB[0]], [bBCOL])
            P.barrier()

        BDB = sbs("BDB", [128, 3, 4, 128], BF16)
        bBDB = Buf("BDB")
        AW = sbs("AW", [128, 4, 32], BF16)
        bAW = Buf("AW")
        BIF = sbs("BIF", [128, 16])
        bBIF = Buf("BIF")
        P.dma("sp", BIF[:, :], bc_rows(b_if, 0, 16), writes=[bBIF])
        CW = sbs("CW", [128, 16])
        bCW = Buf("CW")
        P.dma("sp", CW[:, :], convw.ap(), writes=[bCW])
        XMH = sbs("XMH", [128, 4, 64])
        bXMH = Buf("XMH")
        bPOSRD = Buf("posr_d")
        NTB = 4
        XB = [sbs("XB%d" % i, [128, D]) for i in range(NTB)]
        bXB = [Buf("XB%d" % i) for i in range(NTB)]
        PRB = [sbs("PRB%d" % i, [128, 512]) for i in range(NTB)]
        bPRB = [Buf("PRB%d" % i) for i in range(NTB)]
        X0B = [sbs("X0B%d" % i, [128, D], BF16) for i in range(NTB)]
        bX0B = [Buf("X0B%d" % i) for i in range(NTB)]
        JUNK = [sbs("JUNK%d" % i, [128, D], BF16) for i in range(2)]
        bJUNK = [Buf("JUNK%d" % i) for i in range(2)]
        DG = [sbs("DG%d" % i, [128, 128], BF16) for i in range(NTB)]
        bDG = [Buf("DG%d" % i) for i in range(NTB)]
        SS = sbs("SS", [128, 4 * NTB])
        bSS = [Buf("SS%d" % i) for i in range(NTB)]
        HT = sbs("HT", [128, 8, 512], BF16)
        bHTt = [Buf("HT%d" % i) for i in range(4)]
        PT = PS[0][:, :].bitcast(BF16).rearrange("p (k t) -> p k t", t=128)

        with ExitStack() as st1:
            def sb1(name, shape, dt=F32):
                return st1.enter_context(nc.sbuf_tensor(name, list(shape), dt))
            FREQ = sb1("FREQ", [128, 256])
            bFREQ = Buf("FREQ")
            act(FREQ[:, :], TABS[:, 512:768], AF.Exp, [bTABS], [bFREQ], scale=-math.log(10000.0) / 256.0)
            WS = sb1("WS", [128, 6, 4, 4])
            bWS = Buf("WS")
            for w in range(3):
                P.dma("sp", WS[:, w, :, :], bass.AP(w_qkv, w * 2048, [[4, 128], [512, 4], [1, 4]]), writes=[bWS])
                P.dma("sp", WS[:, 3 + w, :, :], bass.AP(w_qkvT, w * 2048, [[4, 128], [512, 4], [1, 4]]), writes=[bWS])
            BDF = sb1("BDF", [128, 6, 4, 128])
            bBDF = Buf("BDF")
            BDM3 = BDM.rearrange("p (r o) -> p r o", o=4)
            for w in range(6):
                for cc in range(4):
                    tt("dve", BDF[:, w, cc, :].rearrange("p (r o) -> p r o", o=4),
                       WS[:, w, cc, None, :].broadcast_to([128, 32, 4]), BDM3, ALU.mult, [bWS, bCONST], [bBDF])
            cp("dve", BDB[:, :, :, :], BDF[:, 0:3, :, :], [bBDF], [bBDB])
            WIF = sb1("WIF", [128, 12, 16])
            bWIF = Buf("WIF")
            P.dma("sp", WIF[:, :, :], w_if.ap().rearrange("(k p) n -> p k n", p=128), writes=[bWIF])
            for cc in range(4):
                mm(PS[1][:, cc * 32:cc * 32 + 16], BDF[:, 3, cc, :], WIF[:, cc, :], True, False, [bBDF, bWIF], [PB[1]])
                mm(PS[1][:, cc * 32:cc * 32 + 16], BDF[:, 4, cc, :], WIF[:, 4 + cc, :], False, True, [bBDF, bWIF], [PB[1]])
                mm(PS[1][:, cc * 32 + 16:cc * 32 + 32], BDF[:, 5, cc, :], WIF[:, 8 + cc, :], True, True, [bBDF, bWIF], [PB[1]])
            cp("dve", AW[:, :, :], PS[1][:, 0:128].rearrange("p (c n) -> p c n", n=32), [PB[1]], [bAW])

            ANG = sb1("ANG", [128, 512])
            bANG = Buf("ANG")
            KI = sb1("KI", [128, 512], I32)
            bKI = Buf("KI")
            MSK = sb1("MSK", [128, 512])
            bMSK = Buf("MSK")

            def sincos(out, bout, idx):
                ts("dve", ANG[:, 0:256], FREQ[:, :], idx, None, ALU.mult, None, [bFREQ, bTABS], [bANG])
                ts("dve", ANG[:, 256:512], ANG[:, 0:256], math.pi / 2, None, ALU.add, None, [bANG], [bANG])
                ts("dve", KI[:, :], ANG[:, :], 1.0 / TWO_PI, None, ALU.mult, None, [bANG], [bKI])
                stt("dve", ANG[:, :], KI[:, :], -TWO_PI, ANG[:, :], ALU.mult, ALU.add, [bKI, bANG], [bANG])
                ts("dve", MSK[:, :], ANG[:, :], math.pi, TWO_PI, ALU.is_gt, ALU.mult, [bANG], [bMSK])
                tt("dve", ANG[:, :], ANG[:, :], MSK[:, :], ALU.subtract, [bANG, bMSK], [bANG])
                ts("dve", MSK[:, :], ANG[:, :], -math.pi, TWO_PI, ALU.is_lt, ALU.mult, [bANG], [bMSK])
                tt("dve", ANG[:, :], ANG[:, :], MSK[:, :], ALU.add, [bANG, bMSK], [bANG])
                ts("dve", ANG[:, :], ANG[:, :], math.pi, -math.pi, ALU.min, ALU.max, [bANG], [bANG])
                act(out, ANG[:, :], AF.Sin, [bANG], [bout])

            PR = sb1("PR", [128, 2, 512])
            bPR = Buf("PR")
            for a in range(2):
                sincos(PR[:, a, :], bPR, TABS[:, a:a + 1])
            P.dma("sp", posr_d.ap().rearrange("(p a) n -> p a n", a=2), PR[:, :, :], reads=[bPR], writes=[bPOSRD])
            sincos(POSC[:, :], bPOSC, TABS[:, 2:3])
            POSH = sb1("POSH", [128, D])
            bPOSH = Buf("POSH")
            sincos(POSH[:, 0:512], bPOSH, TABS[:, 3:4])
            sincos(POSH[:, 512:1024], bPOSH, TABS[:, 4:5])

            tile_ctr = [0]

            def tile_front(xsrc, pos_mode, ht_dst, bht):
                k = tile_ctr[0]
                tile_ctr[0] += 1
                q = k % NTB
                X, bX = XB[q], bXB[q]
                xb_, bxb_ = X0B[q], bX0B[q]
                dg, bdg = DG[q], bDG[q]
                bss = bSS[q]
                P.dma("sp", X[:, :], xsrc, writes=[bX])
                if pos_mode is not None and pos_mode[0] == "rolled":
                    i = pos_mode[1]
                    PRt, bPRt = PRB[q], bPRB[q]
                    P.dma("sp", PRt[0:64, :], bc_rows(posr_d, 2 * i, 512, parts=64), reads=[bPOSRD], writes=[bPRt])
                    P.dma("sp", PRt[64:128, :], bc_rows(posr_d, 2 * i + 1, 512, parts=64), reads=[bPOSRD], writes=[bPRt])
                    yield
                    tt("pool", xb_[:, 0:512], X[:, 0:512], PRt[:, :], ALU.add, [bX, bPRt], [bxb_])
                    tt("pool", xb_[:, 512:1024], X[:, 512:1024], POSC[:, :], ALU.add, [bX, bPOSC], [bxb_])
                elif pos_mode is not None:
                    tt("pool", xb_[:, :], X[:, :], POSH[:, :], ALU.add, [bX, bPOSH], [bxb_])
                else:
                    yield
                    cp("pool", xb_[:, :], X[:, :], [bX], [bxb_])
                yield
                sc = SS[:, q * 4:q * 4 + 4]
                act(JUNK[k % 2][:, :], xb_[:, :], AF.Square, [bxb_], [bJUNK[k % 2], bss], accum_out=sc[:, 0:1])
                yield
                ts("pool", sc[:, 1:2], sc[:, 0:1], 1.0 / D, EPS, ALU.mult, ALU.add, [bss], [bss])
                tt("pool", sc[:, 2:3], sc[:, 1:2], NEGH, ALU.pow, [bss, bTABS], [bss])
                yield
                ts("dve", dg[:, :], IDF, sc[:, 2:3], None, ALU.mult, None, [bCONST, bss], [bdg])
                yield
                for kc in range(8):
                    pb = kc // 4
                    mm(PS[pb][:, (kc % 4) * 128:(kc % 4 + 1) * 128], xb_[:, kc * 128:(kc + 1) * 128], dg[:, :], True, True, [bxb_, bdg], [PB[pb]])
                cp("act", ht_dst[:, 0:4, :], PS[0][:, :].rearrange("p (k t) -> p k t", t=128), [PB[0]], [bht])
                cp("dve", ht_dst[:, 4:8, :], PS[1][:, :].rearrange("p (k t) -> p k t", t=128), [PB[1]], [bht])
                yield

            HTH = sb1("HTH", [128, 8, 128], BF16)
            bHTH = Buf("HTH")
            interleave([tile_front(x_halo.ap(), ("halo",), HTH[:, :, :], bHTH)])
            for cc in range(4):
                for kc in range(8):
                    mm(PS[2][:, cc * 64:cc * 64 + 64], WIN[:, kc, cc * 128:(cc + 1) * 128], HTH[:, kc, 0:64], kc == 0, kc == 7, [bWIN, bHTH], [PB[2]])
            XMHF = sb1("XMHF", [128, 4, 64])
            bXMHF = Buf("XMHF")
            tt("dve", XMHF[:, :, :], PS[2][:, 0:256].rearrange("p (c n) -> p c n", n=64),
               BCOL[:, 0:4, None].broadcast_to([128, 4, 64]), ALU.add, [PB[2], bBCOL], [bXMHF])
            tt("dve", XMH[:, :, :], XMHF[:, :, :], TABS[:, None, 384:448].broadcast_to([128, 4, 64]), ALU.mult, [bXMHF, bTABS], [bXMH])
            dump("xmh", XMH[:, :, :], [128, 4, 64], [bXMH])
            P.barrier()

        XM = sbs("XM", [128, 4, 514])
        bXM = [Buf("XM%d" % i) for i in range(4)]
        ACC = [sbs("ACC%d" % i, [128, 512]) for i in range(2)]
        bACC = [Buf("ACC%d" % i) for i in range(2)]
        ACTT = [sbs("ACTT%d" % i, [128, 4, 512], BF16) for i in range(2)]
        bACTT = [[Buf("ACTT%d_%d" % (j, i)) for i in range(4)] for j in range(2)]
        XMB = [sbs("XMB%d" % i, [128, 4, 512], BF16) for i in range(2)]
        bXMB = [[Buf("XMB%d_%d" % (j, i)) for i in range(4)] for j in range(2)]
        UT = sbs("UT", [128, 4, 512], BF16)
        bUT = Buf("UT")
        bUD = Buf("u_d")
        KTMG = sbs("KTMG", [128, 4, 512], BF16)
        bKTMG = [Buf("KTMG%d" % i) for i in range(4)]
        VAG = sbs("VAG", [128, 4, 4, 129], BF16)
        bVAG = [Buf("VAG%d" % i) for i in range(4)]
        VS = [sbs("VS%d" % i, [128, 8, 129], BF16) for i in range(2)]
        bVS = [Buf("VS%d" % i) for i in range(2)]
        CBS = sbs("CBS", [128, 4, 129])
        bCBS = Buf("CBS")
        memset("pool", CBS[:, :, :], 0.0, [bCBS])
        CCB = sbs("CCB", [128, 4, 129])
        bCCB = Buf("CCB")
        GT = sbs("GT", [128, 4, 16])
        bGT = Buf("GT")
        GW = sbs("GW", [128, 4, 40])
        bGW = Buf("GW")
        EXI = sbs("EXI", [128, 4, 16])
        bEXI = Buf("EXI")
        EXO = sbs("EXO", [128, 4, 16])
        bEXO = Buf("EXO")
        PALL = sbs("PALL", [128, 5, 4])
        bPALL = Buf("PALL")
        MBT = sbs("MBT", [128, 4, 4])
        bMBT = Buf("MBT")
        WV = sbs("WV", [128, 4, 8])
        bWV = Buf("WV")
        STG = [sbs("STG%d" % i, [128, 512], BF16) for i in range(2)]
        bSTG = [Buf("STG%d" % i) for i in range(2)]
        ZT = sbs("ZT", [128, 512])
        bZT = Buf("ZT")
        bAZ = Buf("az_d")
        stg_ctr = [0]

        memset("pool", VAG[:, :, :, :], 1.0, bVAG)
        memset("pool", VAO[:, :, :, :], 1.0, bVAO)
        memset("pool", PALL[:, :, :], 0.0, [bPALL])

        PSG = PS[6][:, 384:448].rearrange("p (t g) -> p t g", g=16)
        bPSG = PB[6]
        PSB = PS[6][:, 448:512].rearrange("p (t g) -> p t g", g=16)
        bPSB = PB[6]
        DCF = [PS[5][:, 0:129], PS[5][:, 129:258], PS[5][:, 258:387], PS[6][:, 0:129]]
        bDCF = [PB[5], PB[5], PB[5], PB[6]]
        ACCB = [PS[7][:, 0:129], PS[7][:, 129:258], PS[7][:, 258:387], PS[6][:, 129:258]]
        bACCB = [PB[7], PB[7], PB[7], PB[6]]
        u_v = u_d.ap().rearrange("(c p) t -> p c t", p=128)
        LN_QS = math.log(128.0 ** -0.5)

        def front(gi, kind, par):
            n = 2 if kind == "ctx" else 4
            ntok = n * 128
            tgens = []
            for t_ in range(n):
                dst = HT[:, :, t_ * 128:(t_ + 1) * 128]
                if kind == "ctx":
                    tgens.append(tile_front(ctx_in.ap()[t_ * 128:(t_ + 1) * 128, :], None, dst, bHTt[t_]))
                else:
                    i = gi * 4 + t_
                    tgens.append(tile_front(x_rot.ap()[i * 128:(i + 1) * 128, :], ("rolled", i), dst, bHTt[t_]))
            while tgens:
                for g_ in list(tgens):
                    try:
                        next(g_)
                    except StopIteration:
                        tgens.remove(g_)
                yield
            W_, bW_, boff = (WINC, bWINC, 8) if kind == "ctx" else (WIN, bWIN, 0)
            att, batt, xmb, bxmb = ACTT[par], bACTT[par], XMB[par], bXMB[par]
            for cc in range(4):
                pb = 2 + cc % 2
                for kc in range(8):
                    mm(PS[pb][:, 0:ntok], W_[:, kc, cc * 128:(cc + 1) * 128], HT[:, kc, 0:ntok], kc == 0, kc == 7, [bW_] + bHTt, [PB[pb]])
                bias = BCOL[:, boff + cc:boff + cc + 1]
                act(XM[:, cc, 1:1 + ntok], PS[pb][:, 0:ntok], AF.Identity, [PB[pb], bBCOL], [bXM[cc]], bias=bias)
                act(xmb[:, cc, 0:ntok], PS[pb][:, 0:ntok], AF.Identity, [PB[pb], bBCOL], [bxmb[cc]], bias=bias)
                if kind == "ctx":
                    memset("pool", XM[:, cc, 0:1], 0.0, [bXM[cc]])
                    memset("pool", XM[:, cc, 1 + ntok:2 + ntok], 0.0, [bXM[cc]])
                else:
                    cp("pool", XM[:, cc, 0:1], XMH[:, cc, 2 * gi:2 * gi + 1], [bXMH], [bXM[cc]])
                    cp("pool", XM[:, cc, 513:514], XMH[:, cc, 2 * gi + 1:2 * gi + 2], [bXMH], [bXM[cc]])
                yield
                A_, bA_ = ACC[cc % 2], bACC[cc % 2]
                ts("dve", A_[:, 0:ntok], XM[:, cc, 1:1 + ntok], CW[:, cc * 3 + 1:cc * 3 + 2], CW[:, 12 + cc:13 + cc], ALU.mult, ALU.add, [bXM[cc], bCW], [bA_])
                stt("dve", A_[:, 0:ntok], XM[:, cc, 0:ntok], CW[:, cc * 3:cc * 3 + 1], A_[:, 0:ntok], ALU.mult, ALU.add, [bXM[cc], bCW, bA_], [bA_])
                stt("dve", A_[:, 0:ntok], XM[:, cc, 2:2 + ntok], CW[:, cc * 3 + 2:cc * 3 + 3], A_[:, 0:ntok], ALU.mult, ALU.add, [bXM[cc], bCW, bA_], [bA_])
                if cc % 2 == 1:
                    for c2 in (cc - 1, cc):
                        act(att[:, c2, 0:ntok], ACC[c2 % 2][:, 0:ntok], AF.Silu, [bACC[c2 % 2]], [batt[c2]])
                yield
            if kind == "own" and gi == 0:
                dump("actT", att[:, :, 0:128], [128, 4, 128], batt)
            if kind != "ctx":
                for cc in range(4):
                    pb = 2 + cc % 2
                    for kc in range(8):
                        mm(PS[pb][:, :], WIN[:, kc, 1024 + cc * 128:1024 + (cc + 1) * 128], HT[:, kc, :], kc == 0, kc == 7, [bWIN] + bHTt, [PB[pb]])
                    act(UT[:, cc, :], PS[pb][:, :], AF.Identity, [PB[pb], bBCOL], [bUT], bias=BCOL[:, 4 + cc:5 + cc])
                    yield
                P.dma("act", u_v[:, :, gi * 512:(gi + 1) * 512], UT[:, :, :], reads=[bUT], writes=[bUD])
            if kind == "own":
                for w, dstT, bdst in ((0, QT, bQT), (1, KT, bKT)):
                    for cc in range(4):
                        pb = 2 + cc % 2
                        mm(PS[pb][:, :], BDB[:, w, cc, :], att[:, cc, :], True, True, [bBDB, batt[cc]], [PB[pb]])
                        cp("act" if cc % 2 else "dve", dstT[:, cc, gi * 512:(gi + 1) * 512], PS[pb][:, :], [PB[pb]], [bdst])
                    yield
                for t_ in range(4):
                    i = gi * 4 + t_
                    sl = slice(t_ * 128, (t_ + 1) * 128)
                    pb = 2 + t_ % 2
                    for kc in range(8):
                        mm(PS[pb][:, :], HT[:, kc, sl], WIN[:, kc, 512:1024], kc == 0, kc == 7, bHTt + [bWIN], [PB[pb]])
                    tt("dve", ZT[:, :], PS[pb][:, :], BZ[:, :], ALU.add, [PB[pb], bBZ], [bZT])
                    s_ = stg_ctr[0] % 2
                    stg_ctr[0] += 1
                    act(STG[s_][:, :], ZT[:, :], AF.Silu, [bZT], [bSTG[s_]])
                    P.dma("act", az_d.ap()[1, i * 128:(i + 1) * 128, :], STG[s_][:, :], reads=[bSTG[s_]], writes=[bAZ])
                    for cc in range(4):
                        tr(PT[:, cc, :], att[:, cc, sl], IDB[:, :], [batt[cc], bIDB], [PB[0]])
                    s_ = stg_ctr[0] % 2
                    stg_ctr[0] += 1
                    cp("dve", STG[s_][:, :].rearrange("p (c t) -> p c t", t=128), PT[:, 0:4, :], [PB[0]], [bSTG[s_]])
                    P.dma("act", az_d.ap()[0, i * 128:(i + 1) * 128, :], STG[s_][:, :], reads=[bSTG[s_]], writes=[bAZ])
                    yield

        def back(gi, kind, par):
            n = 2 if kind == "ctx" else 4
            att, batt, xmb, bxmb = ACTT[par], bACTT[par], XMB[par], bXMB[par]
            for t_ in range(n):
                sl = slice(t_ * 128, (t_ + 1) * 128)
                if kind == "own":
                    i = gi * 4 + t_
                    ktm, bktm, va, bva = KTMO[:, i, :], bKTMO[i], VAO[:, i, :, :], bVAO[i]
                else:
                    ktm, bktm, va, bva = KTMG[:, t_, :], bKTMG[t_], VAG[:, t_, :, :], bVAG[t_]
                for cc in range(4):
                    mm(PS[4][:, cc * 128:(cc + 1) * 128], att[:, cc, sl], BDB[:, 1, cc, :], True, True, [batt[cc], bBDB], [PB[4]])
                cp("act", ktm, PS[4][:, :], [PB[4]], [bktm])
                yield
                for cc in range(4):
                    mm(PS[4][:, cc * 128:(cc + 1) * 128], xmb[:, cc, sl], BDB[:, 2, cc, :], True, True, [bxmb[cc], bBDB], [PB[4]])
                cp("act", va[:, :, 0:128], PS[4][:, :].rearrange("p (h d) -> p h d", d=128), [PB[4]], [bva])
                for cc in range(4):
                    mm(PSG[:, t_, :], att[:, cc, sl], AW[:, cc, 0:16], cc == 0, False, [batt[cc], bAW], [bPSG])
                for cc in range(4):
                    mm(PSG[:, t_, :], xmb[:, cc, sl], AW[:, cc, 16:32], False, cc == 3, [bxmb[cc], bAW], [bPSG])
                yield
            tt("dve", GT[:, 0:n, :], PSG[:, 0:n, :], BIF[:, None, :].broadcast_to([128, n, 16]), ALU.add, [bPSG, bBIF], [bGT])
            stt("dve", GW[:, 0:n, 0:8], GT[:, 0:n, 8:16], -1.0, GT[:, 0:n, 8:16], ALU.mult, ALU.max, [bGT], [bGW])
            yield
            ts("dve", GW[:, 0:n, 24:32], GT[:, 0:n, 8:16], 0.0, None, ALU.min, None, [bGT], [bGW])
            act(GW[:, 0:n, 8:16], GW[:, 0:n, 0:8], AF.Exp, [bGW], [bGW], scale=-1.0)
            act(GW[:, 0:n, 16:24], GW[:, 0:n, 8:16], AF.Ln, [bGW], [bGW], bias=1.0)
            yield
            tt("dve", GW[:, 0:n, 32:40], GW[:, 0:n, 24:32], GW[:, 0:n, 16:24], ALU.subtract, [bGW], [bGW])
            yield
            for t_ in range(n):
                mm(PSB[:, t_, 0:4], TRIU, GW[:, t_, 32:36], True, True, [bCONST, bGW], [bPSB])
                mm(PSB[:, t_, 4:8], TRIL, GW[:, t_, 36:40], True, True, [bCONST, bGW], [bPSB])
                mm(PSB[:, t_, 8:16], ONES, GW[:, t_, 32:40], True, True, [bCONST, bGW], [bPSB])
            yield
            if kind == "own":
                i0 = gi * 4
                if gi == 0:
                    dump("gt", GT[:, :, :], [128, 4, 16], [bGT])
                    dump("lf", GW[:, :, 32:40], [128, 4, 8], [bGW])
                tt("dve", EXI[:, :, 0:8], GT[:, :, 0:8], PSB[:, :, 0:8], ALU.subtract, [bGT, bPSB], [bEXI])
                yield
                act(OWNG[:, i0:i0 + 4, 0:8], EXI[:, :, 0:8], AF.Exp, [bEXI], [bOWNG])
                act(OWNG[:, i0:i0 + 4, 8:16], PSB[:, :, 0:8], AF.Exp, [bPSB], [bOWNG], bias=LN_QS)
                act(OWNG[:, i0:i0 + 4, 16:24], PSB[:, :, 8:16], AF.Exp, [bPSB], [bOWNG])
                yield
                return
            tt("dve", EXI[:, 0:n, 0:8], GT[:, 0:n, 0:8], PSB[:, 0:n, 8:16], ALU.add, [bGT, bPSB], [bEXI])
            tt("dve", EXI[:, 0:n, 0:8], EXI[:, 0:n, 0:8], PSB[:, 0:n, 0:8], ALU.subtract, [bEXI, bPSB], [bEXI])
            yield
            if kind == "ctx":
                cp("dve", EXI[:, 0:2, 8:16], PSB[:, 0:2, 8:16], [bPSB], [bEXI])
                yield
                act(EXO[:, 0:2, :], EXI[:, 0:2, :], AF.Exp, [bEXI], [bEXO])
                yield
                tt("dve", WV[:, 0, 0:4], EXO[:, 0, 0:4], EXO[:, 1, 8:12], ALU.mult, [bEXO], [bWV])
                cp("dve", WV[:, 1, 0:4], EXO[:, 1, 0:4], [bEXO], [bWV])
                cp("dve", WV[:, 0, 4:8], EXO[:, 0, 4:8], [bEXO], [bWV])
                tt("dve", WV[:, 1, 4:8], EXO[:, 1, 4:8], EXO[:, 0, 12:16], ALU.mult, [bEXO], [bWV])
                yield
            else:
                MF = TABS[:, 128 + 4 * gi:132 + 4 * gi]
                MB = TABS[:, 256 + 4 * gi:260 + 4 * gi]
                MF3 = MF[:, :, None].broadcast_to([128, 4, 4])
                MB3 = MB[:, :, None].broadcast_to([128, 4, 4])
                tt("dve", EXI[:, :, 8:12], PSB[:, :, 8:12], MF3, ALU.mult, [bPSB, bTABS], [bEXI])
                tt("dve", MBT[:, :, :], PSB[:, :, 12:16], MB3, ALU.mult, [bPSB, bTABS], [bMBT])
                yield
                for t_ in range(4):
                    tt("dve", PALL[:, t_ + 1, :], PALL[:, t_, :], MBT[:, t_, :], ALU.add, [bPALL, bMBT], [bPALL])
                    yield
                cp("dve", EXI[:, :, 12:16], PALL[:, 0:4, :], [bPALL], [bEXI])
                yield
                act(EXO[:, :, :], EXI[:, :, :], AF.Exp, [bEXI], [bEXO])
                yield
                tt("dve", WV[:, :, 0:4], EXO[:, :, 0:4], MF3, ALU.mult, [bEXO, bTABS], [bWV])
                tt("dve", WV[:, :, 4:8], EXO[:, :, 4:8], EXO[:, :, 12:16], ALU.mult, [bEXO], [bWV])
                yield
                tt("dve", WV[:, :, 4:8], WV[:, :, 4:8], MB3, ALU.mult, [bWV, bTABS], [bWV])
                cp("dve", PALL[:, 0, :], PALL[:, 4, :], [bPALL], [bPALL])
                yield
            for t_ in range(n):
                s_ = t_ % 2
                tt("pool", VS[s_][:, 0:4, :], VAG[:, t_, :, :], WV[:, t_, 0:4, None].broadcast_to([128, 4, 129]), ALU.mult, [bVAG[t_], bWV], [bVS[s_]])
                tt("pool", VS[s_][:, 4:8, :], VAG[:, t_, :, :], WV[:, t_, 4:8, None].broadcast_to([128, 4, 129]), ALU.mult, [bVAG[t_], bWV], [bVS[s_]])
                yield
                for h in range(4):
                    klhs = KTMG[:, t_, h * 128:(h + 1) * 128]
                    mm(DCF[h], klhs, VS[s_][:, h, :], True, True, [bKTMG[t_], bVS[s_]], [bDCF[h]])
                    mm(ACCB[h], klhs, VS[s_][:, 4 + h, :], True, True, [bKTMG[t_], bVS[s_]], [bACCB[h]])
                yield
                dcf3 = PS[5][:, 0:387].rearrange("p (h d) -> p h d", d=129)
                acb3 = PS[7][:, 0:387].rearrange("p (h d) -> p h d", d=129)
                if kind == "ctx":
                    if t_ == 0:
                        cp("dve", CF[:, 0:3, :], dcf3, [PB[5]], [bCF])
                        cp("dve", CF[:, 3, :], DCF[3], [PB[6]], [bCF])
                        cp("dve", CCB[:, 0:3, :], acb3, [PB[7]], [bCCB])
                        cp("dve", CCB[:, 3, :], ACCB[3], [PB[6]], [bCCB])
                    else:
                        tt("dve", CF[:, 0:3, :], CF[:, 0:3, :], dcf3, ALU.add, [bCF, PB[5]], [bCF])
                        tt("dve", CF[:, 3, :], CF[:, 3, :], DCF[3], ALU.add, [bCF, PB[6]], [bCF])
                        tt("dve", CCB[:, 0:3, :], CCB[:, 0:3, :], acb3, ALU.add, [bCCB, PB[7]], [bCCB])
                        tt("dve", CCB[:, 3, :], CCB[:, 3, :], ACCB[3], ALU.add, [bCCB, PB[6]], [bCCB])
                else:
                    tt("dve", CF[:, :, :], CF[:, :, :], EXO[:, t_, 8:12, None].broadcast_to([128, 4, 129]), ALU.mult, [bCF, bEXO], [bCF])
                    tt("dve", CF[:, 0:3, :], CF[:, 0:3, :], dcf3, ALU.add, [bCF, PB[5]], [bCF])
                    tt("dve", CF[:, 3, :], CF[:, 3, :], DCF[3], ALU.add, [bCF, PB[6]], [bCF])
                    tt("dve", CBS[:, 0:3, :], CBS[:, 0:3, :], acb3, ALU.add, [bCBS, PB[7]], [bCBS])
                    tt("dve", CBS[:, 3, :], CBS[:, 3, :], ACCB[3], ALU.add, [bCBS, PB[6]], [bCBS])
                yield

        seq = [("ctx", 0)] + [("own", g) for g in range(min(OWN // 4, cut))]
        if cut > 4:
            seq += [("oth", g) for g in range(OWN // 4, ngroups)]
        prev = None
        for idx, (kind, gi) in enumerate(seq):
            gens = [front(gi, kind, idx % 2)]
            if prev is not None:
                gens.append(back(*prev))
            interleave(gens)
            prev = (gi, kind, idx % 2)
        interleave([back(*prev)])
        dump("cf_ctx", CF[:, :, :], [128, 4, 129], [bCF])
        act(EXO[:, 0, 0:4], PALL[:, 0, :], AF.Exp, [bPALL], [bEXO])
        for h in range(4):
            if ngroups > OWN // 4 and cut > 4:
                stt("dve", CB[:, h, :], CCB[:, h, :], EXO[:, 0, h:h + 1], CBS[:, h, :], ALU.mult, ALU.add, [bCCB, bEXO, bCBS], [bCB])
            else:
                cp("dve", CB[:, h, :], CCB[:, h, :], [bCCB], [bCB])
        dump("cf_in", CF[:, :, :], [128, 4, 129], [bCF])
        dump("cb_in", CB[:, :, :], [128, 4, 129], [bCB])
        dump("ktm0", KTMO[:, 0, :], [128, 512], [bKTMO[0]])
        dump("va0", VAO[:, 0, :, :], [128, 4, 129], [bVAO[0]])
        dump("owng", OWNG[:, :, :], [128, OWN, 24], [bOWNG])
        dump("qT", QT[:, :, 0:128], [128, 4, 128], [bQT])

        if stage <= 2:
            o_b = Buf("out")
            P.barrier()
            for i in range(OWN):
                t = P.dma("sp", out_d.ap()[i * 128:(i + 1) * 128, 0:512], POSC[:, 0:512], reads=[bPOSC], writes=[o_b])
                final.append((t[0], t[1]))
            P.barrier()
            with nc.Block() as block:
                P.emit(block, final)
            sts.close()
            stm.close()
            return nc

        P.barrier()
        sts.close()
        st6 = ExitStack()

        def sb6(name, shape, dt=F32):
            return st6.enter_context(nc.sbuf_tensor(name, list(shape), dt))

        HD = [sb6("HD%d" % i, [128, OWN, 512], BF16) for i in range(2)]
        bHD = [[Buf("HD%d_%d" % (i, c)) for c in range(OWN)] for i in range(2)]
        HS = [sb6("HS%d" % i, [128, 512]) for i in range(2)]
        bHS = [Buf("HS%d" % i) for i in range(2)]
        SM = [sb6("SM%d" % i, [128, 128], BF16) for i in range(8)]
        bSM = [Buf("SM%d" % i) for i in range(8)]
        VP = [sb6("VP%d" % i, [128, 129], BF16) for i in range(8)]
        bVP = [Buf("VP%d" % i) for i in range(8)]
        CSB = sb6("CSB", [128, 8, 129], BF16)
        bCSB = [Buf("CSB%d" % i) for i in range(8)]
        RD = [sb6("RD%d" % i, [128, 8]) for i in range(8)]
        bRD = [Buf("RD%d" % i) for i in range(8)]
        bST = [Buf("ST%d" % i) for i in range(8)]
        bHSD = Buf("hs_d")

        def chain(d, h):
            q = d * 4 + h
            ST = CF if d == 0 else CB
            bsrc = bCF if d == 0 else bCB
            hsl = slice(h * 128, (h + 1) * 128)
            cp("act", CSB[:, q, :], ST[:, h, :], [bsrc, bST[q]], [bCSB[q], bST[q]])
            yield
            order = range(OWN) if d == 0 else range(OWN - 1, -1, -1)
            for c in order:
                tsl = slice(c * 128, (c + 1) * 128)
                pS, pN, pC = PS[q][:, 0:128], PS[q][:, 128:257], PS[q][:, 257:386]
                mm(pS, KT[:, h, tsl], QT[:, h, tsl], True, True, [bKT, bQT], [PB[q]])
                ts("pool", VP[q][:, :], VAO[:, c, h, :], OWNG[:, c, q:q + 1], None, ALU.mult, None, [bVAO[c], bOWNG], [bVP[q]])
                yield
                tt("dve", SM[q][:, :], pS, TRIU if d == 0 else TRIL, ALU.mult, [PB[q], bCONST], [bSM[q]])
                yield
                mm(pN, SM[q][:, :], VP[q][:, :], True, False, [bSM[q], bVP[q]], [PB[q]])
                mm(pN, QT[:, h, tsl], CSB[:, q, :], False, True, [bQT, bCSB[q]], [PB[q]])
                mm(pC, KTMO[:, c, hsl], VP[q][:, :], True, True, [bKTMO[c], bVP[q]], [PB[q]])
                yield
                eq = OWNG[:, c, 8 + q:9 + q]
                r = RD[q]
                ts("dve", r[:, 0:1], PS[q][:, 256:257], eq, None, ALU.mult, None, [PB[q], bOWNG], [bRD[q]])
                stt("dve", r[:, 1:2], r[:, 0:1], -1.0, r[:, 0:1], ALU.mult, ALU.max, [bRD[q]], [bRD[q]])
                yield
                ts("dve", r[:, 2:3], r[:, 1:2], 1.0, None, ALU.max, None, [bRD[q]], [bRD[q]])
                P.op("dve", lambda e, o=r[:, 3:4], i_=r[:, 2:3]: e.reciprocal(out=o, in_=i_), [bRD[q]], [bRD[q]])
                yield
                tt("dve", r[:, 4:5], r[:, 3:4], eq, ALU.mult, [bRD[q], bOWNG], [bRD[q]])
                tt("dve", ST[:, h, :], ST[:, h, :], pC, ALU.add, [bST[q], PB[q]], [bST[q]])
                yield
                act(HD[d][:, c, hsl], PS[q][:, 128:256], AF.Copy, [PB[q], bRD[q]], [bHD[d][c]], scale=r[:, 4:5])
                ts("dve", ST[:, h, :], ST[:, h, :], OWNG[:, c, 16 + q:17 + q], None, ALU.mult, None, [bST[q], bOWNG], [bST[q]])
                yield
                cp("act", CSB[:, q, :], ST[:, h, :], [bST[q]], [bCSB[q]])
                yield

        interleave([chain(d, h) for d in range(2) for h in range(4)])
        for c in range(OWN):
            tt("pool", HS[c % 2][:, :], HD[0][:, c, :], HD[1][:, c, :], ALU.add, [bHD[0][c], bHD[1][c]], [bHS[c % 2]])
            P.dma("sp", hs_d.ap()[c * 128:(c + 1) * 128, :], HS[c % 2][:, :], reads=[bHS[c % 2]], writes=[bHSD])
            if c in (0, 7, 15):
                dump("hs%d" % c, HS[c % 2][:, :], [128, 512], [bHS[c % 2]])

        if stage <= 3:
            o_b = Buf("out")
            P.barrier()
            for i in range(OWN):
                t = P.dma("sp", out_d.ap()[i * 128:(i + 1) * 128, 0:512], POSC[:, 0:512], reads=[bPOSC], writes=[o_b])
                final.append((t[0], t[1]))
            P.barrier()
            with nc.Block() as block:
                P.emit(block, final)
            st6.close()
            stm.close()
            return nc

        P.barrier()
        st6.close()
        stm.close()

        H2T = sb("H2T", [128, 8, OWN * 128], BF16)
        bH2T = Buf("H2T")
        COMB = sb("COMB", [128, OWN, 16])
        bCOMB = Buf("COMB")
        st7 = ExitStack()

        def sb7(name, shape, dt=F32):
            return st7.enter_context(nc.sbuf_tensor(name, list(shape), dt))

        XCS = sb7("XCS", [128, 512, 2, 16], BF16)
        bXCS = Buf("XCS")
        CS128 = sb7("CS128", [128, 384], BF16)
        bCS = Buf("CS128")
        DI = sb7("DI", [128, 640])
        bDI = Buf("DI")
        P.dma("sp", DI[:, :], dftidx.ap(), writes=[bDI])
        CSF = sb7("CSF", [128, 384])
        bCSF = Buf("CSF")
        act(CSF[:, 0:256], DI[:, 0:256], AF.Sin, [bDI], [bCSF], scale=TWO_PI / 128.0)
        ts("dve", CSF[:, 256:384], CSF[:, 128:256], -1.0, None, ALU.mult, None, [bCSF], [bCSF])
        cp("dve", CS128[:, :], CSF[:, :], [bCSF], [bCS])
        CMY = sb7("CMY", [128, 48], BF16)
        bCMY = Buf("CMY")
        act(CSF[:, 0:32], DI[:, 512:544], AF.Sin, [bDI, bCS], [bCSF], scale=TWO_PI / 128.0)
        ts("dve", CSF[:, 32:48], CSF[:, 16:32], -1.0, None, ALU.mult, None, [bCSF], [bCSF])
        cp("dve", CMY[:, :], CSF[:, 0:48], [bCSF], [bCMY])
        with ExitStack() as st8:
            def sb8(name, shape, dt=F32):
                return st8.enter_context(nc.sbuf_tensor(name, list(shape), dt))
            TW = sb8("TW", [128, 256], BF16)
            bTW = Buf("TW")
            TWF = sb8("TWF", [128, 256])
            bTWF = Buf("TWF")
            act(TWF[:, :], DI[:, 256:512], AF.Sin, [bDI], [bTWF], scale=TWO_PI / 16384.0)
            ts("dve", TW[:, :], TWF[:, :], 1.0 / math.sqrt(16384.0 * 128.0), None, ALU.mult, None, [bTWF], [bTW])
            TC3 = TW[:, None, 0:128].broadcast_to([128, 8, 128])
            TS3 = TW[:, None, 128:256].broadcast_to([128, 8, 128])
            NW_ = 3
            UL = [sb8("UL%d" % i, [128, 16, 128], BF16) for i in range(3)]
            bUL = [Buf("UL%d" % i) for i in range(3)]
            YS = [sb8("YS%d" % i, [128, 8, 256], BF16) for i in range(NW_)]
            bYS = [Buf("YS%d" % i) for i in range(NW_)]
            MT_ = [[sb8("MTW%d_%d" % (j, i), [128, 8, 128], BF16) for i in range(4)] for j in range(NW_)]
            bMT_ = [[Buf("MTW%d_%d" % (j, i)) for i in range(4)] for j in range(NW_)]
            PQ = [sb8("PQ%d" % i, [128, 2, 8, 128], BF16) for i in range(NW_)]
            bPQ = [Buf("PQ%d" % i) for i in range(NW_)]

            def fft_half(hb):
                ub, half = hb // 2, hb % 2
                u_, bu_ = UL[ub % 3], bUL[ub % 3]
                w_ = hb % NW_
                if half == 0:
                    P.dma("sp", u_[:, :, :], bass.AP(u_d, ub * 16 * T, [[128, 128], [T, 16], [1, 128]]), reads=[bUD], writes=[bu_])
                    yield
                y_, by_ = YS[w_], bYS[w_]
                for pr in range(4):
                    pb = 1 + (hb * 4 + pr) % 4
                    for cc in range(2):
                        chl = half * 8 + pr * 2 + cc
                        mm(PS[pb][:, cc * 256:(cc + 1) * 256], u_[:, chl, :], CS128[:, 0:256], True, True, [bu_, bCS], [PB[pb]])
                    cp("act", y_[:, pr * 2:pr * 2 + 2, :], PS[pb][:, :].rearrange("p (c n) -> p c n", n=256), [PB[pb]], [by_])
                    yield
                yr = y_[:, :, 0:128]
                ys_ = y_[:, :, 128:256]
                pq, bpq = PQ[w_], bPQ[w_]
                m_, bm_ = MT_[w_], bMT_[w_]
                tt("dve", m_[0][:, :, :], yr, TC3, ALU.mult, [by_, bTW], [bm_[0]])
                tt("pool", m_[2][:, :, :], yr, TS3, ALU.mult, [by_, bTW], [bm_[2]])
                yield
                tt("dve", m_[1][:, :, :], ys_, TS3, ALU.mult, [by_, bTW], [bm_[1]])
                tt("pool", m_[3][:, :, :], ys_, TC3, ALU.mult, [by_, bTW], [bm_[3]])
                yield
                tt("dve", pq[:, 0, :, :], m_[0][:, :, :], m_[1][:, :, :], ALU.subtract, [bm_[0], bm_[1]], [bpq])
                yield
                tt("dve", pq[:, 1, :, :], m_[2][:, :, :], m_[3][:, :, :], ALU.add, [bm_[2], bm_[3]], [bpq])
                yield
                pb = 5 + hb % 3
                for cc in range(8):
                    o_c = PS[pb][:, cc * 32:cc * 32 + 16]
                    o_s = PS[pb][:, cc * 32 + 16:cc * 32 + 32]
                    mm(o_c, pq[:, 0, cc, :], CMY[:, 0:16], True, False, [bpq, bCMY], [PB[pb]])
                    mm(o_c, pq[:, 1, cc, :], CMY[:, 32:48], False, True, [bpq, bCMY], [PB[pb]])
                    mm(o_s, pq[:, 0, cc, :], CMY[:, 16:32], True, False, [bpq, bCMY], [PB[pb]])
                    mm(o_s, pq[:, 1, cc, :], CMY[:, 0:16], False, True, [bpq, bCMY], [PB[pb]])
                    if cc % 4 == 3:
                        yield
                cp("act", XCS[:, hb * 8:hb * 8 + 8, :, :], PS[pb][:, 0:256].rearrange("p (c s k) -> p c s k", s=2, k=16), [PB[pb]], [bXCS])
                yield

            def window(genfs, w):
                pend = list(genfs)
                act_ = []
                while pend or act_:
                    while pend and len(act_) < w:
                        act_.append(pend.pop(0)())
                    for g_ in list(act_):
                        try:
                            next(g_)
                        except StopIteration:
                            act_.remove(g_)

            window([(lambda hb=hb: fft_half(hb)) for hb in range(64)], NW_)
            P.barrier()
        dump("xcs", XCS[:, :, :, :], [128, 512, 2, 16], [bXCS])

        if stage <= 4:
            o_b = Buf("out")
            P.barrier()
            for i in range(OWN):
                t = P.dma("sp", out_d.ap()[i * 128:(i + 1) * 128, 0:512], POSC[:, 0:512], reads=[bPOSC], writes=[o_b])
                final.append((t[0], t[1]))
            P.barrier()
            with nc.Block() as block:
                P.emit(block, final)
            st7.close()
            return nc

        st9 = ExitStack()

        def sb9(name, shape, dt=F32):
            return st9.enter_context(nc.sbuf_tensor(name, list(shape), dt))

        WOB = sb9("WOB", [128, 8, D], BF16)
        bWOB = Buf("WOB")
        P.dma("pool", WOB[:, :, :], w_out.ap().rearrange("(k p) n -> p k n", p=128), writes=[bWOB])
        WFB = sb9("WFB", [128, 4, 128], BF16)
        bWFB = Buf("WFB")
        P.dma("pool", WFB[:, :, :], w_fourier.ap().rearrange("g c d -> c g d"), writes=[bWFB])
        NWSK = sb9("NWSK", [128, 2, 512])
        bNWSK = Buf("NWSK")
        for i in range(2):
            P.dma("sp", NWSK[:, i, :], bc_rows(nrm_skip, i, 512), writes=[bNWSK])
        WRF = sb9("WRF", [128, 8, 20])
        bWRF = Buf("WRF")
        P.dma("sp", WRF[:, :, :], w_router.ap().rearrange("(k p) n -> p k n", p=128), writes=[bWRF])
        WRH = sb9("WRH", [128, 8, 20], BF16)
        WRL = sb9("WRL", [128, 8, 20], BF16)
        bWR = Buf("WR")
        cp("dve", WRH[:, :, :], WRF[:, :, :], [bWRF], [bWR])
        tt("dve", WRL[:, :, :], WRF[:, :, :], WRH[:, :, :], ALU.subtract, [bWRF, bWR], [bWR])
        MODL = sb9("MODL", [128, 3, D])
        bMODL = Buf("MODL")
        for i_, off_ in enumerate((2 * D, 3 * D, 4 * D)):
            P.dma("sp", MODL[:, i_, :], bc_rows(mod_d, 0, D, off=off_), reads=[bMODD], writes=[bMODL])
        GP1, S2, G2 = MODL[:, 0, :], MODL[:, 1, :], MODL[:, 2, :]
        bMOD = bMODL
        BR = sb9("BR", [128, 20])
        bBR = Buf("BR")
        P.dma("sp", BR[:, :], bc_rows(b_router, 0, 20), writes=[bBR])
        HSt = [sb9("HSt%d" % i, [128, 512]) for i in range(2)]
        bHSt = [Buf("HSt%d" % i) for i in range(2)]
        AZ = [sb9("AZ%d" % i, [128, 2, 512], BF16) for i in range(2)]
        bAZ_ = [Buf("AZ%d" % i) for i in range(2)]
        SM__2 = [sb9("SM__%d" % i, [128, 32]) for i in range(2)]
        bSM__2 = [Buf("SM__%d" % i) for i in range(2)]
        CEN_2 = [sb9("CEN_%d" % i, [128, 512]) for i in range(2)]
        bCEN_2 = [Buf("CEN_%d" % i) for i in range(2)]
        SQ_2 = [sb9("SQ_%d" % i, [128, 512]) for i in range(2)]
        bSQ_2 = [Buf("SQ_%d" % i) for i in range(2)]
        T1_2 = [sb9("T1_%d" % i, [128, 512]) for i in range(2)]
        bT1_2 = [Buf("T1_%d" % i) for i in range(2)]
        T2_2 = [sb9("T2_%d" % i, [128, 512]) for i in range(2)]
        bT2_2 = [Buf("T2_%d" % i) for i in range(2)]
        MBF_2 = [sb9("MBF_%d" % i, [128, 512], BF16) for i in range(2)]
        bMBF_2 = [Buf("MBF_%d" % i) for i in range(2)]
        MTt_2 = [sb9("MTt_%d" % i, [128, 4, 128], BF16) for i in range(2)]
        bMTt_2 = [Buf("MTt_%d" % i) for i in range(2)]
        XT_2 = [sb9("XT_%d" % i, [128, 8, 128], BF16) for i in range(2)]
        bXT_2 = [Buf("XT_%d" % i) for i in range(2)]
        FTB_2 = [sb9("FTB_%d" % i, [128, 4, 128], BF16) for i in range(2)]
        bFTB_2 = [Buf("FTB_%d" % i) for i in range(2)]
        YFT_2 = [sb9("YFT_%d" % i, [128, 4, 128], BF16) for i in range(2)]
        bYFT_2 = [Buf("YFT_%d" % i) for i in range(2)]
        JK_2 = [sb9("JK_%d" % i, [128, D], BF16) for i in range(2)]
        bJK_2 = [Buf("JK_%d" % i) for i in range(2)]
        SY_2 = [sb9("SY_%d" % i, [128, 8]) for i in range(2)]
        bSY_2 = [Buf("SY_%d" % i) for i in range(2)]
        TT__2 = [sb9("TT__%d" % i, [128, D]) for i in range(2)]
        bTT_2 = [Buf("TT__%d" % i) for i in range(2)]
        H2_2 = [sb9("H2_%d" % i, [128, D]) for i in range(2)]
        bH2_2 = [Buf("H2_%d" % i) for i in range(2)]
        H2H_2 = [sb9("H2H_%d" % i, [128, D], BF16) for i in range(2)]
        bH2H_2 = [Buf("H2H_%d" % i) for i in range(2)]
        H2Lw_2 = [sb9("H2Lw_%d" % i, [128, D], BF16) for i in range(2)]
        bH2Lw_2 = [Buf("H2Lw_%d" % i) for i in range(2)]
        H2LT_2 = [sb9("H2LT_%d" % i, [128, 8, 128], BF16) for i in range(2)]
        bH2LT_2 = [Buf("H2LT_%d" % i) for i in range(2)]
        LG_2 = [sb9("LG_%d" % i, [128, 20]) for i in range(2)]
        bLG_2 = [Buf("LG_%d" % i) for i in range(2)]
        RT_2 = [sb9("RT_%d" % i, [128, 96]) for i in range(2)]
        bRT_2 = [Buf("RT_%d" % i) for i in range(2)]
        XR = [sb9("XR%d" % i, [128, D]) for i in range(2)]
        bXR = [Buf("XR%d" % i) for i in range(2)]
        PRr = [sb9("PRr%d" % i, [128, 512]) for i in range(2)]
        bPRr = [Buf("PRr%d" % i) for i in range(2)]
        X1 = [sb9("X1%d" % i, [128, D]) for i in range(2)]
        bX1 = [Buf("X1%d" % i) for i in range(2)]
        bOUT = Buf("out_d")
        BIG = 30000.0
        AX = mybir.AxisListType.X

        def red(eng, out, in_, op, reads, writes):
            return P.op(eng, lambda e: e.tensor_reduce(out=out, in_=in_, axis=AX, op=op), reads, writes)

        def s6_tile(c):
            s_ = c % 2
            rows = slice(c * 128, (c + 1) * 128)
            bk = (0, 1, 2, 3) if s_ == 0 else (4, 5, 6, 7)
            PTl = PS[bk[0]][:, :].bitcast(BF16).rearrange("p (k t) -> p k t", t=128)
            SM_, bSM_ = SM__2[s_], bSM__2[s_]
            CEN, bCEN = CEN_2[s_], bCEN_2[s_]
            SQ, bSQ = SQ_2[s_], bSQ_2[s_]
            T1, bT1 = T1_2[s_], bT1_2[s_]
            T2, bT2 = T2_2[s_], bT2_2[s_]
            MBF, bMBF = MBF_2[s_], bMBF_2[s_]
            MTt, bMTt = MTt_2[s_], bMTt_2[s_]
            XT, bXT = XT_2[s_], bXT_2[s_]
            FTB, bFTB = FTB_2[s_], bFTB_2[s_]
            YFT, bYFT = YFT_2[s_], bYFT_2[s_]
            JK, bJK = JK_2[s_], bJK_2[s_]
            SY, bSY = SY_2[s_], bSY_2[s_]
            TT_, bTT = TT__2[s_], bTT_2[s_]
            H2, bH2 = H2_2[s_], bH2_2[s_]
            H2H, bH2H = H2H_2[s_], bH2H_2[s_]
            H2Lw, bH2Lw = H2Lw_2[s_], bH2Lw_2[s_]
            H2LT, bH2LT = H2LT_2[s_], bH2LT_2[s_]
            LG, bLG = LG_2[s_], bLG_2[s_]
            RT, bRT = RT_2[s_], bRT_2[s_]
            hs, bhs, az, baz = HSt[s_], bHSt[s_], AZ[s_], bAZ_[s_]
            P.dma("sp", hs[:, :], hs_d.ap()[rows, :], reads=[bHSD], writes=[bhs])
            P.dma("sp", az[:, 0, :], az_d.ap()[0, rows, :], reads=[bAZ], writes=[baz])
            P.dma("sp", az[:, 1, :], az_d.ap()[1, rows, :], reads=[bAZ], writes=[baz])
            hs3 = hs[:, :].rearrange("p (h d) -> p h d", d=128)
            cen3 = CEN[:, :].rearrange("p (h d) -> p h d", d=128)
            sq3 = SQ[:, :].rearrange("p (h d) -> p h d", d=128)
            yield
            red("dve", SM_[:, 0:4], hs3, ALU.add, [bhs], [bSM_])
            ts("dve", SM_[:, 4:8], SM_[:, 0:4], 1.0 / 128.0, None, ALU.mult, None, [bSM_], [bSM_])
            tt("dve", cen3, hs3, SM_[:, 4:8, None].broadcast_to([128, 4, 128]), ALU.subtract, [bhs, bSM_], [bCEN])
            yield
            tt("pool", SQ[:, :], CEN[:, :], CEN[:, :], ALU.mult, [bCEN], [bSQ])
            red("dve", SM_[:, 8:12], sq3, ALU.add, [bSQ], [bSM_])
            yield
            ts("pool", SM_[:, 12:16], SM_[:, 8:12], 1.0 / 128.0, EPS, ALU.mult, ALU.add, [bSM_], [bSM_])
            tt("pool", SM_[:, 16:20], SM_[:, 12:16], NEGH.broadcast_to([128, 4]), ALU.pow, [bSM_, bTABS], [bSM_])
            tt("dve", cen3, cen3, SM_[:, 16:20, None].broadcast_to([128, 4, 128]), ALU.mult, [bCEN, bSM_], [bCEN])
            yield
            tt("dve", T1[:, :], CEN[:, :], NWSK[:, 0, :], ALU.mult, [bCEN, bNWSK], [bT1])
            tt("pool", T2[:, :], az[:, 0, :], NWSK[:, 1, :], ALU.mult, [baz, bNWSK], [bT2])
            tt("dve", T1[:, :], T1[:, :], T2[:, :], ALU.add, [bT1, bT2], [bT1])
            yield
            tt("dve", MBF[:, :], T1[:, :], az[:, 1, :], ALU.mult, [bT1, baz], [bMBF])
            if c == 0:
                dump("m0", MBF[:, :], [128, 512], [bMBF])
            yield
            for cc in range(4):
                tr(PTl[:, cc, :], MBF[:, cc * 128:(cc + 1) * 128], IDB[:, :], [bMBF, bIDB], [PB[bk[0]]])
            cp("act", MTt[:, :, :], PTl[:, 0:4, :], [PB[bk[0]]], [bMTt])
            yield
            for sg in range(2):
                for g in range(4):
                    tr(PTl[:, sg * 4 + g, :], XCS[:, g * 128:(g + 1) * 128, sg, c], IDB[:, :], [bXCS, bIDB], [PB[bk[0]]])
            cp("act", XT[:, :, :], PTl, [PB[bk[0]]], [bXT])
            yield
            for g in range(4):
                mm(PS[bk[1]][:, g * 128:(g + 1) * 128], CS128[:, 0:128], XT[:, g, :], True, False, [bCS, bXT], [PB[bk[1]]])
                mm(PS[bk[1]][:, g * 128:(g + 1) * 128], CS128[:, 256:384], XT[:, 4 + g, :], False, True, [bCS, bXT], [PB[bk[1]]])
            cp("dve", FTB[:, :, :], PS[bk[1]][:, :].rearrange("p (g t) -> p g t", t=128), [PB[bk[1]]], [bFTB])
            yield
            for g in range(4):
                mm(PS[bk[1]][:, g * 128:(g + 1) * 128], WFB[:, g, :], FTB[:, g, :], True, True, [bWFB, bFTB], [PB[bk[1]]])
            cp("act", YFT[:, :, :], PS[bk[1]][:, :].rearrange("p (g t) -> p g t", t=128), [PB[bk[1]]], [bYFT])
            yield
            for cb in range(2):
                for kc in range(4):
                    mm(PS[bk[2 + cb]][:, :], MTt[:, kc, :], WOB[:, kc, cb * 512:(cb + 1) * 512], kc == 0, False, [bMTt, bWOB], [PB[bk[2 + cb]]])
                for kc in range(4):
                    mm(PS[bk[2 + cb]][:, :], YFT[:, kc, :], WOB[:, 4 + kc, cb * 512:(cb + 1) * 512], False, kc == 3, [bYFT, bWOB], [PB[bk[2 + cb]]])
            yield
            act(JK[:, 0:512], PS[bk[2]][:, :], AF.Square, [PB[bk[2]]], [bJK, bSY], accum_out=SY[:, 0:1])
            act(JK[:, 512:1024], PS[bk[3]][:, :], AF.Square, [PB[bk[3]]], [bJK, bSY], accum_out=SY[:, 1:2])
            yield
            tt("pool", SY[:, 2:3], SY[:, 0:1], SY[:, 1:2], ALU.add, [bSY], [bSY])
            ts("pool", SY[:, 3:4], SY[:, 2:3], 1.0 / D, EPS, ALU.mult, ALU.add, [bSY], [bSY])
            tt("pool", SY[:, 4:5], SY[:, 3:4], NEGH, ALU.pow, [bSY, bTABS], [bSY])
            yield
            xr, bxr, pr_, bpr_ = XR[s_], bXR[s_], PRr[s_], bPRr[s_]
            P.dma("sp", xr[:, :], x_rot.ap()[rows, :], writes=[bxr])
            P.dma("sp", pr_[0:64, :], bc_rows(posr_d, 2 * c, 512, parts=64), reads=[bPOSRD], writes=[bpr_])
            P.dma("sp", pr_[64:128, :], bc_rows(posr_d, 2 * c + 1, 512, parts=64), reads=[bPOSRD], writes=[bpr_])
            tt("pool", xr[:, 0:512], xr[:, 0:512], pr_[:, :], ALU.add, [bxr, bpr_], [bxr])
            tt("pool", xr[:, 512:1024], xr[:, 512:1024], POSC[:, :], ALU.add, [bxr, bPOSC], [bxr])
            yield
            stt("dve", TT_[:, 0:512], PS[bk[2]][:, :], SY[:, 4:5], GP1[:, 0:512], ALU.mult, ALU.mult, [PB[bk[2]], bSY, bMOD], [bTT])
            stt("dve", TT_[:, 512:1024], PS[bk[3]][:, :], SY[:, 4:5], GP1[:, 512:1024], ALU.mult, ALU.mult, [PB[bk[3]], bSY, bMOD], [bTT])
            yield
            x1, bx1 = X1[s_], bX1[s_]
            tt("pool", x1[:, :], TT_[:, :], xr[:, :], ALU.add, [bTT, bxr], [bx1])
            P.dma("sp", out_d.ap()[rows, :], x1[:, :], reads=[bx1], writes=[bOUT])
            if c == 0:
                dump("x1_0", x1[:, :], [128, D], [bx1])
            yield
            act(JK[:, :], x1[:, :], AF.Square, [bx1], [bJK, bSY], accum_out=SY[:, 5:6])
            yield
            ts("pool", SY[:, 6:7], SY[:, 5:6], 1.0 / D, EPS, ALU.mult, ALU.add, [bSY], [bSY])
            tt("pool", SY[:, 7:8], SY[:, 6:7], NEGH, ALU.pow, [bSY, bTABS], [bSY])
            yield
            stt("dve", TT_[:, :], x1[:, :], SY[:, 7:8], G2, ALU.mult, ALU.mult, [bx1, bSY, bMOD], [bTT])
            tt("pool", H2[:, :], TT_[:, :], S2, ALU.add, [bTT, bMOD], [bH2])
            yield
            cp("act", H2H[:, :], H2[:, :], [bH2], [bH2H])
            tt("dve", H2Lw[:, :], H2[:, :], H2H[:, :], ALU.subtract, [bH2, bH2H], [bH2Lw])
            yield
            for kc in range(8):
                tr(PTl[:, kc, :], H2H[:, kc * 128:(kc + 1) * 128], IDB[:, :], [bH2H, bIDB], [PB[bk[0]]])
            cp("act", H2T[:, :, rows], PTl, [PB[bk[0]]], [bH2T])
            yield
            for kc in range(8):
                tr(PTl[:, kc, :], H2Lw[:, kc * 128:(kc + 1) * 128], IDB[:, :], [bH2Lw, bIDB], [PB[bk[0]]])
            cp("dve", H2LT[:, :, :], PTl, [PB[bk[0]]], [bH2LT])
            yield
            LGp = PS[bk[1]][:, 0:20]
            for kc in range(8):
                mm(LGp, H2T[:, kc, rows], WRH[:, kc, :], kc == 0, False, [bH2T, bWR], [PB[bk[1]]])
            for kc in range(8):
                mm(LGp, H2T[:, kc, rows], WRL[:, kc, :], False, False, [bH2T, bWR], [PB[bk[1]]])
            for kc in range(8):
                mm(LGp, H2LT[:, kc, :], WRH[:, kc, :], False, kc == 7, [bH2LT, bWR], [PB[bk[1]]])
            yield
            tt("dve", LG[:, :], LGp, BR[:, :], ALU.add, [PB[bk[1]], bBR], [bLG])
            if c == 0:
                dump("lg0", LG[:, :], [128, 20], [bLG])
            R = RT
            bR = bRT
            yield
            red("dve", R[:, 0:1], LG[:, 0:4], ALU.max, [bLG], [bR])
            ts("dve", R[:, 1:5], LG[:, 0:4], R[:, 0:1], None, ALU.is_equal, None, [bLG, bR], [bR])
            ts("dve", R[:, 5:6], R[:, 0:1], -1.0, None, ALU.mult, None, [bR], [bR])
            yield
            act(R[:, 6:10], LG[:, 0:4], AF.Exp, [bLG, bR], [bR], bias=R[:, 5:6], accum_out=R[:, 10:11])
            P.op("dve", lambda e, o=R[:, 11:12], i_=R[:, 10:11]: e.reciprocal(out=o, in_=i_), [bR], [bR])
            yield
            ts("dve", R[:, 12:16], R[:, 1:5], BIG, -BIG, ALU.mult, ALU.add, [bR], [bR])
            em = R[:, 16:32]
            tt("dve", em.rearrange("p (g j) -> p g j", j=4), LG[:, 4:20].rearrange("p (g j) -> p g j", j=4),
               R[:, 12:16, None].broadcast_to([128, 4, 4]), ALU.add, [bLG, bR], [bR])
            yield
            red("dve", R[:, 32:33], em, ALU.max, [bR], [bR])
            ts("dve", R[:, 48:64], em, R[:, 32:33], None, ALU.is_equal, None, [bR], [bR])
            stt("dve", R[:, 64:80], R[:, 48:64], -BIG, em, ALU.mult, ALU.add, [bR], [bR])
            yield
            red("dve", R[:, 33:34], R[:, 64:80], ALU.max, [bR], [bR])
            ts("dve", R[:, 80:96], R[:, 64:80], R[:, 33:34], None, ALU.is_equal, None, [bR], [bR])
            tt("dve", R[:, 34:35], R[:, 33:34], R[:, 32:33], ALU.subtract, [bR], [bR])
            yield
            act(R[:, 35:36], R[:, 34:35], AF.Exp, [bR], [bR])
            ts("dve", R[:, 36:37], R[:, 35:36], 1.0, None, ALU.add, None, [bR], [bR])
            P.op("dve", lambda e, o=R[:, 37:38], i_=R[:, 36:37]: e.reciprocal(out=o, in_=i_), [bR], [bR])
            tt("dve", R[:, 38:39], R[:, 37:38], R[:, 35:36], ALU.mult, [bR], [bR])
            yield
            tt("dve", R[:, 39:40], R[:, 37:38], R[:, 11:12], ALU.mult, [bR], [bR])
            tt("dve", R[:, 40:41], R[:, 38:39], R[:, 11:12], ALU.mult, [bR], [bR])
            ts("dve", COMB[:, c, :], R[:, 48:64], R[:, 39:40], None, ALU.mult, None, [bR], [bCOMB])
            stt("dve", COMB[:, c, :], R[:, 80:96], R[:, 40:41], COMB[:, c, :], ALU.mult, ALU.add, [bR, bCOMB], [bCOMB])
            yield

        def window6(genfs, w):
            pend = list(genfs)
            act_ = []
            while pend or act_:
                while pend and len(act_) < w:
                    act_.append(pend.pop(0)())
                for g_ in list(act_):
                    try:
                        next(g_)
                    except StopIteration:
                        act_.remove(g_)

        window6([(lambda c=c: s6_tile(c)) for c in range(OWN)], 2)
        dump("comb", COMB[:, :, :], [128, OWN, 16], [bCOMB])

        if stage <= 5:
            o_b = bOUT
            P.barrier()
            final.append((P.sem["sp"], 0))
            P.barrier()
            with nc.Block() as block:
                P.emit(block, [])
            st9.close()
            st7.close()
            return nc

        P.barrier()
        st9.close()
        st7.close()
        stE = ExitStack()

        def sbE(name, shape, dt=F32):
            return stE.enter_context(nc.sbuf_tensor(name, list(shape), dt))

        YACC = sbE("YACC", [128, OWN, D])
        bYACC = [Buf("YACC%d" % i) for i in range(OWN)]
        WG = [sbE("WG%d" % i, [128, 8, 512], BF16) for i in range(2)]
        WU = [sbE("WU%d" % i, [128, 8, 512], BF16) for i in range(2)]
        WD = [sbE("WD%d" % i, [128, 4, D], BF16) for i in range(2)]
        bWG = [Buf("WG%d" % i) for i in range(2)]
        bWU = [Buf("WU%d" % i) for i in range(2)]
        bWD = [Buf("WD%d" % i) for i in range(2)]
        ATb = [sbE("AT%d" % i, [128, 4, 512], BF16) for i in range(2)]
        bAT = [Buf("AT%d" % i) for i in range(2)]
        SG = [sbE("SG%d" % i, [128, 512]) for i in range(2)]
        bSG = [Buf("SG%d" % i) for i in range(2)]
        NEXP = 16
        gu_ctr = [0]
        dn_ctr = [0]
        for e_ in range(NEXP):
            s_ = e_ % 2
            P.dma("pool", WG[s_][:, :, :], w_gate.ap()[e_].rearrange("(k p) n -> p k n", p=128), writes=[bWG[s_]])
            P.dma("pool", WU[s_][:, :, :], w_up.ap()[e_].rearrange("(k p) n -> p k n", p=128), writes=[bWU[s_]])
            P.dma("pool", WD[s_][:, :, :], w_down.ap()[e_].rearrange("(k p) n -> p k n", p=128), writes=[bWD[s_]])
            for tb in range(OWN // 4):
                tsl = slice(tb * 512, (tb + 1) * 512)
                a_ = (e_ * 4 + tb) % 2
                for fb in range(4):
                    k_ = gu_ctr[0] % 2
                    gu_ctr[0] += 1
                    pg, pu = 2 * k_, 2 * k_ + 1
                    for kc in range(8):
                        mm(PS[pg][:, :], WG[s_][:, kc, fb * 128:(fb + 1) * 128], H2T[:, kc, tsl], kc == 0, kc == 7, [bWG[s_], bH2T], [PB[pg]])
                    for kc in range(8):
                        mm(PS[pu][:, :], WU[s_][:, kc, fb * 128:(fb + 1) * 128], H2T[:, kc, tsl], kc == 0, kc == 7, [bWU[s_], bH2T], [PB[pu]])
                    act(SG[k_][:, :], PS[pg][:, :], AF.Silu, [PB[pg]], [bSG[k_]])
                    tt("dve", ATb[a_][:, fb, :], SG[k_][:, :], PS[pu][:, :], ALU.mult, [bSG[k_], PB[pu]], [bAT[a_]])
                for t_ in range(4):
                    tile = tb * 4 + t_
                    for cb in range(2):
                        pd = 4 + dn_ctr[0] % 4
                        dn_ctr[0] += 1
                        for fb in range(4):
                            mm(PS[pd][:, :], ATb[a_][:, fb, t_ * 128:(t_ + 1) * 128], WD[s_][:, fb, cb * 512:(cb + 1) * 512], fb == 0, fb == 3, [bAT[a_], bWD[s_]], [PB[pd]])
                        ya = YACC[:, tile, cb * 512:(cb + 1) * 512]
                        if e_ == 0:
                            ts("dve", ya, PS[pd][:, :], COMB[:, tile, e_:e_ + 1], None, ALU.mult, None, [PB[pd], bCOMB], [bYACC[tile]])
                        else:
                            stt("dve", ya, PS[pd][:, :], COMB[:, tile, e_:e_ + 1], ya, ALU.mult, ALU.add, [PB[pd], bCOMB, bYACC[tile]], [bYACC[tile]])
        GP2L = sbE("GP2L", [128, D])
        bGP2L = Buf("GP2L")
        P.dma("sp", GP2L[:, :], bc_rows(mod_d, 0, D, off=5 * D), reads=[bMODD], writes=[bGP2L])
        GP2 = GP2L[:, :]
        bMOD = bGP2L
        XF = [sbE("XF%d" % i, [128, D]) for i in range(2)]
        bXF = [Buf("XF%d" % i) for i in range(2)]
        JK2 = sbE("JK2", [128, D], BF16)
        bJK2 = Buf("JK2")
        SF = sbE("SF", [128, 8])
        bSF = Buf("SF")
        OT = [sbE("OT%d" % i, [128, D]) for i in range(2)]
        bOT = [Buf("OT%d" % i) for i in range(2)]
        for c in range(OWN):
            s_ = c % 2
            rows = slice(c * 128, (c + 1) * 128)
            P.dma("sp", XF[s_][:, :], out_d.ap()[rows, :], reads=[bOUT], writes=[bXF[s_]])
            q_ = SF[:, s_ * 4:s_ * 4 + 4]
            act(JK2[:, :], YACC[:, c, :], AF.Square, [bYACC[c]], [bJK2, bSF], accum_out=q_[:, 0:1])
            ts("pool", q_[:, 1:2], q_[:, 0:1], 1.0 / D, EPS, ALU.mult, ALU.add, [bSF], [bSF])
            tt("pool", q_[:, 2:3], q_[:, 1:2], NEGH, ALU.pow, [bSF, bTABS], [bSF])
            stt("dve", OT[s_][:, :], YACC[:, c, :], q_[:, 2:3], GP2, ALU.mult, ALU.mult, [bYACC[c], bSF, bMOD], [bOT[s_]])
            tt("pool", OT[s_][:, :], OT[s_][:, :], XF[s_][:, :], ALU.add, [bOT[s_], bXF[s_]], [bOT[s_]])
            t = P.dma("sp", out_d.ap()[rows, :], OT[s_][:, :], reads=[bOT[s_], bXF[s_]], writes=[bOUT])
            final.append((t[0], t[1]))
        P.barrier()
        with nc.Block() as block:
            P.emit(block, final)
        stE.close()
    return nc


def _centered(idx, n):
    return ((idx + n // 2) % n) - n // 2


def make_inputs(core, x, c, ctx, c_ctx, w_ada, b_ada, g_pre_mix, g_post_mix, g_pre_ffn, g_post_ffn,
                w_in, conv_w, conv_b, w_q, w_k, w_v, w_if_fwd, b_if_fwd, w_if_bwd, b_if_bwd,
                mlstm_norm_w, mlstm_skip, w_fourier, w_out, w_router_group, b_router_group,
                w_router_expert, b_router_expert, w_gate, w_up, w_down, shared):
    f32 = np.float32
    j = core
    xs = x[0]
    m = {}
    m["x_rot"] = np.ascontiguousarray(np.roll(xs, -2048 * j, axis=0))
    halo = np.zeros((128, D), f32)
    hmask = np.zeros(64, f32)
    hrow = np.zeros(128, f32)
    hcol = np.zeros(128, f32)
    for g in range(NG):
        for side, tr_ in ((0, 512 * g - 1), (1, 512 * g + 512)):
            true_t = (tr_ % T + 2048 * j) % T
            own_first_true = ((512 * g) % T + 2048 * j) % T
            if side == 0:
                valid = own_first_true != 0
            else:
                valid = ((512 * g + 511) % T + 2048 * j) % T != T - 1
            halo[2 * g + side] = xs[true_t]
            hmask[2 * g + side] = 1.0 if valid else 0.0
            hrow[2 * g + side] = true_t // 64
            hcol[2 * g + side] = true_t % 64
    m["x_halo"] = halo
    tabs = np.zeros((128, 1024), f32)
    p = np.arange(128)
    for a in range(2):
        tabs[:, a] = ((2 * p + a) + 32 * j) % 256
    tabs[:, 2] = p % 64
    tabs[:, 3] = hrow
    tabs[:, 4] = hcol
    tabs[:, 5] = -0.5
    i = np.arange(128)
    true_c = (i + 16 * j) % 128
    tabs[:, 128:256] = (true_c < 16 * j).astype(f32)[None, :]
    tabs[:, 256:384] = (true_c >= 16 * j + 16).astype(f32)[None, :]
    tabs[:, 384:448] = hmask[None, :]
    tabs[:, 512:768] = np.arange(256, dtype=f32)[None, :]
    m["tabs"] = tabs
    di = np.zeros((128, 640), f32)
    n = np.arange(128)[:, None]
    k = np.arange(128)[None, :]
    di[:, 0:128] = _centered(n * k + 32, 128)
    di[:, 128:256] = _centered(n * k, 128)
    base = n * k + 2048 * j * k
    di[:, 256:384] = _centered(base + 4096, 16384)
    di[:, 384:512] = _centered(base, 16384)
    cc = (16 * j + np.arange(16))[None, :]
    di[:, 512:528] = _centered(n * cc + 32, 128)
    di[:, 528:544] = _centered(n * cc, 128)
    m["dftidx"] = di
    m.update(shared)
    return m


def make_shared(x, c, ctx, c_ctx, w_ada, b_ada, g_pre_mix, g_post_mix, g_pre_ffn, g_post_ffn,
                w_in, conv_w, conv_b, w_q, w_k, w_v, w_if_fwd, b_if_fwd, w_if_bwd, b_if_bwd,
                mlstm_norm_w, mlstm_skip, w_fourier, w_out, w_router_group, b_router_group,
                w_router_expert, b_router_expert, w_gate, w_up, w_down):
    f32 = np.float32
    s = {}
    s["ctx"] = np.ascontiguousarray(ctx[0])
    cT = np.zeros((128, 16), f32)
    cT[:, 0:8] = c[0].reshape(8, 128).T
    cT[:, 8:16] = c_ctx.reshape(8, 128).T
    s["cT"] = cT
    s["w_ada"] = np.ascontiguousarray(w_ada[0])
    s["b_ada"] = np.ascontiguousarray(b_ada[0][None, :])
    s["gains"] = np.stack([g_pre_mix[0], g_post_mix[0], g_pre_ffn[0], g_post_ffn[0]]).astype(f32)
    s["w_in"] = np.ascontiguousarray(w_in[0])
    cw = np.zeros((128, 16), f32)
    for cc in range(4):
        for k in range(3):
            cw[:, cc * 3 + k] = conv_w[0][k, cc * 128:(cc + 1) * 128]
        cw[:, 12 + cc] = conv_b[0][cc * 128:(cc + 1) * 128]
    s["convw"] = cw
    s["w_qkv"] = np.stack([w_q[0].reshape(512, 4), w_k[0].reshape(512, 4), w_v[0].reshape(512, 4)]).astype(f32)
    s["w_qkvT"] = np.stack([np.ascontiguousarray(w.transpose(0, 2, 1)).reshape(512, 4) for w in (w_q[0], w_k[0], w_v[0])]).astype(f32)
    wf, wb = w_if_fwd[0], w_if_bwd[0]
    s["w_if"] = np.ascontiguousarray(np.concatenate([wf[:, 0:4], wb[:, 0:4], wf[:, 4:8], wb[:, 4:8]], axis=1))
    bf, bb = b_if_fwd[0], b_if_bwd[0]
    s["b_if"] = np.concatenate([bf[0:4], bb[0:4], bf[4:8], bb[4:8]])[None, :].astype(f32)
    s["nrm_skip"] = np.stack([mlstm_norm_w[0], mlstm_skip[0]]).astype(f32)
    s["w_fourier"] = np.ascontiguousarray(w_fourier[0])
    s["w_out"] = np.ascontiguousarray(w_out[0])
    s["w_router"] = np.ascontiguousarray(np.concatenate([w_router_group[0], w_router_expert[0]], axis=1))
    s["b_router"] = np.concatenate([b_router_group[0], b_router_expert[0]])[None, :].astype(f32)
    s["w_gate"] = np.ascontiguousarray(w_gate[0])
    s["w_up"] = np.ascontiguousarray(w_up[0])
    s["w_down"] = np.ascontiguousarray(w_down[0])
    cst = np.zeros((128, 640), f32)
    cst[:, 0:128] = np.eye(128)
    a = np.arange(128)
    cst[:, 128:256] = (a[:, None] <= a[None, :])
    cst[:, 256:384] = (a[:, None] >= a[None, :])
    cst[:, 384:512] = 1.0
    cst[:, 512:640] = (a[:, None] // 4 == a[None, :] // 4)
    s["consts"] = cst
    return s


_CACHE = {}


def kernel(**inputs):
    inputs = {k: np.asarray(v) for k, v in inputs.items()}
    if "nc" not in _CACHE:
        _CACHE["nc"] = build()
    nc = _CACHE["nc"]
    shared = make_shared(**inputs)
    in_maps = [make_inputs(core, shared=shared, **inputs) for core in range(NCORES)]
    res = run_bass_kernel_spmd(nc, in_maps, core_ids=list(range(NCORES)))
    out = np.concatenate([res.results[i]["out"] for i in range(NCORES)], axis=0)
    return out.reshape(1, T, D).astype(np.float32)
```
